# Optimizing a Trainium2 kernel written in Bass

```python
import math
import jax, jax.numpy as jnp
from jax import lax
import numpy as np

D_MODEL = 1024
BATCH = 16
SEQ = 2048
DEPTH = 1

DN_HEADS = 4
DN_HEAD_DIM = 128
DN_WIDTH = DN_HEADS * DN_HEAD_DIM
DN_CONV = 4
DN_CHUNK = 64
POOL_WINDOWS = (2, 4, 8, 16)
POOL_GROUPS = len(POOL_WINDOWS)
POOL_WIDTH = D_MODEL // 2
POOL_GROUP_DIM = POOL_WIDTH // POOL_GROUPS
N_BRANCHES = 2
IN_COLS = 4 * DN_WIDTH + 2 * DN_HEADS + POOL_WIDTH + N_BRANCHES * D_MODEL
MOE_GROUPS = 4
MOE_EXPERTS_PER_GROUP = 8
MOE_EXPERTS = MOE_GROUPS * MOE_EXPERTS_PER_GROUP
MOE_TOP_K = 2
MOE_D_FF = D_MODEL // 4
MOE_BLOCK = 128
N_MOD = 6
EPS = 1e-6

kernel_name = "hybrid_deltanet_pool_hmoe_adaln"


def _rmsnorm(x, g):
    x32 = x.astype(jnp.float32)
    y = x32 * lax.rsqrt(jnp.mean(x32 * x32, axis=-1, keepdims=True) + EPS)
    return (y * g.astype(jnp.float32)).astype(x.dtype)


def _l2norm(x):
    return x * lax.rsqrt(jnp.sum(x * x, axis=-1, keepdims=True) + EPS)


def _split_in(p):
    sizes = [3 * DN_WIDTH, DN_WIDTH, DN_HEADS, DN_HEADS, POOL_WIDTH, D_MODEL, D_MODEL]
    idx = [int(v) for v in np.cumsum(sizes)[:-1]]
    return jnp.split(p, idx, axis=-1)


def _causal_dwconv(x, w):
    K, C = w.shape
    return lax.conv_general_dilated(
        x, w[:, None, :].astype(x.dtype), window_strides=(1,), padding=[(K - 1, 0)],
        dimension_numbers=('NWC', 'WIO', 'NWC'), feature_group_count=C)


def _gated_delta_rule(q, k, v, g, beta):
    B, S, H, Dk = q.shape
    Dv = v.shape[-1]
    C = DN_CHUNK
    N = S // C

    def chunks(t):
        return jnp.moveaxis(t.reshape(B, N, C, H, -1), 3, 1)

    q, k, v = chunks(q), chunks(k), chunks(v)
    g = chunks(g[..., None])[..., 0]
    beta = chunks(beta[..., None])[..., 0]
    gc = jnp.cumsum(g, axis=-1)
    causal = jnp.tril(jnp.ones((C, C), bool))
    strict = jnp.tril(jnp.ones((C, C), bool), -1)
    decay = jnp.exp(jnp.where(causal, gc[..., :, None] - gc[..., None, :], -jnp.inf))
    k_beta = k * beta[..., None]
    v_beta = v * beta[..., None]
    Lm = jnp.where(strict, jnp.einsum('bhnid,bhnjd->bhnij', k_beta, k) * decay, 0.0)
    eye = jnp.eye(C, dtype=jnp.float32)
    T = lax.linalg.triangular_solve(Lm + eye, jnp.broadcast_to(eye, Lm.shape),
                                    left_side=True, lower=True, unit_diagonal=True)
    u = jnp.einsum('bhnij,bhnjv->bhniv', T, v_beta)
    w = jnp.einsum('bhnij,bhnjk->bhnik', T, k_beta * jnp.exp(gc)[..., None])
    attn = jnp.einsum('bhnid,bhnjd->bhnij', q, k) * decay
    g_last = gc[..., -1]
    q_dec = q * jnp.exp(gc)[..., None]
    k_dec = k * jnp.exp(g_last[..., None] - gc)[..., None]
    xs = tuple(jnp.moveaxis(t, 2, 0) for t in (u, w, q_dec, k_dec, attn, jnp.exp(g_last)))

    def step(state, inp):
        u_i, w_i, qd_i, kd_i, a_i, dl_i = inp
        v_new = u_i - jnp.einsum('bhck,bhkv->bhcv', w_i, state)
        o_i = jnp.einsum('bhck,bhkv->bhcv', qd_i, state) + jnp.einsum('bhij,bhjv->bhiv', a_i, v_new)
        state = state * dl_i[..., None, None] + jnp.einsum('bhck,bhcv->bhkv', kd_i, v_new)
        return state, o_i

    state0 = jnp.zeros((B, H, Dk, Dv), jnp.float32)
    _, o = lax.scan(step, state0, xs)
    return jnp.transpose(o, (1, 0, 3, 2, 4)).reshape(B, S, H, Dv)


def _multiscale_pool(u, pool_w, pool_scale):
    B, S, _ = u.shape
    u32 = u.astype(jnp.float32)
    cs = jnp.pad(jnp.cumsum(u32, axis=1), ((0, 0), (1, 0), (0, 0)))
    t = jnp.arange(S)
    outs = []
    for gi, win in enumerate(POOL_WINDOWS):
        csg = cs[..., gi * POOL_GROUP_DIM:(gi + 1) * POOL_GROUP_DIM]
        lo = jnp.maximum(t + 1 - win, 0)
        cnt = jnp.minimum(t + 1, win).astype(jnp.float32)
        outs.append((csg[:, 1:] - csg[:, lo]) / cnt[None, :, None])
    pooled = jnp.stack(outs, axis=2) - u32.reshape(B, S, POOL_GROUPS, POOL_GROUP_DIM)
    y = jnp.einsum('bsgc,gcd->bsgd', pooled.astype(u.dtype), pool_w)
    return y.reshape(B, S, POOL_WIDTH) * pool_scale


def _token_mixer(xn, w_in, conv_w, a_log, dt_bias, dn_norm_g, pool_w, pool_scale, w_lift_a, w_lift_b, w_out):
    B, S, _ = xn.shape
    proj = xn @ w_in
    qkv, z, a, b, pu, ga, gb = _split_in(proj)
    qkv = jax.nn.silu(_causal_dwconv(qkv, conv_w)).astype(jnp.float32)
    q, k, v = jnp.split(qkv, 3, axis=-1)
    q = _l2norm(q.reshape(B, S, DN_HEADS, DN_HEAD_DIM)) * (DN_HEAD_DIM ** -0.5)
    k = _l2norm(k.reshape(B, S, DN_HEADS, DN_HEAD_DIM))
    v = v.reshape(B, S, DN_HEADS, DN_HEAD_DIM)
    beta = jax.nn.sigmoid(b.astype(jnp.float32))
    g = -jnp.exp(a_log.astype(jnp.float32)) * jax.nn.softplus(a.astype(jnp.float32) + dt_bias.astype(jnp.float32))
    o = _gated_delta_rule(q, k, v, g, beta)
    zg = jax.nn.silu(z.astype(jnp.float32)).reshape(B, S, DN_HEADS, DN_HEAD_DIM)
    o = o * lax.rsqrt(jnp.mean(o * o, axis=-1, keepdims=True) + EPS) * dn_norm_g.astype(jnp.float32) * zg
    y_a = o.reshape(B, S, DN_WIDTH).astype(xn.dtype)
    y_b = _multiscale_pool(pu, pool_w, pool_scale)
    mixed = jax.nn.sigmoid(ga) * (y_a @ w_lift_a) + jax.nn.sigmoid(gb) * (y_b @ w_lift_b)
    return mixed @ w_out


def _hier_moe(x, w_rg, b_rg, w_re, b_re, w_gate, w_up, w_down):
    B, S, D = x.shape
    N = B * S
    xf = x.reshape(N, D)
    pg = jax.nn.softmax((xf @ w_rg + b_rg).astype(jnp.float32), axis=-1)
    p_grp, grp = lax.top_k(pg, 1)
    le = (xf @ w_re + b_re).astype(jnp.float32).reshape(N, MOE_GROUPS, MOE_EXPERTS_PER_GROUP)
    le = jnp.take_along_axis(le, grp[:, :, None], axis=1)[:, 0]
    top_p, top_i = lax.top_k(jax.nn.softmax(le, axis=-1), MOE_TOP_K)
    wts = p_grp * top_p / jnp.sum(top_p, axis=-1, keepdims=True)
    eid = grp * MOE_EXPERTS_PER_GROUP + top_i
    A = N * MOE_TOP_K
    e_flat = eid.reshape(A)
    tok_flat = jnp.repeat(jnp.arange(N, dtype=jnp.int32), MOE_TOP_K)
    w_flat = wts.reshape(A)
    order = jnp.argsort(e_flat)
    e_s, tok_s, w_s = e_flat[order], tok_flat[order], w_flat[order]
    counts = jnp.bincount(e_flat, length=MOE_EXPERTS)
    padded = (counts + MOE_BLOCK - 1) // MOE_BLOCK * MOE_BLOCK
    start = jnp.cumsum(counts) - counts
    pend = jnp.cumsum(padded)
    pstart = pend - padded
    dest = pstart[e_s] + jnp.arange(A) - start[e_s]
    P = A + MOE_EXPERTS * MOE_BLOCK
    nb = P // MOE_BLOCK
    slot_tok = jnp.full((P,), N, jnp.int32).at[dest].set(tok_s)
    slot_w = jnp.zeros((P,), jnp.float32).at[dest].set(w_s)
    blk_e = jnp.minimum(jnp.searchsorted(pend, jnp.arange(nb) * MOE_BLOCK, side='right'), MOE_EXPERTS - 1)
    x_pad = jnp.concatenate([xf, jnp.zeros((1, D), xf.dtype)], axis=0)
    xb = x_pad[slot_tok].reshape(nb, MOE_BLOCK, D)

    def expert_block(args):
        xblk, e = args
        h = jax.nn.silu(xblk @ w_gate[e]) * (xblk @ w_up[e])
        return h @ w_down[e]

    yb = lax.map(expert_block, (xb, blk_e))
    y = jnp.zeros((N + 1, D), jnp.float32).at[slot_tok].add(yb.reshape(P, D).astype(jnp.float32) * slot_w[:, None])
    return y[:N].astype(x.dtype).reshape(B, S, D)


def setup_inputs(seed: int = 0) -> dict:
    key = jax.random.key(seed)
    ks = jax.random.split(key, 32)
    L, D = DEPTH, D_MODEL
    f32 = jnp.float32

    def nrm(k, shape, scale):
        return jax.random.normal(k, shape, f32) * scale

    dt = jnp.exp(jax.random.uniform(ks[8], (L, DN_HEADS), f32, math.log(1e-3), math.log(1e-1)))
    return {
        "x": nrm(ks[0], (BATCH, SEQ, D), 1.0),
        "c": nrm(ks[1], (BATCH, D), 1.0),
        "w_ada": nrm(ks[2], (L, D, N_MOD * D), 0.5 * D ** -0.5),
        "b_ada": nrm(ks[3], (L, N_MOD * D), 0.02),
        "norm1_g": 1.0 + nrm(ks[4], (L, D), 0.1),
        "w_in": nrm(ks[5], (L, D, IN_COLS), D ** -0.5),
        "conv_w": nrm(ks[6], (L, DN_CONV, 3 * DN_WIDTH), DN_CONV ** -0.5),
        "a_log": jnp.log(jax.random.uniform(ks[7], (L, DN_HEADS), f32, 1.0, 16.0)),
        "dt_bias": dt + jnp.log(-jnp.expm1(-dt)),
        "dn_norm_g": 1.0 + nrm(ks[9], (L, DN_HEAD_DIM), 0.1),
        "pool_w": nrm(ks[10], (L, POOL_GROUPS, POOL_GROUP_DIM, POOL_GROUP_DIM), POOL_GROUP_DIM ** -0.5),
        "pool_scale": 1.0 + nrm(ks[11], (L, POOL_WIDTH), 0.1),
        "w_lift_a": nrm(ks[12], (L, DN_WIDTH, D), DN_WIDTH ** -0.5),
        "w_lift_b": nrm(ks[13], (L, POOL_WIDTH, D), POOL_WIDTH ** -0.5),
        "w_out": nrm(ks[14], (L, D, D), D ** -0.5),
        "norm2_g": 1.0 + nrm(ks[15], (L, D), 0.1),
        "w_router_group": nrm(ks[16], (L, D, MOE_GROUPS), D ** -0.5),
        "b_router_group": nrm(ks[17], (L, MOE_GROUPS), 0.01),
        "w_router_expert": nrm(ks[18], (L, D, MOE_EXPERTS), D ** -0.5),
        "b_router_expert": nrm(ks[19], (L, MOE_EXPERTS), 0.01),
        "w_gate": nrm(ks[20], (L, MOE_EXPERTS, D, MOE_D_FF), D ** -0.5),
        "w_up": nrm(ks[21], (L, MOE_EXPERTS, D, MOE_D_FF), D ** -0.5),
        "w_down": nrm(ks[22], (L, MOE_EXPERTS, MOE_D_FF, D), MOE_D_FF ** -0.5),
        "final_norm_g": 1.0 + nrm(ks[23], (D,), 0.1),
    }


def reference(x, c, w_ada, b_ada, norm1_g, w_in, conv_w, a_log, dt_bias, dn_norm_g, pool_w, pool_scale,
              w_lift_a, w_lift_b, w_out, norm2_g, w_router_group, b_router_group, w_router_expert,
              b_router_expert, w_gate, w_up, w_down, final_norm_g):
    h = x
    c_act = jax.nn.silu(c)
    for l in range(DEPTH):
        mod = c_act @ w_ada[l] + b_ada[l]
        sh1, sc1, gt1, sh2, sc2, gt2 = [m[:, None, :] for m in jnp.split(mod, N_MOD, axis=-1)]
        xn = _rmsnorm(h, norm1_g[l]) * (1 + sc1) + sh1
        h = h + gt1 * _token_mixer(xn, w_in[l], conv_w[l], a_log[l], dt_bias[l], dn_norm_g[l], pool_w[l],
                                   pool_scale[l], w_lift_a[l], w_lift_b[l], w_out[l])
        xn = _rmsnorm(h, norm2_g[l]) * (1 + sc2) + sh2
        h = h + gt2 * _hier_moe(xn, w_router_group[l], b_router_group[l], w_router_expert[l],
                                b_router_expert[l], w_gate[l], w_up[l], w_down[l])
    return _rmsnorm(h, final_norm_g)
```

```python
import types
import numpy as np
import ml_dtypes
from contextlib import ExitStack
import concourse.bass as bass
import concourse.mybir as mybir
from concourse.bass import IndirectOffsetOnAxis
from concourse.bass_utils import run_bass_kernel_spmd

F32 = mybir.dt.float32
BF16 = mybir.dt.bfloat16
I32 = mybir.dt.int32
U32 = mybir.dt.uint32
AF = mybir.ActivationFunctionType
ALU = mybir.AluOpType
AX = mybir.AxisListType

NCORES = 8
D = 1024
TOK = 4096
BLK = 512
NBLK = TOK // BLK
E = 32
CAP = 512
DFF = 256
EPS = 1e-6
NEG = -1.0e30
WIN_RES = 2568
ENGS = ('pe', 'act', 'dve', 'pool', 'sp')


def _freeze(fn):
    if fn.__closure__ is None:
        return fn
    cells = []
    for c in fn.__closure__:
        try:
            cells.append(types.CellType(c.cell_contents))
        except ValueError:
            cells.append(c)
    g = types.FunctionType(fn.__code__, fn.__globals__, fn.__name__, fn.__defaults__, tuple(cells))
    g.__kwdefaults__ = fn.__kwdefaults__
    return g


class Em:
    def __init__(self, nc, es):
        self.nc = nc
        self.streams = {e: [] for e in ENGS}
        self.sem = {e: es.enter_context(nc.semaphore('s_' + e)) for e in ('pe', 'act', 'dve', 'pool')}
        self.cnt = {e: 0 for e in self.sem}
        self.waited = {e: {} for e in ENGS}
        self.lastw = {}
        self.reads = {}
        self.dq = {}
        for q, n in (('sp', 28), ('pool', 14), ('act', 4)):
            sems = [es.enter_context(nc.semaphore('d_%s%d' % (q, i))) for i in range(n)]
            self.dq[q] = dict(sems=sems, vals=[0] * n, nxt=0)

    def _semh(self, key):
        return self.sem[key] if isinstance(key, str) else self.dq[key[0]]['sems'][key[1]]

    def _wait(self, eng, key, val):
        if self.waited[eng].get(key, 0) >= val:
            return
        self.waited[eng][key] = val
        s = self._semh(key)
        self.streams[eng].append(lambda e, s=s, v=val: e.wait_ge(s, v))

    def _deps(self, eng, r, w, pe_inorder=False):
        need = {}

        def add(tok):
            if tok is None:
                return
            k, v = tok
            if need.get(k, 0) < v:
                need[k] = v
        for key in r:
            add(self.lastw.get(key))
        for key in w:
            add(self.lastw.get(key))
            for t in self.reads.get(key, {}).items():
                add(t)
        if SERIAL:
            for k2 in ('pe', 'act', 'dve', 'pool'):
                if self.cnt[k2] > 0:
                    need[k2] = self.cnt[k2]
            for q2, d2 in self.dq.items():
                for i2, v2 in enumerate(d2['vals']):
                    if v2 > 0:
                        need[(q2, i2)] = max(need.get((q2, i2), 0), v2)
        for k, v in need.items():
            if pe_inorder and k == 'pe':
                continue
            self._wait(eng, k, v)

    def _track(self, tok, r, w):
        for key in w:
            self.lastw[key] = tok
            self.reads[key] = {}
        for key in r:
            d = self.reads.setdefault(key, {})
            if d.get(tok[0], 0) < tok[1]:
                d[tok[0]] = tok[1]

    def op(self, eng, fn, r=(), w=()):
        fn = _freeze(fn)
        self._deps(eng, r, w)
        self.cnt[eng] += 1
        tok = (eng, self.cnt[eng])
        s = self.sem[eng]
        self.streams[eng].append(lambda e, fn=fn, s=s: fn(e).then_inc(s, 1))
        self._track(tok, r, w)
        return tok

    def mm(self, fns, r=(), w=()):
        fns = [_freeze(f) for f in fns]
        self._deps('pe', r, w, pe_inorder=True)
        self.cnt['pe'] += 1
        tok = ('pe', self.cnt['pe'])
        s = self.sem['pe']
        for fn in fns[:-1]:
            self.streams['pe'].append(lambda e, fn=fn: fn(e))
        self.streams['pe'].append(lambda e, fn=fns[-1], s=s: fn(e).then_inc(s, 1))
        self._track(tok, r, w)
        return tok

    def dma(self, q, fn, r=(), w=()):
        fn = _freeze(fn)
        d = self.dq[q]
        i = d['nxt']
        d['nxt'] = (i + 1) % len(d['sems'])
        key = (q, i)
        if d['vals'][i] > 0:
            self._wait(q, key, d['vals'][i])
        self._deps(q, r, w)
        d['vals'][i] += 16
        tok = (key, d['vals'][i])
        s = d['sems'][i]
        self.streams[q].append(lambda e, fn=fn, s=s: fn(e).then_inc(s, 16))
        self._track(tok, r, w)
        return tok

    def wait_keys(self, eng, keys):
        self._deps(eng, keys, keys)

    def finish(self):
        nc = self.nc
        st = self.streams
        with nc.Block() as block:
            @block.tensor
            def _(e):
                for f in st['pe']:
                    f(e)

            @block.scalar
            def _(e):
                for f in st['act']:
                    f(e)

            @block.vector
            def _(e):
                for f in st['dve']:
                    f(e)

            @block.gpsimd
            def _(e):
                for f in st['pool']:
                    f(e)

            @block.sync
            def _(e):
                for f in st['sp']:
                    f(e)


class Ring:
    def __init__(self, nc, es, name, n, nbytes):
        self.t = [es.enter_context(nc.sbuf_tensor('%s%d' % (name, i), [128, nbytes // 4], F32)) for i in range(n)]
        self.name = name
        self.n = n
        self.i = 0

    def get(self, dt=F32):
        i = self.i
        self.i = (i + 1) % self.n
        ap = self.t[i][:]
        if dt != F32:
            ap = ap.bitcast(dt)
        return ap, (self.name, i)


def build_nc(dbg=None):
    nc = bass.Bass("TRN2", target_bir_lowering=False)

    def din(name, shape, dt=F32):
        return nc.dram_tensor(name, list(shape), dt, kind="ExternalInput").ap()

    x = din("x", [TOK, D])
    cT = din("cT", [128, 8, 2])
    w_ada = din("w_ada", [D, 6 * D])
    b_ada = din("b_ada", [1, 6 * D])
    w_in = din("w_in", [D, 4616])
    w_la = din("w_lift_a", [512, D])
    w_lb = din("w_lift_b", [512, D])
    w_out = din("w_out", [D, D])
    pool_w = din("pool_w", [4, 128, 128])
    w_gate = din("w_gate", [E, D, DFF])
    w_up = din("w_up", [E, D, DFF])
    w_down = din("w_down", [E, DFF, D])
    p_n1g = din("p_n1g", [128, 8])
    p_convw = din("p_convw", [128, 12, 4])
    p_alog = din("p_alog", [128, 4])
    p_dtb = din("p_dtb", [128, 4])
    p_dng = din("p_dng", [128, 1])
    p_pscale = din("p_pscale", [128, 4])
    p_n2g = din("p_n2g", [128, D])
    p_fng = din("p_fng", [128, D])
    p_brb = din("p_brb", [128, 36])
    p_wr = din("p_wr", [128, 8, 36])
    k_identb = din("k_identb", [128, 128], BF16)
    k_identq = din("k_identq", [128, 512], BF16)
    k_identf = din("k_identf", [128, 128])
    k_onesb = din("k_onesb", [128, 128], BF16)
    k_onesf = din("k_onesf", [128, 128])
    k_triU = din("k_triU", [128, 128])
    k_maskT = din("k_maskT", [128, 128])
    k_maskS = din("k_maskS", [128, 128])
    k_triS = din("k_triS", [128, 128], BF16)
    k_pcorr = din("k_pcorr", [128, 4, 16])
    k_ebase = din("k_ebase", [128, 32])
    k_bmask = din("k_bmask", [128, 7, 128], BF16)
    k_bmaskT = din("k_bmaskT", [128, 7, 128], BF16)
    k_elim = din("k_elim", [128, 32])

    out = nc.dram_tensor("out", [TOK, D], F32, kind="ExternalOutput").ap()
    dbg_t = None
    if dbg is not None:
        dbg_t = nc.dram_tensor("dbg", list(dbg), F32, kind="ExternalOutput").ap()
    modD = nc.dram_tensor("modD", [2, 6 * D], F32).ap()
    wsD = nc.dram_tensor("wsD", [8, 128, 4096], BF16).ap()
    h1D = nc.dram_tensor("h1D", [TOK, D], F32).ap()
    xsD = nc.dram_tensor("xsD", [E * CAP, D], BF16).ap()
    ysD = nc.dram_tensor("ysD", [E * CAP + 128, D], F32).ap()

    es = ExitStack()
    with es:
        em = Em(nc, es)

        def sb(name, shape, dt=F32, stack=es):
            return stack.enter_context(nc.sbuf_tensor(name, list(shape), dt))

        pregs = {}
        em.streams['pool'].append(lambda e: pregs.__setitem__('bc', e.to_reg(E * CAP - 1)))

        psb = [es.enter_context(nc.psum_tensor('ps%d' % i, [128, 512], F32)) for i in range(8)]
        pstate = {'i': 0}

        def ps():
            i = pstate['i']
            pstate['i'] = (i + 1) % 8
            return psb[i][:], psb[i][:].bitcast(BF16), ('ps', i)

        identb = sb('identb', [128, 128], BF16)
        identq = sb('identq', [128, 512], BF16)
        identf = sb('identf', [128, 128])
        onesb = sb('onesb', [128, 128], BF16)
        onesf = sb('onesf', [128, 128])
        triU = sb('triU', [128, 128])
        maskT = sb('maskT', [128, 128])
        maskS = sb('maskS', [128, 128])
        triS = sb('triS', [128, 128], BF16)
        pcorr = sb('pcorr', [128, 4, 16])
        bmask = sb('bmask', [128, 7, 128], BF16)
        bmaskT = sb('bmaskT', [128, 7, 128], BF16)
        n1g = sb('n1g', [128, 8])
        convw = sb('convw', [128, 12, 4])
        alog = sb('alog', [128, 4])
        dtb = sb('dtb', [128, 4])
        dng = sb('dng', [128, 1])
        pscale = sb('pscale', [128, 4])
        brb = sb('brb', [128, 36])
        wr = sb('wr', [128, 8, 36])
        cum = sb('cum', [128, 32])
        elim = sb('elim', [128, 32])
        destall = sb('destall', [128, 64], I32)
        gidxall = sb('gidxall', [128, 64], I32)
        wall = sb('wall', [128, 32, 2])
        cst = [(identb, k_identb), (identq, k_identq), (identf, k_identf), (onesb, k_onesb), (onesf, k_onesf),
               (triU, k_triU), (maskT, k_maskT), (maskS, k_maskS), (triS, k_triS), (pcorr, k_pcorr),
               (n1g, p_n1g), (convw, p_convw), (alog, p_alog), (dtb, p_dtb), (dng, p_dng), (pscale, p_pscale),
               (brb, p_brb), (wr, p_wr), (cum, k_ebase), (elim, k_elim), (bmask, k_bmask), (bmaskT, k_bmaskT)]
        for n_, (t_, src_) in enumerate(cst):
            em.dma('sp', lambda e, t_=t_, src_=src_: e.dma_start(out=t_[:], in_=src_), w=[('c', n_)])
        CK = [('c', n_) for n_ in range(len(cst))]
        cidx = {id(t_): ('c', n_) for n_, (t_, _) in enumerate(cst)}

        def ck(*ts):
            return [cidx[id(t)] for t in ts]

        small = sb('small', [128, 40 * 32])
        smi = {'i': 0}

        def sm(n=1):
            assert n <= 32
            i = smi['i']
            smi['i'] = (i + 1) % 40
            return small[:, i * 32:i * 32 + n], ('sm', i)

        epsc = sb('epsc', [128, 4])
        ctf_t = sb('ctf_t', [128, 16])
        scb_t = sb('scb_t', [128, 16], BF16)
        em.op('dve', lambda e: e.memset(epsc[:, 0:1], EPS), w=['epsc'])
        em.op('dve', lambda e: e.memset(epsc[:, 1:2], 0.0), w=['epsc'])
        em.op('dve', lambda e: e.memset(epsc[:, 2:3], 1.0), w=['epsc'])

        p1 = ExitStack()
        es.enter_context(p1)
        winb = sb('winb', [128, 8, WIN_RES], BF16, p1)
        poolwb = sb('poolwb', [128, 4, 128], BF16, p1)
        wst = [sb('wst%d' % i, [128, 4096], BF16, p1) for i in range(4)]
        wsi = {'i': 0}
        G2b = sb('G2b', [128, D], F32, p1)
        SH2b = sb('SH2b', [128, D], F32, p1)
        GT1b = sb('GT1b', [128, D], F32, p1)
        modp = sb('modp', [128, 2, 2, 8], F32, p1)
        r2k = Ring(nc, p1, 'r2k', 7, 2048)
        r4k = Ring(nc, p1, 'r4k', 4, 4096)
        nq = Ring(nc, p1, 'nq', 12, 1024)
        xnT = sb('xnT', [128, 8, BLK], BF16, p1)
        qT = sb('qT', [128, 4, BLK], BF16, p1)
        kT = sb('kT', [128, 4, BLK], BF16, p1)
        vT = sb('vT', [128, 4, BLK], BF16, p1)
        szT = sb('szT', [128, 4, BLK], BF16, p1)
        puT = sb('puT', [128, 4, 16 + BLK], F32, p1)
        ybT = sb('ybT', [128, 4, BLK], BF16, p1)
        yaT = sb('yaT', [128, 4, BLK], BF16, p1)
        mixedT = sb('mixedT', [128, 8, BLK], BF16, p1)
        halo = sb('halo', [128, 12, 4], F32, p1)
        gtm = sb('gtm', [128, 4, 8], F32, p1)
        S = sb('S', [128, 4, 128], F32, p1)
        Sb = sb('Sb', [128, 4, 128], BF16, p1)
        cq = {n_: sb('cq_' + n_, [128, 4, 128], BF16, p1) for n_ in ('DT', 'Ds', 'Er', 'kbd', 'kdec', 'vb', 'N0', 'Pt0')}
        xn2T = sb('xn2T', [128, 8, 128], F32, p1)
        lgb = sb('lgb', [128, 4, 36], F32, p1)

        w_in_v = w_in.rearrange("(c p) n -> p c n", p=128)
        for kc in range(8):
            for hf in range(2):
                c0 = hf * (WIN_RES // 2)
                c1 = c0 + WIN_RES // 2
                em.dma('pool', lambda e, kc=kc, c0=c0, c1=c1: e.dma_start(out=winb[:, kc, c0:c1], in_=w_in_v[:, kc, c0:c1]),
                       w=[('winb', kc)])
        WINK = [('winb', kc) for kc in range(8)]
        em.dma('pool', lambda e: e.dma_start(out=poolwb[:], in_=pool_w.rearrange("g c d -> c g d")), w=['poolwb'])

        kctf, kscb = 'ctf', 'scb'
        em.dma('sp', lambda e: e.dma_start(out=ctf_t[:, 0:16], in_=cT.rearrange("p c b -> p (c b)")), w=[kctf])
        em.op('act', lambda e: e.activation(out=scb_t[:, 0:16], in_=ctf_t[:, 0:16], func=AF.Silu), r=[kctf], w=[kscb])
        scb = scb_t[:, 0:16].rearrange("p (c b) -> p c b", b=2)

        def wpiece_load(src_ap_fn, wkey):
            i = wsi['i']
            wsi['i'] = (i + 1) % 4
            buf = wst[i]
            em.dma('pool', lambda e: src_ap_fn(e, buf), w=[('wst', i)])
            return buf, ('wst', i)

        w_ada_v = w_ada.rearrange("(c p) n -> p c n", p=128)
        for nt in range(12):
            buf, kb = wpiece_load(lambda e, buf, nt=nt: e.dma_start(
                out=buf[:].rearrange("p (c n) -> p c n", n=512), in_=w_ada_v[:, :, nt * 512:(nt + 1) * 512]), None)
            bt, kbt = r2k.get()
            em.dma('sp', lambda e, bt=bt, nt=nt: e.dma_start(out=bt[0:2, :], in_=b_ada[0:1, nt * 512:(nt + 1) * 512].partition_broadcast(2)),
                   w=[kbt])
            pf, pb, kp = ps()
            bv = buf[:].rearrange("p (c n) -> p c n", n=512)
            em.mm([lambda e, kc=kc, pf=pf, bv=bv: e.matmul(pf[0:2, :], lhsT=scb[:, kc, :], rhs=bv[:, kc, :], start=(kc == 0), stop=(kc == 7))
                   for kc in range(8)], r=[kscb, kb], w=[kp])
            mr, kmr = r2k.get()
            em.op('dve', lambda e, mr=mr, pf=pf, bt=bt: e.tensor_tensor(out=mr[0:2, :], in0=pf[0:2, :], in1=bt[0:2, :], op=ALU.add),
                  r=[kp, kbt], w=[kmr])
            em.dma('sp', lambda e, mr=mr, nt=nt: e.dma_start(out=modD[:, nt * 512:(nt + 1) * 512], in_=mr[0:2, :]), r=[kmr], w=[('modD', nt)])

        w_la_v = w_la.rearrange("(c p) n -> p c n", p=128)
        w_lb_v = w_lb.rearrange("(c p) n -> p c n", p=128)
        w_out_v = w_out.rearrange("(c p) n -> p c n", p=128)
        piece_src = []
        for i in range(4):
            c0 = WIN_RES + i * 512
            piece_src.append((w_in_v[:, :, c0:c0 + 512], 512))
        piece_src.append((w_la_v, 1024))
        piece_src.append((w_lb_v, 1024))
        piece_src.append((w_out_v[:, :, 0:512], 512))
        piece_src.append((w_out_v[:, :, 512:1024], 512))
        for pi, (src_, n_) in enumerate(piece_src):
            buf, kb = wpiece_load(lambda e, buf, src_=src_, n_=n_: e.dma_start(out=buf[:].rearrange("p (c n) -> p c n", n=n_), in_=src_), None)
            em.dma('sp', lambda e, buf=buf, pi=pi: e.dma_start(out=wsD[pi], in_=buf[:]), r=[kb], w=[('wsD', pi)])

        def wpiece(pi, i):
            buf = wst[i]
            em.dma('sp', lambda e: e.dma_start(out=buf[:], in_=wsD[pi]), r=[('wsD', pi)], w=[('wst', i)])
            return buf, ('wst', i)

        MODK = [('modD', nt) for nt in range(12)]

        def load_seq_mod(seq):
            sh1 = modD[seq, 0:1024].rearrange("(c p) -> p c", p=128)
            sc1 = modD[seq, 1024:2048].rearrange("(c p) -> p c", p=128)
            tmp, kt = sm(8)
            em.dma('sp', lambda e: e.dma_start(out=modp[:, seq, 1, :], in_=sh1, allow_slow_non_contiguous=True), r=MODK, w=[('modp', seq, 1)])
            em.dma('sp', lambda e: e.dma_start(out=tmp, in_=sc1, allow_slow_non_contiguous=True), r=MODK, w=[kt])
            em.op('dve', lambda e: e.scalar_tensor_tensor(out=modp[:, seq, 0, :], in0=tmp, scalar=1.0, in1=n1g[:], op0=ALU.add, op1=ALU.mult),
                  r=[kt] + ck(n1g), w=[('modp', seq, 0)])
            em.dma('sp', lambda e: e.dma_start(out=GT1b[:], in_=modD[seq:seq + 1, 2048:3072].partition_broadcast(128)), r=MODK, w=['GT1b'])
            em.dma('sp', lambda e: e.dma_start(out=SH2b[:], in_=modD[seq:seq + 1, 3072:4096].partition_broadcast(128)), r=MODK, w=['SH2b'])
            t4, k4 = r4k.get()
            em.dma('sp', lambda e: e.dma_start(out=t4, in_=modD[seq:seq + 1, 4096:5120].partition_broadcast(128)), r=MODK, w=[k4])
            n2, kn2 = r4k.get()
            em.dma('sp', lambda e: e.dma_start(out=n2, in_=p_n2g), w=[kn2])
            em.op('dve', lambda e: e.scalar_tensor_tensor(out=G2b[:], in0=t4, scalar=1.0, in1=n2, op0=ALU.add, op1=ALU.mult),
                  r=[k4, kn2], w=['G2b'])

        def rstd_from_ss(ss_ap, kss, scale):
            l1, kl1 = sm(1)
            em.op('act', lambda e: e.activation(out=l1, in_=ss_ap, func=AF.Ln, bias=epsc[:, 0:1], scale=scale), r=[kss, 'epsc'], w=[kl1])
            r1, kr1 = sm(1)
            em.op('act', lambda e: e.activation(out=r1, in_=l1, func=AF.Exp, scale=-0.5), r=[kl1], w=[kr1])
            return r1, kr1


        def proj_fm(col0, ncols=128):
            pf, pb, kp = ps()
            em.mm([lambda e, kc=kc, pf=pf: e.matmul(pf[0:ncols, :], lhsT=winb[:, kc, col0:col0 + ncols], rhs=xnT[:, kc, :],
                                                     start=(kc == 0), stop=(kc == 7)) for kc in range(8)],
                  r=WINK + ['xnT'], w=[kp])
            return pf, kp

        dbg_state = {'off': 0}

        def dump(ap_f32_128xN, keys, n):
            if dbg_t is None:
                return
            o = dbg_state['off']
            dbg_state['off'] = o + n
            em.dma('sp', lambda e: e.dma_start(out=dbg_t[:, o:o + n], in_=ap_f32_128xN), r=keys, w=[('dbg', o)])

        def dump_any(ap, keys, n):
            if dbg_t is None:
                return
            t, kt = r2k.get()
            em.op('dve', lambda e: e.tensor_copy(out=t[:, 0:n], in_=ap), r=keys, w=[kt])
            dump(t[:, 0:n], [kt], n)

        try:
            pending_e2 = None
            for b in range(NBLK):
                seq, blk = divmod(b, NBLK // 2)
                t0 = b * BLK
                first = (blk == 0)
                if first and pending_e2 is not None:
                    run_il((pending_e2, 1))
                    pending_e2 = None
                if first:
                    load_seq_mod(seq)
                    em.op('pool', lambda e: e.memset(S[:], 0.0), w=['S'])
                    em.op('pool', lambda e: e.memset(Sb[:], 0.0), w=['Sb'])
                    em.op('pool', lambda e: e.memset(halo[:], 0.0), w=['halo'])
                    em.op('pool', lambda e: e.memset(puT[:, :, 0:16], 0.0), w=['puT'])
                def gen_AB(b=b, seq=seq, blk=blk, t0=t0, first=first):
                    for j in range(4):
                        yield
                        xin, kx = r4k.get()
                        em.dma('sp', lambda e, xin=xin, j=j: e.dma_start(out=xin, in_=x[t0 + j * 128:t0 + (j + 1) * 128, :]), w=[kx])
                        junk, kj = r2k.get(BF16)
                        ss, kss = sm(1)
                        em.op('act', lambda e, xin=xin, junk=junk, ss=ss: e.activation(out=junk, in_=xin, func=AF.Square, accum_out=ss), r=[kx], w=[kj, kss])
                        rs, krs = rstd_from_ss(ss, kss, 1.0 / D)
                        xsb, kxs = r2k.get(BF16)
                        em.op('dve', lambda e, xsb=xsb, xin=xin, rs=rs: e.tensor_scalar(out=xsb, in0=xin, scalar1=rs, scalar2=None, op0=ALU.mult),
                              r=[kx, krs], w=[kxs])
                        pf, pb, kp = ps()
                        em.mm([lambda e, kc=kc, pb=pb, xsb=xsb: e.transpose(pb[:, kc * 128:(kc + 1) * 128], xsb[:, kc * 128:(kc + 1) * 128], identb[:])
                               for kc in range(8)], r=[kxs] + ck(identb), w=[kp])
                        for kc in range(8):
                            em.op('act', lambda e, kc=kc, pb=pb, j=j: e.activation(
                                out=xnT[:, kc, j * 128:(j + 1) * 128], in_=pb[:, kc * 128:(kc + 1) * 128], func=AF.Identity,
                                scale=modp[:, seq, 0, kc:kc + 1], bias=modp[:, seq, 1, kc:kc + 1]),
                                r=[kp, ('modp', seq, 0), ('modp', seq, 1)], w=['xnT'])
                    if dbg_t is not None and b == 0 and 'xnT' in DBGSEL:
                        for kc in range(8):
                            dump_any(xnT[:, kc, :], ['xnT'], 512)

                    if b == 0:
                        chk('A')
                    for ct in range(12):
                        yield
                        pf, kp = proj_fm(ct * 128)
                        pre, kpre = r4k.get()
                        em.op('pool', lambda e, pre=pre, ct=ct: e.tensor_copy(out=pre[:, 0:4], in_=halo[:, ct, :]), r=['halo'], w=[kpre])
                        em.op('act', lambda e, pre=pre, pf=pf: e.activation(out=pre[:, 4:516], in_=pf, func=AF.Copy), r=[kp], w=[kpre])
                        em.op('pool', lambda e, pre=pre, ct=ct: e.tensor_copy(out=halo[:, ct, :], in_=pre[:, 512:516]), r=[kpre], w=['halo'])
                        acc, kacc = r2k.get()
                        em.op('act', lambda e, acc=acc, pf=pf, ct=ct: e.activation(out=acc, in_=pf, func=AF.Copy, scale=convw[:, ct, 3:4]),
                              r=[kp] + ck(convw), w=[kacc])
                        for tap in (2, 1, 0):
                            sh = 3 - tap
                            em.op('dve', lambda e, acc=acc, pre=pre, ct=ct, tap=tap, sh=sh: e.scalar_tensor_tensor(
                                out=acc, in0=pre[:, 4 - sh:516 - sh], scalar=convw[:, ct, tap:tap + 1], in1=acc, op0=ALU.mult, op1=ALU.add),
                                r=[kpre, kacc] + ck(convw), w=[kacc])
                        h = ct % 4
                        if ct >= 8:
                            em.op('act', lambda e, acc=acc, h=h: e.activation(out=vT[:, h, :], in_=acc, func=AF.Silu), r=[kacc], w=['vT'])
                        else:
                            sil, ksil = acc, kacc
                            em.op('act', lambda e, acc=acc, sil=sil: e.activation(out=sil, in_=acc, func=AF.Silu), r=[kacc], w=[ksil])
                            sq, ksq = r2k.get(BF16)
                            em.op('act', lambda e, sq=sq, sil=sil: e.activation(out=sq[:, 0:512], in_=sil, func=AF.Square), r=[ksil], w=[ksq])
                            pf2, pb2, kp2 = ps()
                            em.mm([lambda e, pf2=pf2, sq=sq: e.matmul(pf2, lhsT=onesb[:], rhs=sq[:, 0:512], start=True, stop=True)],
                                  r=[ksq] + ck(onesb), w=[kp2])
                            lnv, kln = r2k.get()
                            em.op('act', lambda e, lnv=lnv, pf2=pf2: e.activation(out=lnv, in_=pf2, func=AF.Ln, bias=epsc[:, 0:1]), r=[kp2, 'epsc'], w=[kln])
                            rinv, kri = lnv, kln
                            qs = (128.0 ** -0.5) if ct < 4 else 1.0
                            em.op('act', lambda e, rinv=rinv, lnv=lnv: e.activation(out=rinv, in_=lnv, func=AF.Exp, scale=-0.5), r=[kln], w=[kri])
                            dst = qT if ct < 4 else kT
                            dk = 'qT' if ct < 4 else 'kT'
                            em.op('dve', lambda e, dst=dst, h=h, sil=sil, rinv=rinv, qs=qs: e.scalar_tensor_tensor(
                                out=dst[:, h, :], in0=sil, scalar=qs, in1=rinv, op0=ALU.mult, op1=ALU.mult), r=[ksil, kri], w=[dk])
                    if b == 0 and dbg_t is not None and 'B' in DBGSEL:
                        for h_ in range(4):
                            dump_any(qT[:, h_, :], ['qT'], 512)
                        for h_ in range(4):
                            dump_any(kT[:, h_, :], ['kT'], 512)
                        for h_ in range(4):
                            dump_any(vT[:, h_, :], ['vT'], 512)
                    if b == 0:
                        chk('B')
                    for h in range(4):
                        yield
                        pf, kp = proj_fm(1536 + h * 128)
                        em.op('act', lambda e, pf=pf, h=h: e.activation(out=szT[:, h, :], in_=pf, func=AF.Silu), r=[kp], w=['szT'])
                    for c in range(4):
                        yield
                        pf, pb, kp = ps()
                        em.mm([lambda e, kc=kc, pf=pf, c=c: e.matmul(pf[:, 0:8], lhsT=xnT[:, kc, c * 128:(c + 1) * 128], rhs=winb[:, kc, 2048:2056],
                                                                      start=(kc == 0), stop=(kc == 7)) for kc in range(8)],
                              r=WINK + ['xnT'], w=[kp])
                        em.op('act', lambda e, pf=pf, c=c: e.activation(out=gtm[:, c, 4:8], in_=pf[:, 4:8], func=AF.Sigmoid), r=[kp], w=[('gtm', c)])
                        xa, kxa = sm(4)
                        em.op('dve', lambda e, xa=xa, pf=pf: e.tensor_tensor(out=xa, in0=pf[:, 0:4], in1=dtb[:], op=ALU.add), r=[kp] + ck(dtb), w=[kxa])
                        ab, kab = sm(4)
                        em.op('act', lambda e, ab=ab, xa=xa: e.activation(out=ab, in_=xa, func=AF.Abs), r=[kxa], w=[kab])
                        ex, kex = sm(4)
                        em.op('act', lambda e, ex=ex, ab=ab: e.activation(out=ex, in_=ab, func=AF.Exp, scale=-1.0), r=[kab], w=[kex])
                        l1p, kl1p = sm(4)
                        em.op('act', lambda e, l1p=l1p, ex=ex: e.activation(out=l1p, in_=ex, func=AF.Ln, bias=epsc[:, 2:3]), r=[kex, 'epsc'], w=[kl1p])
                        sp_, ksp = sm(4)
                        em.op('dve', lambda e, sp_=sp_, xa=xa, l1p=l1p: e.scalar_tensor_tensor(out=sp_, in0=xa, scalar=0.0, in1=l1p, op0=ALU.max, op1=ALU.add),
                              r=[kxa, kl1p], w=[ksp])
                        ea, kea = sm(4)
                        em.op('act', lambda e, ea=ea: e.activation(out=ea, in_=alog[:], func=AF.Exp), r=ck(alog), w=[kea])
                        em.op('dve', lambda e, c=c, sp_=sp_, ea=ea: e.scalar_tensor_tensor(out=gtm[:, c, 0:4], in0=sp_, scalar=-1.0, in1=ea, op0=ALU.mult, op1=ALU.mult),
                              r=[ksp, kea], w=[('gtm', c)])

                    if b == 0 and dbg_t is not None and 'B2' in DBGSEL:
                        dump(gtm[:].rearrange('p a b -> p (a b)'), [('gtm', c_) for c_ in range(4)], 32)
                    if b == 0:
                        chk('B2')
                    yield
                def gen_C(b=b, seq=seq, blk=blk, t0=t0, first=first):
                    for gi, win in enumerate((2, 4, 8, 16)):
                        yield
                        pf, kp = proj_fm(2056 + gi * 128)
                        em.op('act', lambda e, pf=pf, gi=gi: e.activation(out=puT[:, gi, 16:16 + BLK], in_=pf, func=AF.Copy), r=[kp], w=['puT'])
                        cur = puT[:, gi, :]
                        kcur = 'puT'
                        w_ = 1
                        nxt = None
                        while w_ < win:
                            nxt, knxt = r4k.get()
                            em.op('pool', lambda e, nxt=nxt, cur=cur, w_=w_: e.tensor_tensor(out=nxt[:, w_:16 + BLK], in0=cur[:, w_:16 + BLK], in1=cur[:, 0:16 + BLK - w_], op=ALU.add),
                                  r=[kcur], w=[knxt])
                            cur, kcur = nxt, knxt
                            w_ *= 2
                        pl, kpl = r2k.get()
                        em.op('dve', lambda e, pl=pl, cur=cur, gi=gi, win=win: e.scalar_tensor_tensor(
                            out=pl, in0=cur[:, 16:16 + BLK], scalar=1.0 / win, in1=puT[:, gi, 16:16 + BLK], op0=ALU.mult, op1=ALU.subtract),
                            r=[kcur, 'puT'], w=[kpl])
                        if first:
                            t15, k15 = sm(16)
                            em.op('dve', lambda e, t15=t15, cur=cur, gi=gi: e.tensor_tensor(out=t15, in0=cur[:, 16:32], in1=pcorr[:, gi, :], op=ALU.mult),
                                  r=[kcur] + ck(pcorr), w=[k15])
                            em.op('dve', lambda e, t15=t15, pl=pl, gi=gi: e.tensor_tensor(out=pl[:, 0:16], in0=t15, in1=puT[:, gi, 16:32], op=ALU.subtract),
                                  r=[k15, 'puT'], w=[kpl])
                        plb, kplb = r2k.get(BF16)
                        em.op('pool', lambda e, plb=plb, pl=pl: e.tensor_copy(out=plb[:, 0:BLK], in_=pl), r=[kpl], w=[kplb])
                        pf2, pb2, kp2 = ps()
                        em.mm([lambda e, pf2=pf2, plb=plb, gi=gi: e.matmul(pf2, lhsT=poolwb[:, gi, :], rhs=plb[:, 0:BLK], start=True, stop=True)],
                              r=[kplb, 'poolwb'], w=[kp2])
                        em.op('act', lambda e, pf2=pf2, gi=gi: e.activation(out=ybT[:, gi, :], in_=pf2, func=AF.Copy, scale=pscale[:, gi:gi + 1]),
                              r=[kp2] + ck(pscale), w=['ybT'])
                        em.op('pool', lambda e, gi=gi: e.tensor_copy(out=puT[:, gi, 0:16], in_=puT[:, gi, BLK:BLK + 16]), r=['puT', kpl], w=['puT'])

                    if b == 0:
                        chk('C')
                    yield
                def gen_D(b=b, seq=seq, blk=blk, t0=t0, first=first):
                    for c in range(4):
                        tsl = slice(c * 128, (c + 1) * 128)
                        g4 = gtm[:, c, 0:4]
                        b4 = gtm[:, c, 4:8]
                        kg = ('gtm', c)
                        pkf, pkb, kpk = ps()
                        em.mm([lambda e, h=h, pkb=pkb: e.transpose(pkb[:, h * 128:(h + 1) * 128], kT[:, h, tsl], identb[:]) for h in range(4)],
                              r=['kT'] + ck(identb), w=[kpk])
                        pvf, pvb, kpv = ps()
                        em.mm([lambda e, h=h, pvb=pvb: e.transpose(pvb[:, h * 128:(h + 1) * 128], vT[:, h, tsl], identb[:]) for h in range(4)],
                              r=['vT'] + ck(identb), w=[kpv])
                        Gt, kGt = r2k.get()
                        for h in range(4):
                            em.op('pool', lambda e, h=h, Gt=Gt: e.tensor_scalar(out=Gt[:, h * 128:(h + 1) * 128], in0=triU[:], scalar1=g4[:, h:h + 1], scalar2=1.0,
                                                                                op0=ALU.mult, op1=ALU.mult), r=[kg] + ck(triU), w=[kGt])
                        pgr, _, kpgr = ps()
                        em.mm([lambda e, pgr=pgr, Gt=Gt: e.matmul(pgr, lhsT=onesf[:], rhs=Gt, start=True, stop=True)], r=[kGt] + ck(onesf), w=[kpgr])
                        pgc, _, kpgc = ps()
                        em.mm([lambda e, pgc=pgc: e.matmul(pgc[:, 0:4], lhsT=triU[:], rhs=g4, start=True, stop=True)], r=[kg] + ck(triU), w=[kpgc])
                        pgl, _, kpgl = ps()
                        em.mm([lambda e, pgl=pgl: e.matmul(pgl[:, 0:4], lhsT=onesf[:], rhs=g4, start=True, stop=True)], r=[kg] + ck(onesf), w=[kpgl])
                        gcc8, kgcc = sm(8)
                        em.op('act', lambda e, gcc8=gcc8, pgc=pgc: e.activation(out=gcc8[:, 0:4], in_=pgc[:, 0:4], func=AF.Copy), r=[kpgc], w=[kgcc])
                        em.op('act', lambda e, gcc8=gcc8, pgl=pgl: e.activation(out=gcc8[:, 4:8], in_=pgl[:, 0:4], func=AF.Copy), r=[kpgl, kgcc], w=[kgcc])
                        gcc = gcc8[:, 0:4]
                        glv = gcc8[:, 4:8]
                        DTl, kDTl = r2k.get()
                        Dsl, kDsl = r2k.get()
                        for h in range(4):
                            hs = slice(h * 128, (h + 1) * 128)
                            em.op('dve', lambda e, hs=hs, h=h, DTl=DTl, pgr=pgr, gcc=gcc: e.scalar_tensor_tensor(
                                out=DTl[:, hs], in0=pgr[:, hs], scalar=gcc[:, h:h + 1], in1=maskT[:], op0=ALU.subtract, op1=ALU.add),
                                r=[kpgr, kgcc] + ck(maskT), w=[kDTl])
                            em.op('dve', lambda e, hs=hs, h=h, Dsl=Dsl, pgr=pgr, gcc=gcc: e.scalar_tensor_tensor(
                                out=Dsl[:, hs], in0=pgr[:, hs], scalar=gcc[:, h:h + 1], in1=maskS[:], op0=ALU.subtract, op1=ALU.add),
                                r=[kpgr, kgcc] + ck(maskS), w=[kDsl])
                        DT, Ds, Er = cq['DT'], cq['Ds'], cq['Er']
                        fl = lambda t: t[:].rearrange("p h n -> p (h n)")
                        em.op('act', lambda e: e.activation(out=fl(DT), in_=DTl, func=AF.Exp), r=[kDTl], w=['DT'])
                        em.op('act', lambda e: e.activation(out=fl(Ds), in_=Dsl, func=AF.Exp, scale=-1.0), r=[kDsl], w=['Ds'])
                        em.op('act', lambda e, pgr=pgr: e.activation(out=fl(Er), in_=pgr, func=AF.Exp), r=[kpgr], w=['Er'])
                        if b == 0 and c == 0:
                            chk('D1')
                        egc, kegc = sm(4)
                        em.op('act', lambda e, egc=egc, gcc=gcc: e.activation(out=egc, in_=gcc, func=AF.Exp), r=[kgcc], w=[kegc])
                        kbs, kkbs = sm(4)
                        em.op('dve', lambda e, kbs=kbs, egc=egc: e.tensor_tensor(out=kbs, in0=egc, in1=b4, op=ALU.mult), r=[kegc, kg], w=[kkbs])
                        dl, kdl = sm(4)
                        em.op('dve', lambda e, dl=dl, glv=glv, gcc=gcc: e.tensor_tensor(out=dl, in0=glv, in1=gcc, op=ALU.subtract), r=[kgcc], w=[kdl])
                        ekd, kekd = sm(4)
                        em.op('act', lambda e, ekd=ekd, dl=dl: e.activation(out=ekd, in_=dl, func=AF.Exp), r=[kdl], w=[kekd])
                        egl, kegl = sm(4)
                        em.op('act', lambda e, egl=egl, glv=glv: e.activation(out=egl, in_=glv, func=AF.Exp), r=[kgcc], w=[kegl])
                        nb4, knb4 = sm(4)
                        em.op('dve', lambda e, nb4=nb4: e.tensor_scalar(out=nb4, in0=b4, scalar1=-1.0, scalar2=None, op0=ALU.mult), r=[kg], w=[knb4])
                        if b == 0 and c == 0:
                            chk('D1b')
                        kbd, kdec, vb = cq['kbd'], cq['kdec'], cq['vb']
                        for h in range(4):
                            hs = slice(h * 128, (h + 1) * 128)
                            em.op('act', lambda e, h=h, hs=hs, pkb=pkb, kbs=kbs: e.activation(out=kbd[:, h, :], in_=pkb[:, hs], func=AF.Identity, scale=kbs[:, h:h + 1], bias=epsc[:, 1:2]),
                                  r=[kpk, kkbs], w=['kbd'])
                            em.op('act', lambda e, h=h, hs=hs, pkb=pkb, ekd=ekd: e.activation(out=kdec[:, h, :], in_=pkb[:, hs], func=AF.Identity, scale=ekd[:, h:h + 1], bias=epsc[:, 1:2]),
                                  r=[kpk, kekd], w=['kdec'])
                            em.op('act', lambda e, h=h, hs=hs, pvb=pvb: e.activation(out=vb[:, h, :], in_=pvb[:, hs], func=AF.Identity, scale=b4[:, h:h + 1], bias=epsc[:, 1:2]),
                                  r=[kpv, kg], w=['vb'])
                        if b == 0 and c == 0:
                            chk('D2')
                        pkk, _, kpkk = ps()
                        em.mm([lambda e, h=h, pkk=pkk: e.matmul(pkk[:, h * 128:(h + 1) * 128], lhsT=kT[:, h, tsl], rhs=kT[:, h, tsl], start=True, stop=True) for h in range(4)],
                              r=['kT'], w=[kpkk])
                        N0 = cq['N0']
                        for h in range(4):
                            hs = slice(h * 128, (h + 1) * 128)
                            em.op('dve', lambda e, h=h, hs=hs, pkk=pkk, nb4=nb4: e.scalar_tensor_tensor(
                                out=N0[:, h, :], in0=pkk[:, hs], scalar=nb4[:, h:h + 1], in1=Ds[:, h, :], op0=ALU.mult, op1=ALU.mult),
                                r=[kpkk, knb4, 'Ds'], w=['N0'])
                        ptf, ptb, kpt = ps()
                        em.mm([lambda e, h=h, ptb=ptb: e.transpose(ptb[:, h * 128:(h + 1) * 128], N0[:, h, :], identb[:]) for h in range(4)],
                              r=['N0'] + ck(identb), w=[kpt])
                        Pt0 = cq['Pt0']
                        em.op('act', lambda e, ptb=ptb: e.activation(out=fl(Pt0), in_=ptb[:, 0:512], func=AF.Identity, scale=1.0, bias=epsc[:, 1:2]), r=[kpt, 'epsc'], w=['Pt0'])
                        Tt, kTt = nq.get(BF16)
                        if b == 0 and c == 0 and dbg_t is not None and 'D3' in DBGSEL:
                            dump_any(fl(DT), ['DT'], 512)
                            dump_any(fl(Ds), ['Ds'], 512)
                            dump_any(fl(N0), ['N0'], 512)
                        if b == 0 and c == 0:
                            chk('D3')
                        Tq, kTq = nq.get(BF16)
                        Cm, kCm = nq.get(BF16)
                        for h in range(4):
                            hs = slice(h * 128, (h + 1) * 128)
                            em.op('dve', lambda e, h=h, hs=hs, Cm=Cm: e.tensor_tensor(out=Cm[:, hs], in0=N0[:, h, :], in1=bmask[:, 0, :], op=ALU.mult), r=['N0'] + ck(bmask), w=[kCm])
                        em.op('dve', lambda e, Tq=Tq, Cm=Cm: e.tensor_tensor(out=Tq, in0=Cm, in1=identq[:], op=ALU.add), r=[kCm] + ck(identq), w=[kTq])
                        Cmt, kCmt = nq.get(BF16)
                        for h in range(4):
                            hs = slice(h * 128, (h + 1) * 128)
                            em.op('dve', lambda e, h=h, hs=hs, Cmt=Cmt: e.tensor_tensor(out=Cmt[:, hs], in0=Pt0[:, h, :], in1=bmaskT[:, 0, :], op=ALU.mult), r=['Pt0'] + ck(bmaskT), w=[kCmt])
                        em.op('dve', lambda e, Tt=Tt, Cmt=Cmt: e.tensor_tensor(out=Tt, in0=Cmt, in1=identq[:], op=ALU.add), r=[kCmt, kTt] + ck(identq), w=[kTt])
                        if b == 0 and c == 0 and dbg_t is not None and 'D3b' in DBGSEL:
                            dump_any(Cm, [kCm], 512)
                            dump_any(Tq, [kTq], 512)
                            dump_any(Tt, [kTt], 512)
                            dump_any(bmask[:, :, :].rearrange('p a b -> p (a b)')[:, 0:512], ck(bmask), 512)
                        if b == 0 and c == 0:
                            chk('D3b')
                        def masks(lev_):
                            Cm_, kCm_ = nq.get(BF16)
                            Cmt_, kCmt_ = nq.get(BF16)
                            for h in range(4):
                                hs = slice(h * 128, (h + 1) * 128)
                                em.op('dve', lambda e, h=h, hs=hs: e.tensor_tensor(out=Cm_[:, hs], in0=N0[:, h, :], in1=bmask[:, lev_, :], op=ALU.mult), r=['N0'] + ck(bmask), w=[kCm_])
                                if lev_ < 6:
                                    em.op('dve', lambda e, h=h, hs=hs: e.tensor_tensor(out=Cmt_[:, hs], in0=Pt0[:, h, :], in1=bmaskT[:, lev_, :], op=ALU.mult), r=['Pt0'] + ck(bmaskT), w=[kCmt_])
                            return Cm_, kCm_, Cmt_, kCmt_

                        nxt_masks = masks(1)
                        for lev in range(1, 7):
                            yield
                            Cm, kCm, Cmt, kCmt = nxt_masks
                            last = (lev == 6)
                            px2, _, kpx2 = ps()
                            em.mm([lambda e, h=h, px2=px2, Cm=Cm, Tt=Tt: e.matmul(px2[:, h * 128:(h + 1) * 128], lhsT=Cm[:, h * 128:(h + 1) * 128], rhs=Tt[:, h * 128:(h + 1) * 128], start=True, stop=True)
                                   for h in range(4)], r=[kCm, kTt], w=[kpx2])
                            if lev < 6:
                                nxt_masks = masks(lev + 1)
                            Xs2, kXs2 = nq.get(BF16)
                            em.op('act', lambda e, Xs2=Xs2, px2=px2: e.activation(out=Xs2, in_=px2, func=AF.Copy), r=[kpx2], w=[kXs2])
                            if not last:
                                px1, _, kpx1 = ps()
                                em.mm([lambda e, h=h, px1=px1, Cmt=Cmt, Tq=Tq: e.matmul(px1[:, h * 128:(h + 1) * 128], lhsT=Cmt[:, h * 128:(h + 1) * 128], rhs=Tq[:, h * 128:(h + 1) * 128], start=True, stop=True)
                                       for h in range(4)], r=[kCmt, kTq], w=[kpx1])
                                Xs1, kXs1 = nq.get(BF16)
                                em.op('act', lambda e, Xs1=Xs1, px1=px1: e.activation(out=Xs1, in_=px1, func=AF.Copy), r=[kpx1], w=[kXs1])
                            py2, _, kpy2 = ps()
                            em.mm([lambda e, h=h, py2=py2, Tq=Tq, Xs2=Xs2: e.matmul(py2[:, h * 128:(h + 1) * 128], lhsT=Tq[:, h * 128:(h + 1) * 128], rhs=Xs2[:, h * 128:(h + 1) * 128], start=True, stop=True)
                                   for h in range(4)], r=[kTq, kXs2], w=[kpy2])
                            if not last:
                                py1, _, kpy1 = ps()
                                em.mm([lambda e, h=h, py1=py1, Tt=Tt, Xs1=Xs1: e.matmul(py1[:, h * 128:(h + 1) * 128], lhsT=Tt[:, h * 128:(h + 1) * 128], rhs=Xs1[:, h * 128:(h + 1) * 128], start=True, stop=True)
                                       for h in range(4)], r=[kTt, kXs1], w=[kpy1])
                            Ttn, kTtn = nq.get(BF16)
                            em.op('dve', lambda e, Ttn=Ttn, py2=py2, Tt=Tt: e.tensor_tensor(out=Ttn, in0=py2, in1=Tt, op=ALU.add), r=[kpy2, kTt], w=[kTtn])
                            if not last:
                                Tqn, kTqn = nq.get(BF16)
                                em.op('dve', lambda e, Tqn=Tqn, py1=py1, Tq=Tq: e.tensor_tensor(out=Tqn, in0=py1, in1=Tq, op=ALU.add), r=[kpy1, kTq], w=[kTqn])
                                Tq, kTq = Tqn, kTqn
                            Tt, kTt = Ttn, kTtn
                        if b == 0 and c == 0:
                            chk('D4')
                        pw, _, kpw = ps()
                        em.mm([lambda e, h=h, pw=pw, Tt=Tt: e.matmul(pw[:, h * 128:(h + 1) * 128], lhsT=kbd[:, h, :], rhs=Tt[:, h * 128:(h + 1) * 128], start=True, stop=True)
                               for h in range(4)], r=['kbd', kTt], w=[kpw])
                        nwT, knwT = r2k.get(BF16)
                        em.op('act', lambda e, nwT=nwT, pw=pw: e.activation(out=nwT[:, 0:512], in_=pw, func=AF.Copy, scale=-1.0), r=[kpw], w=[knwT])
                        pvn, _, kpvn = ps()
                        fns = []
                        for h in range(4):
                            hs = slice(h * 128, (h + 1) * 128)
                            fns.append(lambda e, h=h, hs=hs, pvn=pvn, Tt=Tt: e.matmul(pvn[:, hs], lhsT=Tt[:, hs], rhs=vb[:, h, :], start=True, stop=False))
                            fns.append(lambda e, h=h, hs=hs, pvn=pvn, nwT=nwT: e.matmul(pvn[:, hs], lhsT=nwT[:, hs], rhs=Sb[:, h, :], start=False, stop=True))
                        em.mm(fns, r=[kTt, 'vb', knwT, 'Sb'], w=[kpvn])
                        vnew, kvnew = r2k.get(BF16)
                        em.op('act', lambda e, vnew=vnew, pvn=pvn: e.activation(out=vnew[:, 0:512], in_=pvn, func=AF.Copy), r=[kpvn], w=[kvnew])
                        if b == 0 and c == 0:
                            chk('D5')
                        pqk, _, kpqk = ps()
                        em.mm([lambda e, h=h, pqk=pqk: e.matmul(pqk[:, h * 128:(h + 1) * 128], lhsT=kT[:, h, tsl], rhs=qT[:, h, tsl], start=True, stop=True) for h in range(4)],
                              r=['kT', 'qT'], w=[kpqk])
                        attnT, kat = r2k.get(BF16)
                        em.op('dve', lambda e, attnT=attnT, pqk=pqk: e.tensor_tensor(out=attnT[:, 0:512], in0=pqk, in1=fl(DT), op=ALU.mult), r=[kpqk, 'DT'], w=[kat])
                        qdT, kqd = r2k.get(BF16)
                        em.op('dve', lambda e, qdT=qdT: e.tensor_tensor(out=qdT[:, 0:512].rearrange("p (h n) -> p h n", n=128), in0=qT[:, :, tsl], in1=Er[:], op=ALU.mult),
                              r=['qT', 'Er'], w=[kqd])
                        po, _, kpo = ps()
                        fns = []
                        for h in range(4):
                            hs = slice(h * 128, (h + 1) * 128)
                            fns.append(lambda e, h=h, hs=hs, po=po, qdT=qdT: e.matmul(po[:, hs], lhsT=Sb[:, h, :], rhs=qdT[:, hs], start=True, stop=False))
                            fns.append(lambda e, h=h, hs=hs, po=po, vnew=vnew, attnT=attnT: e.matmul(po[:, hs], lhsT=vnew[:, hs], rhs=attnT[:, hs], start=False, stop=True))
                        em.mm(fns, r=['Sb', kqd, kvnew, kat], w=[kpo])
                        osq, kosq = r2k.get(BF16)
                        em.op('act', lambda e, osq=osq, po=po: e.activation(out=osq[:, 0:512], in_=po, func=AF.Square), r=[kpo], w=[kosq])
                        pss, _, kpss = ps()
                        em.mm([lambda e, pss=pss, osq=osq: e.matmul(pss, lhsT=onesb[:], rhs=osq[:, 0:512], start=True, stop=True)], r=[kosq] + ck(onesb), w=[kpss])
                        lno, klno = r2k.get()
                        em.op('act', lambda e, lno=lno, pss=pss: e.activation(out=lno, in_=pss, func=AF.Ln, bias=epsc[:, 0:1], scale=1.0 / 128), r=[kpss, 'epsc'], w=[klno])
                        rso, krso = r2k.get()
                        em.op('act', lambda e, rso=rso, lno=lno: e.activation(out=rso, in_=lno, func=AF.Exp, scale=-0.5), r=[klno], w=[krso])
                        t1, kt1 = r2k.get()
                        em.op('dve', lambda e, t1=t1, po=po, rso=rso: e.tensor_tensor(out=t1, in0=po, in1=rso, op=ALU.mult), r=[kpo, krso], w=[kt1])
                        em.op('dve', lambda e, t1=t1: e.scalar_tensor_tensor(out=yaT[:, :, tsl], in0=t1.rearrange("p (h n) -> p h n", n=128), scalar=dng[:, 0:1],
                                                                              in1=szT[:, :, tsl], op0=ALU.mult, op1=ALU.mult),
                              r=[kt1, 'szT'] + ck(dng), w=['yaT'])
                        if b == 0 and c == 0 and dbg_t is not None and 'D6' in DBGSEL:
                            dump_any(Tt, [kTt], 512)
                            dump_any(vnew[:, 0:512], [kvnew], 512)
                            dump_any(attnT[:, 0:512], [kat], 512)
                            dump_any(po, [kpo], 512)
                            dump_any(rso, [krso], 512)
                        if b == 0 and c == 0:
                            chk('D6')
                        pst, _, kpst = ps()
                        em.mm([lambda e, h=h, pst=pst, vnew=vnew: e.matmul(pst[:, h * 128:(h + 1) * 128], lhsT=kdec[:, h, :], rhs=vnew[:, h * 128:(h + 1) * 128], start=True, stop=True)
                               for h in range(4)], r=['kdec', kvnew], w=[kpst])
                        for h in range(4):
                            hs = slice(h * 128, (h + 1) * 128)
                            em.op('dve', lambda e, h=h, hs=hs, pst=pst, egl=egl: e.scalar_tensor_tensor(
                                out=S[:, h, :], in0=S[:, h, :], scalar=egl[:, h:h + 1], in1=pst[:, hs], op0=ALU.mult, op1=ALU.add),
                                r=['S', kegl, kpst, kpo, kpvn], w=['S'])
                        em.op('act', lambda e: e.activation(out=fl(Sb), in_=fl(S), func=AF.Copy), r=['S', kpo, kpvn], w=['Sb'])
                        if b == 0 and c == 0 and dbg_t is not None and 'S1' in DBGSEL:
                            dump(fl(S), ['S'], 512)
                            dump_any(fl(Sb), ['Sb'], 512)
                            dump_any(fl(kdec), ['kdec'], 512)
                        if b == 0 and c == 0:
                            chk('S1')
                    if dbg_t is not None and b == 0 and 'sz' in DBGSEL:
                        for h in range(4):
                            dump_any(szT[:, h, :], ['szT'], 512)
                    if dbg_t is not None and b == 0 and 'ya' in DBGSEL:
                        for h in range(4):
                            dump_any(yaT[:, h, :], ['yaT'], 512)
                        for h in range(4):
                            dump_any(ybT[:, h, :], ['ybT'], 512)

                    if b == 0:
                        chk('D')
                    yield
                def gen_E2(b=b, seq=seq, blk=blk, t0=t0, first=first):
                    wo = [wpiece(6, 2), wpiece(7, 3)]
                    xn2bs = []
                    def fetch_x(j_):
                        xin_, kx_ = r4k.get()
                        em.dma('sp', lambda e: e.dma_start(out=xin_, in_=x[t0 + j_ * 128:t0 + (j_ + 1) * 128, :]), w=[kx_])
                        return xin_, kx_

                    for j in range(4):
                        yield
                        tile_idx = b * 4 + j
                        js = slice(j * 128, (j + 1) * 128)
                        xin, kx = fetch_x(j)
                        h1t, kh1 = r4k.get()
                        for hf in range(2):
                            wv = wo[hf][0][:].rearrange("p (c n) -> p c n", n=512)
                            pw_, _, kpw_ = ps()
                            em.mm([lambda e, kc=kc, pw_=pw_, wv=wv, js=js: e.matmul(pw_, lhsT=mixedT[:, kc, js], rhs=wv[:, kc, :], start=(kc == 0), stop=(kc == 7)) for kc in range(8)],
                                  r=[wo[hf][1], 'mixedT'], w=[kpw_])
                            fs = slice(hf * 512, (hf + 1) * 512)
                            em.op('dve', lambda e, h1t=h1t, pw_=pw_, fs=fs: e.tensor_tensor(out=h1t[:, fs], in0=pw_, in1=GT1b[:, fs], op=ALU.mult), r=[kpw_, 'GT1b'], w=[kh1])
                            em.op('pool', lambda e, h1t=h1t, xin=xin, fs=fs: e.tensor_tensor(out=h1t[:, fs], in0=h1t[:, fs], in1=xin[:, fs], op=ALU.add), r=[kh1, kx], w=[kh1])
                        em.dma('sp', lambda e, h1t=h1t, j=j: e.dma_start(out=h1D[t0 + j * 128:t0 + (j + 1) * 128, :], in_=h1t), r=[kh1], w=[('h1D', tile_idx)])
                        junk, kj = r2k.get(BF16)
                        ss, kss = sm(1)
                        em.op('act', lambda e, h1t=h1t, junk=junk, ss=ss: e.activation(out=junk, in_=h1t, func=AF.Square, accum_out=ss), r=[kh1], w=[kj, kss])
                        rs, krs = rstd_from_ss(ss, kss, 1.0 / D)
                        xn2f, kxf = r4k.get()
                        em.op('dve', lambda e, xn2f=xn2f, h1t=h1t, rs=rs: e.scalar_tensor_tensor(out=xn2f, in0=h1t, scalar=rs, in1=G2b[:], op0=ALU.mult, op1=ALU.mult),
                              r=[kh1, krs, 'G2b'], w=[kxf])
                        em.op('pool', lambda e, xn2f=xn2f: e.tensor_tensor(out=xn2f, in0=xn2f, in1=SH2b[:], op=ALU.add), r=[kxf, 'SH2b'], w=[kxf])
                        xhost, kxb = (yaT, 'yaT') if j < 2 else (ybT, 'ybT')
                        xn2b = xhost[:].rearrange("p h n -> p (h n)")[:, (j % 2) * 1024:(j % 2 + 1) * 1024]
                        em.op('act', lambda e, xn2b=xn2b, xn2f=xn2f: e.activation(out=xn2b, in_=xn2f, func=AF.Copy), r=[kxf], w=[kxb])
                        xn2bs.append((xn2b, kxb))
                        for half in range(2):
                            ptr, _, kptr = ps()
                            em.mm([lambda e, q=q, ptr=ptr, xn2f=xn2f, half=half: e.transpose(ptr[:, q * 128:(q + 1) * 128], xn2f[:, (half * 4 + q) * 128:(half * 4 + q + 1) * 128], identf[:])
                                   for q in range(4)], r=[kxf] + ck(identf), w=[kptr])
                            eng = 'act' if half == 0 else 'dve'
                            if eng == 'act':
                                em.op('act', lambda e, ptr=ptr, half=half: e.activation(out=xn2T[:, half * 4:half * 4 + 4, :].rearrange("p c n -> p (c n)"), in_=ptr, func=AF.Copy),
                                      r=[kptr], w=[('xn2T', half)])
                            else:
                                em.op('dve', lambda e, ptr=ptr, half=half: e.tensor_copy(out=xn2T[:, half * 4:half * 4 + 4, :].rearrange("p c n -> p (c n)"), in_=ptr),
                                      r=[kptr], w=[('xn2T', half)])
                        plg, _, kplg = ps()
                        em.mm([lambda e, kc=kc, plg=plg: e.matmul(plg[:, 0:36], lhsT=xn2T[:, kc, :], rhs=wr[:, kc, :], start=(kc == 0), stop=(kc == 7)) for kc in range(8)],
                              r=[('xn2T', 0), ('xn2T', 1)] + ck(wr), w=[kplg])
                        em.op('dve', lambda e, plg=plg, j=j: e.tensor_tensor(out=lgb[:, j, :], in0=plg[:, 0:36], in1=brb[:], op=ALU.add), r=[kplg] + ck(brb), w=['lgb'])
                        if dbg_t is not None and b == 0 and j == 0 and 'lg' in DBGSEL:
                            dump(lgb[:, 0, :], ['lgb'], 36)
                    routing4(em, sm, lgb, onesb, triS, cum, elim, destall, gidxall, wall, b, ps, ck, r2k)
                    for j in range(4):
                        tile_idx = b * 4 + j
                        xn2b, kxb = xn2bs[j]
                        for k in range(2):
                            em.dma('pool', lambda e, xn2b=xn2b, k=k, tile_idx=tile_idx: e.indirect_dma_start(
                                out=xsD[:, :], out_offset=IndirectOffsetOnAxis(ap=destall[:, tile_idx * 2 + k:tile_idx * 2 + k + 1], axis=0),
                                in_=xn2b, in_offset=None, bounds_check=pregs['bc'], oob_is_err=False),
                                r=[kxb, ('dest', b)], w=['xsD'])
                    yield
                gAB = gen_AB()
                if pending_e2 is not None:
                    run_il((gAB, 5), (pending_e2, 1))
                    pending_e2 = None
                else:
                    run_il((gAB, 1))
                run_il((gen_D(), 2), (gen_C(), 1))
                bufs = {}
                bufs[4] = wpiece(4, 0)
                bufs[5] = wpiece(5, 1)
                bufs[0] = wpiece(0, 2)
                bufs[2] = wpiece(2, 3)
                la_v = bufs[4][0][:].rearrange("p (c n) -> p c n", n=1024)
                lb_v = bufs[5][0][:].rearrange("p (c n) -> p c n", n=1024)
                for mt in range(8):
                    if mt == 4:
                        bufs[1] = wpiece(1, 2)
                        bufs[3] = wpiece(3, 3)
                    gbuf_a, kga = bufs[mt // 4]
                    gbuf_b, kgb = bufs[2 + mt // 4]
                    ga_v = gbuf_a[:].rearrange("p (c n) -> p c n", n=512)
                    gb_v = gbuf_b[:].rearrange("p (c n) -> p c n", n=512)
                    cs = slice((mt % 4) * 128, (mt % 4 + 1) * 128)
                    ms = slice(mt * 128, (mt + 1) * 128)
                    pga, _, kpga = ps()
                    em.mm([lambda e, kc=kc, pga=pga, ga_v=ga_v, cs=cs: e.matmul(pga, lhsT=ga_v[:, kc, cs], rhs=xnT[:, kc, :], start=(kc == 0), stop=(kc == 7)) for kc in range(8)],
                          r=[kga, 'xnT'], w=[kpga])
                    pgb, _, kpgb = ps()
                    em.mm([lambda e, kc=kc, pgb=pgb, gb_v=gb_v, cs=cs: e.matmul(pgb, lhsT=gb_v[:, kc, cs], rhs=xnT[:, kc, :], start=(kc == 0), stop=(kc == 7)) for kc in range(8)],
                          r=[kgb, 'xnT'], w=[kpgb])
                    pla, _, kpla = ps()
                    em.mm([lambda e, kc=kc, pla=pla, ms=ms: e.matmul(pla, lhsT=la_v[:, kc, ms], rhs=yaT[:, kc, :], start=(kc == 0), stop=(kc == 3)) for kc in range(4)],
                          r=[bufs[4][1], 'yaT'], w=[kpla])
                    plb_, _, kplb_ = ps()
                    em.mm([lambda e, kc=kc, plb_=plb_, ms=ms: e.matmul(plb_, lhsT=lb_v[:, kc, ms], rhs=ybT[:, kc, :], start=(kc == 0), stop=(kc == 3)) for kc in range(4)],
                          r=[bufs[5][1], 'ybT'], w=[kplb_])
                    sga, ksga = r2k.get()
                    em.op('act', lambda e, sga=sga, pga=pga: e.activation(out=sga, in_=pga, func=AF.Sigmoid), r=[kpga], w=[ksga])
                    sgb, ksgb = r2k.get()
                    em.op('act', lambda e, sgb=sgb, pgb=pgb: e.activation(out=sgb, in_=pgb, func=AF.Sigmoid), r=[kpgb], w=[ksgb])
                    ma, kma = r2k.get()
                    em.op('dve', lambda e, ma=ma, pla=pla, sga=sga: e.tensor_tensor(out=ma, in0=pla, in1=sga, op=ALU.mult), r=[kpla, ksga], w=[kma])
                    mb, kmb = r2k.get()
                    em.op('dve', lambda e, mb=mb, plb_=plb_, sgb=sgb: e.tensor_tensor(out=mb, in0=plb_, in1=sgb, op=ALU.mult), r=[kplb_, ksgb], w=[kmb])
                    em.op('pool', lambda e, mt=mt, ma=ma, mb=mb: e.tensor_tensor(out=mixedT[:, mt, :], in0=ma, in1=mb, op=ALU.add), r=[kma, kmb], w=['mixedT'])
                if b == 0 and dbg_t is not None and 'E1' in DBGSEL:
                    for h_ in range(8):
                        dump_any(mixedT[:, h_, :], ['mixedT'], 512)
                if b == 0:
                    chk('E1')
                pending_e2 = gen_E2()
                if b == 0:
                    chk('E2')
            if pending_e2 is not None:
                run_il((pending_e2, 1))
                pending_e2 = None
            if dbg_t is not None and 'route' in DBGSEL:
                t, kt = r2k.get()
                em.op('dve', lambda e: e.tensor_copy(out=t[:, 0:64], in_=destall[:]), r=[('dest', i) for i in range(8)], w=[kt])
                dump(t[:, 0:64], [kt], 64)
                dump(wall[:].rearrange("p a b -> p (a b)"), [('dest', i) for i in range(32)], 64)
            chk('P1')
            p1.close()

            p2 = ExitStack()
            es.enter_context(p2)
            wgu = [sb('wgu%d' % i, [128, 8, 512], BF16, p2) for i in range(3)]
            wdn = [sb('wdn%d' % i, [128, 2, D], BF16, p2) for i in range(3)]
            xst = [sb('xst%d' % i, [128, 4, D], BF16, p2) for i in range(2)]
            xsT = [sb('xsT%d' % i, [128, 8, 512], BF16, p2) for i in range(2)]
            hT = [sb('hT%d' % i, [128, 2, 512], BF16, p2) for i in range(2)]
            yst = [sb('yst%d' % i, [128, 4, D], F32, p2) for i in range(2)]
            sgr = Ring(nc, p2, 'sgr', 4, 2048)
            zrow = sb('zrow', [128, D], F32, p2)
            em.op('pool', lambda e: e.memset(zrow[:], 0.0), w=['zrow'])
            em.dma('sp', lambda e: e.dma_start(out=ysD[E * CAP:E * CAP + 128, :], in_=zrow[:]), r=['zrow'], w=[('ysD', 'z')])
            def load_xst(ex_):
                em.dma('sp', lambda e: e.dma_start(out=xst[ex_ % 2][:], in_=xsD[ex_ * CAP:(ex_ + 1) * CAP, :].rearrange("(j p) d -> p j d", p=128)),
                       r=['xsD'], w=[('xst', ex_ % 2)])

            for ex in range(E):
                i2 = ex % 2
                wg_v = w_gate[ex].rearrange("(c p) n -> p c n", p=128)
                wu_v = w_up[ex].rearrange("(c p) n -> p c n", p=128)
                wd_v = w_down[ex].rearrange("(c p) n -> p c n", p=128)
                i3 = ex % 3
                em.dma('pool', lambda e, i3=i3, wg_v=wg_v: e.dma_start(out=wgu[i3][:, :, 0:256], in_=wg_v), w=[('wgu', i3, 0)])
                em.dma('pool', lambda e, i3=i3, wu_v=wu_v: e.dma_start(out=wgu[i3][:, :, 256:512], in_=wu_v), w=[('wgu', i3, 1)])
                em.dma('pool', lambda e, i3=i3, wd_v=wd_v: e.dma_start(out=wdn[i3][:], in_=wd_v), w=[('wdn', i3)])
                if ex == 0:
                    load_xst(0)
                if ex + 1 < E:
                    load_xst(ex + 1)
                for kp_ in range(4):
                    pf, pb, kp = ps()
                    fns = []
                    for q in range(2):
                        kc = kp_ * 2 + q
                        for j in range(4):
                            fns.append(lambda e, q=q, j=j, kc=kc, pb=pb, i2=i2: e.transpose(pb[:, q * 512 + j * 128:q * 512 + (j + 1) * 128], xst[i2][:, j, kc * 128:(kc + 1) * 128], identb[:]))
                    em.mm(fns, r=[('xst', i2)] + ck(identb), w=[kp])
                    dst = xsT[i2][:, kp_ * 2:kp_ * 2 + 2, :].rearrange("p c n -> p (c n)")
                    em.op('act', lambda e, dst=dst, pb=pb: e.activation(out=dst, in_=pb, func=AF.Identity, scale=1.0, bias=epsc[:, 1:2]), r=[kp, 'epsc'], w=[('xsT', i2)])
                pgs = []
                for ft in range(4):
                    pf, pb, kp = ps()
                    em.mm([lambda e, kc=kc, pf=pf, ft=ft, i2=i2, i3=i3: e.matmul(pf, lhsT=wgu[i3][:, kc, ft * 128:(ft + 1) * 128], rhs=xsT[i2][:, kc, :], start=(kc == 0), stop=(kc == 7))
                           for kc in range(8)], r=[('wgu', i3, ft // 2), ('xsT', i2)], w=[kp])
                    pgs.append((pf, kp))
                for f in range(2):
                    sg, ksg = sgr.get()
                    em.op('act', lambda e, sg=sg, f=f, pgs=pgs: e.activation(out=sg, in_=pgs[f][0], func=AF.Silu), r=[pgs[f][1]], w=[ksg])
                    em.op('dve', lambda e, sg=sg, f=f, pgs=pgs, i2=i2: e.tensor_tensor(out=hT[i2][:, f, :], in0=pgs[2 + f][0], in1=sg, op=ALU.mult),
                          r=[pgs[2 + f][1], ksg], w=[('hT', i2)])
                for j in range(4):
                    for hf in range(2):
                        pf, pb, kp = ps()
                        em.mm([lambda e, f=f, pf=pf, j=j, hf=hf, i2=i2, i3=i3: e.matmul(pf, lhsT=hT[i2][:, f, j * 128:(j + 1) * 128], rhs=wdn[i3][:, f, hf * 512:(hf + 1) * 512],
                                                                                  start=(f == 0), stop=(f == 1)) for f in range(2)], r=[('hT', i2), ('wdn', i3)], w=[kp])
                        if (j * 2 + hf) % 2 == 0:
                            em.op('act', lambda e, pf=pf, j=j, hf=hf, i2=i2: e.activation(out=yst[i2][:, j, hf * 512:(hf + 1) * 512], in_=pf, func=AF.Copy), r=[kp], w=[('yst', i2)])
                        else:
                            em.op('dve', lambda e, pf=pf, j=j, hf=hf, i2=i2: e.tensor_copy(out=yst[i2][:, j, hf * 512:(hf + 1) * 512], in_=pf), r=[kp], w=[('yst', i2)])
                em.dma('sp', lambda e, i2=i2, ex=ex: e.dma_start(out=ysD[ex * CAP:(ex + 1) * CAP, :].rearrange("(j p) d -> p j d", p=128), in_=yst[i2][:]),
                       r=[('yst', i2)], w=[('ysD', ex)])
            chk('P2')
            p2.close()

            p3 = ExitStack()
            es.enter_context(p3)
            GT2b = sb('GT2b', [128, D], F32, p3)
            fngb = sb('fngb', [128, D], F32, p3)
            em.dma('sp', lambda e: e.dma_start(out=fngb[:], in_=p_fng), w=['fngb'])
            q4 = Ring(nc, p3, 'q4', 20, 4096)
            q2 = Ring(nc, p3, 'q2', 2, 2048)
            out_toks = []
            def fetch(ti):
                y0, ky0 = q4.get()
                y1, ky1 = q4.get()
                for k, (yy, kyy) in enumerate(((y0, ky0), (y1, ky1))):
                    em.dma('pool', lambda e, yy=yy, k=k, ti=ti: e.indirect_dma_start(
                        out=yy, out_offset=None, in_=ysD[:, :], in_offset=IndirectOffsetOnAxis(ap=gidxall[:, ti * 2 + k:ti * 2 + k + 1], axis=0)),
                        r=[('ysD', 'z')] + [('ysD', ex_) for ex_ in range(E)] + [('dest', ti // 4)], w=[kyy])
                h1t, kh1 = q4.get()
                em.dma('sp', lambda e, h1t=h1t, ti=ti: e.dma_start(out=h1t, in_=h1D[ti * 128:(ti + 1) * 128, :]), r=[('h1D', ti)], w=[kh1])
                return y0, ky0, y1, ky1, h1t, kh1

            pend = [fetch(0), fetch(1)]
            for ti in range(32):
                seq = ti // 16
                if ti % 16 == 0:
                    em.dma('sp', lambda e, seq=seq: e.dma_start(out=GT2b[:], in_=modD[seq:seq + 1, 5120:6144].partition_broadcast(128)), r=MODK, w=['GT2b'])
                y0, ky0, y1, ky1, h1t, kh1 = pend.pop(0)
                if ti + 2 < 32:
                    pend.append(fetch(ti + 2))
                m, km = q4.get()
                em.op('act', lambda e, m=m, y0=y0, ti=ti: e.activation(out=m, in_=y0, func=AF.Copy, scale=wall[:, ti, 0:1]), r=[ky0, ('dest', ti // 4)], w=[km])
                em.op('dve', lambda e, m=m, y1=y1, ti=ti: e.scalar_tensor_tensor(out=m, in0=y1, scalar=wall[:, ti, 1:2], in1=m, op0=ALU.mult, op1=ALU.add),
                      r=[ky1, km, ('dest', ti // 4)], w=[km])
                em.op('pool', lambda e, m=m: e.tensor_tensor(out=m, in0=m, in1=GT2b[:], op=ALU.mult), r=[km, 'GT2b'], w=[km])
                em.op('dve', lambda e, m=m, h1t=h1t: e.tensor_tensor(out=m, in0=m, in1=h1t, op=ALU.add), r=[km, kh1], w=[km])
                junk, kj = q2.get(BF16)
                ss, kss = sm(1)
                em.op('act', lambda e, m=m, junk=junk, ss=ss: e.activation(out=junk, in_=m, func=AF.Square, accum_out=ss), r=[km], w=[kj, kss])
                rs, krs = rstd_from_ss(ss, kss, 1.0 / D)
                o_, ko = q4.get()
                em.op('dve', lambda e, o_=o_, m=m, rs=rs: e.scalar_tensor_tensor(out=o_, in0=m, scalar=rs, in1=fngb[:], op0=ALU.mult, op1=ALU.mult), r=[km, krs, 'fngb'], w=[ko])
                out_toks.append(em.dma('sp', lambda e, o_=o_, ti=ti: e.dma_start(out=out[ti * 128:(ti + 1) * 128, :], in_=o_), r=[ko], w=[('out', ti)]))
        except StopBuild:
            pass
        em.wait_keys('sp', [k for k in em.lastw if isinstance(k, tuple) and k[0] in ('dbg', 'out')])
        for q_ in ('sp', 'pool'):
            d_ = em.dq[q_]
            for i_, v_ in enumerate(d_['vals']):
                if v_ > 0:
                    em._wait('sp', (q_, i_), v_)
        em.finish()
    return nc


DBGSEL = ()
STOP = None
SERIAL = False


class StopBuild(Exception):
    pass


def run_il(*gw):
    gw = [list(x) for x in gw]
    while gw:
        for item in list(gw):
            g, n = item
            for _ in range(n):
                try:
                    next(g)
                except StopIteration:
                    gw.remove(item)
                    break


def chk(name):
    if STOP == name:
        raise StopBuild()


def routing(em, sm, lg, onesb, triS, cum, elim, destall, gidxall, wall, ti, ps, ck, r2k):
    kl = 'lg'
    gmax, kgm = sm(1)
    em.op('dve', lambda e: e.tensor_reduce(out=gmax, in_=lg[:, 0:4], axis=AX.X, op=ALU.max), r=[kl], w=[kgm])
    ohg, kohg = sm(4)
    em.op('dve', lambda e: e.tensor_scalar(out=ohg, in0=lg[:, 0:4], scalar1=gmax, scalar2=None, op0=ALU.is_equal), r=[kl, kgm], w=[kohg])
    ngm, kngm = sm(1)
    em.op('dve', lambda e: e.tensor_scalar(out=ngm, in0=gmax, scalar1=-1.0, scalar2=None, op0=ALU.mult), r=[kgm], w=[kngm])
    eg, keg = sm(4)
    sg, ksg = sm(1)
    em.op('act', lambda e: e.activation(out=eg, in_=lg[:, 0:4], func=AF.Exp, bias=ngm, accum_out=sg), r=[kl, kngm], w=[keg, ksg])
    pg, kpg = sm(1)
    em.op('dve', lambda e: e.reciprocal(out=pg, in_=sg), r=[ksg], w=[kpg])
    les, kles = sm(8)
    em.op('dve', lambda e: e.tensor_scalar(out=les, in0=lg[:, 4:12], scalar1=ohg[:, 0:1], scalar2=None, op0=ALU.mult), r=[kl, kohg], w=[kles])
    for g in range(1, 4):
        em.op('dve', lambda e, g=g: e.scalar_tensor_tensor(out=les, in0=lg[:, 4 + 8 * g:12 + 8 * g], scalar=ohg[:, g:g + 1], in1=les, op0=ALU.mult, op1=ALU.add),
              r=[kl, kohg, kles], w=[kles])
    m8, km8 = sm(8)
    em.op('dve', lambda e: e.max(out=m8, in_=les), r=[kles], w=[km8])
    d21, kd21 = sm(1)
    em.op('dve', lambda e: e.tensor_tensor(out=d21, in0=m8[:, 1:2], in1=m8[:, 0:1], op=ALU.subtract), r=[km8], w=[kd21])
    e21, ke21 = sm(1)
    em.op('act', lambda e: e.activation(out=e21, in_=d21, func=AF.Exp), r=[kd21], w=[ke21])
    den, kden = sm(1)
    em.op('dve', lambda e: e.tensor_scalar(out=den, in0=e21, scalar1=1.0, scalar2=None, op0=ALU.add), r=[ke21], w=[kden])
    rden, krden = sm(1)
    em.op('dve', lambda e: e.reciprocal(out=rden, in_=den), r=[kden], w=[krden])
    w1, kw1 = sm(1)
    em.op('dve', lambda e: e.tensor_tensor(out=w1, in0=pg, in1=rden, op=ALU.mult), r=[kpg, krden], w=[kw1])
    w2, kw2 = sm(1)
    em.op('dve', lambda e: e.tensor_tensor(out=w2, in0=w1, in1=e21, op=ALU.mult), r=[kw1, ke21], w=[kw2])
    ohs = []
    for k in range(2):
        sel, ksel = sm(8)
        em.op('dve', lambda e, k=k, sel=sel: e.tensor_scalar(out=sel, in0=les, scalar1=m8[:, k:k + 1], scalar2=None, op0=ALU.is_equal), r=[kles, km8], w=[ksel])
        oh, koh = sm(32)
        for g in range(4):
            em.op('dve', lambda e, g=g, oh=oh, sel=sel: e.tensor_scalar(out=oh[:, g * 8:(g + 1) * 8], in0=sel, scalar1=ohg[:, g:g + 1], scalar2=None, op0=ALU.mult),
                  r=[ksel, kohg], w=[koh])
        ohs.append((oh, koh))
    ohsum, kohs = r2k.get(BF16)
    em.op('dve', lambda e: e.tensor_tensor(out=ohsum[:, 0:32], in0=ohs[0][0], in1=ohs[1][0], op=ALU.add), r=[ohs[0][1], ohs[1][1]], w=[kohs])
    pr, _, kpr = ps()
    em.mm([lambda e: e.matmul(pr[:, 0:32], lhsT=triS[:], rhs=ohsum[:, 0:32], start=True, stop=True),
           lambda e: e.matmul(pr[:, 32:64], lhsT=onesb[:], rhs=ohsum[:, 0:32], start=True, stop=True)], r=[kohs] + ck(triS, onesb), w=[kpr])
    rk, krk = sm(32)
    em.op('dve', lambda e: e.tensor_tensor(out=rk, in0=pr[:, 0:32], in1=cum[:], op=ALU.add), r=[kpr, 'cum'], w=[krk])
    em.op('dve', lambda e: e.tensor_tensor(out=cum[:], in0=pr[:, 32:64], in1=cum[:], op=ALU.add), r=[kpr, 'cum', krk], w=['cum'])
    for k in range(2):
        oh, koh = ohs[k]
        t32, kt32 = sm(32)
        dst, kdst = sm(1)
        em.op('dve', lambda e, t32=t32, oh=oh: e.tensor_tensor(out=t32, in0=oh, in1=rk, op=ALU.mult), r=[koh, krk], w=[kt32])
        em.op('dve', lambda e, t32=t32, dst=dst: e.tensor_reduce(out=dst, in_=t32, axis=AX.X, op=ALU.add), r=[kt32], w=[kdst])
        l32, kl32 = sm(32)
        lim, klim = sm(1)
        em.op('dve', lambda e, l32=l32, oh=oh: e.tensor_tensor(out=l32, in0=oh, in1=elim[:], op=ALU.mult), r=[koh] + ck(elim), w=[kl32])
        em.op('dve', lambda e, l32=l32, lim=lim: e.tensor_reduce(out=lim, in_=l32, axis=AX.X, op=ALU.add), r=[kl32], w=[klim])
        ok, kok = sm(1)
        em.op('dve', lambda e, ok=ok, dst=dst, lim=lim: e.tensor_tensor(out=ok, in0=dst, in1=lim, op=ALU.is_lt), r=[kdst, klim], w=[kok])
        nok, knok = sm(1)
        em.op('dve', lambda e, nok=nok, ok=ok: e.tensor_scalar(out=nok, in0=ok, scalar1=-1.0, scalar2=1.0, op0=ALU.mult, op1=ALU.add), r=[kok], w=[knok])
        dv, kdv = sm(1)
        em.op('dve', lambda e, dv=dv, dst=dst, ok=ok: e.tensor_tensor(out=dv, in0=dst, in1=ok, op=ALU.mult), r=[kdst, kok], w=[kdv])
        si, ksi = sm(1)
        em.op('dve', lambda e, si=si, nok=nok, dv=dv: e.scalar_tensor_tensor(out=si, in0=nok, scalar=float(E * CAP + 64), in1=dv, op0=ALU.mult, op1=ALU.add), r=[knok, kdv], w=[ksi])
        gi_, kgi = sm(1)
        em.op('dve', lambda e, gi_=gi_, nok=nok, dv=dv: e.scalar_tensor_tensor(out=gi_, in0=nok, scalar=float(E * CAP), in1=dv, op0=ALU.mult, op1=ALU.add), r=[knok, kdv], w=[kgi])
        em.op('dve', lambda e, k=k, si=si: e.tensor_copy(out=destall[:, ti * 2 + k:ti * 2 + k + 1], in_=si), r=[ksi], w=[('dest', ti)])
        em.op('dve', lambda e, k=k, gi_=gi_: e.tensor_copy(out=gidxall[:, ti * 2 + k:ti * 2 + k + 1], in_=gi_), r=[kgi], w=[('dest', ti)])
        wk, kwk = (w1, kw1) if k == 0 else (w2, kw2)
        em.op('dve', lambda e, k=k, wk=wk, ok=ok: e.tensor_tensor(out=wall[:, ti, k:k + 1], in0=wk, in1=ok, op=ALU.mult), r=[kwk, kok], w=[('dest', ti)])


def _consts():
    bf = ml_dtypes.bfloat16
    i = np.arange(128)
    c = {}
    c['k_identb'] = np.eye(128, dtype=np.float32).astype(bf)
    c['k_identq'] = np.tile(np.eye(128, dtype=np.float32), (1, 4)).astype(bf)
    c['k_identf'] = np.eye(128, dtype=np.float32)
    c['k_onesb'] = np.ones((128, 128), np.float32).astype(bf)
    c['k_onesf'] = np.ones((128, 128), np.float32)
    c['k_triU'] = (i[:, None] <= i[None, :]).astype(np.float32)
    c['k_maskT'] = np.where(i[None, :] >= i[:, None], 0.0, NEG).astype(np.float32)
    c['k_maskS'] = np.where(i[:, None] > i[None, :], 0.0, -NEG).astype(np.float32)
    c['k_triS'] = (i[:, None] < i[None, :]).astype(np.float32).astype(bf)
    pc = np.zeros((128, 4, 16), np.float32)
    for gi, win in enumerate((2, 4, 8, 16)):
        t = np.arange(16)
        pc[:, gi, :] = 1.0 / np.minimum(t + 1, win)
    c['k_pcorr'] = pc
    bm = np.zeros((128, 7, 128), np.float32)
    for l in range(7):
        s_ = 1 << l
        bm[:, l, :] = ((i[:, None] // (2 * s_) == i[None, :] // (2 * s_)) & (i[:, None] % (2 * s_) >= s_) & (i[None, :] % (2 * s_) < s_))
    c['k_bmask'] = bm.astype(bf)
    c['k_bmaskT'] = np.ascontiguousarray(bm.transpose(2, 1, 0)).astype(bf)
    c['k_ebase'] = np.tile((np.arange(32) * CAP).astype(np.float32), (128, 1))
    c['k_elim'] = np.tile(((np.arange(32) + 1) * CAP).astype(np.float32), (128, 1))
    return c


def _prep_inputs(inp):
    f = lambda a: np.ascontiguousarray(np.asarray(a, dtype=np.float32))
    shared = {}
    shared['w_ada'] = f(inp['w_ada'][0])
    shared['b_ada'] = f(inp['b_ada'][0]).reshape(1, -1)
    shared['w_in'] = f(inp['w_in'][0])
    shared['w_lift_a'] = f(inp['w_lift_a'][0])
    shared['w_lift_b'] = f(inp['w_lift_b'][0])
    shared['w_out'] = f(inp['w_out'][0])
    shared['pool_w'] = f(inp['pool_w'][0])
    shared['w_gate'] = f(inp['w_gate'][0])
    shared['w_up'] = f(inp['w_up'][0])
    shared['w_down'] = f(inp['w_down'][0])
    shared['p_n1g'] = f(np.asarray(inp['norm1_g'][0]).reshape(8, 128).T)
    shared['p_convw'] = f(np.asarray(inp['conv_w'][0]).reshape(4, 12, 128).transpose(2, 1, 0))
    shared['p_alog'] = f(np.broadcast_to(np.asarray(inp['a_log'][0]).reshape(1, 4), (128, 4)))
    shared['p_dtb'] = f(np.broadcast_to(np.asarray(inp['dt_bias'][0]).reshape(1, 4), (128, 4)))
    shared['p_dng'] = f(np.asarray(inp['dn_norm_g'][0]).reshape(128, 1))
    shared['p_pscale'] = f(np.asarray(inp['pool_scale'][0]).reshape(4, 128).T)
    shared['p_n2g'] = f(np.broadcast_to(np.asarray(inp['norm2_g'][0]).reshape(1, D), (128, D)))
    shared['p_fng'] = f(np.broadcast_to(np.asarray(inp['final_norm_g']).reshape(1, D), (128, D)))
    br = np.concatenate([np.asarray(inp['b_router_group'][0]), np.asarray(inp['b_router_expert'][0])]).reshape(1, 36)
    shared['p_brb'] = f(np.broadcast_to(br, (128, 36)))
    wrc = np.concatenate([np.asarray(inp['w_router_group'][0]), np.asarray(inp['w_router_expert'][0])], axis=1)
    shared['p_wr'] = f(wrc.reshape(8, 128, 36).transpose(1, 0, 2))
    shared.update(_consts())
    xs = np.asarray(inp['x'], dtype=np.float32)
    cs = np.asarray(inp['c'], dtype=np.float32)
    in_maps = []
    for i in range(NCORES):
        m = dict(shared)
        m['x'] = np.ascontiguousarray(xs[2 * i:2 * i + 2].reshape(TOK, D))
        m['cT'] = np.ascontiguousarray(cs[2 * i:2 * i + 2].reshape(2, 8, 128).transpose(2, 1, 0))
        in_maps.append(m)
    return in_maps


_NC_CACHE = {}


def kernel(**inputs):
    in_maps = _prep_inputs(inputs)
    if 'nc' not in _NC_CACHE:
        _NC_CACHE['nc'] = build_nc()
    nc = _NC_CACHE['nc']
    res = run_bass_kernel_spmd(nc, in_maps, core_ids=list(range(NCORES)))
    outs = [np.asarray(r['out'], dtype=np.float32).reshape(2, 2048, D) for r in res.results]
    return np.concatenate(outs, axis=0)


def routing4(em, sm, lgb, onesb, triS, cum, elim, destall, gidxall, wall, b, ps, ck, r2k):
    kl = 'lgb'
    T = 4

    def v3(ap, n):
        return ap.rearrange("p (t n) -> p t n", n=n)

    def bc_last(ap, n):
        return ap.unsqueeze(2).broadcast_to([128, T, n])
    lgg = lgb[:, :, 0:4]
    gmax, kgm = sm(T)
    em.op('dve', lambda e: e.tensor_reduce(out=gmax, in_=lgg, axis=AX.X, op=ALU.max), r=[kl], w=[kgm])
    ohg_, kohg = sm(16)
    ohg = v3(ohg_, 4)
    em.op('dve', lambda e: e.tensor_tensor(out=ohg, in0=lgg, in1=bc_last(gmax, 4), op=ALU.is_equal), r=[kl, kgm], w=[kohg])
    sub_, ksub = sm(16)
    em.op('dve', lambda e: e.tensor_tensor(out=v3(sub_, 4), in0=lgg, in1=bc_last(gmax, 4), op=ALU.subtract), r=[kl, kgm], w=[ksub])
    eg_, keg = sm(16)
    em.op('act', lambda e: e.activation(out=eg_, in_=sub_, func=AF.Exp), r=[ksub], w=[keg])
    sg, ksg = sm(T)
    em.op('dve', lambda e: e.tensor_reduce(out=sg, in_=v3(eg_, 4), axis=AX.X, op=ALU.add), r=[keg], w=[ksg])
    pg, kpg = sm(T)
    em.op('dve', lambda e: e.reciprocal(out=pg, in_=sg), r=[ksg], w=[kpg])
    prod_, kprod = r2k.get()
    prod = prod_[:, 0:T * 32]
    le4 = lgb[:, :, 4:36].rearrange("p t (g j) -> p t g j", j=8)
    em.op('dve', lambda e: e.tensor_tensor(out=prod.rearrange("p (t g j) -> p t g j", g=4, j=8), in0=le4,
                                           in1=ohg.unsqueeze(3).broadcast_to([128, T, 4, 8]), op=ALU.mult), r=[kl, kohg], w=[kprod])
    les_, kles = sm(32)
    les = v3(les_, 8)
    em.op('dve', lambda e: e.tensor_reduce(out=les, in_=prod.rearrange("p (t g j) -> p t j g", g=4, j=8), axis=AX.X, op=ALU.add), r=[kprod], w=[kles])
    m1, km1 = sm(T)
    em.op('dve', lambda e: e.tensor_reduce(out=m1, in_=les, axis=AX.X, op=ALU.max), r=[kles], w=[km1])
    sel1_, ksel1 = sm(32)
    sel1 = v3(sel1_, 8)
    em.op('dve', lambda e: e.tensor_tensor(out=sel1, in0=les, in1=bc_last(m1, 8), op=ALU.is_equal), r=[kles, km1], w=[ksel1])
    les2_, kles2 = sm(32)
    les2 = v3(les2_, 8)
    em.op('dve', lambda e: e.scalar_tensor_tensor(out=les2, in0=sel1, scalar=NEG, in1=les, op0=ALU.mult, op1=ALU.add), r=[ksel1, kles], w=[kles2])
    m2, km2 = sm(T)
    em.op('dve', lambda e: e.tensor_reduce(out=m2, in_=les2, axis=AX.X, op=ALU.max), r=[kles2], w=[km2])
    sel2_, ksel2 = sm(32)
    sel2 = v3(sel2_, 8)
    em.op('dve', lambda e: e.tensor_tensor(out=sel2, in0=les2, in1=bc_last(m2, 8), op=ALU.is_equal), r=[kles2, km2], w=[ksel2])
    d21, kd21 = sm(T)
    em.op('dve', lambda e: e.tensor_tensor(out=d21, in0=m2, in1=m1, op=ALU.subtract), r=[km1, km2], w=[kd21])
    e21, ke21 = sm(T)
    em.op('act', lambda e: e.activation(out=e21, in_=d21, func=AF.Exp), r=[kd21], w=[ke21])
    den, kden = sm(T)
    em.op('dve', lambda e: e.tensor_scalar(out=den, in0=e21, scalar1=1.0, scalar2=None, op0=ALU.add), r=[ke21], w=[kden])
    rden, krden = sm(T)
    em.op('dve', lambda e: e.reciprocal(out=rden, in_=den), r=[kden], w=[krden])
    w1, kw1 = sm(T)
    em.op('dve', lambda e: e.tensor_tensor(out=w1, in0=pg, in1=rden, op=ALU.mult), r=[kpg, krden], w=[kw1])
    w2, kw2 = sm(T)
    em.op('dve', lambda e: e.tensor_tensor(out=w2, in0=w1, in1=e21, op=ALU.mult), r=[kw1, ke21], w=[kw2])
    ohs = []
    for k, (sel, ksel) in enumerate(((sel1, ksel1), (sel2, ksel2))):
        oh_, koh = r2k.get()
        oh = oh_[:, 0:T * 32]
        em.op('dve', lambda e, oh=oh, sel=sel: e.tensor_tensor(out=oh.rearrange("p (t g j) -> p t g j", g=4, j=8),
                                                               in0=ohg.unsqueeze(3).broadcast_to([128, T, 4, 8]),
                                                               in1=sel.unsqueeze(2).broadcast_to([128, T, 4, 8]), op=ALU.mult), r=[kohg, ksel], w=[koh])
        ohs.append((oh, koh))
    ohsum_, kohs = r2k.get(BF16)
    ohsum = ohsum_[:, 0:T * 32]
    em.op('dve', lambda e: e.tensor_tensor(out=ohsum, in0=ohs[0][0], in1=ohs[1][0], op=ALU.add), r=[ohs[0][1], ohs[1][1]], w=[kohs])
    pr, _, kpr = ps()
    fns = []
    for t in range(T):
        terms = [(triS, t)] + [(onesb, t2) for t2 in range(t)]
        for i_, (lt, t2) in enumerate(terms):
            fns.append(lambda e, t=t, lt=lt, t2=t2, i_=i_, n_=len(terms): e.matmul(pr[:, t * 32:(t + 1) * 32], lhsT=lt[:], rhs=ohsum[:, t2 * 32:(t2 + 1) * 32],
                                                                                  start=(i_ == 0), stop=(i_ == n_ - 1)))
    for t in range(T):
        fns.append(lambda e, t=t: e.matmul(pr[:, 128:160], lhsT=onesb[:], rhs=ohsum[:, t * 32:(t + 1) * 32], start=(t == 0), stop=(t == T - 1)))
    em.mm(fns, r=[kohs] + ck(triS, onesb), w=[kpr])
    rk_, krk = r2k.get()
    rk = rk_[:, 0:T * 32]
    em.op('dve', lambda e: e.tensor_tensor(out=v3(rk, 32), in0=v3(pr[:, 0:128], 32), in1=cum[:].unsqueeze(1).broadcast_to([128, T, 32]), op=ALU.add),
          r=[kpr, 'cum'], w=[krk])
    em.op('dve', lambda e: e.tensor_tensor(out=cum[:], in0=pr[:, 128:160], in1=cum[:], op=ALU.add), r=[kpr, 'cum', krk], w=['cum'])
    dsl = slice(b * 8, (b + 1) * 8)
    for k in range(2):
        oh, koh = ohs[k]
        t32_, kt32 = r2k.get()
        t32 = t32_[:, 0:T * 32]
        em.op('dve', lambda e, t32=t32, oh=oh: e.tensor_tensor(out=t32, in0=oh, in1=rk, op=ALU.mult), r=[koh, krk], w=[kt32])
        dst, kdst = sm(T)
        em.op('dve', lambda e, t32=t32, dst=dst: e.tensor_reduce(out=dst, in_=v3(t32, 32), axis=AX.X, op=ALU.add), r=[kt32], w=[kdst])
        l32_, kl32 = r2k.get()
        l32 = l32_[:, 0:T * 32]
        em.op('dve', lambda e, l32=l32, oh=oh: e.tensor_tensor(out=v3(l32, 32), in0=v3(oh, 32), in1=elim[:].unsqueeze(1).broadcast_to([128, T, 32]), op=ALU.mult),
              r=[koh] + ck(elim), w=[kl32])
        lim, klim = sm(T)
        em.op('dve', lambda e, l32=l32, lim=lim: e.tensor_reduce(out=lim, in_=v3(l32, 32), axis=AX.X, op=ALU.add), r=[kl32], w=[klim])
        ok, kok = sm(T)
        em.op('dve', lambda e, ok=ok, dst=dst, lim=lim: e.tensor_tensor(out=ok, in0=dst, in1=lim, op=ALU.is_lt), r=[kdst, klim], w=[kok])
        nok, knok = sm(T)
        em.op('dve', lambda e, nok=nok, ok=ok: e.tensor_scalar(out=nok, in0=ok, scalar1=-1.0, scalar2=1.0, op0=ALU.mult, op1=ALU.add), r=[kok], w=[knok])
        dv, kdv = sm(T)
        em.op('dve', lambda e, dv=dv, dst=dst, ok=ok: e.tensor_tensor(out=dv, in0=dst, in1=ok, op=ALU.mult), r=[kdst, kok], w=[kdv])
        si, ksi = sm(T)
        em.op('dve', lambda e, si=si, nok=nok, dv=dv: e.scalar_tensor_tensor(out=si, in0=nok, scalar=float(E * CAP + 64), in1=dv, op0=ALU.mult, op1=ALU.add), r=[knok, kdv], w=[ksi])
        gi_, kgi = sm(T)
        em.op('dve', lambda e, gi_=gi_, nok=nok, dv=dv: e.scalar_tensor_tensor(out=gi_, in0=nok, scalar=float(E * CAP), in1=dv, op0=ALU.mult, op1=ALU.add), r=[knok, kdv], w=[kgi])
        em.op('dve', lambda e, k=k, si=si: e.tensor_copy(out=destall[:, dsl].rearrange("p (t k) -> p t k", k=2)[:, :, k], in_=si), r=[ksi], w=[('dest', b)])
        em.op('dve', lambda e, k=k, gi_=gi_: e.tensor_copy(out=gidxall[:, dsl].rearrange("p (t k) -> p t k", k=2)[:, :, k], in_=gi_), r=[kgi], w=[('dest', b)])
        wk, kwk = (w1, kw1) if k == 0 else (w2, kw2)
        em.op('dve', lambda e, k=k, wk=wk, ok=ok: e.tensor_tensor(out=wall[:, b * 4:(b + 1) * 4, k], in0=wk, in1=ok, op=ALU.mult), r=[kwk, kok], w=[('dest', b)])
```

```python
import types
import numpy as np
import ml_dtypes
from contextlib import ExitStack
import concourse.bass as bass
import concourse.mybir as mybir
from concourse.bass import IndirectOffsetOnAxis
from concourse.bass_utils import run_bass_kernel_spmd

F32 = mybir.dt.float32
BF16 = mybir.dt.bfloat16
I32 = mybir.dt.int32
U32 = mybir.dt.uint32
AF = mybir.ActivationFunctionType
ALU = mybir.AluOpType
AX = mybir.AxisListType

NCORES = 8
D = 1024
TOK = 4096
BLK = 512
NBLK = TOK // BLK
E = 32
CAP = 512
DFF = 256
EPS = 1e-6
NEG = -1.0e30
WIN_RES = 2568
ENGS = ('pe', 'act', 'dve', 'pool', 'sp')


def _freeze(fn):
    if fn.__closure__ is None:
        return fn
    cells = []
    for c in fn.__closure__:
        try:
            cells.append(types.CellType(c.cell_contents))
        except ValueError:
            cells.append(c)
    g = types.FunctionType(fn.__code__, fn.__globals__, fn.__name__, fn.__defaults__, tuple(cells))
    g.__kwdefaults__ = fn.__kwdefaults__
    return g


class Em:
    def __init__(self, nc, es):
        self.nc = nc
        self.streams = {e: [] for e in ENGS}
        self.sem = {e: es.enter_context(nc.semaphore('s_' + e)) for e in ('pe', 'act', 'dve', 'pool')}
        self.cnt = {e: 0 for e in self.sem}
        self.waited = {e: {} for e in ENGS}
        self.lastw = {}
        self.reads = {}
        self.dq = {}
        for q, n in (('sp', 28), ('pool', 14), ('act', 4)):
            sems = [es.enter_context(nc.semaphore('d_%s%d' % (q, i))) for i in range(n)]
            self.dq[q] = dict(sems=sems, vals=[0] * n, nxt=0)

    def _semh(self, key):
        return self.sem[key] if isinstance(key, str) else self.dq[key[0]]['sems'][key[1]]

    def _wait(self, eng, key, val):
        if self.waited[eng].get(key, 0) >= val:
            return
        self.waited[eng][key] = val
        s = self._semh(key)
        self.streams[eng].append(lambda e, s=s, v=val: e.wait_ge(s, v))

    def _deps(self, eng, r, w, pe_inorder=False):
        need = {}

        def add(tok):
            if tok is None:
                return
            k, v = tok
            if need.get(k, 0) < v:
                need[k] = v
        for key in r:
            add(self.lastw.get(key))
        for key in w:
            add(self.lastw.get(key))
            for t in self.reads.get(key, {}).items():
                add(t)
        if SERIAL:
            for k2 in ('pe', 'act', 'dve', 'pool'):
                if self.cnt[k2] > 0:
                    need[k2] = self.cnt[k2]
            for q2, d2 in self.dq.items():
                for i2, v2 in enumerate(d2['vals']):
                    if v2 > 0:
                        need[(q2, i2)] = max(need.get((q2, i2), 0), v2)
        for k, v in need.items():
            if pe_inorder and k == 'pe':
                continue
            self._wait(eng, k, v)

    def _track(self, tok, r, w):
        for key in w:
            self.lastw[key] = tok
            self.reads[key] = {}
        for key in r:
            d = self.reads.setdefault(key, {})
            if d.get(tok[0], 0) < tok[1]:
                d[tok[0]] = tok[1]

    def op(self, eng, fn, r=(), w=()):
        fn = _freeze(fn)
        self._deps(eng, r, w)
        self.cnt[eng] += 1
        tok = (eng, self.cnt[eng])
        s = self.sem[eng]
        self.streams[eng].append(lambda e, fn=fn, s=s: fn(e).then_inc(s, 1))
        self._track(tok, r, w)
        return tok

    def mm(self, fns, r=(), w=()):
        fns = [_freeze(f) for f in fns]
        self._deps('pe', r, w, pe_inorder=True)
        self.cnt['pe'] += 1
        tok = ('pe', self.cnt['pe'])
        s = self.sem['pe']
        for fn in fns[:-1]:
            self.streams['pe'].append(lambda e, fn=fn: fn(e))
        self.streams['pe'].append(lambda e, fn=fns[-1], s=s: fn(e).then_inc(s, 1))
        self._track(tok, r, w)
        return tok

    def dma(self, q, fn, r=(), w=()):
        fn = _freeze(fn)
        d = self.dq[q]
        i = d['nxt']
        d['nxt'] = (i + 1) % len(d['sems'])
        key = (q, i)
        if d['vals'][i] > 0:
            self._wait(q, key, d['vals'][i])
        self._deps(q, r, w)
        d['vals'][i] += 16
        tok = (key, d['vals'][i])
        s = d['sems'][i]
        self.streams[q].append(lambda e, fn=fn, s=s: fn(e).then_inc(s, 16))
        self._track(tok, r, w)
        return tok

    def wait_keys(self, eng, keys):
        self._deps(eng, keys, keys)

    def finish(self):
        nc = self.nc
        st = self.streams
        with nc.Block() as block:
            @block.tensor
            def _(e):
                for f in st['pe']:
                    f(e)

            @block.scalar
            def _(e):
                for f in st['act']:
                    f(e)

            @block.vector
            def _(e):
                for f in st['dve']:
                    f(e)

            @block.gpsimd
            def _(e):
                for f in st['pool']:
                    f(e)

            @block.sync
            def _(e):
                for f in st['sp']:
                    f(e)


class Ring:
    def __init__(self, nc, es, name, n, nbytes):
        self.t = [es.enter_context(nc.sbuf_tensor('%s%d' % (name, i), [128, nbytes // 4], F32)) for i in range(n)]
        self.name = name
        self.n = n
        self.i = 0

    def get(self, dt=F32):
        i = self.i
        self.i = (i + 1) % self.n
        ap = self.t[i][:]
        if dt != F32:
            ap = ap.bitcast(dt)
        return ap, (self.name, i)


def build_nc(dbg=None):
    nc = bass.Bass("TRN2", target_bir_lowering=False)

    def din(name, shape, dt=F32):
        return nc.dram_tensor(name, list(shape), dt, kind="ExternalInput").ap()

    x = din("x", [TOK, D])
    cT = din("cT", [128, 8, 2])
    w_ada = din("w_ada", [D, 6 * D])
    b_ada = din("b_ada", [1, 6 * D])
    w_in = din("w_in", [D, 4616])
    w_la = din("w_lift_a", [512, D])
    w_lb = din("w_lift_b", [512, D])
    w_out = din("w_out", [D, D])
    pool_w = din("pool_w", [4, 128, 128])
    w_gate = din("w_gate", [E, D, DFF])
    w_up = din("w_up", [E, D, DFF])
    w_down = din("w_down", [E, DFF, D])
    p_n1g = din("p_n1g", [128, 8])
    p_convw = din("p_convw", [128, 12, 4])
    p_alog = din("p_alog", [128, 4])
    p_dtb = din("p_dtb", [128, 4])
    p_dng = din("p_dng", [128, 1])
    p_pscale = din("p_pscale", [128, 4])
    p_n2g = din("p_n2g", [128, D])
    p_fng = din("p_fng", [128, D])
    p_brb = din("p_brb", [128, 36])
    p_wr = din("p_wr", [128, 8, 36])
    k_identb = din("k_identb", [128, 128], BF16)
    k_identq = din("k_identq", [128, 512], BF16)
    k_identf = din("k_identf", [128, 128])
    k_onesb = din("k_onesb", [128, 128], BF16)
    k_onesf = din("k_onesf", [128, 128])
    k_triU = din("k_triU", [128, 128])
    k_maskT = din("k_maskT", [128, 128])
    k_maskS = din("k_maskS", [128, 128])
    k_triS = din("k_triS", [128, 128], BF16)
    k_pcorr = din("k_pcorr", [128, 4, 16])
    k_ebase = din("k_ebase", [128, 32])
    k_bmask = din("k_bmask", [128, 7, 128], BF16)
    k_bmaskT = din("k_bmaskT", [128, 7, 128], BF16)
    k_elim = din("k_elim", [128, 32])

    out = nc.dram_tensor("out", [TOK, D], F32, kind="ExternalOutput").ap()
    dbg_t = None
    if dbg is not None:
        dbg_t = nc.dram_tensor("dbg", list(dbg), F32, kind="ExternalOutput").ap()
    modD = nc.dram_tensor("modD", [2, 6 * D], F32).ap()
    wsD = nc.dram_tensor("wsD", [8, 128, 4096], BF16).ap()
    h1D = nc.dram_tensor("h1D", [TOK, D], F32).ap()
    xsD = nc.dram_tensor("xsD", [E * CAP, D], BF16).ap()
    ysD = nc.dram_tensor("ysD", [E * CAP + 128, D], F32).ap()

    es = ExitStack()
    with es:
        em = Em(nc, es)

        def sb(name, shape, dt=F32, stack=es):
            return stack.enter_context(nc.sbuf_tensor(name, list(shape), dt))

        pregs = {}
        em.streams['pool'].append(lambda e: pregs.__setitem__('bc', e.to_reg(E * CAP - 1)))

        psb = [es.enter_context(nc.psum_tensor('ps%d' % i, [128, 512], F32)) for i in range(8)]
        pstate = {'i': 0}

        def ps():
            i = pstate['i']
            pstate['i'] = (i + 1) % 8
            return psb[i][:], psb[i][:].bitcast(BF16), ('ps', i)

        identb = sb('identb', [128, 128], BF16)
        identq = sb('identq', [128, 512], BF16)
        identf = sb('identf', [128, 128])
        onesb = sb('onesb', [128, 128], BF16)
        onesf = sb('onesf', [128, 128])
        triU = sb('triU', [128, 128])
        maskT = sb('maskT', [128, 128])
        maskS = sb('maskS', [128, 128])
        triS = sb('triS', [128, 128], BF16)
        pcorr = sb('pcorr', [128, 4, 16])
        bmask = sb('bmask', [128, 7, 128], BF16)
        bmaskT = sb('bmaskT', [128, 7, 128], BF16)
        n1g = sb('n1g', [128, 8])
        convw = sb('convw', [128, 12, 4])
        alog = sb('alog', [128, 4])
        dtb = sb('dtb', [128, 4])
        dng = sb('dng', [128, 1])
        pscale = sb('pscale', [128, 4])
        brb = sb('brb', [128, 36])
        wr = sb('wr', [128, 8, 36])
        cum = sb('cum', [128, 32])
        elim = sb('elim', [128, 32])
        destall = sb('destall', [128, 64], I32)
        gidxall = sb('gidxall', [128, 64], I32)
        wall = sb('wall', [128, 32, 2])
        cst = [(identb, k_identb), (identq, k_identq), (identf, k_identf), (onesb, k_onesb), (onesf, k_onesf),
               (triU, k_triU), (maskT, k_maskT), (maskS, k_maskS), (triS, k_triS), (pcorr, k_pcorr),
               (n1g, p_n1g), (convw, p_convw), (alog, p_alog), (dtb, p_dtb), (dng, p_dng), (pscale, p_pscale),
               (brb, p_brb), (wr, p_wr), (cum, k_ebase), (elim, k_elim), (bmask, k_bmask), (bmaskT, k_bmaskT)]
        for n_, (t_, src_) in enumerate(cst):
            em.dma('sp', lambda e, t_=t_, src_=src_: e.dma_start(out=t_[:], in_=src_), w=[('c', n_)])
        CK = [('c', n_) for n_ in range(len(cst))]
        cidx = {id(t_): ('c', n_) for n_, (t_, _) in enumerate(cst)}

        def ck(*ts):
            return [cidx[id(t)] for t in ts]

        small = sb('small', [128, 40 * 32])
        smi = {'i': 0}

        def sm(n=1):
            assert n <= 32
            i = smi['i']
            smi['i'] = (i + 1) % 40
            return small[:, i * 32:i * 32 + n], ('sm', i)

        epsc = sb('epsc', [128, 4])
        ctf_t = sb('ctf_t', [128, 16])
        scb_t = sb('scb_t', [128, 16], BF16)
        em.op('dve', lambda e: e.memset(epsc[:, 0:1], EPS), w=['epsc'])
        em.op('dve', lambda e: e.memset(epsc[:, 1:2], 0.0), w=['epsc'])
        em.op('dve', lambda e: e.memset(epsc[:, 2:3], 1.0), w=['epsc'])

        p1 = ExitStack()
        es.enter_context(p1)
        winb = sb('winb', [128, 8, WIN_RES], BF16, p1)
        poolwb = sb('poolwb', [128, 4, 128], BF16, p1)
        wst = [sb('wst%d' % i, [128, 4096], BF16, p1) for i in range(4)]
        wsi = {'i': 0}
        G2b = sb('G2b', [128, D], F32, p1)
        SH2b = sb('SH2b', [128, D], F32, p1)
        GT1b = sb('GT1b', [128, D], F32, p1)
        modp = sb('modp', [128, 2, 2, 8], F32, p1)
        r2k = Ring(nc, p1, 'r2k', 7, 2048)
        r4k = Ring(nc, p1, 'r4k', 4, 4096)
        nq = Ring(nc, p1, 'nq', 12, 1024)
        xnT = sb('xnT', [128, 8, BLK], BF16, p1)
        qT = sb('qT', [128, 4, BLK], BF16, p1)
        kT = sb('kT', [128, 4, BLK], BF16, p1)
        vT = sb('vT', [128, 4, BLK], BF16, p1)
        szT = sb('szT', [128, 4, BLK], BF16, p1)
        puT = sb('puT', [128, 4, 16 + BLK], F32, p1)
        ybT = sb('ybT', [128, 4, BLK], BF16, p1)
        yaT = sb('yaT', [128, 4, BLK], BF16, p1)
        mixedT = sb('mixedT', [128, 8, BLK], BF16, p1)
        halo = sb('halo', [128, 12, 4], F32, p1)
        gtm = sb('gtm', [128, 4, 8], F32, p1)
        S = sb('S', [128, 4, 128], F32, p1)
        Sb = sb('Sb', [128, 4, 128], BF16, p1)
        cq = {n_: sb('cq_' + n_, [128, 4, 128], BF16, p1) for n_ in ('DT', 'Ds', 'Er', 'kbd', 'kdec', 'vb', 'N0', 'Pt0')}
        xn2T = sb('xn2T', [128, 8, 128], F32, p1)
        lgb = sb('lgb', [128, 4, 36], F32, p1)

        w_in_v = w_in.rearrange("(c p) n -> p c n", p=128)
        for kc in range(8):
            for hf in range(2):
                c0 = hf * (WIN_RES // 2)
                c1 = c0 + WIN_RES // 2
                em.dma('pool', lambda e, kc=kc, c0=c0, c1=c1: e.dma_start(out=winb[:, kc, c0:c1], in_=w_in_v[:, kc, c0:c1]),
                       w=[('winb', kc)])
        WINK = [('winb', kc) for kc in range(8)]
        em.dma('pool', lambda e: e.dma_start(out=poolwb[:], in_=pool_w.rearrange("g c d -> c g d")), w=['poolwb'])

        kctf, kscb = 'ctf', 'scb'
        em.dma('sp', lambda e: e.dma_start(out=ctf_t[:, 0:16], in_=cT.rearrange("p c b -> p (c b)")), w=[kctf])
        em.op('act', lambda e: e.activation(out=scb_t[:, 0:16], in_=ctf_t[:, 0:16], func=AF.Silu), r=[kctf], w=[kscb])
        scb = scb_t[:, 0:16].rearrange("p (c b) -> p c b", b=2)

        def wpiece_load(src_ap_fn, wkey):
            i = wsi['i']
            wsi['i'] = (i + 1) % 4
            buf = wst[i]
            em.dma('pool', lambda e: src_ap_fn(e, buf), w=[('wst', i)])
            return buf, ('wst', i)

        w_ada_v = w_ada.rearrange("(c p) n -> p c n", p=128)
        for nt in range(12):
            buf, kb = wpiece_load(lambda e, buf, nt=nt: e.dma_start(
                out=buf[:].rearrange("p (c n) -> p c n", n=512), in_=w_ada_v[:, :, nt * 512:(nt + 1) * 512]), None)
            bt, kbt = r2k.get()
            em.dma('sp', lambda e, bt=bt, nt=nt: e.dma_start(out=bt[0:2, :], in_=b_ada[0:1, nt * 512:(nt + 1) * 512].partition_broadcast(2)),
                   w=[kbt])
            pf, pb, kp = ps()
            bv = buf[:].rearrange("p (c n) -> p c n", n=512)
            em.mm([lambda e, kc=kc, pf=pf, bv=bv: e.matmul(pf[0:2, :], lhsT=scb[:, kc, :], rhs=bv[:, kc, :], start=(kc == 0), stop=(kc == 7))
                   for kc in range(8)], r=[kscb, kb], w=[kp])
            mr, kmr = r2k.get()
            em.op('dve', lambda e, mr=mr, pf=pf, bt=bt: e.tensor_tensor(out=mr[0:2, :], in0=pf[0:2, :], in1=bt[0:2, :], op=ALU.add),
                  r=[kp, kbt], w=[kmr])
            em.dma('sp', lambda e, mr=mr, nt=nt: e.dma_start(out=modD[:, nt * 512:(nt + 1) * 512], in_=mr[0:2, :]), r=[kmr], w=[('modD', nt)])

        w_la_v = w_la.rearrange("(c p) n -> p c n", p=128)
        w_lb_v = w_lb.rearrange("(c p) n -> p c n", p=128)
        w_out_v = w_out.rearrange("(c p) n -> p c n", p=128)
        piece_src = []
        for i in range(4):
            c0 = WIN_RES + i * 512
            piece_src.append((w_in_v[:, :, c0:c0 + 512], 512))
        piece_src.append((w_la_v, 1024))
        piece_src.append((w_lb_v, 1024))
        piece_src.append((w_out_v[:, :, 0:512], 512))
        piece_src.append((w_out_v[:, :, 512:1024], 512))
        for pi, (src_, n_) in enumerate(piece_src):
            buf, kb = wpiece_load(lambda e, buf, src_=src_, n_=n_: e.dma_start(out=buf[:].rearrange("p (c n) -> p c n", n=n_), in_=src_), None)
            em.dma('sp', lambda e, buf=buf, pi=pi: e.dma_start(out=wsD[pi], in_=buf[:]), r=[kb], w=[('wsD', pi)])

        def wpiece(pi, i):
            buf = wst[i]
            em.dma('sp', lambda e: e.dma_start(out=buf[:], in_=wsD[pi]), r=[('wsD', pi)], w=[('wst', i)])
            return buf, ('wst', i)

        MODK = [('modD', nt) for nt in range(12)]

        def load_seq_mod(seq):
            sh1 = modD[seq, 0:1024].rearrange("(c p) -> p c", p=128)
            sc1 = modD[seq, 1024:2048].rearrange("(c p) -> p c", p=128)
            tmp, kt = sm(8)
            em.dma('sp', lambda e: e.dma_start(out=modp[:, seq, 1, :], in_=sh1, allow_slow_non_contiguous=True), r=MODK, w=[('modp', seq, 1)])
            em.dma('sp', lambda e: e.dma_start(out=tmp, in_=sc1, allow_slow_non_contiguous=True), r=MODK, w=[kt])
            em.op('dve', lambda e: e.scalar_tensor_tensor(out=modp[:, seq, 0, :], in0=tmp, scalar=1.0, in1=n1g[:], op0=ALU.add, op1=ALU.mult),
                  r=[kt] + ck(n1g), w=[('modp', seq, 0)])
            em.dma('sp', lambda e: e.dma_start(out=GT1b[:], in_=modD[seq:seq + 1, 2048:3072].partition_broadcast(128)), r=MODK, w=['GT1b'])
            em.dma('sp', lambda e: e.dma_start(out=SH2b[:], in_=modD[seq:seq + 1, 3072:4096].partition_broadcast(128)), r=MODK, w=['SH2b'])
            t4, k4 = r4k.get()
            em.dma('sp', lambda e: e.dma_start(out=t4, in_=modD[seq:seq + 1, 4096:5120].partition_broadcast(128)), r=MODK, w=[k4])
            n2, kn2 = r4k.get()
            em.dma('sp', lambda e: e.dma_start(out=n2, in_=p_n2g), w=[kn2])
            em.op('dve', lambda e: e.scalar_tensor_tensor(out=G2b[:], in0=t4, scalar=1.0, in1=n2, op0=ALU.add, op1=ALU.mult),
                  r=[k4, kn2], w=['G2b'])

        def rstd_from_ss(ss_ap, kss, scale):
            l1, kl1 = sm(1)
            em.op('act', lambda e: e.activation(out=l1, in_=ss_ap, func=AF.Ln, bias=epsc[:, 0:1], scale=scale), r=[kss, 'epsc'], w=[kl1])
            r1, kr1 = sm(1)
            em.op('act', lambda e: e.activation(out=r1, in_=l1, func=AF.Exp, scale=-0.5), r=[kl1], w=[kr1])
            return r1, kr1


        def proj_fm(col0, ncols=128):
            pf, pb, kp = ps()
            em.mm([lambda e, kc=kc, pf=pf: e.matmul(pf[0:ncols, :], lhsT=winb[:, kc, col0:col0 + ncols], rhs=xnT[:, kc, :],
                                                     start=(kc == 0), stop=(kc == 7)) for kc in range(8)],
                  r=WINK + ['xnT'], w=[kp])
            return pf, kp

        dbg_state = {'off': 0}

        def dump(ap_f32_128xN, keys, n):
            if dbg_t is None:
                return
            o = dbg_state['off']
            dbg_state['off'] = o + n
            em.dma('sp', lambda e: e.dma_start(out=dbg_t[:, o:o + n], in_=ap_f32_128xN), r=keys, w=[('dbg', o)])

        def dump_any(ap, keys, n):
            if dbg_t is None:
                return
            t, kt = r2k.get()
            em.op('dve', lambda e: e.tensor_copy(out=t[:, 0:n], in_=ap), r=keys, w=[kt])
            dump(t[:, 0:n], [kt], n)

        try:
            for b in range(NBLK):
                seq, blk = divmod(b, NBLK // 2)
                t0 = b * BLK
                first = (blk == 0)
                if first:
                    load_seq_mod(seq)
                    em.op('pool', lambda e: e.memset(S[:], 0.0), w=['S'])
                    em.op('pool', lambda e: e.memset(Sb[:], 0.0), w=['Sb'])
                    em.op('pool', lambda e: e.memset(halo[:], 0.0), w=[('halo', ct_) for ct_ in range(12)])
                    em.op('pool', lambda e: e.memset(puT[:, :, 0:16], 0.0), w=[('puT', g_) for g_ in range(4)])
                for j in range(4):
                    xin, kx = r4k.get()
                    em.dma('sp', lambda e, xin=xin, j=j: e.dma_start(out=xin, in_=x[t0 + j * 128:t0 + (j + 1) * 128, :]), w=[kx])
                    junk, kj = r2k.get(BF16)
                    ss, kss = sm(1)
                    em.op('act', lambda e, xin=xin, junk=junk, ss=ss: e.activation(out=junk, in_=xin, func=AF.Square, accum_out=ss), r=[kx], w=[kj, kss])
                    rs, krs = rstd_from_ss(ss, kss, 1.0 / D)
                    xsb, kxs = r2k.get(BF16)
                    em.op('dve', lambda e, xsb=xsb, xin=xin, rs=rs: e.tensor_scalar(out=xsb, in0=xin, scalar1=rs, scalar2=None, op0=ALU.mult),
                          r=[kx, krs], w=[kxs])
                    pf, pb, kp = ps()
                    em.mm([lambda e, kc=kc, pb=pb, xsb=xsb: e.transpose(pb[:, kc * 128:(kc + 1) * 128], xsb[:, kc * 128:(kc + 1) * 128], identb[:])
                           for kc in range(8)], r=[kxs] + ck(identb), w=[kp])
                    for kc in range(8):
                        em.op('act', lambda e, kc=kc, pb=pb, j=j: e.activation(
                            out=xnT[:, kc, j * 128:(j + 1) * 128], in_=pb[:, kc * 128:(kc + 1) * 128], func=AF.Identity,
                            scale=modp[:, seq, 0, kc:kc + 1], bias=modp[:, seq, 1, kc:kc + 1]),
                            r=[kp, ('modp', seq, 0), ('modp', seq, 1)], w=['xnT'])
                if dbg_t is not None and b == 0 and 'xnT' in DBGSEL:
                    for kc in range(8):
                        dump_any(xnT[:, kc, :], ['xnT'], 512)

                if b == 0:
                    chk('A')
                for ct in range(12):
                    pf, kp = proj_fm(ct * 128)
                    pre, kpre = r4k.get()
                    em.op('pool', lambda e, pre=pre, ct=ct: e.tensor_copy(out=pre[:, 0:4], in_=halo[:, ct, :]), r=[('halo', ct)], w=[kpre])
                    em.op('act', lambda e, pre=pre, pf=pf: e.activation(out=pre[:, 4:516], in_=pf, func=AF.Copy), r=[kp], w=[kpre])
                    em.op('pool', lambda e, pre=pre, ct=ct: e.tensor_copy(out=halo[:, ct, :], in_=pre[:, 512:516]), r=[kpre], w=[('halo', ct)])
                    acc, kacc = r2k.get()
                    em.op('act', lambda e, acc=acc, pf=pf, ct=ct: e.activation(out=acc, in_=pf, func=AF.Copy, scale=convw[:, ct, 3:4]),
                          r=[kp] + ck(convw), w=[kacc])
                    for tap in (2, 1, 0):
                        sh = 3 - tap
                        em.op('dve', lambda e, acc=acc, pre=pre, ct=ct, tap=tap, sh=sh: e.scalar_tensor_tensor(
                            out=acc, in0=pre[:, 4 - sh:516 - sh], scalar=convw[:, ct, tap:tap + 1], in1=acc, op0=ALU.mult, op1=ALU.add),
                            r=[kpre, kacc] + ck(convw), w=[kacc])
                    h = ct % 4
                    if ct >= 8:
                        em.op('act', lambda e, acc=acc, h=h: e.activation(out=vT[:, h, :], in_=acc, func=AF.Silu), r=[kacc], w=['vT'])
                    else:
                        sil, ksil = acc, kacc
                        em.op('act', lambda e, acc=acc, sil=sil: e.activation(out=sil, in_=acc, func=AF.Silu), r=[kacc], w=[ksil])
                        sq, ksq = r2k.get(BF16)
                        em.op('act', lambda e, sq=sq, sil=sil: e.activation(out=sq[:, 0:512], in_=sil, func=AF.Square), r=[ksil], w=[ksq])
                        pf2, pb2, kp2 = ps()
                        em.mm([lambda e, pf2=pf2, sq=sq: e.matmul(pf2, lhsT=onesb[:], rhs=sq[:, 0:512], start=True, stop=True)],
                              r=[ksq] + ck(onesb), w=[kp2])
                        lnv, kln = r2k.get()
                        em.op('act', lambda e, lnv=lnv, pf2=pf2: e.activation(out=lnv, in_=pf2, func=AF.Ln, bias=epsc[:, 0:1]), r=[kp2, 'epsc'], w=[kln])
                        rinv, kri = lnv, kln
                        qs = (128.0 ** -0.5) if ct < 4 else 1.0
                        em.op('act', lambda e, rinv=rinv, lnv=lnv: e.activation(out=rinv, in_=lnv, func=AF.Exp, scale=-0.5), r=[kln], w=[kri])
                        dst = qT if ct < 4 else kT
                        dk = 'qT' if ct < 4 else 'kT'
                        em.op('dve', lambda e, dst=dst, h=h, sil=sil, rinv=rinv, qs=qs: e.scalar_tensor_tensor(
                            out=dst[:, h, :], in0=sil, scalar=qs, in1=rinv, op0=ALU.mult, op1=ALU.mult), r=[ksil, kri], w=[dk])
                if b == 0 and dbg_t is not None and 'B' in DBGSEL:
                    for h_ in range(4):
                        dump_any(qT[:, h_, :], ['qT'], 512)
                    for h_ in range(4):
                        dump_any(kT[:, h_, :], ['kT'], 512)
                    for h_ in range(4):
                        dump_any(vT[:, h_, :], ['vT'], 512)
                if b == 0:
                    chk('B')
                for h in range(4):
                    pf, kp = proj_fm(1536 + h * 128)
                    em.op('act', lambda e, pf=pf, h=h: e.activation(out=szT[:, h, :], in_=pf, func=AF.Silu), r=[kp], w=['szT'])
                for c in range(4):
                    pf, pb, kp = ps()
                    em.mm([lambda e, kc=kc, pf=pf, c=c: e.matmul(pf[:, 0:8], lhsT=xnT[:, kc, c * 128:(c + 1) * 128], rhs=winb[:, kc, 2048:2056],
                                                                  start=(kc == 0), stop=(kc == 7)) for kc in range(8)],
                          r=WINK + ['xnT'], w=[kp])
                    em.op('act', lambda e, pf=pf, c=c: e.activation(out=gtm[:, c, 4:8], in_=pf[:, 4:8], func=AF.Sigmoid), r=[kp], w=[('gtm', c)])
                    xa, kxa = sm(4)
                    em.op('dve', lambda e, xa=xa, pf=pf: e.tensor_tensor(out=xa, in0=pf[:, 0:4], in1=dtb[:], op=ALU.add), r=[kp] + ck(dtb), w=[kxa])
                    ab, kab = sm(4)
                    em.op('act', lambda e, ab=ab, xa=xa: e.activation(out=ab, in_=xa, func=AF.Abs), r=[kxa], w=[kab])
                    ex, kex = sm(4)
                    em.op('act', lambda e, ex=ex, ab=ab: e.activation(out=ex, in_=ab, func=AF.Exp, scale=-1.0), r=[kab], w=[kex])
                    l1p, kl1p = sm(4)
                    em.op('act', lambda e, l1p=l1p, ex=ex: e.activation(out=l1p, in_=ex, func=AF.Ln, bias=epsc[:, 2:3]), r=[kex, 'epsc'], w=[kl1p])
                    sp_, ksp = sm(4)
                    em.op('dve', lambda e, sp_=sp_, xa=xa, l1p=l1p: e.scalar_tensor_tensor(out=sp_, in0=xa, scalar=0.0, in1=l1p, op0=ALU.max, op1=ALU.add),
                          r=[kxa, kl1p], w=[ksp])
                    ea, kea = sm(4)
                    em.op('act', lambda e, ea=ea: e.activation(out=ea, in_=alog[:], func=AF.Exp), r=ck(alog), w=[kea])
                    em.op('dve', lambda e, c=c, sp_=sp_, ea=ea: e.scalar_tensor_tensor(out=gtm[:, c, 0:4], in0=sp_, scalar=-1.0, in1=ea, op0=ALU.mult, op1=ALU.mult),
                          r=[ksp, kea], w=[('gtm', c)])

                if b == 0 and dbg_t is not None and 'B2' in DBGSEL:
                    dump(gtm[:].rearrange('p a b -> p (a b)'), [('gtm', c_) for c_ in range(4)], 32)
                if b == 0:
                    chk('B2')
                for gi, win in enumerate((2, 4, 8, 16)):
                    pf, kp = proj_fm(2056 + gi * 128)
                    em.op('act', lambda e, pf=pf, gi=gi: e.activation(out=puT[:, gi, 16:16 + BLK], in_=pf, func=AF.Copy), r=[kp], w=[('puT', gi)])
                    cur = puT[:, gi, :]
                    kcur = ('puT', gi)
                    w_ = 1
                    nxt = None
                    while w_ < win:
                        nxt, knxt = r4k.get()
                        em.op('pool', lambda e, nxt=nxt, cur=cur, w_=w_: e.tensor_tensor(out=nxt[:, w_:16 + BLK], in0=cur[:, w_:16 + BLK], in1=cur[:, 0:16 + BLK - w_], op=ALU.add),
                              r=[kcur], w=[knxt])
                        cur, kcur = nxt, knxt
                        w_ *= 2
                    pl, kpl = r2k.get()
                    em.op('dve', lambda e, pl=pl, cur=cur, gi=gi, win=win: e.scalar_tensor_tensor(
                        out=pl, in0=cur[:, 16:16 + BLK], scalar=1.0 / win, in1=puT[:, gi, 16:16 + BLK], op0=ALU.mult, op1=ALU.subtract),
                        r=[kcur, ('puT', gi)], w=[kpl])
                    if first:
                        t15, k15 = sm(16)
                        em.op('dve', lambda e, t15=t15, cur=cur, gi=gi: e.tensor_tensor(out=t15, in0=cur[:, 16:32], in1=pcorr[:, gi, :], op=ALU.mult),
                              r=[kcur] + ck(pcorr), w=[k15])
                        em.op('dve', lambda e, t15=t15, pl=pl, gi=gi: e.tensor_tensor(out=pl[:, 0:16], in0=t15, in1=puT[:, gi, 16:32], op=ALU.subtract),
                              r=[k15, ('puT', gi)], w=[kpl])
                    plb, kplb = r2k.get(BF16)
                    em.op('pool', lambda e, plb=plb, pl=pl: e.tensor_copy(out=plb[:, 0:BLK], in_=pl), r=[kpl], w=[kplb])
                    pf2, pb2, kp2 = ps()
                    em.mm([lambda e, pf2=pf2, plb=plb, gi=gi: e.matmul(pf2, lhsT=poolwb[:, gi, :], rhs=plb[:, 0:BLK], start=True, stop=True)],
                          r=[kplb, 'poolwb'], w=[kp2])
                    em.op('act', lambda e, pf2=pf2, gi=gi: e.activation(out=ybT[:, gi, :], in_=pf2, func=AF.Copy, scale=pscale[:, gi:gi + 1]),
                          r=[kp2] + ck(pscale), w=['ybT'])
                    em.op('pool', lambda e, gi=gi: e.tensor_copy(out=puT[:, gi, 0:16], in_=puT[:, gi, BLK:BLK + 16]), r=[('puT', gi), kpl], w=[('puT', gi)])

                if b == 0:
                    chk('C')
                for c in range(4):
                    tsl = slice(c * 128, (c + 1) * 128)
                    g4 = gtm[:, c, 0:4]
                    b4 = gtm[:, c, 4:8]
                    kg = ('gtm', c)
                    pkf, pkb, kpk = ps()
                    em.mm([lambda e, h=h, pkb=pkb: e.transpose(pkb[:, h * 128:(h + 1) * 128], kT[:, h, tsl], identb[:]) for h in range(4)],
                          r=['kT'] + ck(identb), w=[kpk])
                    pvf, pvb, kpv = ps()
                    em.mm([lambda e, h=h, pvb=pvb: e.transpose(pvb[:, h * 128:(h + 1) * 128], vT[:, h, tsl], identb[:]) for h in range(4)],
                          r=['vT'] + ck(identb), w=[kpv])
                    Gt, kGt = r2k.get()
                    for h in range(4):
                        em.op('pool', lambda e, h=h, Gt=Gt: e.tensor_scalar(out=Gt[:, h * 128:(h + 1) * 128], in0=triU[:], scalar1=g4[:, h:h + 1], scalar2=1.0,
                                                                            op0=ALU.mult, op1=ALU.mult), r=[kg] + ck(triU), w=[kGt])
                    pgr, _, kpgr = ps()
                    em.mm([lambda e, pgr=pgr, Gt=Gt: e.matmul(pgr, lhsT=onesf[:], rhs=Gt, start=True, stop=True)], r=[kGt] + ck(onesf), w=[kpgr])
                    pgc, _, kpgc = ps()
                    em.mm([lambda e, pgc=pgc: e.matmul(pgc[:, 0:4], lhsT=triU[:], rhs=g4, start=True, stop=True)], r=[kg] + ck(triU), w=[kpgc])
                    pgl, _, kpgl = ps()
                    em.mm([lambda e, pgl=pgl: e.matmul(pgl[:, 0:4], lhsT=onesf[:], rhs=g4, start=True, stop=True)], r=[kg] + ck(onesf), w=[kpgl])
                    gcc8, kgcc = sm(8)
                    em.op('act', lambda e, gcc8=gcc8, pgc=pgc: e.activation(out=gcc8[:, 0:4], in_=pgc[:, 0:4], func=AF.Copy), r=[kpgc], w=[kgcc])
                    em.op('act', lambda e, gcc8=gcc8, pgl=pgl: e.activation(out=gcc8[:, 4:8], in_=pgl[:, 0:4], func=AF.Copy), r=[kpgl, kgcc], w=[kgcc])
                    gcc = gcc8[:, 0:4]
                    glv = gcc8[:, 4:8]
                    DTl, kDTl = r2k.get()
                    Dsl, kDsl = r2k.get()
                    for h in range(4):
                        hs = slice(h * 128, (h + 1) * 128)
                        em.op('dve', lambda e, hs=hs, h=h, DTl=DTl, pgr=pgr, gcc=gcc: e.scalar_tensor_tensor(
                            out=DTl[:, hs], in0=pgr[:, hs], scalar=gcc[:, h:h + 1], in1=maskT[:], op0=ALU.subtract, op1=ALU.add),
                            r=[kpgr, kgcc] + ck(maskT), w=[kDTl])
                        em.op('dve', lambda e, hs=hs, h=h, Dsl=Dsl, pgr=pgr, gcc=gcc: e.scalar_tensor_tensor(
                            out=Dsl[:, hs], in0=pgr[:, hs], scalar=gcc[:, h:h + 1], in1=maskS[:], op0=ALU.subtract, op1=ALU.add),
                            r=[kpgr, kgcc] + ck(maskS), w=[kDsl])
                    DT, Ds, Er = cq['DT'], cq['Ds'], cq['Er']
                    fl = lambda t: t[:].rearrange("p h n -> p (h n)")
                    em.op('act', lambda e: e.activation(out=fl(DT), in_=DTl, func=AF.Exp), r=[kDTl], w=['DT'])
                    em.op('act', lambda e: e.activation(out=fl(Ds), in_=Dsl, func=AF.Exp, scale=-1.0), r=[kDsl], w=['Ds'])
                    em.op('act', lambda e, pgr=pgr: e.activation(out=fl(Er), in_=pgr, func=AF.Exp), r=[kpgr], w=['Er'])
                    if b == 0 and c == 0:
                        chk('D1')
                    egc, kegc = sm(4)
                    em.op('act', lambda e, egc=egc, gcc=gcc: e.activation(out=egc, in_=gcc, func=AF.Exp), r=[kgcc], w=[kegc])
                    kbs, kkbs = sm(4)
                    em.op('dve', lambda e, kbs=kbs, egc=egc: e.tensor_tensor(out=kbs, in0=egc, in1=b4, op=ALU.mult), r=[kegc, kg], w=[kkbs])
                    dl, kdl = sm(4)
                    em.op('dve', lambda e, dl=dl, glv=glv, gcc=gcc: e.tensor_tensor(out=dl, in0=glv, in1=gcc, op=ALU.subtract), r=[kgcc], w=[kdl])
                    ekd, kekd = sm(4)
                    em.op('act', lambda e, ekd=ekd, dl=dl: e.activation(out=ekd, in_=dl, func=AF.Exp), r=[kdl], w=[kekd])
                    egl, kegl = sm(4)
                    em.op('act', lambda e, egl=egl, glv=glv: e.activation(out=egl, in_=glv, func=AF.Exp), r=[kgcc], w=[kegl])
                    nb4, knb4 = sm(4)
                    em.op('dve', lambda e, nb4=nb4: e.tensor_scalar(out=nb4, in0=b4, scalar1=-1.0, scalar2=None, op0=ALU.mult), r=[kg], w=[knb4])
                    if b == 0 and c == 0:
                        chk('D1b')
                    kbd, kdec, vb = cq['kbd'], cq['kdec'], cq['vb']
                    for h in range(4):
                        hs = slice(h * 128, (h + 1) * 128)
                        em.op('act', lambda e, h=h, hs=hs, pkb=pkb, kbs=kbs: e.activation(out=kbd[:, h, :], in_=pkb[:, hs], func=AF.Identity, scale=kbs[:, h:h + 1], bias=epsc[:, 1:2]),
                              r=[kpk, kkbs], w=['kbd'])
                        em.op('act', lambda e, h=h, hs=hs, pkb=pkb, ekd=ekd: e.activation(out=kdec[:, h, :], in_=pkb[:, hs], func=AF.Identity, scale=ekd[:, h:h + 1], bias=epsc[:, 1:2]),
                              r=[kpk, kekd], w=['kdec'])
                        em.op('act', lambda e, h=h, hs=hs, pvb=pvb: e.activation(out=vb[:, h, :], in_=pvb[:, hs], func=AF.Identity, scale=b4[:, h:h + 1], bias=epsc[:, 1:2]),
                              r=[kpv, kg], w=['vb'])
                    if b == 0 and c == 0:
                        chk('D2')
                    pkk, _, kpkk = ps()
                    em.mm([lambda e, h=h, pkk=pkk: e.matmul(pkk[:, h * 128:(h + 1) * 128], lhsT=kT[:, h, tsl], rhs=kT[:, h, tsl], start=True, stop=True) for h in range(4)],
                          r=['kT'], w=[kpkk])
                    N0 = cq['N0']
                    for h in range(4):
                        hs = slice(h * 128, (h + 1) * 128)
                        em.op('dve', lambda e, h=h, hs=hs, pkk=pkk, nb4=nb4: e.scalar_tensor_tensor(
                            out=N0[:, h, :], in0=pkk[:, hs], scalar=nb4[:, h:h + 1], in1=Ds[:, h, :], op0=ALU.mult, op1=ALU.mult),
                            r=[kpkk, knb4, 'Ds'], w=['N0'])
                    ptf, ptb, kpt = ps()
                    em.mm([lambda e, h=h, ptb=ptb: e.transpose(ptb[:, h * 128:(h + 1) * 128], N0[:, h, :], identb[:]) for h in range(4)],
                          r=['N0'] + ck(identb), w=[kpt])
                    Pt0 = cq['Pt0']
                    em.op('act', lambda e, ptb=ptb: e.activation(out=fl(Pt0), in_=ptb[:, 0:512], func=AF.Identity, scale=1.0, bias=epsc[:, 1:2]), r=[kpt, 'epsc'], w=['Pt0'])
                    Tt, kTt = nq.get(BF16)
                    if b == 0 and c == 0 and dbg_t is not None and 'D3' in DBGSEL:
                        dump_any(fl(DT), ['DT'], 512)
                        dump_any(fl(Ds), ['Ds'], 512)
                        dump_any(fl(N0), ['N0'], 512)
                    if b == 0 and c == 0:
                        chk('D3')
                    Tq, kTq = nq.get(BF16)
                    Cm, kCm = nq.get(BF16)
                    for h in range(4):
                        hs = slice(h * 128, (h + 1) * 128)
                        em.op('dve', lambda e, h=h, hs=hs, Cm=Cm: e.tensor_tensor(out=Cm[:, hs], in0=N0[:, h, :], in1=bmask[:, 0, :], op=ALU.mult), r=['N0'] + ck(bmask), w=[kCm])
                    em.op('dve', lambda e, Tq=Tq, Cm=Cm: e.tensor_tensor(out=Tq, in0=Cm, in1=identq[:], op=ALU.add), r=[kCm] + ck(identq), w=[kTq])
                    Cmt, kCmt = nq.get(BF16)
                    for h in range(4):
                        hs = slice(h * 128, (h + 1) * 128)
                        em.op('dve', lambda e, h=h, hs=hs, Cmt=Cmt: e.tensor_tensor(out=Cmt[:, hs], in0=Pt0[:, h, :], in1=bmaskT[:, 0, :], op=ALU.mult), r=['Pt0'] + ck(bmaskT), w=[kCmt])
                    em.op('dve', lambda e, Tt=Tt, Cmt=Cmt: e.tensor_tensor(out=Tt, in0=Cmt, in1=identq[:], op=ALU.add), r=[kCmt, kTt] + ck(identq), w=[kTt])
                    if b == 0 and c == 0 and dbg_t is not None and 'D3b' in DBGSEL:
                        dump_any(Cm, [kCm], 512)
                        dump_any(Tq, [kTq], 512)
                        dump_any(Tt, [kTt], 512)
                        dump_any(bmask[:, :, :].rearrange('p a b -> p (a b)')[:, 0:512], ck(bmask), 512)
                    if b == 0 and c == 0:
                        chk('D3b')
                    def masks(lev_):
                        Cm_, kCm_ = nq.get(BF16)
                        Cmt_, kCmt_ = nq.get(BF16)
                        for h in range(4):
                            hs = slice(h * 128, (h + 1) * 128)
                            em.op('dve', lambda e, h=h, hs=hs: e.tensor_tensor(out=Cm_[:, hs], in0=N0[:, h, :], in1=bmask[:, lev_, :], op=ALU.mult), r=['N0'] + ck(bmask), w=[kCm_])
                            if lev_ < 6:
                                em.op('dve', lambda e, h=h, hs=hs: e.tensor_tensor(out=Cmt_[:, hs], in0=Pt0[:, h, :], in1=bmaskT[:, lev_, :], op=ALU.mult), r=['Pt0'] + ck(bmaskT), w=[kCmt_])
                        return Cm_, kCm_, Cmt_, kCmt_

                    nxt_masks = masks(1)
                    for lev in range(1, 7):
                        Cm, kCm, Cmt, kCmt = nxt_masks
                        last = (lev == 6)
                        px2, _, kpx2 = ps()
                        em.mm([lambda e, h=h, px2=px2, Cm=Cm, Tt=Tt: e.matmul(px2[:, h * 128:(h + 1) * 128], lhsT=Cm[:, h * 128:(h + 1) * 128], rhs=Tt[:, h * 128:(h + 1) * 128], start=True, stop=True)
                               for h in range(4)], r=[kCm, kTt], w=[kpx2])
                        if lev < 6:
                            nxt_masks = masks(lev + 1)
                        Xs2, kXs2 = nq.get(BF16)
                        em.op('act', lambda e, Xs2=Xs2, px2=px2: e.activation(out=Xs2, in_=px2, func=AF.Copy), r=[kpx2], w=[kXs2])
                        if not last:
                            px1, _, kpx1 = ps()
                            em.mm([lambda e, h=h, px1=px1, Cmt=Cmt, Tq=Tq: e.matmul(px1[:, h * 128:(h + 1) * 128], lhsT=Cmt[:, h * 128:(h + 1) * 128], rhs=Tq[:, h * 128:(h + 1) * 128], start=True, stop=True)
                                   for h in range(4)], r=[kCmt, kTq], w=[kpx1])
                            Xs1, kXs1 = nq.get(BF16)
                            em.op('act', lambda e, Xs1=Xs1, px1=px1: e.activation(out=Xs1, in_=px1, func=AF.Copy), r=[kpx1], w=[kXs1])
                        py2, _, kpy2 = ps()
                        em.mm([lambda e, h=h, py2=py2, Tq=Tq, Xs2=Xs2: e.matmul(py2[:, h * 128:(h + 1) * 128], lhsT=Tq[:, h * 128:(h + 1) * 128], rhs=Xs2[:, h * 128:(h + 1) * 128], start=True, stop=True)
                               for h in range(4)], r=[kTq, kXs2], w=[kpy2])
                        if not last:
                            py1, _, kpy1 = ps()
                            em.mm([lambda e, h=h, py1=py1, Tt=Tt, Xs1=Xs1: e.matmul(py1[:, h * 128:(h + 1) * 128], lhsT=Tt[:, h * 128:(h + 1) * 128], rhs=Xs1[:, h * 128:(h + 1) * 128], start=True, stop=True)
                                   for h in range(4)], r=[kTt, kXs1], w=[kpy1])
                        Ttn, kTtn = nq.get(BF16)
                        em.op('dve', lambda e, Ttn=Ttn, py2=py2, Tt=Tt: e.tensor_tensor(out=Ttn, in0=py2, in1=Tt, op=ALU.add), r=[kpy2, kTt], w=[kTtn])
                        if not last:
                            Tqn, kTqn = nq.get(BF16)
                            em.op('dve', lambda e, Tqn=Tqn, py1=py1, Tq=Tq: e.tensor_tensor(out=Tqn, in0=py1, in1=Tq, op=ALU.add), r=[kpy1, kTq], w=[kTqn])
                            Tq, kTq = Tqn, kTqn
                        Tt, kTt = Ttn, kTtn
                    if b == 0 and c == 0:
                        chk('D4')
                    pw, _, kpw = ps()
                    em.mm([lambda e, h=h, pw=pw, Tt=Tt: e.matmul(pw[:, h * 128:(h + 1) * 128], lhsT=kbd[:, h, :], rhs=Tt[:, h * 128:(h + 1) * 128], start=True, stop=True)
                           for h in range(4)], r=['kbd', kTt], w=[kpw])
                    nwT, knwT = r2k.get(BF16)
                    em.op('act', lambda e, nwT=nwT, pw=pw: e.activation(out=nwT[:, 0:512], in_=pw, func=AF.Copy, scale=-1.0), r=[kpw], w=[knwT])
                    pvn, _, kpvn = ps()
                    fns = []
                    for h in range(4):
                        hs = slice(h * 128, (h + 1) * 128)
                        fns.append(lambda e, h=h, hs=hs, pvn=pvn, Tt=Tt: e.matmul(pvn[:, hs], lhsT=Tt[:, hs], rhs=vb[:, h, :], start=True, stop=False))
                        fns.append(lambda e, h=h, hs=hs, pvn=pvn, nwT=nwT: e.matmul(pvn[:, hs], lhsT=nwT[:, hs], rhs=Sb[:, h, :], start=False, stop=True))
                    em.mm(fns, r=[kTt, 'vb', knwT, 'Sb'], w=[kpvn])
                    vnew, kvnew = r2k.get(BF16)
                    em.op('act', lambda e, vnew=vnew, pvn=pvn: e.activation(out=vnew[:, 0:512], in_=pvn, func=AF.Copy), r=[kpvn], w=[kvnew])
                    if b == 0 and c == 0:
                        chk('D5')
                    pqk, _, kpqk = ps()
                    em.mm([lambda e, h=h, pqk=pqk: e.matmul(pqk[:, h * 128:(h + 1) * 128], lhsT=kT[:, h, tsl], rhs=qT[:, h, tsl], start=True, stop=True) for h in range(4)],
                          r=['kT', 'qT'], w=[kpqk])
                    attnT, kat = r2k.get(BF16)
                    em.op('dve', lambda e, attnT=attnT, pqk=pqk: e.tensor_tensor(out=attnT[:, 0:512], in0=pqk, in1=fl(DT), op=ALU.mult), r=[kpqk, 'DT'], w=[kat])
                    qdT, kqd = r2k.get(BF16)
                    em.op('dve', lambda e, qdT=qdT: e.tensor_tensor(out=qdT[:, 0:512].rearrange("p (h n) -> p h n", n=128), in0=qT[:, :, tsl], in1=Er[:], op=ALU.mult),
                          r=['qT', 'Er'], w=[kqd])
                    po, _, kpo = ps()
                    fns = []
                    for h in range(4):
                        hs = slice(h * 128, (h + 1) * 128)
                        fns.append(lambda e, h=h, hs=hs, po=po, qdT=qdT: e.matmul(po[:, hs], lhsT=Sb[:, h, :], rhs=qdT[:, hs], start=True, stop=False))
                        fns.append(lambda e, h=h, hs=hs, po=po, vnew=vnew, attnT=attnT: e.matmul(po[:, hs], lhsT=vnew[:, hs], rhs=attnT[:, hs], start=False, stop=True))
                    em.mm(fns, r=['Sb', kqd, kvnew, kat], w=[kpo])
                    osq, kosq = r2k.get(BF16)
                    em.op('act', lambda e, osq=osq, po=po: e.activation(out=osq[:, 0:512], in_=po, func=AF.Square), r=[kpo], w=[kosq])
                    pss, _, kpss = ps()
                    em.mm([lambda e, pss=pss, osq=osq: e.matmul(pss, lhsT=onesb[:], rhs=osq[:, 0:512], start=True, stop=True)], r=[kosq] + ck(onesb), w=[kpss])
                    lno, klno = r2k.get()
                    em.op('act', lambda e, lno=lno, pss=pss: e.activation(out=lno, in_=pss, func=AF.Ln, bias=epsc[:, 0:1], scale=1.0 / 128), r=[kpss, 'epsc'], w=[klno])
                    rso, krso = r2k.get()
                    em.op('act', lambda e, rso=rso, lno=lno: e.activation(out=rso, in_=lno, func=AF.Exp, scale=-0.5), r=[klno], w=[krso])
                    t1, kt1 = r2k.get()
                    em.op('dve', lambda e, t1=t1, po=po, rso=rso: e.tensor_tensor(out=t1, in0=po, in1=rso, op=ALU.mult), r=[kpo, krso], w=[kt1])
                    em.op('dve', lambda e, t1=t1: e.scalar_tensor_tensor(out=yaT[:, :, tsl], in0=t1.rearrange("p (h n) -> p h n", n=128), scalar=dng[:, 0:1],
                                                                          in1=szT[:, :, tsl], op0=ALU.mult, op1=ALU.mult),
                          r=[kt1, 'szT'] + ck(dng), w=['yaT'])
                    if b == 0 and c == 0 and dbg_t is not None and 'D6' in DBGSEL:
                        dump_any(Tt, [kTt], 512)
                        dump_any(vnew[:, 0:512], [kvnew], 512)
                        dump_any(attnT[:, 0:512], [kat], 512)
                        dump_any(po, [kpo], 512)
                        dump_any(rso, [krso], 512)
                    if b == 0 and c == 0:
                        chk('D6')
                    pst, _, kpst = ps()
                    em.mm([lambda e, h=h, pst=pst, vnew=vnew: e.matmul(pst[:, h * 128:(h + 1) * 128], lhsT=kdec[:, h, :], rhs=vnew[:, h * 128:(h + 1) * 128], start=True, stop=True)
                           for h in range(4)], r=['kdec', kvnew], w=[kpst])
                    for h in range(4):
                        hs = slice(h * 128, (h + 1) * 128)
                        em.op('dve', lambda e, h=h, hs=hs, pst=pst, egl=egl: e.scalar_tensor_tensor(
                            out=S[:, h, :], in0=S[:, h, :], scalar=egl[:, h:h + 1], in1=pst[:, hs], op0=ALU.mult, op1=ALU.add),
                            r=['S', kegl, kpst, kpo, kpvn], w=['S'])
                    em.op('act', lambda e: e.activation(out=fl(Sb), in_=fl(S), func=AF.Copy), r=['S', kpo, kpvn], w=['Sb'])
                    if b == 0 and c == 0 and dbg_t is not None and 'S1' in DBGSEL:
                        dump(fl(S), ['S'], 512)
                        dump_any(fl(Sb), ['Sb'], 512)
                        dump_any(fl(kdec), ['kdec'], 512)
                    if b == 0 and c == 0:
                        chk('S1')
                if dbg_t is not None and b == 0 and 'sz' in DBGSEL:
                    for h in range(4):
                        dump_any(szT[:, h, :], ['szT'], 512)
                if dbg_t is not None and b == 0 and 'ya' in DBGSEL:
                    for h in range(4):
                        dump_any(yaT[:, h, :], ['yaT'], 512)
                    for h in range(4):
                        dump_any(ybT[:, h, :], ['ybT'], 512)

                if b == 0:
                    chk('D')
                bufs = {}
                bufs[4] = wpiece(4, 0)
                bufs[5] = wpiece(5, 1)
                bufs[0] = wpiece(0, 2)
                bufs[2] = wpiece(2, 3)
                la_v = bufs[4][0][:].rearrange("p (c n) -> p c n", n=1024)
                lb_v = bufs[5][0][:].rearrange("p (c n) -> p c n", n=1024)
                for mt in range(8):
                    if mt == 4:
                        bufs[1] = wpiece(1, 2)
                        bufs[3] = wpiece(3, 3)
                    gbuf_a, kga = bufs[mt // 4]
                    gbuf_b, kgb = bufs[2 + mt // 4]
                    ga_v = gbuf_a[:].rearrange("p (c n) -> p c n", n=512)
                    gb_v = gbuf_b[:].rearrange("p (c n) -> p c n", n=512)
                    cs = slice((mt % 4) * 128, (mt % 4 + 1) * 128)
                    ms = slice(mt * 128, (mt + 1) * 128)
                    pga, _, kpga = ps()
                    em.mm([lambda e, kc=kc, pga=pga, ga_v=ga_v, cs=cs: e.matmul(pga, lhsT=ga_v[:, kc, cs], rhs=xnT[:, kc, :], start=(kc == 0), stop=(kc == 7)) for kc in range(8)],
                          r=[kga, 'xnT'], w=[kpga])
                    pgb, _, kpgb = ps()
                    em.mm([lambda e, kc=kc, pgb=pgb, gb_v=gb_v, cs=cs: e.matmul(pgb, lhsT=gb_v[:, kc, cs], rhs=xnT[:, kc, :], start=(kc == 0), stop=(kc == 7)) for kc in range(8)],
                          r=[kgb, 'xnT'], w=[kpgb])
                    pla, _, kpla = ps()
                    em.mm([lambda e, kc=kc, pla=pla, ms=ms: e.matmul(pla, lhsT=la_v[:, kc, ms], rhs=yaT[:, kc, :], start=(kc == 0), stop=(kc == 3)) for kc in range(4)],
                          r=[bufs[4][1], 'yaT'], w=[kpla])
                    plb_, _, kplb_ = ps()
                    em.mm([lambda e, kc=kc, plb_=plb_, ms=ms: e.matmul(plb_, lhsT=lb_v[:, kc, ms], rhs=ybT[:, kc, :], start=(kc == 0), stop=(kc == 3)) for kc in range(4)],
                          r=[bufs[5][1], 'ybT'], w=[kplb_])
                    sga, ksga = r2k.get()
                    em.op('act', lambda e, sga=sga, pga=pga: e.activation(out=sga, in_=pga, func=AF.Sigmoid), r=[kpga], w=[ksga])
                    sgb, ksgb = r2k.get()
                    em.op('act', lambda e, sgb=sgb, pgb=pgb: e.activation(out=sgb, in_=pgb, func=AF.Sigmoid), r=[kpgb], w=[ksgb])
                    ma, kma = r2k.get()
                    em.op('dve', lambda e, ma=ma, pla=pla, sga=sga: e.tensor_tensor(out=ma, in0=pla, in1=sga, op=ALU.mult), r=[kpla, ksga], w=[kma])
                    mb, kmb = r2k.get()
                    em.op('dve', lambda e, mb=mb, plb_=plb_, sgb=sgb: e.tensor_tensor(out=mb, in0=plb_, in1=sgb, op=ALU.mult), r=[kplb_, ksgb], w=[kmb])
                    em.op('pool', lambda e, mt=mt, ma=ma, mb=mb: e.tensor_tensor(out=mixedT[:, mt, :], in0=ma, in1=mb, op=ALU.add), r=[kma, kmb], w=['mixedT'])
                if b == 0 and dbg_t is not None and 'E1' in DBGSEL:
                    for h_ in range(8):
                        dump_any(mixedT[:, h_, :], ['mixedT'], 512)
                if b == 0:
                    chk('E1')
                wo = [wpiece(6, 2), wpiece(7, 3)]
                xn2bs = []
                def fetch_x(j_):
                    xin_, kx_ = r4k.get()
                    em.dma('sp', lambda e: e.dma_start(out=xin_, in_=x[t0 + j_ * 128:t0 + (j_ + 1) * 128, :]), w=[kx_])
                    return xin_, kx_

                nxt_x = fetch_x(0)
                for j in range(4):
                    tile_idx = b * 4 + j
                    js = slice(j * 128, (j + 1) * 128)
                    xin, kx = nxt_x
                    h1t, kh1 = r4k.get()
                    if j + 1 < 4:
                        nxt_x = fetch_x(j + 1)
                    for hf in range(2):
                        wv = wo[hf][0][:].rearrange("p (c n) -> p c n", n=512)
                        pw_, _, kpw_ = ps()
                        em.mm([lambda e, kc=kc, pw_=pw_, wv=wv, js=js: e.matmul(pw_, lhsT=mixedT[:, kc, js], rhs=wv[:, kc, :], start=(kc == 0), stop=(kc == 7)) for kc in range(8)],
                              r=[wo[hf][1], 'mixedT'], w=[kpw_])
                        fs = slice(hf * 512, (hf + 1) * 512)
                        em.op('dve', lambda e, h1t=h1t, pw_=pw_, fs=fs: e.tensor_tensor(out=h1t[:, fs], in0=pw_, in1=GT1b[:, fs], op=ALU.mult), r=[kpw_, 'GT1b'], w=[kh1])
                        em.op('pool', lambda e, h1t=h1t, xin=xin, fs=fs: e.tensor_tensor(out=h1t[:, fs], in0=h1t[:, fs], in1=xin[:, fs], op=ALU.add), r=[kh1, kx], w=[kh1])
                    em.dma('sp', lambda e, h1t=h1t, j=j: e.dma_start(out=h1D[t0 + j * 128:t0 + (j + 1) * 128, :], in_=h1t), r=[kh1], w=[('h1D', tile_idx)])
                    junk, kj = r2k.get(BF16)
                    ss, kss = sm(1)
                    em.op('act', lambda e, h1t=h1t, junk=junk, ss=ss: e.activation(out=junk, in_=h1t, func=AF.Square, accum_out=ss), r=[kh1], w=[kj, kss])
                    rs, krs = rstd_from_ss(ss, kss, 1.0 / D)
                    xn2f, kxf = r4k.get()
                    em.op('dve', lambda e, xn2f=xn2f, h1t=h1t, rs=rs: e.scalar_tensor_tensor(out=xn2f, in0=h1t, scalar=rs, in1=G2b[:], op0=ALU.mult, op1=ALU.mult),
                          r=[kh1, krs, 'G2b'], w=[kxf])
                    em.op('pool', lambda e, xn2f=xn2f: e.tensor_tensor(out=xn2f, in0=xn2f, in1=SH2b[:], op=ALU.add), r=[kxf, 'SH2b'], w=[kxf])
                    xhost, kxb = (yaT, 'yaT') if j < 2 else (ybT, 'ybT')
                    xn2b = xhost[:].rearrange("p h n -> p (h n)")[:, (j % 2) * 1024:(j % 2 + 1) * 1024]
                    em.op('act', lambda e, xn2b=xn2b, xn2f=xn2f: e.activation(out=xn2b, in_=xn2f, func=AF.Copy), r=[kxf], w=[kxb])
                    xn2bs.append((xn2b, kxb))
                    for half in range(2):
                        ptr, _, kptr = ps()
                        em.mm([lambda e, q=q, ptr=ptr, xn2f=xn2f, half=half: e.transpose(ptr[:, q * 128:(q + 1) * 128], xn2f[:, (half * 4 + q) * 128:(half * 4 + q + 1) * 128], identf[:])
                               for q in range(4)], r=[kxf] + ck(identf), w=[kptr])
                        eng = 'act' if half == 0 else 'dve'
                        if eng == 'act':
                            em.op('act', lambda e, ptr=ptr, half=half: e.activation(out=xn2T[:, half * 4:half * 4 + 4, :].rearrange("p c n -> p (c n)"), in_=ptr, func=AF.Copy),
                                  r=[kptr], w=[('xn2T', half)])
                        else:
                            em.op('dve', lambda e, ptr=ptr, half=half: e.tensor_copy(out=xn2T[:, half * 4:half * 4 + 4, :].rearrange("p c n -> p (c n)"), in_=ptr),
                                  r=[kptr], w=[('xn2T', half)])
                    plg, _, kplg = ps()
                    em.mm([lambda e, kc=kc, plg=plg: e.matmul(plg[:, 0:36], lhsT=xn2T[:, kc, :], rhs=wr[:, kc, :], start=(kc == 0), stop=(kc == 7)) for kc in range(8)],
                          r=[('xn2T', 0), ('xn2T', 1)] + ck(wr), w=[kplg])
                    em.op('dve', lambda e, plg=plg, j=j: e.tensor_tensor(out=lgb[:, j, :], in0=plg[:, 0:36], in1=brb[:], op=ALU.add), r=[kplg] + ck(brb), w=['lgb'])
                    if dbg_t is not None and b == 0 and j == 0 and 'lg' in DBGSEL:
                        dump(lgb[:, 0, :], ['lgb'], 36)
                routing4(em, sm, lgb, onesb, triS, cum, elim, destall, gidxall, wall, b, ps, ck, r2k)
                for j in range(4):
                    tile_idx = b * 4 + j
                    xn2b, kxb = xn2bs[j]
                    for k in range(2):
                        em.dma('pool', lambda e, xn2b=xn2b, k=k, tile_idx=tile_idx: e.indirect_dma_start(
                            out=xsD[:, :], out_offset=IndirectOffsetOnAxis(ap=destall[:, tile_idx * 2 + k:tile_idx * 2 + k + 1], axis=0),
                            in_=xn2b, in_offset=None, bounds_check=pregs['bc'], oob_is_err=False),
                            r=[kxb, ('dest', b)], w=['xsD'])
                if b == 0:
                    chk('E2')
            if dbg_t is not None and 'route' in DBGSEL:
                t, kt = r2k.get()
                em.op('dve', lambda e: e.tensor_copy(out=t[:, 0:64], in_=destall[:]), r=[('dest', i) for i in range(8)], w=[kt])
                dump(t[:, 0:64], [kt], 64)
                dump(wall[:].rearrange("p a b -> p (a b)"), [('dest', i) for i in range(32)], 64)
            chk('P1')
            p1.close()

            p2 = ExitStack()
            es.enter_context(p2)
            wgu = [sb('wgu%d' % i, [128, 8, 512], BF16, p2) for i in range(3)]
            wdn = [sb('wdn%d' % i, [128, 2, D], BF16, p2) for i in range(3)]
            xst = [sb('xst%d' % i, [128, 4, D], BF16, p2) for i in range(2)]
            xsT = [sb('xsT%d' % i, [128, 8, 512], BF16, p2) for i in range(2)]
            hT = [sb('hT%d' % i, [128, 2, 512], BF16, p2) for i in range(2)]
            yst = [sb('yst%d' % i, [128, 4, D], F32, p2) for i in range(2)]
            sgr = Ring(nc, p2, 'sgr', 4, 2048)
            zrow = sb('zrow', [128, D], F32, p2)
            em.op('pool', lambda e: e.memset(zrow[:], 0.0), w=['zrow'])
            em.dma('sp', lambda e: e.dma_start(out=ysD[E * CAP:E * CAP + 128, :], in_=zrow[:]), r=['zrow'], w=[('ysD', 'z')])
            def load_xst(ex_):
                em.dma('sp', lambda e: e.dma_start(out=xst[ex_ % 2][:], in_=xsD[ex_ * CAP:(ex_ + 1) * CAP, :].rearrange("(j p) d -> p j d", p=128)),
                       r=['xsD'], w=[('xst', ex_ % 2)])

            for ex in range(E):
                i2 = ex % 2
                wg_v = w_gate[ex].rearrange("(c p) n -> p c n", p=128)
                wu_v = w_up[ex].rearrange("(c p) n -> p c n", p=128)
                wd_v = w_down[ex].rearrange("(c p) n -> p c n", p=128)
                i3 = ex % 3
                em.dma('pool', lambda e, i3=i3, wg_v=wg_v: e.dma_start(out=wgu[i3][:, :, 0:256], in_=wg_v), w=[('wgu', i3, 0)])
                em.dma('pool', lambda e, i3=i3, wu_v=wu_v: e.dma_start(out=wgu[i3][:, :, 256:512], in_=wu_v), w=[('wgu', i3, 1)])
                em.dma('pool', lambda e, i3=i3, wd_v=wd_v: e.dma_start(out=wdn[i3][:], in_=wd_v), w=[('wdn', i3)])
                if ex == 0:
                    load_xst(0)
                if ex + 1 < E:
                    load_xst(ex + 1)
                for kp_ in range(4):
                    pf, pb, kp = ps()
                    fns = []
                    for q in range(2):
                        kc = kp_ * 2 + q
                        for j in range(4):
                            fns.append(lambda e, q=q, j=j, kc=kc, pb=pb, i2=i2: e.transpose(pb[:, q * 512 + j * 128:q * 512 + (j + 1) * 128], xst[i2][:, j, kc * 128:(kc + 1) * 128], identb[:]))
                    em.mm(fns, r=[('xst', i2)] + ck(identb), w=[kp])
                    dst = xsT[i2][:, kp_ * 2:kp_ * 2 + 2, :].rearrange("p c n -> p (c n)")
                    em.op('act', lambda e, dst=dst, pb=pb: e.activation(out=dst, in_=pb, func=AF.Identity, scale=1.0, bias=epsc[:, 1:2]), r=[kp, 'epsc'], w=[('xsT', i2)])
                pgs = []
                for ft in range(4):
                    pf, pb, kp = ps()
                    em.mm([lambda e, kc=kc, pf=pf, ft=ft, i2=i2, i3=i3: e.matmul(pf, lhsT=wgu[i3][:, kc, ft * 128:(ft + 1) * 128], rhs=xsT[i2][:, kc, :], start=(kc == 0), stop=(kc == 7))
                           for kc in range(8)], r=[('wgu', i3, ft // 2), ('xsT', i2)], w=[kp])
                    pgs.append((pf, kp))
                for f in range(2):
                    sg, ksg = sgr.get()
                    em.op('act', lambda e, sg=sg, f=f, pgs=pgs: e.activation(out=sg, in_=pgs[f][0], func=AF.Silu), r=[pgs[f][1]], w=[ksg])
                    em.op('dve', lambda e, sg=sg, f=f, pgs=pgs, i2=i2: e.tensor_tensor(out=hT[i2][:, f, :], in0=pgs[2 + f][0], in1=sg, op=ALU.mult),
                          r=[pgs[2 + f][1], ksg], w=[('hT', i2)])
                for j in range(4):
                    for hf in range(2):
                        pf, pb, kp = ps()
                        em.mm([lambda e, f=f, pf=pf, j=j, hf=hf, i2=i2, i3=i3: e.matmul(pf, lhsT=hT[i2][:, f, j * 128:(j + 1) * 128], rhs=wdn[i3][:, f, hf * 512:(hf + 1) * 512],
                                                                                  start=(f == 0), stop=(f == 1)) for f in range(2)], r=[('hT', i2), ('wdn', i3)], w=[kp])
                        if (j * 2 + hf) % 2 == 0:
                            em.op('act', lambda e, pf=pf, j=j, hf=hf, i2=i2: e.activation(out=yst[i2][:, j, hf * 512:(hf + 1) * 512], in_=pf, func=AF.Copy), r=[kp], w=[('yst', i2)])
                        else:
                            em.op('dve', lambda e, pf=pf, j=j, hf=hf, i2=i2: e.tensor_copy(out=yst[i2][:, j, hf * 512:(hf + 1) * 512], in_=pf), r=[kp], w=[('yst', i2)])
                em.dma('sp', lambda e, i2=i2, ex=ex: e.dma_start(out=ysD[ex * CAP:(ex + 1) * CAP, :].rearrange("(j p) d -> p j d", p=128), in_=yst[i2][:]),
                       r=[('yst', i2)], w=[('ysD', ex)])
            chk('P2')
            p2.close()

            p3 = ExitStack()
            es.enter_context(p3)
            GT2b = sb('GT2b', [128, D], F32, p3)
            fngb = sb('fngb', [128, D], F32, p3)
            em.dma('sp', lambda e: e.dma_start(out=fngb[:], in_=p_fng), w=['fngb'])
            q4 = Ring(nc, p3, 'q4', 20, 4096)
            q2 = Ring(nc, p3, 'q2', 2, 2048)
            out_toks = []
            def fetch(ti):
                y0, ky0 = q4.get()
                y1, ky1 = q4.get()
                for k, (yy, kyy) in enumerate(((y0, ky0), (y1, ky1))):
                    em.dma('pool', lambda e, yy=yy, k=k, ti=ti: e.indirect_dma_start(
                        out=yy, out_offset=None, in_=ysD[:, :], in_offset=IndirectOffsetOnAxis(ap=gidxall[:, ti * 2 + k:ti * 2 + k + 1], axis=0)),
                        r=[('ysD', 'z')] + [('ysD', ex_) for ex_ in range(E)] + [('dest', ti // 4)], w=[kyy])
                h1t, kh1 = q4.get()
                em.dma('sp', lambda e, h1t=h1t, ti=ti: e.dma_start(out=h1t, in_=h1D[ti * 128:(ti + 1) * 128, :]), r=[('h1D', ti)], w=[kh1])
                return y0, ky0, y1, ky1, h1t, kh1

            pend = [fetch(0), fetch(1)]
            for ti in range(32):
                seq = ti // 16
                if ti % 16 == 0:
                    em.dma('sp', lambda e, seq=seq: e.dma_start(out=GT2b[:], in_=modD[seq:seq + 1, 5120:6144].partition_broadcast(128)), r=MODK, w=['GT2b'])
                y0, ky0, y1, ky1, h1t, kh1 = pend.pop(0)
                if ti + 2 < 32:
                    pend.append(fetch(ti + 2))
                m, km = q4.get()
                em.op('act', lambda e, m=m, y0=y0, ti=ti: e.activation(out=m, in_=y0, func=AF.Copy, scale=wall[:, ti, 0:1]), r=[ky0, ('dest', ti // 4)], w=[km])
                em.op('dve', lambda e, m=m, y1=y1, ti=ti: e.scalar_tensor_tensor(out=m, in0=y1, scalar=wall[:, ti, 1:2], in1=m, op0=ALU.mult, op1=ALU.add),
                      r=[ky1, km, ('dest', ti // 4)], w=[km])
                em.op('pool', lambda e, m=m: e.tensor_tensor(out=m, in0=m, in1=GT2b[:], op=ALU.mult), r=[km, 'GT2b'], w=[km])
                em.op('dve', lambda e, m=m, h1t=h1t: e.tensor_tensor(out=m, in0=m, in1=h1t, op=ALU.add), r=[km, kh1], w=[km])
                junk, kj = q2.get(BF16)
                ss, kss = sm(1)
                em.op('act', lambda e, m=m, junk=junk, ss=ss: e.activation(out=junk, in_=m, func=AF.Square, accum_out=ss), r=[km], w=[kj, kss])
                rs, krs = rstd_from_ss(ss, kss, 1.0 / D)
                o_, ko = q4.get()
                em.op('dve', lambda e, o_=o_, m=m, rs=rs: e.scalar_tensor_tensor(out=o_, in0=m, scalar=rs, in1=fngb[:], op0=ALU.mult, op1=ALU.mult), r=[km, krs, 'fngb'], w=[ko])
                out_toks.append(em.dma('sp', lambda e, o_=o_, ti=ti: e.dma_start(out=out[ti * 128:(ti + 1) * 128, :], in_=o_), r=[ko], w=[('out', ti)]))
        except StopBuild:
            pass
        em.wait_keys('sp', [k for k in em.lastw if isinstance(k, tuple) and k[0] in ('dbg', 'out')])
        for q_ in ('sp', 'pool'):
            d_ = em.dq[q_]
            for i_, v_ in enumerate(d_['vals']):
                if v_ > 0:
                    em._wait('sp', (q_, i_), v_)
        em.finish()
    return nc


DBGSEL = ()
STOP = None
SERIAL = False


class StopBuild(Exception):
    pass


def chk(name):
    if STOP == name:
        raise StopBuild()


def routing(em, sm, lg, onesb, triS, cum, elim, destall, gidxall, wall, ti, ps, ck, r2k):
    kl = 'lg'
    gmax, kgm = sm(1)
    em.op('dve', lambda e: e.tensor_reduce(out=gmax, in_=lg[:, 0:4], axis=AX.X, op=ALU.max), r=[kl], w=[kgm])
    ohg, kohg = sm(4)
    em.op('dve', lambda e: e.tensor_scalar(out=ohg, in0=lg[:, 0:4], scalar1=gmax, scalar2=None, op0=ALU.is_equal), r=[kl, kgm], w=[kohg])
    ngm, kngm = sm(1)
    em.op('dve', lambda e: e.tensor_scalar(out=ngm, in0=gmax, scalar1=-1.0, scalar2=None, op0=ALU.mult), r=[kgm], w=[kngm])
    eg, keg = sm(4)
    sg, ksg = sm(1)
    em.op('act', lambda e: e.activation(out=eg, in_=lg[:, 0:4], func=AF.Exp, bias=ngm, accum_out=sg), r=[kl, kngm], w=[keg, ksg])
    pg, kpg = sm(1)
    em.op('dve', lambda e: e.reciprocal(out=pg, in_=sg), r=[ksg], w=[kpg])
    les, kles = sm(8)
    em.op('dve', lambda e: e.tensor_scalar(out=les, in0=lg[:, 4:12], scalar1=ohg[:, 0:1], scalar2=None, op0=ALU.mult), r=[kl, kohg], w=[kles])
    for g in range(1, 4):
        em.op('dve', lambda e, g=g: e.scalar_tensor_tensor(out=les, in0=lg[:, 4 + 8 * g:12 + 8 * g], scalar=ohg[:, g:g + 1], in1=les, op0=ALU.mult, op1=ALU.add),
              r=[kl, kohg, kles], w=[kles])
    m8, km8 = sm(8)
    em.op('dve', lambda e: e.max(out=m8, in_=les), r=[kles], w=[km8])
    d21, kd21 = sm(1)
    em.op('dve', lambda e: e.tensor_tensor(out=d21, in0=m8[:, 1:2], in1=m8[:, 0:1], op=ALU.subtract), r=[km8], w=[kd21])
    e21, ke21 = sm(1)
    em.op('act', lambda e: e.activation(out=e21, in_=d21, func=AF.Exp), r=[kd21], w=[ke21])
    den, kden = sm(1)
    em.op('dve', lambda e: e.tensor_scalar(out=den, in0=e21, scalar1=1.0, scalar2=None, op0=ALU.add), r=[ke21], w=[kden])
    rden, krden = sm(1)
    em.op('dve', lambda e: e.reciprocal(out=rden, in_=den), r=[kden], w=[krden])
    w1, kw1 = sm(1)
    em.op('dve', lambda e: e.tensor_tensor(out=w1, in0=pg, in1=rden, op=ALU.mult), r=[kpg, krden], w=[kw1])
    w2, kw2 = sm(1)
    em.op('dve', lambda e: e.tensor_tensor(out=w2, in0=w1, in1=e21, op=ALU.mult), r=[kw1, ke21], w=[kw2])
    ohs = []
    for k in range(2):
        sel, ksel = sm(8)
        em.op('dve', lambda e, k=k, sel=sel: e.tensor_scalar(out=sel, in0=les, scalar1=m8[:, k:k + 1], scalar2=None, op0=ALU.is_equal), r=[kles, km8], w=[ksel])
        oh, koh = sm(32)
        for g in range(4):
            em.op('dve', lambda e, g=g, oh=oh, sel=sel: e.tensor_scalar(out=oh[:, g * 8:(g + 1) * 8], in0=sel, scalar1=ohg[:, g:g + 1], scalar2=None, op0=ALU.mult),
                  r=[ksel, kohg], w=[koh])
        ohs.append((oh, koh))
    ohsum, kohs = r2k.get(BF16)
    em.op('dve', lambda e: e.tensor_tensor(out=ohsum[:, 0:32], in0=ohs[0][0], in1=ohs[1][0], op=ALU.add), r=[ohs[0][1], ohs[1][1]], w=[kohs])
    pr, _, kpr = ps()
    em.mm([lambda e: e.matmul(pr[:, 0:32], lhsT=triS[:], rhs=ohsum[:, 0:32], start=True, stop=True),
           lambda e: e.matmul(pr[:, 32:64], lhsT=onesb[:], rhs=ohsum[:, 0:32], start=True, stop=True)], r=[kohs] + ck(triS, onesb), w=[kpr])
    rk, krk = sm(32)
    em.op('dve', lambda e: e.tensor_tensor(out=rk, in0=pr[:, 0:32], in1=cum[:], op=ALU.add), r=[kpr, 'cum'], w=[krk])
    em.op('dve', lambda e: e.tensor_tensor(out=cum[:], in0=pr[:, 32:64], in1=cum[:], op=ALU.add), r=[kpr, 'cum', krk], w=['cum'])
    for k in range(2):
        oh, koh = ohs[k]
        t32, kt32 = sm(32)
        dst, kdst = sm(1)
        em.op('dve', lambda e, t32=t32, oh=oh: e.tensor_tensor(out=t32, in0=oh, in1=rk, op=ALU.mult), r=[koh, krk], w=[kt32])
        em.op('dve', lambda e, t32=t32, dst=dst: e.tensor_reduce(out=dst, in_=t32, axis=AX.X, op=ALU.add), r=[kt32], w=[kdst])
        l32, kl32 = sm(32)
        lim, klim = sm(1)
        em.op('dve', lambda e, l32=l32, oh=oh: e.tensor_tensor(out=l32, in0=oh, in1=elim[:], op=ALU.mult), r=[koh] + ck(elim), w=[kl32])
        em.op('dve', lambda e, l32=l32, lim=lim: e.tensor_reduce(out=lim, in_=l32, axis=AX.X, op=ALU.add), r=[kl32], w=[klim])
        ok, kok = sm(1)
        em.op('dve', lambda e, ok=ok, dst=dst, lim=lim: e.tensor_tensor(out=ok, in0=dst, in1=lim, op=ALU.is_lt), r=[kdst, klim], w=[kok])
        nok, knok = sm(1)
        em.op('dve', lambda e, nok=nok, ok=ok: e.tensor_scalar(out=nok, in0=ok, scalar1=-1.0, scalar2=1.0, op0=ALU.mult, op1=ALU.add), r=[kok], w=[knok])
        dv, kdv = sm(1)
        em.op('dve', lambda e, dv=dv, dst=dst, ok=ok: e.tensor_tensor(out=dv, in0=dst, in1=ok, op=ALU.mult), r=[kdst, kok], w=[kdv])
        si, ksi = sm(1)
        em.op('dve', lambda e, si=si, nok=nok, dv=dv: e.scalar_tensor_tensor(out=si, in0=nok, scalar=float(E * CAP + 64), in1=dv, op0=ALU.mult, op1=ALU.add), r=[knok, kdv], w=[ksi])
        gi_, kgi = sm(1)
        em.op('dve', lambda e, gi_=gi_, nok=nok, dv=dv: e.scalar_tensor_tensor(out=gi_, in0=nok, scalar=float(E * CAP), in1=dv, op0=ALU.mult, op1=ALU.add), r=[knok, kdv], w=[kgi])
        em.op('dve', lambda e, k=k, si=si: e.tensor_copy(out=destall[:, ti * 2 + k:ti * 2 + k + 1], in_=si), r=[ksi], w=[('dest', ti)])
        em.op('dve', lambda e, k=k, gi_=gi_: e.tensor_copy(out=gidxall[:, ti * 2 + k:ti * 2 + k + 1], in_=gi_), r=[kgi], w=[('dest', ti)])
        wk, kwk = (w1, kw1) if k == 0 else (w2, kw2)
        em.op('dve', lambda e, k=k, wk=wk, ok=ok: e.tensor_tensor(out=wall[:, ti, k:k + 1], in0=wk, in1=ok, op=ALU.mult), r=[kwk, kok], w=[('dest', ti)])


def _consts():
    bf = ml_dtypes.bfloat16
    i = np.arange(128)
    c = {}
    c['k_identb'] = np.eye(128, dtype=np.float32).astype(bf)
    c['k_identq'] = np.tile(np.eye(128, dtype=np.float32), (1, 4)).astype(bf)
    c['k_identf'] = np.eye(128, dtype=np.float32)
    c['k_onesb'] = np.ones((128, 128), np.float32).astype(bf)
    c['k_onesf'] = np.ones((128, 128), np.float32)
    c['k_triU'] = (i[:, None] <= i[None, :]).astype(np.float32)
    c['k_maskT'] = np.where(i[None, :] >= i[:, None], 0.0, NEG).astype(np.float32)
    c['k_maskS'] = np.where(i[:, None] > i[None, :], 0.0, -NEG).astype(np.float32)
    c['k_triS'] = (i[:, None] < i[None, :]).astype(np.float32).astype(bf)
    pc = np.zeros((128, 4, 16), np.float32)
    for gi, win in enumerate((2, 4, 8, 16)):
        t = np.arange(16)
        pc[:, gi, :] = 1.0 / np.minimum(t + 1, win)
    c['k_pcorr'] = pc
    bm = np.zeros((128, 7, 128), np.float32)
    for l in range(7):
        s_ = 1 << l
        bm[:, l, :] = ((i[:, None] // (2 * s_) == i[None, :] // (2 * s_)) & (i[:, None] % (2 * s_) >= s_) & (i[None, :] % (2 * s_) < s_))
    c['k_bmask'] = bm.astype(bf)
    c['k_bmaskT'] = np.ascontiguousarray(bm.transpose(2, 1, 0)).astype(bf)
    c['k_ebase'] = np.tile((np.arange(32) * CAP).astype(np.float32), (128, 1))
    c['k_elim'] = np.tile(((np.arange(32) + 1) * CAP).astype(np.float32), (128, 1))
    return c


def _prep_inputs(inp):
    f = lambda a: np.ascontiguousarray(np.asarray(a, dtype=np.float32))
    shared = {}
    shared['w_ada'] = f(inp['w_ada'][0])
    shared['b_ada'] = f(inp['b_ada'][0]).reshape(1, -1)
    shared['w_in'] = f(inp['w_in'][0])
    shared['w_lift_a'] = f(inp['w_lift_a'][0])
    shared['w_lift_b'] = f(inp['w_lift_b'][0])
    shared['w_out'] = f(inp['w_out'][0])
    shared['pool_w'] = f(inp['pool_w'][0])
    shared['w_gate'] = f(inp['w_gate'][0])
    shared['w_up'] = f(inp['w_up'][0])
    shared['w_down'] = f(inp['w_down'][0])
    shared['p_n1g'] = f(np.asarray(inp['norm1_g'][0]).reshape(8, 128).T)
    shared['p_convw'] = f(np.asarray(inp['conv_w'][0]).reshape(4, 12, 128).transpose(2, 1, 0))
    shared['p_alog'] = f(np.broadcast_to(np.asarray(inp['a_log'][0]).reshape(1, 4), (128, 4)))
    shared['p_dtb'] = f(np.broadcast_to(np.asarray(inp['dt_bias'][0]).reshape(1, 4), (128, 4)))
    shared['p_dng'] = f(np.asarray(inp['dn_norm_g'][0]).reshape(128, 1))
    shared['p_pscale'] = f(np.asarray(inp['pool_scale'][0]).reshape(4, 128).T)
    shared['p_n2g'] = f(np.broadcast_to(np.asarray(inp['norm2_g'][0]).reshape(1, D), (128, D)))
    shared['p_fng'] = f(np.broadcast_to(np.asarray(inp['final_norm_g']).reshape(1, D), (128, D)))
    br = np.concatenate([np.asarray(inp['b_router_group'][0]), np.asarray(inp['b_router_expert'][0])]).reshape(1, 36)
    shared['p_brb'] = f(np.broadcast_to(br, (128, 36)))
    wrc = np.concatenate([np.asarray(inp['w_router_group'][0]), np.asarray(inp['w_router_expert'][0])], axis=1)
    shared['p_wr'] = f(wrc.reshape(8, 128, 36).transpose(1, 0, 2))
    shared.update(_consts())
    xs = np.asarray(inp['x'], dtype=np.float32)
    cs = np.asarray(inp['c'], dtype=np.float32)
    in_maps = []
    for i in range(NCORES):
        m = dict(shared)
        m['x'] = np.ascontiguousarray(xs[2 * i:2 * i + 2].reshape(TOK, D))
        m['cT'] = np.ascontiguousarray(cs[2 * i:2 * i + 2].reshape(2, 8, 128).transpose(2, 1, 0))
        in_maps.append(m)
    return in_maps


_NC_CACHE = {}


def kernel(**inputs):
    in_maps = _prep_inputs(inputs)
    if 'nc' not in _NC_CACHE:
        _NC_CACHE['nc'] = build_nc()
    nc = _NC_CACHE['nc']
    res = run_bass_kernel_spmd(nc, in_maps, core_ids=list(range(NCORES)))
    outs = [np.asarray(r['out'], dtype=np.float32).reshape(2, 2048, D) for r in res.results]
    return np.concatenate(outs, axis=0)


def routing4(em, sm, lgb, onesb, triS, cum, elim, destall, gidxall, wall, b, ps, ck, r2k):
    kl = 'lgb'
    T = 4

    def v3(ap, n):
        return ap.rearrange("p (t n) -> p t n", n=n)

    def bc_last(ap, n):
        return ap.unsqueeze(2).broadcast_to([128, T, n])
    lgg = lgb[:, :, 0:4]
    gmax, kgm = sm(T)
    em.op('dve', lambda e: e.tensor_reduce(out=gmax, in_=lgg, axis=AX.X, op=ALU.max), r=[kl], w=[kgm])
    ohg_, kohg = sm(16)
    ohg = v3(ohg_, 4)
    em.op('dve', lambda e: e.tensor_tensor(out=ohg, in0=lgg, in1=bc_last(gmax, 4), op=ALU.is_equal), r=[kl, kgm], w=[kohg])
    sub_, ksub = sm(16)
    em.op('dve', lambda e: e.tensor_tensor(out=v3(sub_, 4), in0=lgg, in1=bc_last(gmax, 4), op=ALU.subtract), r=[kl, kgm], w=[ksub])
    eg_, keg = sm(16)
    em.op('act', lambda e: e.activation(out=eg_, in_=sub_, func=AF.Exp), r=[ksub], w=[keg])
    sg, ksg = sm(T)
    em.op('dve', lambda e: e.tensor_reduce(out=sg, in_=v3(eg_, 4), axis=AX.X, op=ALU.add), r=[keg], w=[ksg])
    pg, kpg = sm(T)
    em.op('dve', lambda e: e.reciprocal(out=pg, in_=sg), r=[ksg], w=[kpg])
    prod_, kprod = r2k.get()
    prod = prod_[:, 0:T * 32]
    le4 = lgb[:, :, 4:36].rearrange("p t (g j) -> p t g j", j=8)
    em.op('dve', lambda e: e.tensor_tensor(out=prod.rearrange("p (t g j) -> p t g j", g=4, j=8), in0=le4,
                                           in1=ohg.unsqueeze(3).broadcast_to([128, T, 4, 8]), op=ALU.mult), r=[kl, kohg], w=[kprod])
    les_, kles = sm(32)
    les = v3(les_, 8)
    em.op('dve', lambda e: e.tensor_reduce(out=les, in_=prod.rearrange("p (t g j) -> p t j g", g=4, j=8), axis=AX.X, op=ALU.add), r=[kprod], w=[kles])
    m1, km1 = sm(T)
    em.op('dve', lambda e: e.tensor_reduce(out=m1, in_=les, axis=AX.X, op=ALU.max), r=[kles], w=[km1])
    sel1_, ksel1 = sm(32)
    sel1 = v3(sel1_, 8)
    em.op('dve', lambda e: e.tensor_tensor(out=sel1, in0=les, in1=bc_last(m1, 8), op=ALU.is_equal), r=[kles, km1], w=[ksel1])
    les2_, kles2 = sm(32)
    les2 = v3(les2_, 8)
    em.op('dve', lambda e: e.scalar_tensor_tensor(out=les2, in0=sel1, scalar=NEG, in1=les, op0=ALU.mult, op1=ALU.add), r=[ksel1, kles], w=[kles2])
    m2, km2 = sm(T)
    em.op('dve', lambda e: e.tensor_reduce(out=m2, in_=les2, axis=AX.X, op=ALU.max), r=[kles2], w=[km2])
    sel2_, ksel2 = sm(32)
    sel2 = v3(sel2_, 8)
    em.op('dve', lambda e: e.tensor_tensor(out=sel2, in0=les2, in1=bc_last(m2, 8), op=ALU.is_equal), r=[kles2, km2], w=[ksel2])
    d21, kd21 = sm(T)
    em.op('dve', lambda e: e.tensor_tensor(out=d21, in0=m2, in1=m1, op=ALU.subtract), r=[km1, km2], w=[kd21])
    e21, ke21 = sm(T)
    em.op('act', lambda e: e.activation(out=e21, in_=d21, func=AF.Exp), r=[kd21], w=[ke21])
    den, kden = sm(T)
    em.op('dve', lambda e: e.tensor_scalar(out=den, in0=e21, scalar1=1.0, scalar2=None, op0=ALU.add), r=[ke21], w=[kden])
    rden, krden = sm(T)
    em.op('dve', lambda e: e.reciprocal(out=rden, in_=den), r=[kden], w=[krden])
    w1, kw1 = sm(T)
    em.op('dve', lambda e: e.tensor_tensor(out=w1, in0=pg, in1=rden, op=ALU.mult), r=[kpg, krden], w=[kw1])
    w2, kw2 = sm(T)
    em.op('dve', lambda e: e.tensor_tensor(out=w2, in0=w1, in1=e21, op=ALU.mult), r=[kw1, ke21], w=[kw2])
    ohs = []
    for k, (sel, ksel) in enumerate(((sel1, ksel1), (sel2, ksel2))):
        oh_, koh = r2k.get()
        oh = oh_[:, 0:T * 32]
        em.op('dve', lambda e, oh=oh, sel=sel: e.tensor_tensor(out=oh.rearrange("p (t g j) -> p t g j", g=4, j=8),
                                                               in0=ohg.unsqueeze(3).broadcast_to([128, T, 4, 8]),
                                                               in1=sel.unsqueeze(2).broadcast_to([128, T, 4, 8]), op=ALU.mult), r=[kohg, ksel], w=[koh])
        ohs.append((oh, koh))
    ohsum_, kohs = r2k.get(BF16)
    ohsum = ohsum_[:, 0:T * 32]
    em.op('dve', lambda e: e.tensor_tensor(out=ohsum, in0=ohs[0][0], in1=ohs[1][0], op=ALU.add), r=[ohs[0][1], ohs[1][1]], w=[kohs])
    pr, _, kpr = ps()
    fns = []
    for t in range(T):
        terms = [(triS, t)] + [(onesb, t2) for t2 in range(t)]
        for i_, (lt, t2) in enumerate(terms):
            fns.append(lambda e, t=t, lt=lt, t2=t2, i_=i_, n_=len(terms): e.matmul(pr[:, t * 32:(t + 1) * 32], lhsT=lt[:], rhs=ohsum[:, t2 * 32:(t2 + 1) * 32],
                                                                                  start=(i_ == 0), stop=(i_ == n_ - 1)))
    for t in range(T):
        fns.append(lambda e, t=t: e.matmul(pr[:, 128:160], lhsT=onesb[:], rhs=ohsum[:, t * 32:(t + 1) * 32], start=(t == 0), stop=(t == T - 1)))
    em.mm(fns, r=[kohs] + ck(triS, onesb), w=[kpr])
    rk_, krk = r2k.get()
    rk = rk_[:, 0:T * 32]
    em.op('dve', lambda e: e.tensor_tensor(out=v3(rk, 32), in0=v3(pr[:, 0:128], 32), in1=cum[:].unsqueeze(1).broadcast_to([128, T, 32]), op=ALU.add),
          r=[kpr, 'cum'], w=[krk])
    em.op('dve', lambda e: e.tensor_tensor(out=cum[:], in0=pr[:, 128:160], in1=cum[:], op=ALU.add), r=[kpr, 'cum', krk], w=['cum'])
    dsl = slice(b * 8, (b + 1) * 8)
    for k in range(2):
        oh, koh = ohs[k]
        t32_, kt32 = r2k.get()
        t32 = t32_[:, 0:T * 32]
        em.op('dve', lambda e, t32=t32, oh=oh: e.tensor_tensor(out=t32, in0=oh, in1=rk, op=ALU.mult), r=[koh, krk], w=[kt32])
        dst, kdst = sm(T)
        em.op('dve', lambda e, t32=t32, dst=dst: e.tensor_reduce(out=dst, in_=v3(t32, 32), axis=AX.X, op=ALU.add), r=[kt32], w=[kdst])
        l32_, kl32 = r2k.get()
        l32 = l32_[:, 0:T * 32]
        em.op('dve', lambda e, l32=l32, oh=oh: e.tensor_tensor(out=v3(l32, 32), in0=v3(oh, 32), in1=elim[:].unsqueeze(1).broadcast_to([128, T, 32]), op=ALU.mult),
              r=[koh] + ck(elim), w=[kl32])
        lim, klim = sm(T)
        em.op('dve', lambda e, l32=l32, lim=lim: e.tensor_reduce(out=lim, in_=v3(l32, 32), axis=AX.X, op=ALU.add), r=[kl32], w=[klim])
        ok, kok = sm(T)
        em.op('dve', lambda e, ok=ok, dst=dst, lim=lim: e.tensor_tensor(out=ok, in0=dst, in1=lim, op=ALU.is_lt), r=[kdst, klim], w=[kok])
        nok, knok = sm(T)
        em.op('dve', lambda e, nok=nok, ok=ok: e.tensor_scalar(out=nok, in0=ok, scalar1=-1.0, scalar2=1.0, op0=ALU.mult, op1=ALU.add), r=[kok], w=[knok])
        dv, kdv = sm(T)
        em.op('dve', lambda e, dv=dv, dst=dst, ok=ok: e.tensor_tensor(out=dv, in0=dst, in1=ok, op=ALU.mult), r=[kdst, kok], w=[kdv])
        si, ksi = sm(T)
        em.op('dve', lambda e, si=si, nok=nok, dv=dv: e.scalar_tensor_tensor(out=si, in0=nok, scalar=float(E * CAP + 64), in1=dv, op0=ALU.mult, op1=ALU.add), r=[knok, kdv], w=[ksi])
        gi_, kgi = sm(T)
        em.op('dve', lambda e, gi_=gi_, nok=nok, dv=dv: e.scalar_tensor_tensor(out=gi_, in0=nok, scalar=float(E * CAP), in1=dv, op0=ALU.mult, op1=ALU.add), r=[knok, kdv], w=[kgi])
        em.op('dve', lambda e, k=k, si=si: e.tensor_copy(out=destall[:, dsl].rearrange("p (t k) -> p t k", k=2)[:, :, k], in_=si), r=[ksi], w=[('dest', b)])
        em.op('dve', lambda e, k=k, gi_=gi_: e.tensor_copy(out=gidxall[:, dsl].rearrange("p (t k) -> p t k", k=2)[:, :, k], in_=gi_), r=[kgi], w=[('dest', b)])
        wk, kwk = (w1, kw1) if k == 0 else (w2, kw2)
        em.op('dve', lambda e, k=k, wk=wk, ok=ok: e.tensor_tensor(out=wall[:, b * 4:(b + 1) * 4, k], in0=wk, in1=ok, op=ALU.mult), r=[kwk, kok], w=[('dest', b)])
```

```python
import types
import numpy as np
import ml_dtypes
from contextlib import ExitStack
import concourse.bass as bass
import concourse.mybir as mybir
from concourse.bass import IndirectOffsetOnAxis
from concourse.bass_utils import run_bass_kernel_spmd

F32 = mybir.dt.float32
BF16 = mybir.dt.bfloat16
I32 = mybir.dt.int32
U32 = mybir.dt.uint32
AF = mybir.ActivationFunctionType
ALU = mybir.AluOpType
AX = mybir.AxisListType

NCORES = 8
D = 1024
TOK = 4096
BLK = 512
NBLK = TOK // BLK
E = 32
CAP = 512
DFF = 256
EPS = 1e-6
NEG = -1.0e30
WIN_RES = 2568
ENGS = ('pe', 'act', 'dve', 'pool', 'sp')


def _freeze(fn):
    if fn.__closure__ is None:
        return fn
    cells = []
    for c in fn.__closure__:
        try:
            cells.append(types.CellType(c.cell_contents))
        except ValueError:
            cells.append(c)
    g = types.FunctionType(fn.__code__, fn.__globals__, fn.__name__, fn.__defaults__, tuple(cells))
    g.__kwdefaults__ = fn.__kwdefaults__
    return g


class Em:
    def __init__(self, nc, es):
        self.nc = nc
        self.streams = {e: [] for e in ENGS}
        self.sem = {e: es.enter_context(nc.semaphore('s_' + e)) for e in ('pe', 'act', 'dve', 'pool')}
        self.cnt = {e: 0 for e in self.sem}
        self.waited = {e: {} for e in ENGS}
        self.lastw = {}
        self.reads = {}
        self.dq = {}
        for q, n in (('sp', 28), ('pool', 14), ('act', 4)):
            sems = [es.enter_context(nc.semaphore('d_%s%d' % (q, i))) for i in range(n)]
            self.dq[q] = dict(sems=sems, vals=[0] * n, nxt=0)

    def _semh(self, key):
        return self.sem[key] if isinstance(key, str) else self.dq[key[0]]['sems'][key[1]]

    def _wait(self, eng, key, val):
        if self.waited[eng].get(key, 0) >= val:
            return
        self.waited[eng][key] = val
        s = self._semh(key)
        self.streams[eng].append(lambda e, s=s, v=val: e.wait_ge(s, v))

    def _deps(self, eng, r, w, pe_inorder=False):
        need = {}

        def add(tok):
            if tok is None:
                return
            k, v = tok
            if need.get(k, 0) < v:
                need[k] = v
        for key in r:
            add(self.lastw.get(key))
        for key in w:
            add(self.lastw.get(key))
            for t in self.reads.get(key, {}).items():
                add(t)
        if SERIAL:
            for k2 in ('pe', 'act', 'dve', 'pool'):
                if self.cnt[k2] > 0:
                    need[k2] = self.cnt[k2]
            for q2, d2 in self.dq.items():
                for i2, v2 in enumerate(d2['vals']):
                    if v2 > 0:
                        need[(q2, i2)] = max(need.get((q2, i2), 0), v2)
        for k, v in need.items():
            if pe_inorder and k == 'pe':
                continue
            self._wait(eng, k, v)

    def _track(self, tok, r, w):
        for key in w:
            self.lastw[key] = tok
            self.reads[key] = {}
        for key in r:
            d = self.reads.setdefault(key, {})
            if d.get(tok[0], 0) < tok[1]:
                d[tok[0]] = tok[1]

    def op(self, eng, fn, r=(), w=()):
        fn = _freeze(fn)
        self._deps(eng, r, w)
        self.cnt[eng] += 1
        tok = (eng, self.cnt[eng])
        s = self.sem[eng]
        self.streams[eng].append(lambda e, fn=fn, s=s: fn(e).then_inc(s, 1))
        self._track(tok, r, w)
        return tok

    def mm(self, fns, r=(), w=()):
        fns = [_freeze(f) for f in fns]
        self._deps('pe', r, w, pe_inorder=True)
        self.cnt['pe'] += 1
        tok = ('pe', self.cnt['pe'])
        s = self.sem['pe']
        for fn in fns[:-1]:
            self.streams['pe'].append(lambda e, fn=fn: fn(e))
        self.streams['pe'].append(lambda e, fn=fns[-1], s=s: fn(e).then_inc(s, 1))
        self._track(tok, r, w)
        return tok

    def dma(self, q, fn, r=(), w=()):
        fn = _freeze(fn)
        d = self.dq[q]
        i = d['nxt']
        d['nxt'] = (i + 1) % len(d['sems'])
        key = (q, i)
        if d['vals'][i] > 0:
            self._wait(q, key, d['vals'][i])
        self._deps(q, r, w)
        d['vals'][i] += 16
        tok = (key, d['vals'][i])
        s = d['sems'][i]
        self.streams[q].append(lambda e, fn=fn, s=s: fn(e).then_inc(s, 16))
        self._track(tok, r, w)
        return tok

    def wait_keys(self, eng, keys):
        self._deps(eng, keys, keys)

    def finish(self):
        nc = self.nc
        st = self.streams
        with nc.Block() as block:
            @block.tensor
            def _(e):
                for f in st['pe']:
                    f(e)

            @block.scalar
            def _(e):
                for f in st['act']:
                    f(e)

            @block.vector
            def _(e):
                for f in st['dve']:
                    f(e)

            @block.gpsimd
            def _(e):
                for f in st['pool']:
                    f(e)

            @block.sync
            def _(e):
                for f in st['sp']:
                    f(e)


class Ring:
    def __init__(self, nc, es, name, n, nbytes):
        self.t = [es.enter_context(nc.sbuf_tensor('%s%d' % (name, i), [128, nbytes // 4], F32)) for i in range(n)]
        self.name = name
        self.n = n
        self.i = 0

    def get(self, dt=F32):
        i = self.i
        self.i = (i + 1) % self.n
        ap = self.t[i][:]
        if dt != F32:
            ap = ap.bitcast(dt)
        return ap, (self.name, i)


def build_nc(dbg=None):
    nc = bass.Bass("TRN2", target_bir_lowering=False)

    def din(name, shape, dt=F32):
        return nc.dram_tensor(name, list(shape), dt, kind="ExternalInput").ap()

    x = din("x", [TOK, D])
    cT = din("cT", [128, 8, 2])
    w_ada = din("w_ada", [D, 6 * D])
    b_ada = din("b_ada", [1, 6 * D])
    w_in = din("w_in", [D, 4616])
    w_la = din("w_lift_a", [512, D])
    w_lb = din("w_lift_b", [512, D])
    w_out = din("w_out", [D, D])
    pool_w = din("pool_w", [4, 128, 128])
    w_gate = din("w_gate", [E, D, DFF])
    w_up = din("w_up", [E, D, DFF])
    w_down = din("w_down", [E, DFF, D])
    p_n1g = din("p_n1g", [128, 8])
    p_convw = din("p_convw", [128, 12, 4])
    p_alog = din("p_alog", [128, 4])
    p_dtb = din("p_dtb", [128, 4])
    p_dng = din("p_dng", [128, 1])
    p_pscale = din("p_pscale", [128, 4])
    p_n2g = din("p_n2g", [128, D])
    p_fng = din("p_fng", [128, D])
    p_brb = din("p_brb", [128, 36])
    p_wr = din("p_wr", [128, 8, 36])
    k_identb = din("k_identb", [128, 128], BF16)
    k_identq = din("k_identq", [128, 512], BF16)
    k_identf = din("k_identf", [128, 128])
    k_onesb = din("k_onesb", [128, 128], BF16)
    k_onesf = din("k_onesf", [128, 128])
    k_triU = din("k_triU", [128, 128])
    k_maskT = din("k_maskT", [128, 128])
    k_maskS = din("k_maskS", [128, 128])
    k_triS = din("k_triS", [128, 128], BF16)
    k_pcorr = din("k_pcorr", [128, 4, 16])
    k_ebase = din("k_ebase", [128, 32])
    k_bmask = din("k_bmask", [128, 7, 128], BF16)
    k_bmaskT = din("k_bmaskT", [128, 7, 128], BF16)
    k_elim = din("k_elim", [128, 32])

    out = nc.dram_tensor("out", [TOK, D], F32, kind="ExternalOutput").ap()
    dbg_t = None
    if dbg is not None:
        dbg_t = nc.dram_tensor("dbg", list(dbg), F32, kind="ExternalOutput").ap()
    modD = nc.dram_tensor("modD", [2, 6 * D], F32).ap()
    wsD = nc.dram_tensor("wsD", [8, 128, 4096], BF16).ap()
    h1D = nc.dram_tensor("h1D", [TOK, D], F32).ap()
    xsD = nc.dram_tensor("xsD", [E * CAP, D], BF16).ap()
    ysD = nc.dram_tensor("ysD", [E * CAP + 128, D], F32).ap()

    es = ExitStack()
    with es:
        em = Em(nc, es)

        def sb(name, shape, dt=F32, stack=es):
            return stack.enter_context(nc.sbuf_tensor(name, list(shape), dt))

        pregs = {}
        em.streams['pool'].append(lambda e: pregs.__setitem__('bc', e.to_reg(E * CAP - 1)))

        psb = [es.enter_context(nc.psum_tensor('ps%d' % i, [128, 512], F32)) for i in range(8)]
        pstate = {'i': 0}

        def ps():
            i = pstate['i']
            pstate['i'] = (i + 1) % 8
            return psb[i][:], psb[i][:].bitcast(BF16), ('ps', i)

        identb = sb('identb', [128, 128], BF16)
        identq = sb('identq', [128, 512], BF16)
        identf = sb('identf', [128, 128])
        onesb = sb('onesb', [128, 128], BF16)
        onesf = sb('onesf', [128, 128])
        triU = sb('triU', [128, 128])
        maskT = sb('maskT', [128, 128])
        maskS = sb('maskS', [128, 128])
        triS = sb('triS', [128, 128], BF16)
        pcorr = sb('pcorr', [128, 4, 16])
        bmask = sb('bmask', [128, 7, 128], BF16)
        bmaskT = sb('bmaskT', [128, 7, 128], BF16)
        n1g = sb('n1g', [128, 8])
        convw = sb('convw', [128, 12, 4])
        alog = sb('alog', [128, 4])
        dtb = sb('dtb', [128, 4])
        dng = sb('dng', [128, 1])
        pscale = sb('pscale', [128, 4])
        brb = sb('brb', [128, 36])
        wr = sb('wr', [128, 8, 36])
        cum = sb('cum', [128, 32])
        elim = sb('elim', [128, 32])
        destall = sb('destall', [128, 64], I32)
        gidxall = sb('gidxall', [128, 64], I32)
        wall = sb('wall', [128, 32, 2])
        cst = [(identb, k_identb), (identq, k_identq), (identf, k_identf), (onesb, k_onesb), (onesf, k_onesf),
               (triU, k_triU), (maskT, k_maskT), (maskS, k_maskS), (triS, k_triS), (pcorr, k_pcorr),
               (n1g, p_n1g), (convw, p_convw), (alog, p_alog), (dtb, p_dtb), (dng, p_dng), (pscale, p_pscale),
               (brb, p_brb), (wr, p_wr), (cum, k_ebase), (elim, k_elim), (bmask, k_bmask), (bmaskT, k_bmaskT)]
        for n_, (t_, src_) in enumerate(cst):
            em.dma('sp', lambda e, t_=t_, src_=src_: e.dma_start(out=t_[:], in_=src_), w=[('c', n_)])
        CK = [('c', n_) for n_ in range(len(cst))]
        cidx = {id(t_): ('c', n_) for n_, (t_, _) in enumerate(cst)}

        def ck(*ts):
            return [cidx[id(t)] for t in ts]

        small = sb('small', [128, 40 * 32])
        smi = {'i': 0}

        def sm(n=1):
            assert n <= 32
            i = smi['i']
            smi['i'] = (i + 1) % 40
            return small[:, i * 32:i * 32 + n], ('sm', i)

        epsc = sb('epsc', [128, 4])
        ctf_t = sb('ctf_t', [128, 16])
        scb_t = sb('scb_t', [128, 16], BF16)
        em.op('dve', lambda e: e.memset(epsc[:, 0:1], EPS), w=['epsc'])
        em.op('dve', lambda e: e.memset(epsc[:, 1:2], 0.0), w=['epsc'])
        em.op('dve', lambda e: e.memset(epsc[:, 2:3], 1.0), w=['epsc'])

        p1 = ExitStack()
        es.enter_context(p1)
        winb = sb('winb', [128, 8, WIN_RES], BF16, p1)
        poolwb = sb('poolwb', [128, 4, 128], BF16, p1)
        wst = [sb('wst%d' % i, [128, 4096], BF16, p1) for i in range(4)]
        wsi = {'i': 0}
        G2b = sb('G2b', [128, D], F32, p1)
        SH2b = sb('SH2b', [128, D], F32, p1)
        GT1b = sb('GT1b', [128, D], F32, p1)
        modp = sb('modp', [128, 2, 2, 8], F32, p1)
        r2k = Ring(nc, p1, 'r2k', 8, 2048)
        r4k = Ring(nc, p1, 'r4k', 4, 4096)
        nq = Ring(nc, p1, 'nq', 10, 1024)
        xnT = sb('xnT', [128, 8, BLK], BF16, p1)
        qT = sb('qT', [128, 4, BLK], BF16, p1)
        kT = sb('kT', [128, 4, BLK], BF16, p1)
        vT = sb('vT', [128, 4, BLK], BF16, p1)
        szT = sb('szT', [128, 4, BLK], BF16, p1)
        puT = sb('puT', [128, 4, 16 + BLK], F32, p1)
        ybT = sb('ybT', [128, 4, BLK], BF16, p1)
        yaT = sb('yaT', [128, 4, BLK], BF16, p1)
        mixedT = sb('mixedT', [128, 8, BLK], BF16, p1)
        halo = sb('halo', [128, 12, 4], F32, p1)
        gtm = sb('gtm', [128, 4, 8], F32, p1)
        S = sb('S', [128, 4, 128], F32, p1)
        Sb = sb('Sb', [128, 4, 128], BF16, p1)
        cq = {n_: sb('cq_' + n_, [128, 4, 128], BF16, p1) for n_ in ('DT', 'Ds', 'Er', 'kbd', 'kdec', 'vb', 'N0', 'Pt0')}
        xn2T = sb('xn2T', [128, 8, 128], F32, p1)
        lgb = sb('lgb', [128, 4, 36], F32, p1)

        w_in_v = w_in.rearrange("(c p) n -> p c n", p=128)
        for kc in range(8):
            for hf in range(2):
                c0 = hf * (WIN_RES // 2)
                c1 = c0 + WIN_RES // 2
                em.dma('pool', lambda e, kc=kc, c0=c0, c1=c1: e.dma_start(out=winb[:, kc, c0:c1], in_=w_in_v[:, kc, c0:c1]),
                       w=[('winb', kc)])
        WINK = [('winb', kc) for kc in range(8)]
        em.dma('pool', lambda e: e.dma_start(out=poolwb[:], in_=pool_w.rearrange("g c d -> c g d")), w=['poolwb'])

        kctf, kscb = 'ctf', 'scb'
        em.dma('sp', lambda e: e.dma_start(out=ctf_t[:, 0:16], in_=cT.rearrange("p c b -> p (c b)")), w=[kctf])
        em.op('act', lambda e: e.activation(out=scb_t[:, 0:16], in_=ctf_t[:, 0:16], func=AF.Silu), r=[kctf], w=[kscb])
        scb = scb_t[:, 0:16].rearrange("p (c b) -> p c b", b=2)

        def wpiece_load(src_ap_fn, wkey):
            i = wsi['i']
            wsi['i'] = (i + 1) % 4
            buf = wst[i]
            em.dma('pool', lambda e: src_ap_fn(e, buf), w=[('wst', i)])
            return buf, ('wst', i)

        w_ada_v = w_ada.rearrange("(c p) n -> p c n", p=128)
        for nt in range(12):
            buf, kb = wpiece_load(lambda e, buf, nt=nt: e.dma_start(
                out=buf[:].rearrange("p (c n) -> p c n", n=512), in_=w_ada_v[:, :, nt * 512:(nt + 1) * 512]), None)
            bt, kbt = r2k.get()
            em.dma('sp', lambda e, bt=bt, nt=nt: e.dma_start(out=bt[0:2, :], in_=b_ada[0:1, nt * 512:(nt + 1) * 512].partition_broadcast(2)),
                   w=[kbt])
            pf, pb, kp = ps()
            bv = buf[:].rearrange("p (c n) -> p c n", n=512)
            em.mm([lambda e, kc=kc, pf=pf, bv=bv: e.matmul(pf[0:2, :], lhsT=scb[:, kc, :], rhs=bv[:, kc, :], start=(kc == 0), stop=(kc == 7))
                   for kc in range(8)], r=[kscb, kb], w=[kp])
            mr, kmr = r2k.get()
            em.op('dve', lambda e, mr=mr, pf=pf, bt=bt: e.tensor_tensor(out=mr[0:2, :], in0=pf[0:2, :], in1=bt[0:2, :], op=ALU.add),
                  r=[kp, kbt], w=[kmr])
            em.dma('sp', lambda e, mr=mr, nt=nt: e.dma_start(out=modD[:, nt * 512:(nt + 1) * 512], in_=mr[0:2, :]), r=[kmr], w=[('modD', nt)])

        w_la_v = w_la.rearrange("(c p) n -> p c n", p=128)
        w_lb_v = w_lb.rearrange("(c p) n -> p c n", p=128)
        w_out_v = w_out.rearrange("(c p) n -> p c n", p=128)
        piece_src = []
        for i in range(4):
            c0 = WIN_RES + i * 512
            piece_src.append((w_in_v[:, :, c0:c0 + 512], 512))
        piece_src.append((w_la_v, 1024))
        piece_src.append((w_lb_v, 1024))
        piece_src.append((w_out_v[:, :, 0:512], 512))
        piece_src.append((w_out_v[:, :, 512:1024], 512))
        for pi, (src_, n_) in enumerate(piece_src):
            buf, kb = wpiece_load(lambda e, buf, src_=src_, n_=n_: e.dma_start(out=buf[:].rearrange("p (c n) -> p c n", n=n_), in_=src_), None)
            em.dma('sp', lambda e, buf=buf, pi=pi: e.dma_start(out=wsD[pi], in_=buf[:]), r=[kb], w=[('wsD', pi)])

        def wpiece(pi, i):
            buf = wst[i]
            em.dma('sp', lambda e: e.dma_start(out=buf[:], in_=wsD[pi]), r=[('wsD', pi)], w=[('wst', i)])
            return buf, ('wst', i)

        MODK = [('modD', nt) for nt in range(12)]

        def load_seq_mod(seq):
            sh1 = modD[seq, 0:1024].rearrange("(c p) -> p c", p=128)
            sc1 = modD[seq, 1024:2048].rearrange("(c p) -> p c", p=128)
            tmp, kt = sm(8)
            em.dma('sp', lambda e: e.dma_start(out=modp[:, seq, 1, :], in_=sh1, allow_slow_non_contiguous=True), r=MODK, w=[('modp', seq, 1)])
            em.dma('sp', lambda e: e.dma_start(out=tmp, in_=sc1, allow_slow_non_contiguous=True), r=MODK, w=[kt])
            em.op('dve', lambda e: e.scalar_tensor_tensor(out=modp[:, seq, 0, :], in0=tmp, scalar=1.0, in1=n1g[:], op0=ALU.add, op1=ALU.mult),
                  r=[kt] + ck(n1g), w=[('modp', seq, 0)])
            em.dma('sp', lambda e: e.dma_start(out=GT1b[:], in_=modD[seq:seq + 1, 2048:3072].partition_broadcast(128)), r=MODK, w=['GT1b'])
            em.dma('sp', lambda e: e.dma_start(out=SH2b[:], in_=modD[seq:seq + 1, 3072:4096].partition_broadcast(128)), r=MODK, w=['SH2b'])
            t4, k4 = r4k.get()
            em.dma('sp', lambda e: e.dma_start(out=t4, in_=modD[seq:seq + 1, 4096:5120].partition_broadcast(128)), r=MODK, w=[k4])
            n2, kn2 = r4k.get()
            em.dma('sp', lambda e: e.dma_start(out=n2, in_=p_n2g), w=[kn2])
            em.op('dve', lambda e: e.scalar_tensor_tensor(out=G2b[:], in0=t4, scalar=1.0, in1=n2, op0=ALU.add, op1=ALU.mult),
                  r=[k4, kn2], w=['G2b'])

        def rstd_from_ss(ss_ap, kss, scale):
            l1, kl1 = sm(1)
            em.op('act', lambda e: e.activation(out=l1, in_=ss_ap, func=AF.Ln, bias=epsc[:, 0:1], scale=scale), r=[kss, 'epsc'], w=[kl1])
            r1, kr1 = sm(1)
            em.op('act', lambda e: e.activation(out=r1, in_=l1, func=AF.Exp, scale=-0.5), r=[kl1], w=[kr1])
            return r1, kr1


        def proj_fm(col0, ncols=128):
            pf, pb, kp = ps()
            em.mm([lambda e, kc=kc, pf=pf: e.matmul(pf[0:ncols, :], lhsT=winb[:, kc, col0:col0 + ncols], rhs=xnT[:, kc, :],
                                                     start=(kc == 0), stop=(kc == 7)) for kc in range(8)],
                  r=WINK + ['xnT'], w=[kp])
            return pf, kp

        dbg_state = {'off': 0}

        def dump(ap_f32_128xN, keys, n):
            if dbg_t is None:
                return
            o = dbg_state['off']
            dbg_state['off'] = o + n
            em.dma('sp', lambda e: e.dma_start(out=dbg_t[:, o:o + n], in_=ap_f32_128xN), r=keys, w=[('dbg', o)])

        def dump_any(ap, keys, n):
            if dbg_t is None:
                return
            t, kt = r2k.get()
            em.op('dve', lambda e: e.tensor_copy(out=t[:, 0:n], in_=ap), r=keys, w=[kt])
            dump(t[:, 0:n], [kt], n)

        try:
            for b in range(NBLK):
                seq, blk = divmod(b, NBLK // 2)
                t0 = b * BLK
                first = (blk == 0)
                if first:
                    load_seq_mod(seq)
                    em.op('pool', lambda e: e.memset(S[:], 0.0), w=['S'])
                    em.op('pool', lambda e: e.memset(Sb[:], 0.0), w=['Sb'])
                    em.op('pool', lambda e: e.memset(halo[:], 0.0), w=[('halo', ct_) for ct_ in range(12)])
                    em.op('pool', lambda e: e.memset(puT[:, :, 0:16], 0.0), w=[('puT', g_) for g_ in range(4)])
                for j in range(4):
                    xin, kx = r4k.get()
                    em.dma('sp', lambda e, xin=xin, j=j: e.dma_start(out=xin, in_=x[t0 + j * 128:t0 + (j + 1) * 128, :]), w=[kx])
                    junk, kj = r2k.get(BF16)
                    ss, kss = sm(1)
                    em.op('act', lambda e, xin=xin, junk=junk, ss=ss: e.activation(out=junk, in_=xin, func=AF.Square, accum_out=ss), r=[kx], w=[kj, kss])
                    rs, krs = rstd_from_ss(ss, kss, 1.0 / D)
                    xsb, kxs = r2k.get(BF16)
                    em.op('dve', lambda e, xsb=xsb, xin=xin, rs=rs: e.tensor_scalar(out=xsb, in0=xin, scalar1=rs, scalar2=None, op0=ALU.mult),
                          r=[kx, krs], w=[kxs])
                    pf, pb, kp = ps()
                    em.mm([lambda e, kc=kc, pb=pb, xsb=xsb: e.transpose(pb[:, kc * 128:(kc + 1) * 128], xsb[:, kc * 128:(kc + 1) * 128], identb[:])
                           for kc in range(8)], r=[kxs] + ck(identb), w=[kp])
                    for kc in range(8):
                        em.op('act', lambda e, kc=kc, pb=pb, j=j: e.activation(
                            out=xnT[:, kc, j * 128:(j + 1) * 128], in_=pb[:, kc * 128:(kc + 1) * 128], func=AF.Identity,
                            scale=modp[:, seq, 0, kc:kc + 1], bias=modp[:, seq, 1, kc:kc + 1]),
                            r=[kp, ('modp', seq, 0), ('modp', seq, 1)], w=['xnT'])
                if dbg_t is not None and b == 0 and 'xnT' in DBGSEL:
                    for kc in range(8):
                        dump_any(xnT[:, kc, :], ['xnT'], 512)

                if b == 0:
                    chk('A')
                def st_C(ct):
                    pf, kp = proj_fm(ct * 128)
                    pre, kpre = r4k.get()
                    em.op('pool', lambda e: e.tensor_copy(out=pre[:, 0:4], in_=halo[:, ct, :]), r=[('halo', ct)], w=[kpre])
                    em.op('act', lambda e: e.activation(out=pre[:, 4:516], in_=pf, func=AF.Copy), r=[kp], w=[kpre])
                    em.op('pool', lambda e: e.tensor_copy(out=halo[:, ct, :], in_=pre[:, 512:516]), r=[kpre], w=[('halo', ct)])
                    acc, kacc = r2k.get()
                    em.op('act', lambda e: e.activation(out=acc, in_=pf, func=AF.Copy, scale=convw[:, ct, 3:4]), r=[kp] + ck(convw), w=[kacc])
                    return dict(ct=ct, pre=pre, kpre=kpre, acc=acc, kacc=kacc)

                def st_M(st):
                    ct, pre, kpre, acc, kacc = st['ct'], st['pre'], st['kpre'], st['acc'], st['kacc']
                    for tap in (2, 1, 0):
                        sh = 3 - tap
                        em.op('dve', lambda e, tap=tap, sh=sh: e.scalar_tensor_tensor(
                            out=acc, in0=pre[:, 4 - sh:516 - sh], scalar=convw[:, ct, tap:tap + 1], in1=acc, op0=ALU.mult, op1=ALU.add),
                            r=[kpre, kacc] + ck(convw), w=[kacc])

                def st_S(st):
                    ct, acc, kacc = st['ct'], st['acc'], st['kacc']
                    h = ct % 4
                    if ct >= 8:
                        em.op('act', lambda e: e.activation(out=vT[:, h, :], in_=acc, func=AF.Silu), r=[kacc], w=['vT'])
                        return
                    em.op('act', lambda e: e.activation(out=acc, in_=acc, func=AF.Silu), r=[kacc], w=[kacc])
                    i_ = r2k.i
                    sqb, ksq = r2k.get(BF16)
                    st['sqb'], st['ksq'], st['sqf'] = sqb, ksq, r2k.t[i_][:]
                    em.op('act', lambda e: e.activation(out=sqb[:, 0:512], in_=acc, func=AF.Square), r=[kacc], w=[ksq])

                def st_N(st):
                    if st['ct'] >= 8:
                        return
                    sqb, ksq = st['sqb'], st['ksq']
                    pf2, pb2, kp2 = ps()
                    st['pf2'], st['kp2'] = pf2, kp2
                    em.mm([lambda e: e.matmul(pf2, lhsT=onesb[:], rhs=sqb[:, 0:512], start=True, stop=True)], r=[ksq] + ck(onesb), w=[kp2])

                def st_L(st):
                    if st['ct'] >= 8:
                        return
                    lnv, ksq, pf2, kp2 = st['sqf'], st['ksq'], st['pf2'], st['kp2']
                    em.op('act', lambda e: e.activation(out=lnv, in_=pf2, func=AF.Ln, bias=epsc[:, 0:1]), r=[kp2, 'epsc'], w=[ksq])
                    em.op('act', lambda e: e.activation(out=lnv, in_=lnv, func=AF.Exp, scale=-0.5), r=[ksq], w=[ksq])

                def st_F(st):
                    ct = st['ct']
                    if ct >= 8:
                        return
                    h = ct % 4
                    acc, kacc, rinv, ksq = st['acc'], st['kacc'], st['sqf'], st['ksq']
                    qs = (128.0 ** -0.5) if ct < 4 else 1.0
                    dst = qT if ct < 4 else kT
                    dk = 'qT' if ct < 4 else 'kT'
                    em.op('dve', lambda e: e.scalar_tensor_tensor(out=dst[:, h, :], in0=acc, scalar=qs, in1=rinv, op0=ALU.mult, op1=ALU.mult),
                          r=[kacc, ksq], w=[dk])

                prev_pair = None
                for pr in ((0, 1), (2, 3), (4, 5), (6, 7), (8, 9), (10, 11)):
                    cur_pair = [st_C(ct) for ct in pr]
                    for st in cur_pair:
                        st_M(st)
                    if prev_pair is not None:
                        for fn_ in (st_S, st_N, st_L, st_F):
                            for st in prev_pair:
                                fn_(st)
                    prev_pair = cur_pair
                for fn_ in (st_S, st_N, st_L, st_F):
                    for st in prev_pair:
                        fn_(st)
                if b == 0 and dbg_t is not None and 'B' in DBGSEL:
                    for h_ in range(4):
                        dump_any(qT[:, h_, :], ['qT'], 512)
                    for h_ in range(4):
                        dump_any(kT[:, h_, :], ['kT'], 512)
                    for h_ in range(4):
                        dump_any(vT[:, h_, :], ['vT'], 512)
                if b == 0:
                    chk('B')
                for h in range(4):
                    pf, kp = proj_fm(1536 + h * 128)
                    em.op('act', lambda e, pf=pf, h=h: e.activation(out=szT[:, h, :], in_=pf, func=AF.Silu), r=[kp], w=['szT'])
                for c in range(4):
                    pf, pb, kp = ps()
                    em.mm([lambda e, kc=kc, pf=pf, c=c: e.matmul(pf[:, 0:8], lhsT=xnT[:, kc, c * 128:(c + 1) * 128], rhs=winb[:, kc, 2048:2056],
                                                                  start=(kc == 0), stop=(kc == 7)) for kc in range(8)],
                          r=WINK + ['xnT'], w=[kp])
                    em.op('act', lambda e, pf=pf, c=c: e.activation(out=gtm[:, c, 4:8], in_=pf[:, 4:8], func=AF.Sigmoid), r=[kp], w=[('gtm', c)])
                    xa, kxa = sm(4)
                    em.op('dve', lambda e, xa=xa, pf=pf: e.tensor_tensor(out=xa, in0=pf[:, 0:4], in1=dtb[:], op=ALU.add), r=[kp] + ck(dtb), w=[kxa])
                    ab, kab = sm(4)
                    em.op('act', lambda e, ab=ab, xa=xa: e.activation(out=ab, in_=xa, func=AF.Abs), r=[kxa], w=[kab])
                    ex, kex = sm(4)
                    em.op('act', lambda e, ex=ex, ab=ab: e.activation(out=ex, in_=ab, func=AF.Exp, scale=-1.0), r=[kab], w=[kex])
                    l1p, kl1p = sm(4)
                    em.op('act', lambda e, l1p=l1p, ex=ex: e.activation(out=l1p, in_=ex, func=AF.Ln, bias=epsc[:, 2:3]), r=[kex, 'epsc'], w=[kl1p])
                    sp_, ksp = sm(4)
                    em.op('dve', lambda e, sp_=sp_, xa=xa, l1p=l1p: e.scalar_tensor_tensor(out=sp_, in0=xa, scalar=0.0, in1=l1p, op0=ALU.max, op1=ALU.add),
                          r=[kxa, kl1p], w=[ksp])
                    ea, kea = sm(4)
                    em.op('act', lambda e, ea=ea: e.activation(out=ea, in_=alog[:], func=AF.Exp), r=ck(alog), w=[kea])
                    em.op('dve', lambda e, c=c, sp_=sp_, ea=ea: e.scalar_tensor_tensor(out=gtm[:, c, 0:4], in0=sp_, scalar=-1.0, in1=ea, op0=ALU.mult, op1=ALU.mult),
                          r=[ksp, kea], w=[('gtm', c)])

                if b == 0 and dbg_t is not None and 'B2' in DBGSEL:
                    dump(gtm[:].rearrange('p a b -> p (a b)'), [('gtm', c_) for c_ in range(4)], 32)
                if b == 0:
                    chk('B2')
                for gi, win in enumerate((2, 4, 8, 16)):
                    pf, kp = proj_fm(2056 + gi * 128)
                    em.op('act', lambda e, pf=pf, gi=gi: e.activation(out=puT[:, gi, 16:16 + BLK], in_=pf, func=AF.Copy), r=[kp], w=[('puT', gi)])
                    cur = puT[:, gi, :]
                    kcur = ('puT', gi)
                    w_ = 1
                    nxt = None
                    while w_ < win:
                        nxt, knxt = r4k.get()
                        em.op('pool', lambda e, nxt=nxt, cur=cur, w_=w_: e.tensor_tensor(out=nxt[:, w_:16 + BLK], in0=cur[:, w_:16 + BLK], in1=cur[:, 0:16 + BLK - w_], op=ALU.add),
                              r=[kcur], w=[knxt])
                        cur, kcur = nxt, knxt
                        w_ *= 2
                    pl, kpl = r2k.get()
                    em.op('dve', lambda e, pl=pl, cur=cur, gi=gi, win=win: e.scalar_tensor_tensor(
                        out=pl, in0=cur[:, 16:16 + BLK], scalar=1.0 / win, in1=puT[:, gi, 16:16 + BLK], op0=ALU.mult, op1=ALU.subtract),
                        r=[kcur, ('puT', gi)], w=[kpl])
                    if first:
                        t15, k15 = sm(16)
                        em.op('dve', lambda e, t15=t15, cur=cur, gi=gi: e.tensor_tensor(out=t15, in0=cur[:, 16:32], in1=pcorr[:, gi, :], op=ALU.mult),
                              r=[kcur] + ck(pcorr), w=[k15])
                        em.op('dve', lambda e, t15=t15, pl=pl, gi=gi: e.tensor_tensor(out=pl[:, 0:16], in0=t15, in1=puT[:, gi, 16:32], op=ALU.subtract),
                              r=[k15, ('puT', gi)], w=[kpl])
                    plb, kplb = r2k.get(BF16)
                    em.op('pool', lambda e, plb=plb, pl=pl: e.tensor_copy(out=plb[:, 0:BLK], in_=pl), r=[kpl], w=[kplb])
                    pf2, pb2, kp2 = ps()
                    em.mm([lambda e, pf2=pf2, plb=plb, gi=gi: e.matmul(pf2, lhsT=poolwb[:, gi, :], rhs=plb[:, 0:BLK], start=True, stop=True)],
                          r=[kplb, 'poolwb'], w=[kp2])
                    em.op('act', lambda e, pf2=pf2, gi=gi: e.activation(out=ybT[:, gi, :], in_=pf2, func=AF.Copy, scale=pscale[:, gi:gi + 1]),
                          r=[kp2] + ck(pscale), w=['ybT'])
                    em.op('pool', lambda e, gi=gi: e.tensor_copy(out=puT[:, gi, 0:16], in_=puT[:, gi, BLK:BLK + 16]), r=[('puT', gi), kpl], w=[('puT', gi)])

                if b == 0:
                    chk('C')
                for c in range(4):
                    tsl = slice(c * 128, (c + 1) * 128)
                    g4 = gtm[:, c, 0:4]
                    b4 = gtm[:, c, 4:8]
                    kg = ('gtm', c)
                    pkf, pkb, kpk = ps()
                    em.mm([lambda e, h=h, pkb=pkb: e.transpose(pkb[:, h * 128:(h + 1) * 128], kT[:, h, tsl], identb[:]) for h in range(4)],
                          r=['kT'] + ck(identb), w=[kpk])
                    pvf, pvb, kpv = ps()
                    em.mm([lambda e, h=h, pvb=pvb: e.transpose(pvb[:, h * 128:(h + 1) * 128], vT[:, h, tsl], identb[:]) for h in range(4)],
                          r=['vT'] + ck(identb), w=[kpv])
                    Gt, kGt = r2k.get()
                    for h in range(4):
                        em.op('pool', lambda e, h=h, Gt=Gt: e.tensor_scalar(out=Gt[:, h * 128:(h + 1) * 128], in0=triU[:], scalar1=g4[:, h:h + 1], scalar2=1.0,
                                                                            op0=ALU.mult, op1=ALU.mult), r=[kg] + ck(triU), w=[kGt])
                    pgr, _, kpgr = ps()
                    em.mm([lambda e, pgr=pgr, Gt=Gt: e.matmul(pgr, lhsT=onesf[:], rhs=Gt, start=True, stop=True)], r=[kGt] + ck(onesf), w=[kpgr])
                    pgc, _, kpgc = ps()
                    em.mm([lambda e, pgc=pgc: e.matmul(pgc[:, 0:4], lhsT=triU[:], rhs=g4, start=True, stop=True)], r=[kg] + ck(triU), w=[kpgc])
                    pgl, _, kpgl = ps()
                    em.mm([lambda e, pgl=pgl: e.matmul(pgl[:, 0:4], lhsT=onesf[:], rhs=g4, start=True, stop=True)], r=[kg] + ck(onesf), w=[kpgl])
                    gcc8, kgcc = sm(8)
                    em.op('act', lambda e, gcc8=gcc8, pgc=pgc: e.activation(out=gcc8[:, 0:4], in_=pgc[:, 0:4], func=AF.Copy), r=[kpgc], w=[kgcc])
                    em.op('act', lambda e, gcc8=gcc8, pgl=pgl: e.activation(out=gcc8[:, 4:8], in_=pgl[:, 0:4], func=AF.Copy), r=[kpgl, kgcc], w=[kgcc])
                    gcc = gcc8[:, 0:4]
                    glv = gcc8[:, 4:8]
                    DTl, kDTl = r2k.get()
                    Dsl, kDsl = r2k.get()
                    for h in range(4):
                        hs = slice(h * 128, (h + 1) * 128)
                        em.op('dve', lambda e, hs=hs, h=h, DTl=DTl, pgr=pgr, gcc=gcc: e.scalar_tensor_tensor(
                            out=DTl[:, hs], in0=pgr[:, hs], scalar=gcc[:, h:h + 1], in1=maskT[:], op0=ALU.subtract, op1=ALU.add),
                            r=[kpgr, kgcc] + ck(maskT), w=[kDTl])
                        em.op('dve', lambda e, hs=hs, h=h, Dsl=Dsl, pgr=pgr, gcc=gcc: e.scalar_tensor_tensor(
                            out=Dsl[:, hs], in0=pgr[:, hs], scalar=gcc[:, h:h + 1], in1=maskS[:], op0=ALU.subtract, op1=ALU.add),
                            r=[kpgr, kgcc] + ck(maskS), w=[kDsl])
                    DT, Ds, Er = cq['DT'], cq['Ds'], cq['Er']
                    fl = lambda t: t[:].rearrange("p h n -> p (h n)")
                    em.op('act', lambda e: e.activation(out=fl(DT), in_=DTl, func=AF.Exp), r=[kDTl], w=['DT'])
                    em.op('act', lambda e: e.activation(out=fl(Ds), in_=Dsl, func=AF.Exp, scale=-1.0), r=[kDsl], w=['Ds'])
                    em.op('act', lambda e, pgr=pgr: e.activation(out=fl(Er), in_=pgr, func=AF.Exp), r=[kpgr], w=['Er'])
                    if b == 0 and c == 0:
                        chk('D1')
                    egc, kegc = sm(4)
                    em.op('act', lambda e, egc=egc, gcc=gcc: e.activation(out=egc, in_=gcc, func=AF.Exp), r=[kgcc], w=[kegc])
                    kbs, kkbs = sm(4)
                    em.op('dve', lambda e, kbs=kbs, egc=egc: e.tensor_tensor(out=kbs, in0=egc, in1=b4, op=ALU.mult), r=[kegc, kg], w=[kkbs])
                    dl, kdl = sm(4)
                    em.op('dve', lambda e, dl=dl, glv=glv, gcc=gcc: e.tensor_tensor(out=dl, in0=glv, in1=gcc, op=ALU.subtract), r=[kgcc], w=[kdl])
                    ekd, kekd = sm(4)
                    em.op('act', lambda e, ekd=ekd, dl=dl: e.activation(out=ekd, in_=dl, func=AF.Exp), r=[kdl], w=[kekd])
                    egl, kegl = sm(4)
                    em.op('act', lambda e, egl=egl, glv=glv: e.activation(out=egl, in_=glv, func=AF.Exp), r=[kgcc], w=[kegl])
                    nb4, knb4 = sm(4)
                    em.op('dve', lambda e, nb4=nb4: e.tensor_scalar(out=nb4, in0=b4, scalar1=-1.0, scalar2=None, op0=ALU.mult), r=[kg], w=[knb4])
                    if b == 0 and c == 0:
                        chk('D1b')
                    kbd, kdec, vb = cq['kbd'], cq['kdec'], cq['vb']
                    for h in range(4):
                        hs = slice(h * 128, (h + 1) * 128)
                        em.op('act', lambda e, h=h, hs=hs, pkb=pkb, kbs=kbs: e.activation(out=kbd[:, h, :], in_=pkb[:, hs], func=AF.Identity, scale=kbs[:, h:h + 1], bias=epsc[:, 1:2]),
                              r=[kpk, kkbs], w=['kbd'])
                        em.op('act', lambda e, h=h, hs=hs, pkb=pkb, ekd=ekd: e.activation(out=kdec[:, h, :], in_=pkb[:, hs], func=AF.Identity, scale=ekd[:, h:h + 1], bias=epsc[:, 1:2]),
                              r=[kpk, kekd], w=['kdec'])
                        em.op('act', lambda e, h=h, hs=hs, pvb=pvb: e.activation(out=vb[:, h, :], in_=pvb[:, hs], func=AF.Identity, scale=b4[:, h:h + 1], bias=epsc[:, 1:2]),
                              r=[kpv, kg], w=['vb'])
                    if b == 0 and c == 0:
                        chk('D2')
                    pkk, _, kpkk = ps()
                    em.mm([lambda e, h=h, pkk=pkk: e.matmul(pkk[:, h * 128:(h + 1) * 128], lhsT=kT[:, h, tsl], rhs=kT[:, h, tsl], start=True, stop=True) for h in range(4)],
                          r=['kT'], w=[kpkk])
                    N0 = cq['N0']
                    for h in range(4):
                        hs = slice(h * 128, (h + 1) * 128)
                        em.op('dve', lambda e, h=h, hs=hs, pkk=pkk, nb4=nb4: e.scalar_tensor_tensor(
                            out=N0[:, h, :], in0=pkk[:, hs], scalar=nb4[:, h:h + 1], in1=Ds[:, h, :], op0=ALU.mult, op1=ALU.mult),
                            r=[kpkk, knb4, 'Ds'], w=['N0'])
                    ptf, ptb, kpt = ps()
                    em.mm([lambda e, h=h, ptb=ptb: e.transpose(ptb[:, h * 128:(h + 1) * 128], N0[:, h, :], identb[:]) for h in range(4)],
                          r=['N0'] + ck(identb), w=[kpt])
                    Pt0 = cq['Pt0']
                    em.op('act', lambda e, ptb=ptb: e.activation(out=fl(Pt0), in_=ptb[:, 0:512], func=AF.Identity, scale=1.0, bias=epsc[:, 1:2]), r=[kpt, 'epsc'], w=['Pt0'])
                    Tt, kTt = nq.get(BF16)
                    if b == 0 and c == 0 and dbg_t is not None and 'D3' in DBGSEL:
                        dump_any(fl(DT), ['DT'], 512)
                        dump_any(fl(Ds), ['Ds'], 512)
                        dump_any(fl(N0), ['N0'], 512)
                    if b == 0 and c == 0:
                        chk('D3')
                    Tq, kTq = nq.get(BF16)
                    Cm, kCm = nq.get(BF16)
                    for h in range(4):
                        hs = slice(h * 128, (h + 1) * 128)
                        em.op('dve', lambda e, h=h, hs=hs, Cm=Cm: e.tensor_tensor(out=Cm[:, hs], in0=N0[:, h, :], in1=bmask[:, 0, :], op=ALU.mult), r=['N0'] + ck(bmask), w=[kCm])
                    em.op('dve', lambda e, Tq=Tq, Cm=Cm: e.tensor_tensor(out=Tq, in0=Cm, in1=identq[:], op=ALU.add), r=[kCm] + ck(identq), w=[kTq])
                    Cmt, kCmt = nq.get(BF16)
                    for h in range(4):
                        hs = slice(h * 128, (h + 1) * 128)
                        em.op('dve', lambda e, h=h, hs=hs, Cmt=Cmt: e.tensor_tensor(out=Cmt[:, hs], in0=Pt0[:, h, :], in1=bmaskT[:, 0, :], op=ALU.mult), r=['Pt0'] + ck(bmaskT), w=[kCmt])
                    em.op('dve', lambda e, Tt=Tt, Cmt=Cmt: e.tensor_tensor(out=Tt, in0=Cmt, in1=identq[:], op=ALU.add), r=[kCmt, kTt] + ck(identq), w=[kTt])
                    if b == 0 and c == 0 and dbg_t is not None and 'D3b' in DBGSEL:
                        dump_any(Cm, [kCm], 512)
                        dump_any(Tq, [kTq], 512)
                        dump_any(Tt, [kTt], 512)
                        dump_any(bmask[:, :, :].rearrange('p a b -> p (a b)')[:, 0:512], ck(bmask), 512)
                    if b == 0 and c == 0:
                        chk('D3b')
                    def masks(lev_):
                        Cm_, kCm_ = nq.get(BF16)
                        Cmt_, kCmt_ = nq.get(BF16)
                        for h in range(4):
                            hs = slice(h * 128, (h + 1) * 128)
                            em.op('dve', lambda e, h=h, hs=hs: e.tensor_tensor(out=Cm_[:, hs], in0=N0[:, h, :], in1=bmask[:, lev_, :], op=ALU.mult), r=['N0'] + ck(bmask), w=[kCm_])
                            if lev_ < 6:
                                em.op('dve', lambda e, h=h, hs=hs: e.tensor_tensor(out=Cmt_[:, hs], in0=Pt0[:, h, :], in1=bmaskT[:, lev_, :], op=ALU.mult), r=['Pt0'] + ck(bmaskT), w=[kCmt_])
                        return Cm_, kCm_, Cmt_, kCmt_

                    nxt_masks = masks(1)
                    for lev in range(1, 7):
                        Cm, kCm, Cmt, kCmt = nxt_masks
                        last = (lev == 6)
                        px2, _, kpx2 = ps()
                        em.mm([lambda e, h=h, px2=px2, Cm=Cm, Tt=Tt: e.matmul(px2[:, h * 128:(h + 1) * 128], lhsT=Cm[:, h * 128:(h + 1) * 128], rhs=Tt[:, h * 128:(h + 1) * 128], start=True, stop=True)
                               for h in range(4)], r=[kCm, kTt], w=[kpx2])
                        if lev < 6:
                            nxt_masks = masks(lev + 1)
                        Xs2, kXs2 = nq.get(BF16)
                        em.op('act', lambda e, Xs2=Xs2, px2=px2: e.activation(out=Xs2, in_=px2, func=AF.Copy), r=[kpx2], w=[kXs2])
                        if not last:
                            px1, _, kpx1 = ps()
                            em.mm([lambda e, h=h, px1=px1, Cmt=Cmt, Tq=Tq: e.matmul(px1[:, h * 128:(h + 1) * 128], lhsT=Cmt[:, h * 128:(h + 1) * 128], rhs=Tq[:, h * 128:(h + 1) * 128], start=True, stop=True)
                                   for h in range(4)], r=[kCmt, kTq], w=[kpx1])
                            Xs1, kXs1 = nq.get(BF16)
                            em.op('act', lambda e, Xs1=Xs1, px1=px1: e.activation(out=Xs1, in_=px1, func=AF.Copy), r=[kpx1], w=[kXs1])
                        py2, _, kpy2 = ps()
                        em.mm([lambda e, h=h, py2=py2, Tq=Tq, Xs2=Xs2: e.matmul(py2[:, h * 128:(h + 1) * 128], lhsT=Tq[:, h * 128:(h + 1) * 128], rhs=Xs2[:, h * 128:(h + 1) * 128], start=True, stop=True)
                               for h in range(4)], r=[kTq, kXs2], w=[kpy2])
                        if not last:
                            py1, _, kpy1 = ps()
                            em.mm([lambda e, h=h, py1=py1, Tt=Tt, Xs1=Xs1: e.matmul(py1[:, h * 128:(h + 1) * 128], lhsT=Tt[:, h * 128:(h + 1) * 128], rhs=Xs1[:, h * 128:(h + 1) * 128], start=True, stop=True)
                                   for h in range(4)], r=[kTt, kXs1], w=[kpy1])
                        Ttn, kTtn = nq.get(BF16)
                        em.op('dve', lambda e, Ttn=Ttn, py2=py2, Tt=Tt: e.tensor_tensor(out=Ttn, in0=py2, in1=Tt, op=ALU.add), r=[kpy2, kTt], w=[kTtn])
                        if not last:
                            Tqn, kTqn = nq.get(BF16)
                            em.op('dve', lambda e, Tqn=Tqn, py1=py1, Tq=Tq: e.tensor_tensor(out=Tqn, in0=py1, in1=Tq, op=ALU.add), r=[kpy1, kTq], w=[kTqn])
                            Tq, kTq = Tqn, kTqn
                        Tt, kTt = Ttn, kTtn
                    if b == 0 and c == 0:
                        chk('D4')
                    pw, _, kpw = ps()
                    em.mm([lambda e, h=h, pw=pw, Tt=Tt: e.matmul(pw[:, h * 128:(h + 1) * 128], lhsT=kbd[:, h, :], rhs=Tt[:, h * 128:(h + 1) * 128], start=True, stop=True)
                           for h in range(4)], r=['kbd', kTt], w=[kpw])
                    nwT, knwT = r2k.get(BF16)
                    em.op('act', lambda e, nwT=nwT, pw=pw: e.activation(out=nwT[:, 0:512], in_=pw, func=AF.Copy, scale=-1.0), r=[kpw], w=[knwT])
                    pvn, _, kpvn = ps()
                    fns = []
                    for h in range(4):
                        hs = slice(h * 128, (h + 1) * 128)
                        fns.append(lambda e, h=h, hs=hs, pvn=pvn, Tt=Tt: e.matmul(pvn[:, hs], lhsT=Tt[:, hs], rhs=vb[:, h, :], start=True, stop=False))
                        fns.append(lambda e, h=h, hs=hs, pvn=pvn, nwT=nwT: e.matmul(pvn[:, hs], lhsT=nwT[:, hs], rhs=Sb[:, h, :], start=False, stop=True))
                    em.mm(fns, r=[kTt, 'vb', knwT, 'Sb'], w=[kpvn])
                    vnew, kvnew = r2k.get(BF16)
                    em.op('act', lambda e, vnew=vnew, pvn=pvn: e.activation(out=vnew[:, 0:512], in_=pvn, func=AF.Copy), r=[kpvn], w=[kvnew])
                    if b == 0 and c == 0:
                        chk('D5')
                    pqk, _, kpqk = ps()
                    em.mm([lambda e, h=h, pqk=pqk: e.matmul(pqk[:, h * 128:(h + 1) * 128], lhsT=kT[:, h, tsl], rhs=qT[:, h, tsl], start=True, stop=True) for h in range(4)],
                          r=['kT', 'qT'], w=[kpqk])
                    attnT, kat = r2k.get(BF16)
                    em.op('dve', lambda e, attnT=attnT, pqk=pqk: e.tensor_tensor(out=attnT[:, 0:512], in0=pqk, in1=fl(DT), op=ALU.mult), r=[kpqk, 'DT'], w=[kat])
                    qdT, kqd = r2k.get(BF16)
                    em.op('dve', lambda e, qdT=qdT: e.tensor_tensor(out=qdT[:, 0:512].rearrange("p (h n) -> p h n", n=128), in0=qT[:, :, tsl], in1=Er[:], op=ALU.mult),
                          r=['qT', 'Er'], w=[kqd])
                    po, _, kpo = ps()
                    fns = []
                    for h in range(4):
                        hs = slice(h * 128, (h + 1) * 128)
                        fns.append(lambda e, h=h, hs=hs, po=po, qdT=qdT: e.matmul(po[:, hs], lhsT=Sb[:, h, :], rhs=qdT[:, hs], start=True, stop=False))
                        fns.append(lambda e, h=h, hs=hs, po=po, vnew=vnew, attnT=attnT: e.matmul(po[:, hs], lhsT=vnew[:, hs], rhs=attnT[:, hs], start=False, stop=True))
                    em.mm(fns, r=['Sb', kqd, kvnew, kat], w=[kpo])
                    osq, kosq = r2k.get(BF16)
                    em.op('act', lambda e, osq=osq, po=po: e.activation(out=osq[:, 0:512], in_=po, func=AF.Square), r=[kpo], w=[kosq])
                    pss, _, kpss = ps()
                    em.mm([lambda e, pss=pss, osq=osq: e.matmul(pss, lhsT=onesb[:], rhs=osq[:, 0:512], start=True, stop=True)], r=[kosq] + ck(onesb), w=[kpss])
                    lno, klno = r2k.get()
                    em.op('act', lambda e, lno=lno, pss=pss: e.activation(out=lno, in_=pss, func=AF.Ln, bias=epsc[:, 0:1], scale=1.0 / 128), r=[kpss, 'epsc'], w=[klno])
                    rso, krso = r2k.get()
                    em.op('act', lambda e, rso=rso, lno=lno: e.activation(out=rso, in_=lno, func=AF.Exp, scale=-0.5), r=[klno], w=[krso])
                    t1, kt1 = r2k.get()
                    em.op('dve', lambda e, t1=t1, po=po, rso=rso: e.tensor_tensor(out=t1, in0=po, in1=rso, op=ALU.mult), r=[kpo, krso], w=[kt1])
                    em.op('dve', lambda e, t1=t1: e.scalar_tensor_tensor(out=yaT[:, :, tsl], in0=t1.rearrange("p (h n) -> p h n", n=128), scalar=dng[:, 0:1],
                                                                          in1=szT[:, :, tsl], op0=ALU.mult, op1=ALU.mult),
                          r=[kt1, 'szT'] + ck(dng), w=['yaT'])
                    if b == 0 and c == 0 and dbg_t is not None and 'D6' in DBGSEL:
                        dump_any(Tt, [kTt], 512)
                        dump_any(vnew[:, 0:512], [kvnew], 512)
                        dump_any(attnT[:, 0:512], [kat], 512)
                        dump_any(po, [kpo], 512)
                        dump_any(rso, [krso], 512)
                    if b == 0 and c == 0:
                        chk('D6')
                    pst, _, kpst = ps()
                    em.mm([lambda e, h=h, pst=pst, vnew=vnew: e.matmul(pst[:, h * 128:(h + 1) * 128], lhsT=kdec[:, h, :], rhs=vnew[:, h * 128:(h + 1) * 128], start=True, stop=True)
                           for h in range(4)], r=['kdec', kvnew], w=[kpst])
                    for h in range(4):
                        hs = slice(h * 128, (h + 1) * 128)
                        em.op('dve', lambda e, h=h, hs=hs, pst=pst, egl=egl: e.scalar_tensor_tensor(
                            out=S[:, h, :], in0=S[:, h, :], scalar=egl[:, h:h + 1], in1=pst[:, hs], op0=ALU.mult, op1=ALU.add),
                            r=['S', kegl, kpst, kpo, kpvn], w=['S'])
                    em.op('act', lambda e: e.activation(out=fl(Sb), in_=fl(S), func=AF.Copy), r=['S', kpo, kpvn], w=['Sb'])
                    if b == 0 and c == 0 and dbg_t is not None and 'S1' in DBGSEL:
                        dump(fl(S), ['S'], 512)
                        dump_any(fl(Sb), ['Sb'], 512)
                        dump_any(fl(kdec), ['kdec'], 512)
                    if b == 0 and c == 0:
                        chk('S1')
                if dbg_t is not None and b == 0 and 'sz' in DBGSEL:
                    for h in range(4):
                        dump_any(szT[:, h, :], ['szT'], 512)
                if dbg_t is not None and b == 0 and 'ya' in DBGSEL:
                    for h in range(4):
                        dump_any(yaT[:, h, :], ['yaT'], 512)
                    for h in range(4):
                        dump_any(ybT[:, h, :], ['ybT'], 512)

                if b == 0:
                    chk('D')
                bufs = {}
                bufs[4] = wpiece(4, 0)
                bufs[5] = wpiece(5, 1)
                bufs[0] = wpiece(0, 2)
                bufs[2] = wpiece(2, 3)
                la_v = bufs[4][0][:].rearrange("p (c n) -> p c n", n=1024)
                lb_v = bufs[5][0][:].rearrange("p (c n) -> p c n", n=1024)
                for mt in range(8):
                    if mt == 4:
                        bufs[1] = wpiece(1, 2)
                        bufs[3] = wpiece(3, 3)
                    gbuf_a, kga = bufs[mt // 4]
                    gbuf_b, kgb = bufs[2 + mt // 4]
                    ga_v = gbuf_a[:].rearrange("p (c n) -> p c n", n=512)
                    gb_v = gbuf_b[:].rearrange("p (c n) -> p c n", n=512)
                    cs = slice((mt % 4) * 128, (mt % 4 + 1) * 128)
                    ms = slice(mt * 128, (mt + 1) * 128)
                    pga, _, kpga = ps()
                    em.mm([lambda e, kc=kc, pga=pga, ga_v=ga_v, cs=cs: e.matmul(pga, lhsT=ga_v[:, kc, cs], rhs=xnT[:, kc, :], start=(kc == 0), stop=(kc == 7)) for kc in range(8)],
                          r=[kga, 'xnT'], w=[kpga])
                    pgb, _, kpgb = ps()
                    em.mm([lambda e, kc=kc, pgb=pgb, gb_v=gb_v, cs=cs: e.matmul(pgb, lhsT=gb_v[:, kc, cs], rhs=xnT[:, kc, :], start=(kc == 0), stop=(kc == 7)) for kc in range(8)],
                          r=[kgb, 'xnT'], w=[kpgb])
                    pla, _, kpla = ps()
                    em.mm([lambda e, kc=kc, pla=pla, ms=ms: e.matmul(pla, lhsT=la_v[:, kc, ms], rhs=yaT[:, kc, :], start=(kc == 0), stop=(kc == 3)) for kc in range(4)],
                          r=[bufs[4][1], 'yaT'], w=[kpla])
                    plb_, _, kplb_ = ps()
                    em.mm([lambda e, kc=kc, plb_=plb_, ms=ms: e.matmul(plb_, lhsT=lb_v[:, kc, ms], rhs=ybT[:, kc, :], start=(kc == 0), stop=(kc == 3)) for kc in range(4)],
                          r=[bufs[5][1], 'ybT'], w=[kplb_])
                    sga, ksga = r2k.get()
                    em.op('act', lambda e, sga=sga, pga=pga: e.activation(out=sga, in_=pga, func=AF.Sigmoid), r=[kpga], w=[ksga])
                    sgb, ksgb = r2k.get()
                    em.op('act', lambda e, sgb=sgb, pgb=pgb: e.activation(out=sgb, in_=pgb, func=AF.Sigmoid), r=[kpgb], w=[ksgb])
                    ma, kma = r2k.get()
                    em.op('dve', lambda e, ma=ma, pla=pla, sga=sga: e.tensor_tensor(out=ma, in0=pla, in1=sga, op=ALU.mult), r=[kpla, ksga], w=[kma])
                    mb, kmb = r2k.get()
                    em.op('dve', lambda e, mb=mb, plb_=plb_, sgb=sgb: e.tensor_tensor(out=mb, in0=plb_, in1=sgb, op=ALU.mult), r=[kplb_, ksgb], w=[kmb])
                    em.op('pool', lambda e, mt=mt, ma=ma, mb=mb: e.tensor_tensor(out=mixedT[:, mt, :], in0=ma, in1=mb, op=ALU.add), r=[kma, kmb], w=['mixedT'])
                if b == 0 and dbg_t is not None and 'E1' in DBGSEL:
                    for h_ in range(8):
                        dump_any(mixedT[:, h_, :], ['mixedT'], 512)
                if b == 0:
                    chk('E1')
                wo = [wpiece(6, 2), wpiece(7, 3)]
                xn2bs = []
                def fetch_x(j_):
                    xin_, kx_ = r4k.get()
                    em.dma('sp', lambda e: e.dma_start(out=xin_, in_=x[t0 + j_ * 128:t0 + (j_ + 1) * 128, :]), w=[kx_])
                    return xin_, kx_

                nxt_x = fetch_x(0)
                for j in range(4):
                    tile_idx = b * 4 + j
                    js = slice(j * 128, (j + 1) * 128)
                    xin, kx = nxt_x
                    h1t, kh1 = r4k.get()
                    if j + 1 < 4:
                        nxt_x = fetch_x(j + 1)
                    for hf in range(2):
                        wv = wo[hf][0][:].rearrange("p (c n) -> p c n", n=512)
                        pw_, _, kpw_ = ps()
                        em.mm([lambda e, kc=kc, pw_=pw_, wv=wv, js=js: e.matmul(pw_, lhsT=mixedT[:, kc, js], rhs=wv[:, kc, :], start=(kc == 0), stop=(kc == 7)) for kc in range(8)],
                              r=[wo[hf][1], 'mixedT'], w=[kpw_])
                        fs = slice(hf * 512, (hf + 1) * 512)
                        em.op('dve', lambda e, h1t=h1t, pw_=pw_, fs=fs: e.tensor_tensor(out=h1t[:, fs], in0=pw_, in1=GT1b[:, fs], op=ALU.mult), r=[kpw_, 'GT1b'], w=[kh1])
                        em.op('pool', lambda e, h1t=h1t, xin=xin, fs=fs: e.tensor_tensor(out=h1t[:, fs], in0=h1t[:, fs], in1=xin[:, fs], op=ALU.add), r=[kh1, kx], w=[kh1])
                    em.dma('sp', lambda e, h1t=h1t, j=j: e.dma_start(out=h1D[t0 + j * 128:t0 + (j + 1) * 128, :], in_=h1t), r=[kh1], w=[('h1D', tile_idx)])
                    junk, kj = r2k.get(BF16)
                    ss, kss = sm(1)
                    em.op('act', lambda e, h1t=h1t, junk=junk, ss=ss: e.activation(out=junk, in_=h1t, func=AF.Square, accum_out=ss), r=[kh1], w=[kj, kss])
                    rs, krs = rstd_from_ss(ss, kss, 1.0 / D)
                    xn2f, kxf = r4k.get()
                    em.op('dve', lambda e, xn2f=xn2f, h1t=h1t, rs=rs: e.scalar_tensor_tensor(out=xn2f, in0=h1t, scalar=rs, in1=G2b[:], op0=ALU.mult, op1=ALU.mult),
                          r=[kh1, krs, 'G2b'], w=[kxf])
                    em.op('pool', lambda e, xn2f=xn2f: e.tensor_tensor(out=xn2f, in0=xn2f, in1=SH2b[:], op=ALU.add), r=[kxf, 'SH2b'], w=[kxf])
                    xhost, kxb = (yaT, 'yaT') if j < 2 else (ybT, 'ybT')
                    xn2b = xhost[:].rearrange("p h n -> p (h n)")[:, (j % 2) * 1024:(j % 2 + 1) * 1024]
                    em.op('act', lambda e, xn2b=xn2b, xn2f=xn2f: e.activation(out=xn2b, in_=xn2f, func=AF.Copy), r=[kxf], w=[kxb])
                    xn2bs.append((xn2b, kxb))
                    for half in range(2):
                        ptr, _, kptr = ps()
                        em.mm([lambda e, q=q, ptr=ptr, xn2f=xn2f, half=half: e.transpose(ptr[:, q * 128:(q + 1) * 128], xn2f[:, (half * 4 + q) * 128:(half * 4 + q + 1) * 128], identf[:])
                               for q in range(4)], r=[kxf] + ck(identf), w=[kptr])
                        eng = 'act' if half == 0 else 'dve'
                        if eng == 'act':
                            em.op('act', lambda e, ptr=ptr, half=half: e.activation(out=xn2T[:, half * 4:half * 4 + 4, :].rearrange("p c n -> p (c n)"), in_=ptr, func=AF.Copy),
                                  r=[kptr], w=[('xn2T', half)])
                        else:
                            em.op('dve', lambda e, ptr=ptr, half=half: e.tensor_copy(out=xn2T[:, half * 4:half * 4 + 4, :].rearrange("p c n -> p (c n)"), in_=ptr),
                                  r=[kptr], w=[('xn2T', half)])
                    plg, _, kplg = ps()
                    em.mm([lambda e, kc=kc, plg=plg: e.matmul(plg[:, 0:36], lhsT=xn2T[:, kc, :], rhs=wr[:, kc, :], start=(kc == 0), stop=(kc == 7)) for kc in range(8)],
                          r=[('xn2T', 0), ('xn2T', 1)] + ck(wr), w=[kplg])
                    em.op('dve', lambda e, plg=plg, j=j: e.tensor_tensor(out=lgb[:, j, :], in0=plg[:, 0:36], in1=brb[:], op=ALU.add), r=[kplg] + ck(brb), w=['lgb'])
                    if dbg_t is not None and b == 0 and j == 0 and 'lg' in DBGSEL:
                        dump(lgb[:, 0, :], ['lgb'], 36)
                routing4(em, sm, lgb, onesb, triS, cum, elim, destall, gidxall, wall, b, ps, ck, r2k)
                for j in range(4):
                    tile_idx = b * 4 + j
                    xn2b, kxb = xn2bs[j]
                    for k in range(2):
                        em.dma('pool', lambda e, xn2b=xn2b, k=k, tile_idx=tile_idx: e.indirect_dma_start(
                            out=xsD[:, :], out_offset=IndirectOffsetOnAxis(ap=destall[:, tile_idx * 2 + k:tile_idx * 2 + k + 1], axis=0),
                            in_=xn2b, in_offset=None, bounds_check=pregs['bc'], oob_is_err=False),
                            r=[kxb, ('dest', b)], w=['xsD'])
                if b == 0:
                    chk('E2')
            if dbg_t is not None and 'route' in DBGSEL:
                t, kt = r2k.get()
                em.op('dve', lambda e: e.tensor_copy(out=t[:, 0:64], in_=destall[:]), r=[('dest', i) for i in range(8)], w=[kt])
                dump(t[:, 0:64], [kt], 64)
                dump(wall[:].rearrange("p a b -> p (a b)"), [('dest', i) for i in range(32)], 64)
            chk('P1')
            p1.close()

            p2 = ExitStack()
            es.enter_context(p2)
            wgu = [sb('wgu%d' % i, [128, 8, 512], BF16, p2) for i in range(3)]
            wdn = [sb('wdn%d' % i, [128, 2, D], BF16, p2) for i in range(3)]
            xst = [sb('xst%d' % i, [128, 4, D], BF16, p2) for i in range(2)]
            xsT = [sb('xsT%d' % i, [128, 8, 512], BF16, p2) for i in range(2)]
            hT = [sb('hT%d' % i, [128, 2, 512], BF16, p2) for i in range(2)]
            yst = [sb('yst%d' % i, [128, 4, D], F32, p2) for i in range(2)]
            sgr = Ring(nc, p2, 'sgr', 4, 2048)
            zrow = sb('zrow', [128, D], F32, p2)
            em.op('pool', lambda e: e.memset(zrow[:], 0.0), w=['zrow'])
            em.dma('sp', lambda e: e.dma_start(out=ysD[E * CAP:E * CAP + 128, :], in_=zrow[:]), r=['zrow'], w=[('ysD', 'z')])
            def load_xst(ex_):
                em.dma('sp', lambda e: e.dma_start(out=xst[ex_ % 2][:], in_=xsD[ex_ * CAP:(ex_ + 1) * CAP, :].rearrange("(j p) d -> p j d", p=128)),
                       r=['xsD'], w=[('xst', ex_ % 2)])

            for ex in range(E):
                i2 = ex % 2
                wg_v = w_gate[ex].rearrange("(c p) n -> p c n", p=128)
                wu_v = w_up[ex].rearrange("(c p) n -> p c n", p=128)
                wd_v = w_down[ex].rearrange("(c p) n -> p c n", p=128)
                i3 = ex % 3
                em.dma('pool', lambda e, i3=i3, wg_v=wg_v: e.dma_start(out=wgu[i3][:, :, 0:256], in_=wg_v), w=[('wgu', i3, 0)])
                em.dma('pool', lambda e, i3=i3, wu_v=wu_v: e.dma_start(out=wgu[i3][:, :, 256:512], in_=wu_v), w=[('wgu', i3, 1)])
                em.dma('pool', lambda e, i3=i3, wd_v=wd_v: e.dma_start(out=wdn[i3][:], in_=wd_v), w=[('wdn', i3)])
                if ex == 0:
                    load_xst(0)
                if ex + 1 < E:
                    load_xst(ex + 1)
                for kp_ in range(4):
                    pf, pb, kp = ps()
                    fns = []
                    for q in range(2):
                        kc = kp_ * 2 + q
                        for j in range(4):
                            fns.append(lambda e, q=q, j=j, kc=kc, pb=pb, i2=i2: e.transpose(pb[:, q * 512 + j * 128:q * 512 + (j + 1) * 128], xst[i2][:, j, kc * 128:(kc + 1) * 128], identb[:]))
                    em.mm(fns, r=[('xst', i2)] + ck(identb), w=[kp])
                    dst = xsT[i2][:, kp_ * 2:kp_ * 2 + 2, :].rearrange("p c n -> p (c n)")
                    em.op('act', lambda e, dst=dst, pb=pb: e.activation(out=dst, in_=pb, func=AF.Identity, scale=1.0, bias=epsc[:, 1:2]), r=[kp, 'epsc'], w=[('xsT', i2)])
                pgs = []
                for ft in range(4):
                    pf, pb, kp = ps()
                    em.mm([lambda e, kc=kc, pf=pf, ft=ft, i2=i2, i3=i3: e.matmul(pf, lhsT=wgu[i3][:, kc, ft * 128:(ft + 1) * 128], rhs=xsT[i2][:, kc, :], start=(kc == 0), stop=(kc == 7))
                           for kc in range(8)], r=[('wgu', i3, ft // 2), ('xsT', i2)], w=[kp])
                    pgs.append((pf, kp))
                for f in range(2):
                    sg, ksg = sgr.get()
                    em.op('act', lambda e, sg=sg, f=f, pgs=pgs: e.activation(out=sg, in_=pgs[f][0], func=AF.Silu), r=[pgs[f][1]], w=[ksg])
                    em.op('dve', lambda e, sg=sg, f=f, pgs=pgs, i2=i2: e.tensor_tensor(out=hT[i2][:, f, :], in0=pgs[2 + f][0], in1=sg, op=ALU.mult),
                          r=[pgs[2 + f][1], ksg], w=[('hT', i2)])
                for j in range(4):
                    for hf in range(2):
                        pf, pb, kp = ps()
                        em.mm([lambda e, f=f, pf=pf, j=j, hf=hf, i2=i2, i3=i3: e.matmul(pf, lhsT=hT[i2][:, f, j * 128:(j + 1) * 128], rhs=wdn[i3][:, f, hf * 512:(hf + 1) * 512],
                                                                                  start=(f == 0), stop=(f == 1)) for f in range(2)], r=[('hT', i2), ('wdn', i3)], w=[kp])
                        if (j * 2 + hf) % 2 == 0:
                            em.op('act', lambda e, pf=pf, j=j, hf=hf, i2=i2: e.activation(out=yst[i2][:, j, hf * 512:(hf + 1) * 512], in_=pf, func=AF.Copy), r=[kp], w=[('yst', i2)])
                        else:
                            em.op('dve', lambda e, pf=pf, j=j, hf=hf, i2=i2: e.tensor_copy(out=yst[i2][:, j, hf * 512:(hf + 1) * 512], in_=pf), r=[kp], w=[('yst', i2)])
                em.dma('sp', lambda e, i2=i2, ex=ex: e.dma_start(out=ysD[ex * CAP:(ex + 1) * CAP, :].rearrange("(j p) d -> p j d", p=128), in_=yst[i2][:]),
                       r=[('yst', i2)], w=[('ysD', ex)])
            chk('P2')
            p2.close()

            p3 = ExitStack()
            es.enter_context(p3)
            GT2b = sb('GT2b', [128, D], F32, p3)
            fngb = sb('fngb', [128, D], F32, p3)
            em.dma('sp', lambda e: e.dma_start(out=fngb[:], in_=p_fng), w=['fngb'])
            q4 = Ring(nc, p3, 'q4', 20, 4096)
            q2 = Ring(nc, p3, 'q2', 2, 2048)
            out_toks = []
            def fetch(ti):
                y0, ky0 = q4.get()
                y1, ky1 = q4.get()
                for k, (yy, kyy) in enumerate(((y0, ky0), (y1, ky1))):
                    em.dma('pool', lambda e, yy=yy, k=k, ti=ti: e.indirect_dma_start(
                        out=yy, out_offset=None, in_=ysD[:, :], in_offset=IndirectOffsetOnAxis(ap=gidxall[:, ti * 2 + k:ti * 2 + k + 1], axis=0)),
                        r=[('ysD', 'z')] + [('ysD', ex_) for ex_ in range(E)] + [('dest', ti // 4)], w=[kyy])
                h1t, kh1 = q4.get()
                em.dma('sp', lambda e, h1t=h1t, ti=ti: e.dma_start(out=h1t, in_=h1D[ti * 128:(ti + 1) * 128, :]), r=[('h1D', ti)], w=[kh1])
                return y0, ky0, y1, ky1, h1t, kh1

            pend = [fetch(0), fetch(1)]
            for ti in range(32):
                seq = ti // 16
                if ti % 16 == 0:
                    em.dma('sp', lambda e, seq=seq: e.dma_start(out=GT2b[:], in_=modD[seq:seq + 1, 5120:6144].partition_broadcast(128)), r=MODK, w=['GT2b'])
                y0, ky0, y1, ky1, h1t, kh1 = pend.pop(0)
                if ti + 2 < 32:
                    pend.append(fetch(ti + 2))
                m, km = q4.get()
                em.op('act', lambda e, m=m, y0=y0, ti=ti: e.activation(out=m, in_=y0, func=AF.Copy, scale=wall[:, ti, 0:1]), r=[ky0, ('dest', ti // 4)], w=[km])
                em.op('dve', lambda e, m=m, y1=y1, ti=ti: e.scalar_tensor_tensor(out=m, in0=y1, scalar=wall[:, ti, 1:2], in1=m, op0=ALU.mult, op1=ALU.add),
                      r=[ky1, km, ('dest', ti // 4)], w=[km])
                em.op('pool', lambda e, m=m: e.tensor_tensor(out=m, in0=m, in1=GT2b[:], op=ALU.mult), r=[km, 'GT2b'], w=[km])
                em.op('dve', lambda e, m=m, h1t=h1t: e.tensor_tensor(out=m, in0=m, in1=h1t, op=ALU.add), r=[km, kh1], w=[km])
                junk, kj = q2.get(BF16)
                ss, kss = sm(1)
                em.op('act', lambda e, m=m, junk=junk, ss=ss: e.activation(out=junk, in_=m, func=AF.Square, accum_out=ss), r=[km], w=[kj, kss])
                rs, krs = rstd_from_ss(ss, kss, 1.0 / D)
                o_, ko = q4.get()
                em.op('dve', lambda e, o_=o_, m=m, rs=rs: e.scalar_tensor_tensor(out=o_, in0=m, scalar=rs, in1=fngb[:], op0=ALU.mult, op1=ALU.mult), r=[km, krs, 'fngb'], w=[ko])
                out_toks.append(em.dma('sp', lambda e, o_=o_, ti=ti: e.dma_start(out=out[ti * 128:(ti + 1) * 128, :], in_=o_), r=[ko], w=[('out', ti)]))
        except StopBuild:
            pass
        em.wait_keys('sp', [k for k in em.lastw if isinstance(k, tuple) and k[0] in ('dbg', 'out')])
        for q_ in ('sp', 'pool'):
            d_ = em.dq[q_]
            for i_, v_ in enumerate(d_['vals']):
                if v_ > 0:
                    em._wait('sp', (q_, i_), v_)
        em.finish()
    return nc


DBGSEL = ()
STOP = None
SERIAL = False


class StopBuild(Exception):
    pass


def chk(name):
    if STOP == name:
        raise StopBuild()


def routing(em, sm, lg, onesb, triS, cum, elim, destall, gidxall, wall, ti, ps, ck, r2k):
    kl = 'lg'
    gmax, kgm = sm(1)
    em.op('dve', lambda e: e.tensor_reduce(out=gmax, in_=lg[:, 0:4], axis=AX.X, op=ALU.max), r=[kl], w=[kgm])
    ohg, kohg = sm(4)
    em.op('dve', lambda e: e.tensor_scalar(out=ohg, in0=lg[:, 0:4], scalar1=gmax, scalar2=None, op0=ALU.is_equal), r=[kl, kgm], w=[kohg])
    ngm, kngm = sm(1)
    em.op('dve', lambda e: e.tensor_scalar(out=ngm, in0=gmax, scalar1=-1.0, scalar2=None, op0=ALU.mult), r=[kgm], w=[kngm])
    eg, keg = sm(4)
    sg, ksg = sm(1)
    em.op('act', lambda e: e.activation(out=eg, in_=lg[:, 0:4], func=AF.Exp, bias=ngm, accum_out=sg), r=[kl, kngm], w=[keg, ksg])
    pg, kpg = sm(1)
    em.op('dve', lambda e: e.reciprocal(out=pg, in_=sg), r=[ksg], w=[kpg])
    les, kles = sm(8)
    em.op('dve', lambda e: e.tensor_scalar(out=les, in0=lg[:, 4:12], scalar1=ohg[:, 0:1], scalar2=None, op0=ALU.mult), r=[kl, kohg], w=[kles])
    for g in range(1, 4):
        em.op('dve', lambda e, g=g: e.scalar_tensor_tensor(out=les, in0=lg[:, 4 + 8 * g:12 + 8 * g], scalar=ohg[:, g:g + 1], in1=les, op0=ALU.mult, op1=ALU.add),
              r=[kl, kohg, kles], w=[kles])
    m8, km8 = sm(8)
    em.op('dve', lambda e: e.max(out=m8, in_=les), r=[kles], w=[km8])
    d21, kd21 = sm(1)
    em.op('dve', lambda e: e.tensor_tensor(out=d21, in0=m8[:, 1:2], in1=m8[:, 0:1], op=ALU.subtract), r=[km8], w=[kd21])
    e21, ke21 = sm(1)
    em.op('act', lambda e: e.activation(out=e21, in_=d21, func=AF.Exp), r=[kd21], w=[ke21])
    den, kden = sm(1)
    em.op('dve', lambda e: e.tensor_scalar(out=den, in0=e21, scalar1=1.0, scalar2=None, op0=ALU.add), r=[ke21], w=[kden])
    rden, krden = sm(1)
    em.op('dve', lambda e: e.reciprocal(out=rden, in_=den), r=[kden], w=[krden])
    w1, kw1 = sm(1)
    em.op('dve', lambda e: e.tensor_tensor(out=w1, in0=pg, in1=rden, op=ALU.mult), r=[kpg, krden], w=[kw1])
    w2, kw2 = sm(1)
    em.op('dve', lambda e: e.tensor_tensor(out=w2, in0=w1, in1=e21, op=ALU.mult), r=[kw1, ke21], w=[kw2])
    ohs = []
    for k in range(2):
        sel, ksel = sm(8)
        em.op('dve', lambda e, k=k, sel=sel: e.tensor_scalar(out=sel, in0=les, scalar1=m8[:, k:k + 1], scalar2=None, op0=ALU.is_equal), r=[kles, km8], w=[ksel])
        oh, koh = sm(32)
        for g in range(4):
            em.op('dve', lambda e, g=g, oh=oh, sel=sel: e.tensor_scalar(out=oh[:, g * 8:(g + 1) * 8], in0=sel, scalar1=ohg[:, g:g + 1], scalar2=None, op0=ALU.mult),
                  r=[ksel, kohg], w=[koh])
        ohs.append((oh, koh))
    ohsum, kohs = r2k.get(BF16)
    em.op('dve', lambda e: e.tensor_tensor(out=ohsum[:, 0:32], in0=ohs[0][0], in1=ohs[1][0], op=ALU.add), r=[ohs[0][1], ohs[1][1]], w=[kohs])
    pr, _, kpr = ps()
    em.mm([lambda e: e.matmul(pr[:, 0:32], lhsT=triS[:], rhs=ohsum[:, 0:32], start=True, stop=True),
           lambda e: e.matmul(pr[:, 32:64], lhsT=onesb[:], rhs=ohsum[:, 0:32], start=True, stop=True)], r=[kohs] + ck(triS, onesb), w=[kpr])
    rk, krk = sm(32)
    em.op('dve', lambda e: e.tensor_tensor(out=rk, in0=pr[:, 0:32], in1=cum[:], op=ALU.add), r=[kpr, 'cum'], w=[krk])
    em.op('dve', lambda e: e.tensor_tensor(out=cum[:], in0=pr[:, 32:64], in1=cum[:], op=ALU.add), r=[kpr, 'cum', krk], w=['cum'])
    for k in range(2):
        oh, koh = ohs[k]
        t32, kt32 = sm(32)
        dst, kdst = sm(1)
        em.op('dve', lambda e, t32=t32, oh=oh: e.tensor_tensor(out=t32, in0=oh, in1=rk, op=ALU.mult), r=[koh, krk], w=[kt32])
        em.op('dve', lambda e, t32=t32, dst=dst: e.tensor_reduce(out=dst, in_=t32, axis=AX.X, op=ALU.add), r=[kt32], w=[kdst])
        l32, kl32 = sm(32)
        lim, klim = sm(1)
        em.op('dve', lambda e, l32=l32, oh=oh: e.tensor_tensor(out=l32, in0=oh, in1=elim[:], op=ALU.mult), r=[koh] + ck(elim), w=[kl32])
        em.op('dve', lambda e, l32=l32, lim=lim: e.tensor_reduce(out=lim, in_=l32, axis=AX.X, op=ALU.add), r=[kl32], w=[klim])
        ok, kok = sm(1)
        em.op('dve', lambda e, ok=ok, dst=dst, lim=lim: e.tensor_tensor(out=ok, in0=dst, in1=lim, op=ALU.is_lt), r=[kdst, klim], w=[kok])
        nok, knok = sm(1)
        em.op('dve', lambda e, nok=nok, ok=ok: e.tensor_scalar(out=nok, in0=ok, scalar1=-1.0, scalar2=1.0, op0=ALU.mult, op1=ALU.add), r=[kok], w=[knok])
        dv, kdv = sm(1)
        em.op('dve', lambda e, dv=dv, dst=dst, ok=ok: e.tensor_tensor(out=dv, in0=dst, in1=ok, op=ALU.mult), r=[kdst, kok], w=[kdv])
        si, ksi = sm(1)
        em.op('dve', lambda e, si=si, nok=nok, dv=dv: e.scalar_tensor_tensor(out=si, in0=nok, scalar=float(E * CAP + 64), in1=dv, op0=ALU.mult, op1=ALU.add), r=[knok, kdv], w=[ksi])
        gi_, kgi = sm(1)
        em.op('dve', lambda e, gi_=gi_, nok=nok, dv=dv: e.scalar_tensor_tensor(out=gi_, in0=nok, scalar=float(E * CAP), in1=dv, op0=ALU.mult, op1=ALU.add), r=[knok, kdv], w=[kgi])
        em.op('dve', lambda e, k=k, si=si: e.tensor_copy(out=destall[:, ti * 2 + k:ti * 2 + k + 1], in_=si), r=[ksi], w=[('dest', ti)])
        em.op('dve', lambda e, k=k, gi_=gi_: e.tensor_copy(out=gidxall[:, ti * 2 + k:ti * 2 + k + 1], in_=gi_), r=[kgi], w=[('dest', ti)])
        wk, kwk = (w1, kw1) if k == 0 else (w2, kw2)
        em.op('dve', lambda e, k=k, wk=wk, ok=ok: e.tensor_tensor(out=wall[:, ti, k:k + 1], in0=wk, in1=ok, op=ALU.mult), r=[kwk, kok], w=[('dest', ti)])


def _consts():
    bf = ml_dtypes.bfloat16
    i = np.arange(128)
    c = {}
    c['k_identb'] = np.eye(128, dtype=np.float32).astype(bf)
    c['k_identq'] = np.tile(np.eye(128, dtype=np.float32), (1, 4)).astype(bf)
    c['k_identf'] = np.eye(128, dtype=np.float32)
    c['k_onesb'] = np.ones((128, 128), np.float32).astype(bf)
    c['k_onesf'] = np.ones((128, 128), np.float32)
    c['k_triU'] = (i[:, None] <= i[None, :]).astype(np.float32)
    c['k_maskT'] = np.where(i[None, :] >= i[:, None], 0.0, NEG).astype(np.float32)
    c['k_maskS'] = np.where(i[:, None] > i[None, :], 0.0, -NEG).astype(np.float32)
    c['k_triS'] = (i[:, None] < i[None, :]).astype(np.float32).astype(bf)
    pc = np.zeros((128, 4, 16), np.float32)
    for gi, win in enumerate((2, 4, 8, 16)):
        t = np.arange(16)
        pc[:, gi, :] = 1.0 / np.minimum(t + 1, win)
    c['k_pcorr'] = pc
    bm = np.zeros((128, 7, 128), np.float32)
    for l in range(7):
        s_ = 1 << l
        bm[:, l, :] = ((i[:, None] // (2 * s_) == i[None, :] // (2 * s_)) & (i[:, None] % (2 * s_) >= s_) & (i[None, :] % (2 * s_) < s_))
    c['k_bmask'] = bm.astype(bf)
    c['k_bmaskT'] = np.ascontiguousarray(bm.transpose(2, 1, 0)).astype(bf)
    c['k_ebase'] = np.tile((np.arange(32) * CAP).astype(np.float32), (128, 1))
    c['k_elim'] = np.tile(((np.arange(32) + 1) * CAP).astype(np.float32), (128, 1))
    return c


def _prep_inputs(inp):
    f = lambda a: np.ascontiguousarray(np.asarray(a, dtype=np.float32))
    shared = {}
    shared['w_ada'] = f(inp['w_ada'][0])
    shared['b_ada'] = f(inp['b_ada'][0]).reshape(1, -1)
    shared['w_in'] = f(inp['w_in'][0])
    shared['w_lift_a'] = f(inp['w_lift_a'][0])
    shared['w_lift_b'] = f(inp['w_lift_b'][0])
    shared['w_out'] = f(inp['w_out'][0])
    shared['pool_w'] = f(inp['pool_w'][0])
    shared['w_gate'] = f(inp['w_gate'][0])
    shared['w_up'] = f(inp['w_up'][0])
    shared['w_down'] = f(inp['w_down'][0])
    shared['p_n1g'] = f(np.asarray(inp['norm1_g'][0]).reshape(8, 128).T)
    shared['p_convw'] = f(np.asarray(inp['conv_w'][0]).reshape(4, 12, 128).transpose(2, 1, 0))
    shared['p_alog'] = f(np.broadcast_to(np.asarray(inp['a_log'][0]).reshape(1, 4), (128, 4)))
    shared['p_dtb'] = f(np.broadcast_to(np.asarray(inp['dt_bias'][0]).reshape(1, 4), (128, 4)))
    shared['p_dng'] = f(np.asarray(inp['dn_norm_g'][0]).reshape(128, 1))
    shared['p_pscale'] = f(np.asarray(inp['pool_scale'][0]).reshape(4, 128).T)
    shared['p_n2g'] = f(np.broadcast_to(np.asarray(inp['norm2_g'][0]).reshape(1, D), (128, D)))
    shared['p_fng'] = f(np.broadcast_to(np.asarray(inp['final_norm_g']).reshape(1, D), (128, D)))
    br = np.concatenate([np.asarray(inp['b_router_group'][0]), np.asarray(inp['b_router_expert'][0])]).reshape(1, 36)
    shared['p_brb'] = f(np.broadcast_to(br, (128, 36)))
    wrc = np.concatenate([np.asarray(inp['w_router_group'][0]), np.asarray(inp['w_router_expert'][0])], axis=1)
    shared['p_wr'] = f(wrc.reshape(8, 128, 36).transpose(1, 0, 2))
    shared.update(_consts())
    xs = np.asarray(inp['x'], dtype=np.float32)
    cs = np.asarray(inp['c'], dtype=np.float32)
    in_maps = []
    for i in range(NCORES):
        m = dict(shared)
        m['x'] = np.ascontiguousarray(xs[2 * i:2 * i + 2].reshape(TOK, D))
        m['cT'] = np.ascontiguousarray(cs[2 * i:2 * i + 2].reshape(2, 8, 128).transpose(2, 1, 0))
        in_maps.append(m)
    return in_maps


_NC_CACHE = {}


def kernel(**inputs):
    in_maps = _prep_inputs(inputs)
    if 'nc' not in _NC_CACHE:
        _NC_CACHE['nc'] = build_nc()
    nc = _NC_CACHE['nc']
    res = run_bass_kernel_spmd(nc, in_maps, core_ids=list(range(NCORES)))
    outs = [np.asarray(r['out'], dtype=np.float32).reshape(2, 2048, D) for r in res.results]
    return np.concatenate(outs, axis=0)


def routing4(em, sm, lgb, onesb, triS, cum, elim, destall, gidxall, wall, b, ps, ck, r2k):
    kl = 'lgb'
    T = 4

    def v3(ap, n):
        return ap.rearrange("p (t n) -> p t n", n=n)

    def bc_last(ap, n):
        return ap.unsqueeze(2).broadcast_to([128, T, n])
    lgg = lgb[:, :, 0:4]
    gmax, kgm = sm(T)
    em.op('dve', lambda e: e.tensor_reduce(out=gmax, in_=lgg, axis=AX.X, op=ALU.max), r=[kl], w=[kgm])
    ohg_, kohg = sm(16)
    ohg = v3(ohg_, 4)
    em.op('dve', lambda e: e.tensor_tensor(out=ohg, in0=lgg, in1=bc_last(gmax, 4), op=ALU.is_equal), r=[kl, kgm], w=[kohg])
    sub_, ksub = sm(16)
    em.op('dve', lambda e: e.tensor_tensor(out=v3(sub_, 4), in0=lgg, in1=bc_last(gmax, 4), op=ALU.subtract), r=[kl, kgm], w=[ksub])
    eg_, keg = sm(16)
    em.op('act', lambda e: e.activation(out=eg_, in_=sub_, func=AF.Exp), r=[ksub], w=[keg])
    sg, ksg = sm(T)
    em.op('dve', lambda e: e.tensor_reduce(out=sg, in_=v3(eg_, 4), axis=AX.X, op=ALU.add), r=[keg], w=[ksg])
    pg, kpg = sm(T)
    em.op('dve', lambda e: e.reciprocal(out=pg, in_=sg), r=[ksg], w=[kpg])
    prod_, kprod = r2k.get()
    prod = prod_[:, 0:T * 32]
    le4 = lgb[:, :, 4:36].rearrange("p t (g j) -> p t g j", j=8)
    em.op('dve', lambda e: e.tensor_tensor(out=prod.rearrange("p (t g j) -> p t g j", g=4, j=8), in0=le4,
                                           in1=ohg.unsqueeze(3).broadcast_to([128, T, 4, 8]), op=ALU.mult), r=[kl, kohg], w=[kprod])
    les_, kles = sm(32)
    les = v3(les_, 8)
    em.op('dve', lambda e: e.tensor_reduce(out=les, in_=prod.rearrange("p (t g j) -> p t j g", g=4, j=8), axis=AX.X, op=ALU.add), r=[kprod], w=[kles])
    m1, km1 = sm(T)
    em.op('dve', lambda e: e.tensor_reduce(out=m1, in_=les, axis=AX.X, op=ALU.max), r=[kles], w=[km1])
    sel1_, ksel1 = sm(32)
    sel1 = v3(sel1_, 8)
    em.op('dve', lambda e: e.tensor_tensor(out=sel1, in0=les, in1=bc_last(m1, 8), op=ALU.is_equal), r=[kles, km1], w=[ksel1])
    les2_, kles2 = sm(32)
    les2 = v3(les2_, 8)
    em.op('dve', lambda e: e.scalar_tensor_tensor(out=les2, in0=sel1, scalar=NEG, in1=les, op0=ALU.mult, op1=ALU.add), r=[ksel1, kles], w=[kles2])
    m2, km2 = sm(T)
    em.op('dve', lambda e: e.tensor_reduce(out=m2, in_=les2, axis=AX.X, op=ALU.max), r=[kles2], w=[km2])
    sel2_, ksel2 = sm(32)
    sel2 = v3(sel2_, 8)
    em.op('dve', lambda e: e.tensor_tensor(out=sel2, in0=les2, in1=bc_last(m2, 8), op=ALU.is_equal), r=[kles2, km2], w=[ksel2])
    d21, kd21 = sm(T)
    em.op('dve', lambda e: e.tensor_tensor(out=d21, in0=m2, in1=m1, op=ALU.subtract), r=[km1, km2], w=[kd21])
    e21, ke21 = sm(T)
    em.op('act', lambda e: e.activation(out=e21, in_=d21, func=AF.Exp), r=[kd21], w=[ke21])
    den, kden = sm(T)
    em.op('dve', lambda e: e.tensor_scalar(out=den, in0=e21, scalar1=1.0, scalar2=None, op0=ALU.add), r=[ke21], w=[kden])
    rden, krden = sm(T)
    em.op('dve', lambda e: e.reciprocal(out=rden, in_=den), r=[kden], w=[krden])
    w1, kw1 = sm(T)
    em.op('dve', lambda e: e.tensor_tensor(out=w1, in0=pg, in1=rden, op=ALU.mult), r=[kpg, krden], w=[kw1])
    w2, kw2 = sm(T)
    em.op('dve', lambda e: e.tensor_tensor(out=w2, in0=w1, in1=e21, op=ALU.mult), r=[kw1, ke21], w=[kw2])
    ohs = []
    for k, (sel, ksel) in enumerate(((sel1, ksel1), (sel2, ksel2))):
        oh_, koh = r2k.get()
        oh = oh_[:, 0:T * 32]
        em.op('dve', lambda e, oh=oh, sel=sel: e.tensor_tensor(out=oh.rearrange("p (t g j) -> p t g j", g=4, j=8),
                                                               in0=ohg.unsqueeze(3).broadcast_to([128, T, 4, 8]),
                                                               in1=sel.unsqueeze(2).broadcast_to([128, T, 4, 8]), op=ALU.mult), r=[kohg, ksel], w=[koh])
        ohs.append((oh, koh))
    ohsum_, kohs = r2k.get(BF16)
    ohsum = ohsum_[:, 0:T * 32]
    em.op('dve', lambda e: e.tensor_tensor(out=ohsum, in0=ohs[0][0], in1=ohs[1][0], op=ALU.add), r=[ohs[0][1], ohs[1][1]], w=[kohs])
    pr, _, kpr = ps()
    fns = []
    for t in range(T):
        terms = [(triS, t)] + [(onesb, t2) for t2 in range(t)]
        for i_, (lt, t2) in enumerate(terms):
            fns.append(lambda e, t=t, lt=lt, t2=t2, i_=i_, n_=len(terms): e.matmul(pr[:, t * 32:(t + 1) * 32], lhsT=lt[:], rhs=ohsum[:, t2 * 32:(t2 + 1) * 32],
                                                                                  start=(i_ == 0), stop=(i_ == n_ - 1)))
    for t in range(T):
        fns.append(lambda e, t=t: e.matmul(pr[:, 128:160], lhsT=onesb[:], rhs=ohsum[:, t * 32:(t + 1) * 32], start=(t == 0), stop=(t == T - 1)))
    em.mm(fns, r=[kohs] + ck(triS, onesb), w=[kpr])
    rk_, krk = r2k.get()
    rk = rk_[:, 0:T * 32]
    em.op('dve', lambda e: e.tensor_tensor(out=v3(rk, 32), in0=v3(pr[:, 0:128], 32), in1=cum[:].unsqueeze(1).broadcast_to([128, T, 32]), op=ALU.add),
          r=[kpr, 'cum'], w=[krk])
    em.op('dve', lambda e: e.tensor_tensor(out=cum[:], in0=pr[:, 128:160], in1=cum[:], op=ALU.add), r=[kpr, 'cum', krk], w=['cum'])
    dsl = slice(b * 8, (b + 1) * 8)
    for k in range(2):
        oh, koh = ohs[k]
        t32_, kt32 = r2k.get()
        t32 = t32_[:, 0:T * 32]
        em.op('dve', lambda e, t32=t32, oh=oh: e.tensor_tensor(out=t32, in0=oh, in1=rk, op=ALU.mult), r=[koh, krk], w=[kt32])
        dst, kdst = sm(T)
        em.op('dve', lambda e, t32=t32, dst=dst: e.tensor_reduce(out=dst, in_=v3(t32, 32), axis=AX.X, op=ALU.add), r=[kt32], w=[kdst])
        l32_, kl32 = r2k.get()
        l32 = l32_[:, 0:T * 32]
        em.op('dve', lambda e, l32=l32, oh=oh: e.tensor_tensor(out=v3(l32, 32), in0=v3(oh, 32), in1=elim[:].unsqueeze(1).broadcast_to([128, T, 32]), op=ALU.mult),
              r=[koh] + ck(elim), w=[kl32])
        lim, klim = sm(T)
        em.op('dve', lambda e, l32=l32, lim=lim: e.tensor_reduce(out=lim, in_=v3(l32, 32), axis=AX.X, op=ALU.add), r=[kl32], w=[klim])
        ok, kok = sm(T)
        em.op('dve', lambda e, ok=ok, dst=dst, lim=lim: e.tensor_tensor(out=ok, in0=dst, in1=lim, op=ALU.is_lt), r=[kdst, klim], w=[kok])
        nok, knok = sm(T)
        em.op('dve', lambda e, nok=nok, ok=ok: e.tensor_scalar(out=nok, in0=ok, scalar1=-1.0, scalar2=1.0, op0=ALU.mult, op1=ALU.add), r=[kok], w=[knok])
        dv, kdv = sm(T)
        em.op('dve', lambda e, dv=dv, dst=dst, ok=ok: e.tensor_tensor(out=dv, in0=dst, in1=ok, op=ALU.mult), r=[kdst, kok], w=[kdv])
        si, ksi = sm(T)
        em.op('dve', lambda e, si=si, nok=nok, dv=dv: e.scalar_tensor_tensor(out=si, in0=nok, scalar=float(E * CAP + 64), in1=dv, op0=ALU.mult, op1=ALU.add), r=[knok, kdv], w=[ksi])
        gi_, kgi = sm(T)
        em.op('dve', lambda e, gi_=gi_, nok=nok, dv=dv: e.scalar_tensor_tensor(out=gi_, in0=nok, scalar=float(E * CAP), in1=dv, op0=ALU.mult, op1=ALU.add), r=[knok, kdv], w=[kgi])
        em.op('dve', lambda e, k=k, si=si: e.tensor_copy(out=destall[:, dsl].rearrange("p (t k) -> p t k", k=2)[:, :, k], in_=si), r=[ksi], w=[('dest', b)])
        em.op('dve', lambda e, k=k, gi_=gi_: e.tensor_copy(out=gidxall[:, dsl].rearrange("p (t k) -> p t k", k=2)[:, :, k], in_=gi_), r=[kgi], w=[('dest', b)])
        wk, kwk = (w1, kw1) if k == 0 else (w2, kw2)
        em.op('dve', lambda e, k=k, wk=wk, ok=ok: e.tensor_tensor(out=wall[:, b * 4:(b + 1) * 4, k], in0=wk, in1=ok, op=ALU.mult), r=[kwk, kok], w=[('dest', b)])
```

```python
import types
import numpy as np
import ml_dtypes
from contextlib import ExitStack
import concourse.bass as bass
import concourse.mybir as mybir
from concourse.bass import IndirectOffsetOnAxis
from concourse.bass_utils import run_bass_kernel_spmd

F32 = mybir.dt.float32
BF16 = mybir.dt.bfloat16
I32 = mybir.dt.int32
U32 = mybir.dt.uint32
AF = mybir.ActivationFunctionType
ALU = mybir.AluOpType
AX = mybir.AxisListType

NCORES = 8
D = 1024
TOK = 4096
BLK = 512
NBLK = TOK // BLK
E = 32
CAP = 512
DFF = 256
EPS = 1e-6
NEG = -1.0e30
WIN_RES = 2568
ENGS = ('pe', 'act', 'dve', 'pool', 'sp')


def _freeze(fn):
    if fn.__closure__ is None:
        return fn
    cells = []
    for c in fn.__closure__:
        try:
            cells.append(types.CellType(c.cell_contents))
        except ValueError:
            cells.append(c)
    g = types.FunctionType(fn.__code__, fn.__globals__, fn.__name__, fn.__defaults__, tuple(cells))
    g.__kwdefaults__ = fn.__kwdefaults__
    return g


class Em:
    def __init__(self, nc, es):
        self.nc = nc
        self.streams = {e: [] for e in ENGS}
        self.sem = {e: es.enter_context(nc.semaphore('s_' + e)) for e in ('pe', 'act', 'dve', 'pool')}
        self.cnt = {e: 0 for e in self.sem}
        self.waited = {e: {} for e in ENGS}
        self.lastw = {}
        self.reads = {}
        self.dq = {}
        for q, n in (('sp', 28), ('pool', 14), ('act', 4)):
            sems = [es.enter_context(nc.semaphore('d_%s%d' % (q, i))) for i in range(n)]
            self.dq[q] = dict(sems=sems, vals=[0] * n, nxt=0)

    def _semh(self, key):
        return self.sem[key] if isinstance(key, str) else self.dq[key[0]]['sems'][key[1]]

    def _wait(self, eng, key, val):
        if self.waited[eng].get(key, 0) >= val:
            return
        self.waited[eng][key] = val
        s = self._semh(key)
        self.streams[eng].append(lambda e, s=s, v=val: e.wait_ge(s, v))

    def _deps(self, eng, r, w, pe_inorder=False):
        need = {}

        def add(tok):
            if tok is None:
                return
            k, v = tok
            if need.get(k, 0) < v:
                need[k] = v
        for key in r:
            add(self.lastw.get(key))
        for key in w:
            add(self.lastw.get(key))
            for t in self.reads.get(key, {}).items():
                add(t)
        if SERIAL:
            for k2 in ('pe', 'act', 'dve', 'pool'):
                if self.cnt[k2] > 0:
                    need[k2] = self.cnt[k2]
            for q2, d2 in self.dq.items():
                for i2, v2 in enumerate(d2['vals']):
                    if v2 > 0:
                        need[(q2, i2)] = max(need.get((q2, i2), 0), v2)
        for k, v in need.items():
            if pe_inorder and k == 'pe':
                continue
            self._wait(eng, k, v)

    def _track(self, tok, r, w):
        for key in w:
            self.lastw[key] = tok
            self.reads[key] = {}
        for key in r:
            d = self.reads.setdefault(key, {})
            if d.get(tok[0], 0) < tok[1]:
                d[tok[0]] = tok[1]

    def op(self, eng, fn, r=(), w=()):
        fn = _freeze(fn)
        self._deps(eng, r, w)
        self.cnt[eng] += 1
        tok = (eng, self.cnt[eng])
        s = self.sem[eng]
        self.streams[eng].append(lambda e, fn=fn, s=s: fn(e).then_inc(s, 1))
        self._track(tok, r, w)
        return tok

    def mm(self, fns, r=(), w=()):
        fns = [_freeze(f) for f in fns]
        self._deps('pe', r, w, pe_inorder=True)
        self.cnt['pe'] += 1
        tok = ('pe', self.cnt['pe'])
        s = self.sem['pe']
        for fn in fns[:-1]:
            self.streams['pe'].append(lambda e, fn=fn: fn(e))
        self.streams['pe'].append(lambda e, fn=fns[-1], s=s: fn(e).then_inc(s, 1))
        self._track(tok, r, w)
        return tok

    def dma(self, q, fn, r=(), w=()):
        fn = _freeze(fn)
        d = self.dq[q]
        i = d['nxt']
        d['nxt'] = (i + 1) % len(d['sems'])
        key = (q, i)
        if d['vals'][i] > 0:
            self._wait(q, key, d['vals'][i])
        self._deps(q, r, w)
        d['vals'][i] += 16
        tok = (key, d['vals'][i])
        s = d['sems'][i]
        self.streams[q].append(lambda e, fn=fn, s=s: fn(e).then_inc(s, 16))
        self._track(tok, r, w)
        return tok

    def wait_keys(self, eng, keys):
        self._deps(eng, keys, keys)

    def finish(self):
        nc = self.nc
        st = self.streams
        with nc.Block() as block:
            @block.tensor
            def _(e):
                for f in st['pe']:
                    f(e)

            @block.scalar
            def _(e):
                for f in st['act']:
                    f(e)

            @block.vector
            def _(e):
                for f in st['dve']:
                    f(e)

            @block.gpsimd
            def _(e):
                for f in st['pool']:
                    f(e)

            @block.sync
            def _(e):
                for f in st['sp']:
                    f(e)


class Ring:
    def __init__(self, nc, es, name, n, nbytes):
        self.t = [es.enter_context(nc.sbuf_tensor('%s%d' % (name, i), [128, nbytes // 4], F32)) for i in range(n)]
        self.name = name
        self.n = n
        self.i = 0

    def get(self, dt=F32):
        i = self.i
        self.i = (i + 1) % self.n
        ap = self.t[i][:]
        if dt != F32:
            ap = ap.bitcast(dt)
        return ap, (self.name, i)


def build_nc(dbg=None):
    nc = bass.Bass("TRN2", target_bir_lowering=False)

    def din(name, shape, dt=F32):
        return nc.dram_tensor(name, list(shape), dt, kind="ExternalInput").ap()

    x = din("x", [TOK, D])
    cT = din("cT", [128, 8, 2])
    w_ada = din("w_ada", [D, 6 * D])
    b_ada = din("b_ada", [1, 6 * D])
    w_in = din("w_in", [D, 4616])
    w_la = din("w_lift_a", [512, D])
    w_lb = din("w_lift_b", [512, D])
    w_out = din("w_out", [D, D])
    pool_w = din("pool_w", [4, 128, 128])
    w_gate = din("w_gate", [E, D, DFF])
    w_up = din("w_up", [E, D, DFF])
    w_down = din("w_down", [E, DFF, D])
    p_n1g = din("p_n1g", [128, 8])
    p_convw = din("p_convw", [128, 12, 4])
    p_alog = din("p_alog", [128, 4])
    p_dtb = din("p_dtb", [128, 4])
    p_dng = din("p_dng", [128, 1])
    p_pscale = din("p_pscale", [128, 4])
    p_n2g = din("p_n2g", [128, D])
    p_fng = din("p_fng", [128, D])
    p_brb = din("p_brb", [128, 36])
    p_wr = din("p_wr", [128, 8, 36])
    k_identb = din("k_identb", [128, 128], BF16)
    k_identq = din("k_identq", [128, 512], BF16)
    k_identf = din("k_identf", [128, 128])
    k_onesb = din("k_onesb", [128, 128], BF16)
    k_onesf = din("k_onesf", [128, 128])
    k_triU = din("k_triU", [128, 128])
    k_maskT = din("k_maskT", [128, 128])
    k_maskS = din("k_maskS", [128, 128])
    k_triS = din("k_triS", [128, 128], BF16)
    k_pcorr = din("k_pcorr", [128, 4, 16])
    k_ebase = din("k_ebase", [128, 32])
    k_bmask = din("k_bmask", [128, 7, 128], BF16)
    k_bmaskT = din("k_bmaskT", [128, 7, 128], BF16)
    k_elim = din("k_elim", [128, 32])

    out = nc.dram_tensor("out", [TOK, D], F32, kind="ExternalOutput").ap()
    dbg_t = None
    if dbg is not None:
        dbg_t = nc.dram_tensor("dbg", list(dbg), F32, kind="ExternalOutput").ap()
    modD = nc.dram_tensor("modD", [2, 6 * D], F32).ap()
    wsD = nc.dram_tensor("wsD", [8, 128, 4096], BF16).ap()
    h1D = nc.dram_tensor("h1D", [TOK, D], F32).ap()
    xsD = nc.dram_tensor("xsD", [E * CAP, D], BF16).ap()
    ysD = nc.dram_tensor("ysD", [E * CAP + 128, D], F32).ap()

    es = ExitStack()
    with es:
        em = Em(nc, es)

        def sb(name, shape, dt=F32, stack=es):
            return stack.enter_context(nc.sbuf_tensor(name, list(shape), dt))

        pregs = {}
        em.streams['pool'].append(lambda e: pregs.__setitem__('bc', e.to_reg(E * CAP - 1)))

        psb = [es.enter_context(nc.psum_tensor('ps%d' % i, [128, 512], F32)) for i in range(8)]
        pstate = {'i': 0}

        def ps():
            i = pstate['i']
            pstate['i'] = (i + 1) % 8
            return psb[i][:], psb[i][:].bitcast(BF16), ('ps', i)

        identb = sb('identb', [128, 128], BF16)
        identq = sb('identq', [128, 512], BF16)
        identf = sb('identf', [128, 128])
        onesb = sb('onesb', [128, 128], BF16)
        onesf = sb('onesf', [128, 128])
        triU = sb('triU', [128, 128])
        maskT = sb('maskT', [128, 128])
        maskS = sb('maskS', [128, 128])
        triS = sb('triS', [128, 128], BF16)
        pcorr = sb('pcorr', [128, 4, 16])
        bmask = sb('bmask', [128, 7, 128], BF16)
        bmaskT = sb('bmaskT', [128, 7, 128], BF16)
        n1g = sb('n1g', [128, 8])
        convw = sb('convw', [128, 12, 4])
        alog = sb('alog', [128, 4])
        dtb = sb('dtb', [128, 4])
        dng = sb('dng', [128, 1])
        pscale = sb('pscale', [128, 4])
        brb = sb('brb', [128, 36])
        wr = sb('wr', [128, 8, 36])
        cum = sb('cum', [128, 32])
        elim = sb('elim', [128, 32])
        destall = sb('destall', [128, 64], I32)
        gidxall = sb('gidxall', [128, 64], I32)
        wall = sb('wall', [128, 32, 2])
        cst = [(identb, k_identb), (identq, k_identq), (identf, k_identf), (onesb, k_onesb), (onesf, k_onesf),
               (triU, k_triU), (maskT, k_maskT), (maskS, k_maskS), (triS, k_triS), (pcorr, k_pcorr),
               (n1g, p_n1g), (convw, p_convw), (alog, p_alog), (dtb, p_dtb), (dng, p_dng), (pscale, p_pscale),
               (brb, p_brb), (wr, p_wr), (cum, k_ebase), (elim, k_elim), (bmask, k_bmask), (bmaskT, k_bmaskT)]
        for n_, (t_, src_) in enumerate(cst):
            em.dma('sp', lambda e, t_=t_, src_=src_: e.dma_start(out=t_[:], in_=src_), w=[('c', n_)])
        CK = [('c', n_) for n_ in range(len(cst))]
        cidx = {id(t_): ('c', n_) for n_, (t_, _) in enumerate(cst)}

        def ck(*ts):
            return [cidx[id(t)] for t in ts]

        small = sb('small', [128, 40 * 32])
        smi = {'i': 0}

        def sm(n=1):
            assert n <= 32
            i = smi['i']
            smi['i'] = (i + 1) % 40
            return small[:, i * 32:i * 32 + n], ('sm', i)

        epsc = sb('epsc', [128, 4])
        ctf_t = sb('ctf_t', [128, 16])
        scb_t = sb('scb_t', [128, 16], BF16)
        em.op('dve', lambda e: e.memset(epsc[:, 0:1], EPS), w=['epsc'])
        em.op('dve', lambda e: e.memset(epsc[:, 1:2], 0.0), w=['epsc'])
        em.op('dve', lambda e: e.memset(epsc[:, 2:3], 1.0), w=['epsc'])

        p1 = ExitStack()
        es.enter_context(p1)
        winb = sb('winb', [128, 8, WIN_RES], BF16, p1)
        poolwb = sb('poolwb', [128, 4, 128], BF16, p1)
        wst = [sb('wst%d' % i, [128, 4096], BF16, p1) for i in range(4)]
        wsi = {'i': 0}
        G2b = sb('G2b', [128, D], F32, p1)
        SH2b = sb('SH2b', [128, D], F32, p1)
        GT1b = sb('GT1b', [128, D], F32, p1)
        modp = sb('modp', [128, 2, 2, 8], F32, p1)
        r2k = Ring(nc, p1, 'r2k', 8, 2048)
        r4k = Ring(nc, p1, 'r4k', 4, 4096)
        nq = Ring(nc, p1, 'nq', 10, 1024)
        xnT = sb('xnT', [128, 8, BLK], BF16, p1)
        qT = sb('qT', [128, 4, BLK], BF16, p1)
        kT = sb('kT', [128, 4, BLK], BF16, p1)
        vT = sb('vT', [128, 4, BLK], BF16, p1)
        szT = sb('szT', [128, 4, BLK], BF16, p1)
        puT = sb('puT', [128, 4, 16 + BLK], F32, p1)
        ybT = sb('ybT', [128, 4, BLK], BF16, p1)
        yaT = sb('yaT', [128, 4, BLK], BF16, p1)
        mixedT = sb('mixedT', [128, 8, BLK], BF16, p1)
        halo = sb('halo', [128, 12, 4], F32, p1)
        gtm = sb('gtm', [128, 4, 8], F32, p1)
        S = sb('S', [128, 4, 128], F32, p1)
        Sb = sb('Sb', [128, 4, 128], BF16, p1)
        cq = {n_: sb('cq_' + n_, [128, 4, 128], BF16, p1) for n_ in ('DT', 'Ds', 'Er', 'kbd', 'kdec', 'vb', 'N0', 'Pt0')}
        xn2T = sb('xn2T', [128, 8, 128], F32, p1)
        lgb = sb('lgb', [128, 4, 36], F32, p1)

        w_in_v = w_in.rearrange("(c p) n -> p c n", p=128)
        for kc in range(8):
            for hf in range(2):
                c0 = hf * (WIN_RES // 2)
                c1 = c0 + WIN_RES // 2
                em.dma('pool', lambda e, kc=kc, c0=c0, c1=c1: e.dma_start(out=winb[:, kc, c0:c1], in_=w_in_v[:, kc, c0:c1]),
                       w=[('winb', kc)])
        WINK = [('winb', kc) for kc in range(8)]
        em.dma('pool', lambda e: e.dma_start(out=poolwb[:], in_=pool_w.rearrange("g c d -> c g d")), w=['poolwb'])

        kctf, kscb = 'ctf', 'scb'
        em.dma('sp', lambda e: e.dma_start(out=ctf_t[:, 0:16], in_=cT.rearrange("p c b -> p (c b)")), w=[kctf])
        em.op('act', lambda e: e.activation(out=scb_t[:, 0:16], in_=ctf_t[:, 0:16], func=AF.Silu), r=[kctf], w=[kscb])
        scb = scb_t[:, 0:16].rearrange("p (c b) -> p c b", b=2)

        def wpiece_load(src_ap_fn, wkey):
            i = wsi['i']
            wsi['i'] = (i + 1) % 4
            buf = wst[i]
            em.dma('pool', lambda e: src_ap_fn(e, buf), w=[('wst', i)])
            return buf, ('wst', i)

        w_ada_v = w_ada.rearrange("(c p) n -> p c n", p=128)
        for nt in range(12):
            buf, kb = wpiece_load(lambda e, buf, nt=nt: e.dma_start(
                out=buf[:].rearrange("p (c n) -> p c n", n=512), in_=w_ada_v[:, :, nt * 512:(nt + 1) * 512]), None)
            bt, kbt = r2k.get()
            em.dma('sp', lambda e, bt=bt, nt=nt: e.dma_start(out=bt[0:2, :], in_=b_ada[0:1, nt * 512:(nt + 1) * 512].partition_broadcast(2)),
                   w=[kbt])
            pf, pb, kp = ps()
            bv = buf[:].rearrange("p (c n) -> p c n", n=512)
            em.mm([lambda e, kc=kc, pf=pf, bv=bv: e.matmul(pf[0:2, :], lhsT=scb[:, kc, :], rhs=bv[:, kc, :], start=(kc == 0), stop=(kc == 7))
                   for kc in range(8)], r=[kscb, kb], w=[kp])
            mr, kmr = r2k.get()
            em.op('dve', lambda e, mr=mr, pf=pf, bt=bt: e.tensor_tensor(out=mr[0:2, :], in0=pf[0:2, :], in1=bt[0:2, :], op=ALU.add),
                  r=[kp, kbt], w=[kmr])
            em.dma('sp', lambda e, mr=mr, nt=nt: e.dma_start(out=modD[:, nt * 512:(nt + 1) * 512], in_=mr[0:2, :]), r=[kmr], w=[('modD', nt)])

        w_la_v = w_la.rearrange("(c p) n -> p c n", p=128)
        w_lb_v = w_lb.rearrange("(c p) n -> p c n", p=128)
        w_out_v = w_out.rearrange("(c p) n -> p c n", p=128)
        piece_src = []
        for i in range(4):
            c0 = WIN_RES + i * 512
            piece_src.append((w_in_v[:, :, c0:c0 + 512], 512))
        piece_src.append((w_la_v, 1024))
        piece_src.append((w_lb_v, 1024))
        piece_src.append((w_out_v[:, :, 0:512], 512))
        piece_src.append((w_out_v[:, :, 512:1024], 512))
        for pi, (src_, n_) in enumerate(piece_src):
            buf, kb = wpiece_load(lambda e, buf, src_=src_, n_=n_: e.dma_start(out=buf[:].rearrange("p (c n) -> p c n", n=n_), in_=src_), None)
            em.dma('sp', lambda e, buf=buf, pi=pi: e.dma_start(out=wsD[pi], in_=buf[:]), r=[kb], w=[('wsD', pi)])

        def wpiece(pi, i):
            buf = wst[i]
            em.dma('sp', lambda e: e.dma_start(out=buf[:], in_=wsD[pi]), r=[('wsD', pi)], w=[('wst', i)])
            return buf, ('wst', i)

        MODK = [('modD', nt) for nt in range(12)]

        def load_seq_mod(seq):
            sh1 = modD[seq, 0:1024].rearrange("(c p) -> p c", p=128)
            sc1 = modD[seq, 1024:2048].rearrange("(c p) -> p c", p=128)
            tmp, kt = sm(8)
            em.dma('sp', lambda e: e.dma_start(out=modp[:, seq, 1, :], in_=sh1, allow_slow_non_contiguous=True), r=MODK, w=[('modp', seq, 1)])
            em.dma('sp', lambda e: e.dma_start(out=tmp, in_=sc1, allow_slow_non_contiguous=True), r=MODK, w=[kt])
            em.op('dve', lambda e: e.scalar_tensor_tensor(out=modp[:, seq, 0, :], in0=tmp, scalar=1.0, in1=n1g[:], op0=ALU.add, op1=ALU.mult),
                  r=[kt] + ck(n1g), w=[('modp', seq, 0)])
            em.dma('sp', lambda e: e.dma_start(out=GT1b[:], in_=modD[seq:seq + 1, 2048:3072].partition_broadcast(128)), r=MODK, w=['GT1b'])
            em.dma('sp', lambda e: e.dma_start(out=SH2b[:], in_=modD[seq:seq + 1, 3072:4096].partition_broadcast(128)), r=MODK, w=['SH2b'])
            t4, k4 = r4k.get()
            em.dma('sp', lambda e: e.dma_start(out=t4, in_=modD[seq:seq + 1, 4096:5120].partition_broadcast(128)), r=MODK, w=[k4])
            n2, kn2 = r4k.get()
            em.dma('sp', lambda e: e.dma_start(out=n2, in_=p_n2g), w=[kn2])
            em.op('dve', lambda e: e.scalar_tensor_tensor(out=G2b[:], in0=t4, scalar=1.0, in1=n2, op0=ALU.add, op1=ALU.mult),
                  r=[k4, kn2], w=['G2b'])

        def rstd_from_ss(ss_ap, kss, scale):
            l1, kl1 = sm(1)
            em.op('act', lambda e: e.activation(out=l1, in_=ss_ap, func=AF.Ln, bias=epsc[:, 0:1], scale=scale), r=[kss, 'epsc'], w=[kl1])
            r1, kr1 = sm(1)
            em.op('act', lambda e: e.activation(out=r1, in_=l1, func=AF.Exp, scale=-0.5), r=[kl1], w=[kr1])
            return r1, kr1


        def proj_fm(col0, ncols=128):
            pf, pb, kp = ps()
            em.mm([lambda e, kc=kc, pf=pf: e.matmul(pf[0:ncols, :], lhsT=winb[:, kc, col0:col0 + ncols], rhs=xnT[:, kc, :],
                                                     start=(kc == 0), stop=(kc == 7)) for kc in range(8)],
                  r=WINK + ['xnT'], w=[kp])
            return pf, kp

        dbg_state = {'off': 0}

        def dump(ap_f32_128xN, keys, n):
            if dbg_t is None:
                return
            o = dbg_state['off']
            dbg_state['off'] = o + n
            em.dma('sp', lambda e: e.dma_start(out=dbg_t[:, o:o + n], in_=ap_f32_128xN), r=keys, w=[('dbg', o)])

        def dump_any(ap, keys, n):
            if dbg_t is None:
                return
            t, kt = r2k.get()
            em.op('dve', lambda e: e.tensor_copy(out=t[:, 0:n], in_=ap), r=keys, w=[kt])
            dump(t[:, 0:n], [kt], n)

        try:
            for b in range(NBLK):
                seq, blk = divmod(b, NBLK // 2)
                t0 = b * BLK
                first = (blk == 0)
                if first:
                    load_seq_mod(seq)
                    em.op('pool', lambda e: e.memset(S[:], 0.0), w=['S'])
                    em.op('pool', lambda e: e.memset(Sb[:], 0.0), w=['Sb'])
                    em.op('pool', lambda e: e.memset(halo[:], 0.0), w=[('halo', ct_) for ct_ in range(12)])
                    em.op('pool', lambda e: e.memset(puT[:, :, 0:16], 0.0), w=[('puT', g_) for g_ in range(4)])
                for j in range(4):
                    xin, kx = r4k.get()
                    em.dma('sp', lambda e, xin=xin, j=j: e.dma_start(out=xin, in_=x[t0 + j * 128:t0 + (j + 1) * 128, :]), w=[kx])
                    junk, kj = r2k.get(BF16)
                    ss, kss = sm(1)
                    em.op('act', lambda e, xin=xin, junk=junk, ss=ss: e.activation(out=junk, in_=xin, func=AF.Square, accum_out=ss), r=[kx], w=[kj, kss])
                    rs, krs = rstd_from_ss(ss, kss, 1.0 / D)
                    xsb, kxs = r2k.get(BF16)
                    em.op('dve', lambda e, xsb=xsb, xin=xin, rs=rs: e.tensor_scalar(out=xsb, in0=xin, scalar1=rs, scalar2=None, op0=ALU.mult),
                          r=[kx, krs], w=[kxs])
                    pf, pb, kp = ps()
                    em.mm([lambda e, kc=kc, pb=pb, xsb=xsb: e.transpose(pb[:, kc * 128:(kc + 1) * 128], xsb[:, kc * 128:(kc + 1) * 128], identb[:])
                           for kc in range(8)], r=[kxs] + ck(identb), w=[kp])
                    for kc in range(8):
                        em.op('act', lambda e, kc=kc, pb=pb, j=j: e.activation(
                            out=xnT[:, kc, j * 128:(j + 1) * 128], in_=pb[:, kc * 128:(kc + 1) * 128], func=AF.Identity,
                            scale=modp[:, seq, 0, kc:kc + 1], bias=modp[:, seq, 1, kc:kc + 1]),
                            r=[kp, ('modp', seq, 0), ('modp', seq, 1)], w=['xnT'])
                if dbg_t is not None and b == 0 and 'xnT' in DBGSEL:
                    for kc in range(8):
                        dump_any(xnT[:, kc, :], ['xnT'], 512)

                if b == 0:
                    chk('A')
                def st_C(ct):
                    pf, kp = proj_fm(ct * 128)
                    pre, kpre = r4k.get()
                    em.op('pool', lambda e: e.tensor_copy(out=pre[:, 0:4], in_=halo[:, ct, :]), r=[('halo', ct)], w=[kpre])
                    em.op('act', lambda e: e.activation(out=pre[:, 4:516], in_=pf, func=AF.Copy), r=[kp], w=[kpre])
                    em.op('pool', lambda e: e.tensor_copy(out=halo[:, ct, :], in_=pre[:, 512:516]), r=[kpre], w=[('halo', ct)])
                    acc, kacc = r2k.get()
                    em.op('act', lambda e: e.activation(out=acc, in_=pf, func=AF.Copy, scale=convw[:, ct, 3:4]), r=[kp] + ck(convw), w=[kacc])
                    return dict(ct=ct, pre=pre, kpre=kpre, acc=acc, kacc=kacc)

                def st_M(st):
                    ct, pre, kpre, acc, kacc = st['ct'], st['pre'], st['kpre'], st['acc'], st['kacc']
                    for tap in (2, 1, 0):
                        sh = 3 - tap
                        em.op('dve', lambda e, tap=tap, sh=sh: e.scalar_tensor_tensor(
                            out=acc, in0=pre[:, 4 - sh:516 - sh], scalar=convw[:, ct, tap:tap + 1], in1=acc, op0=ALU.mult, op1=ALU.add),
                            r=[kpre, kacc] + ck(convw), w=[kacc])

                def st_S(st):
                    ct, acc, kacc = st['ct'], st['acc'], st['kacc']
                    h = ct % 4
                    if ct >= 8:
                        em.op('act', lambda e: e.activation(out=vT[:, h, :], in_=acc, func=AF.Silu), r=[kacc], w=['vT'])
                        return
                    em.op('act', lambda e: e.activation(out=acc, in_=acc, func=AF.Silu), r=[kacc], w=[kacc])
                    i_ = r2k.i
                    sqb, ksq = r2k.get(BF16)
                    st['sqb'], st['ksq'], st['sqf'] = sqb, ksq, r2k.t[i_][:]
                    em.op('act', lambda e: e.activation(out=sqb[:, 0:512], in_=acc, func=AF.Square), r=[kacc], w=[ksq])

                def st_N(st):
                    if st['ct'] >= 8:
                        return
                    sqb, ksq = st['sqb'], st['ksq']
                    pf2, pb2, kp2 = ps()
                    st['pf2'], st['kp2'] = pf2, kp2
                    em.mm([lambda e: e.matmul(pf2, lhsT=onesb[:], rhs=sqb[:, 0:512], start=True, stop=True)], r=[ksq] + ck(onesb), w=[kp2])

                def st_L(st):
                    if st['ct'] >= 8:
                        return
                    lnv, ksq, pf2, kp2 = st['sqf'], st['ksq'], st['pf2'], st['kp2']
                    em.op('act', lambda e: e.activation(out=lnv, in_=pf2, func=AF.Ln, bias=epsc[:, 0:1]), r=[kp2, 'epsc'], w=[ksq])
                    em.op('act', lambda e: e.activation(out=lnv, in_=lnv, func=AF.Exp, scale=-0.5), r=[ksq], w=[ksq])

                def st_F(st):
                    ct = st['ct']
                    if ct >= 8:
                        return
                    h = ct % 4
                    acc, kacc, rinv, ksq = st['acc'], st['kacc'], st['sqf'], st['ksq']
                    qs = (128.0 ** -0.5) if ct < 4 else 1.0
                    dst = qT if ct < 4 else kT
                    dk = 'qT' if ct < 4 else 'kT'
                    em.op('dve', lambda e: e.scalar_tensor_tensor(out=dst[:, h, :], in0=acc, scalar=qs, in1=rinv, op0=ALU.mult, op1=ALU.mult),
                          r=[kacc, ksq], w=[dk])

                prev_pair = None
                for pr in ((0, 1), (2, 3), (4, 5), (6, 7), (8, 9), (10, 11)):
                    cur_pair = [st_C(ct) for ct in pr]
                    for st in cur_pair:
                        st_M(st)
                    if prev_pair is not None:
                        for fn_ in (st_S, st_N, st_L, st_F):
                            for st in prev_pair:
                                fn_(st)
                    prev_pair = cur_pair
                for fn_ in (st_S, st_N, st_L, st_F):
                    for st in prev_pair:
                        fn_(st)
                if b == 0 and dbg_t is not None and 'B' in DBGSEL:
                    for h_ in range(4):
                        dump_any(qT[:, h_, :], ['qT'], 512)
                    for h_ in range(4):
                        dump_any(kT[:, h_, :], ['kT'], 512)
                    for h_ in range(4):
                        dump_any(vT[:, h_, :], ['vT'], 512)
                if b == 0:
                    chk('B')
                for h in range(4):
                    pf, kp = proj_fm(1536 + h * 128)
                    em.op('act', lambda e, pf=pf, h=h: e.activation(out=szT[:, h, :], in_=pf, func=AF.Silu), r=[kp], w=['szT'])
                for c in range(4):
                    pf, pb, kp = ps()
                    em.mm([lambda e, kc=kc, pf=pf, c=c: e.matmul(pf[:, 0:8], lhsT=xnT[:, kc, c * 128:(c + 1) * 128], rhs=winb[:, kc, 2048:2056],
                                                                  start=(kc == 0), stop=(kc == 7)) for kc in range(8)],
                          r=WINK + ['xnT'], w=[kp])
                    em.op('act', lambda e, pf=pf, c=c: e.activation(out=gtm[:, c, 4:8], in_=pf[:, 4:8], func=AF.Sigmoid), r=[kp], w=[('gtm', c)])
                    xa, kxa = sm(4)
                    em.op('dve', lambda e, xa=xa, pf=pf: e.tensor_tensor(out=xa, in0=pf[:, 0:4], in1=dtb[:], op=ALU.add), r=[kp] + ck(dtb), w=[kxa])
                    ab, kab = sm(4)
                    em.op('act', lambda e, ab=ab, xa=xa: e.activation(out=ab, in_=xa, func=AF.Abs), r=[kxa], w=[kab])
                    ex, kex = sm(4)
                    em.op('act', lambda e, ex=ex, ab=ab: e.activation(out=ex, in_=ab, func=AF.Exp, scale=-1.0), r=[kab], w=[kex])
                    l1p, kl1p = sm(4)
                    em.op('act', lambda e, l1p=l1p, ex=ex: e.activation(out=l1p, in_=ex, func=AF.Ln, bias=epsc[:, 2:3]), r=[kex, 'epsc'], w=[kl1p])
                    sp_, ksp = sm(4)
                    em.op('dve', lambda e, sp_=sp_, xa=xa, l1p=l1p: e.scalar_tensor_tensor(out=sp_, in0=xa, scalar=0.0, in1=l1p, op0=ALU.max, op1=ALU.add),
                          r=[kxa, kl1p], w=[ksp])
                    ea, kea = sm(4)
                    em.op('act', lambda e, ea=ea: e.activation(out=ea, in_=alog[:], func=AF.Exp), r=ck(alog), w=[kea])
                    em.op('dve', lambda e, c=c, sp_=sp_, ea=ea: e.scalar_tensor_tensor(out=gtm[:, c, 0:4], in0=sp_, scalar=-1.0, in1=ea, op0=ALU.mult, op1=ALU.mult),
                          r=[ksp, kea], w=[('gtm', c)])

                if b == 0 and dbg_t is not None and 'B2' in DBGSEL:
                    dump(gtm[:].rearrange('p a b -> p (a b)'), [('gtm', c_) for c_ in range(4)], 32)
                if b == 0:
                    chk('B2')
                def pc_H(gi, win):
                    pf, kp = proj_fm(2056 + gi * 128)
                    em.op('act', lambda e: e.activation(out=puT[:, gi, 16:16 + BLK], in_=pf, func=AF.Copy), r=[kp], w=[('puT', gi)])
                    cur = puT[:, gi, :]
                    kcur = ('puT', gi)
                    slots = [r4k.get()]
                    if win > 2:
                        slots.append(r4k.get())
                    w_ = 1
                    k_ = 0
                    while w_ < win:
                        nxt, knxt = slots[k_ % len(slots)]
                        em.op('pool', lambda e, nxt=nxt, cur=cur, w_=w_: e.tensor_tensor(out=nxt[:, w_:16 + BLK], in0=cur[:, w_:16 + BLK], in1=cur[:, 0:16 + BLK - w_], op=ALU.add),
                              r=[kcur], w=[knxt])
                        cur, kcur = nxt, knxt
                        w_ *= 2
                        k_ += 1
                    return dict(gi=gi, win=win, cur=cur, kcur=kcur)

                def pc_T(st):
                    gi, win, cur, kcur = st['gi'], st['win'], st['cur'], st['kcur']
                    plb, kplb = r2k.get(BF16)
                    em.op('dve', lambda e: e.scalar_tensor_tensor(
                        out=plb[:, 0:BLK], in0=cur[:, 16:16 + BLK], scalar=1.0 / win, in1=puT[:, gi, 16:16 + BLK], op0=ALU.mult, op1=ALU.subtract),
                        r=[kcur, ('puT', gi)], w=[kplb])
                    if first:
                        t15, k15 = sm(16)
                        em.op('dve', lambda e: e.tensor_tensor(out=t15, in0=cur[:, 16:32], in1=pcorr[:, gi, :], op=ALU.mult), r=[kcur] + ck(pcorr), w=[k15])
                        em.op('dve', lambda e: e.tensor_tensor(out=plb[:, 0:16], in0=t15, in1=puT[:, gi, 16:32], op=ALU.subtract), r=[k15, ('puT', gi)], w=[kplb])
                    pf2, pb2, kp2 = ps()
                    em.mm([lambda e: e.matmul(pf2, lhsT=poolwb[:, gi, :], rhs=plb[:, 0:BLK], start=True, stop=True)], r=[kplb, 'poolwb'], w=[kp2])
                    em.op('act', lambda e: e.activation(out=ybT[:, gi, :], in_=pf2, func=AF.Copy, scale=pscale[:, gi:gi + 1]), r=[kp2] + ck(pscale), w=['ybT'])
                    em.op('pool', lambda e: e.tensor_copy(out=puT[:, gi, 0:16], in_=puT[:, gi, BLK:BLK + 16]), r=[('puT', gi), kplb], w=[('puT', gi)])

                wins = (2, 4, 8, 16)
                pcs = [pc_H(0, wins[0]), pc_H(1, wins[1])]
                pc_T(pcs[0])
                pcs.append(pc_H(2, wins[2]))
                pc_T(pcs[1])
                pcs.append(pc_H(3, wins[3]))
                pc_T(pcs[2])
                pc_T(pcs[3])

                if b == 0:
                    chk('C')
                for c in range(4):
                    tsl = slice(c * 128, (c + 1) * 128)
                    g4 = gtm[:, c, 0:4]
                    b4 = gtm[:, c, 4:8]
                    kg = ('gtm', c)
                    pkf, pkb, kpk = ps()
                    em.mm([lambda e, h=h, pkb=pkb: e.transpose(pkb[:, h * 128:(h + 1) * 128], kT[:, h, tsl], identb[:]) for h in range(4)],
                          r=['kT'] + ck(identb), w=[kpk])
                    pvf, pvb, kpv = ps()
                    em.mm([lambda e, h=h, pvb=pvb: e.transpose(pvb[:, h * 128:(h + 1) * 128], vT[:, h, tsl], identb[:]) for h in range(4)],
                          r=['vT'] + ck(identb), w=[kpv])
                    Gt, kGt = r2k.get()
                    for h in range(4):
                        em.op('pool', lambda e, h=h, Gt=Gt: e.tensor_scalar(out=Gt[:, h * 128:(h + 1) * 128], in0=triU[:], scalar1=g4[:, h:h + 1], scalar2=1.0,
                                                                            op0=ALU.mult, op1=ALU.mult), r=[kg] + ck(triU), w=[kGt])
                    pgr, _, kpgr = ps()
                    em.mm([lambda e, pgr=pgr, Gt=Gt: e.matmul(pgr, lhsT=onesf[:], rhs=Gt, start=True, stop=True)], r=[kGt] + ck(onesf), w=[kpgr])
                    pgc, _, kpgc = ps()
                    em.mm([lambda e, pgc=pgc: e.matmul(pgc[:, 0:4], lhsT=triU[:], rhs=g4, start=True, stop=True)], r=[kg] + ck(triU), w=[kpgc])
                    pgl, _, kpgl = ps()
                    em.mm([lambda e, pgl=pgl: e.matmul(pgl[:, 0:4], lhsT=onesf[:], rhs=g4, start=True, stop=True)], r=[kg] + ck(onesf), w=[kpgl])
                    gcc8, kgcc = sm(8)
                    em.op('act', lambda e, gcc8=gcc8, pgc=pgc: e.activation(out=gcc8[:, 0:4], in_=pgc[:, 0:4], func=AF.Copy), r=[kpgc], w=[kgcc])
                    em.op('act', lambda e, gcc8=gcc8, pgl=pgl: e.activation(out=gcc8[:, 4:8], in_=pgl[:, 0:4], func=AF.Copy), r=[kpgl, kgcc], w=[kgcc])
                    gcc = gcc8[:, 0:4]
                    glv = gcc8[:, 4:8]
                    DTl, kDTl = r2k.get()
                    Dsl, kDsl = r2k.get()
                    for h in range(4):
                        hs = slice(h * 128, (h + 1) * 128)
                        em.op('dve', lambda e, hs=hs, h=h, DTl=DTl, pgr=pgr, gcc=gcc: e.scalar_tensor_tensor(
                            out=DTl[:, hs], in0=pgr[:, hs], scalar=gcc[:, h:h + 1], in1=maskT[:], op0=ALU.subtract, op1=ALU.add),
                            r=[kpgr, kgcc] + ck(maskT), w=[kDTl])
                        em.op('dve', lambda e, hs=hs, h=h, Dsl=Dsl, pgr=pgr, gcc=gcc: e.scalar_tensor_tensor(
                            out=Dsl[:, hs], in0=pgr[:, hs], scalar=gcc[:, h:h + 1], in1=maskS[:], op0=ALU.subtract, op1=ALU.add),
                            r=[kpgr, kgcc] + ck(maskS), w=[kDsl])
                    DT, Ds, Er = cq['DT'], cq['Ds'], cq['Er']
                    fl = lambda t: t[:].rearrange("p h n -> p (h n)")
                    em.op('act', lambda e: e.activation(out=fl(DT), in_=DTl, func=AF.Exp), r=[kDTl], w=['DT'])
                    em.op('act', lambda e: e.activation(out=fl(Ds), in_=Dsl, func=AF.Exp, scale=-1.0), r=[kDsl], w=['Ds'])
                    em.op('act', lambda e, pgr=pgr: e.activation(out=fl(Er), in_=pgr, func=AF.Exp), r=[kpgr], w=['Er'])
                    if b == 0 and c == 0:
                        chk('D1')
                    egc, kegc = sm(4)
                    em.op('act', lambda e, egc=egc, gcc=gcc: e.activation(out=egc, in_=gcc, func=AF.Exp), r=[kgcc], w=[kegc])
                    kbs, kkbs = sm(4)
                    em.op('dve', lambda e, kbs=kbs, egc=egc: e.tensor_tensor(out=kbs, in0=egc, in1=b4, op=ALU.mult), r=[kegc, kg], w=[kkbs])
                    dl, kdl = sm(4)
                    em.op('dve', lambda e, dl=dl, glv=glv, gcc=gcc: e.tensor_tensor(out=dl, in0=glv, in1=gcc, op=ALU.subtract), r=[kgcc], w=[kdl])
                    ekd, kekd = sm(4)
                    em.op('act', lambda e, ekd=ekd, dl=dl: e.activation(out=ekd, in_=dl, func=AF.Exp), r=[kdl], w=[kekd])
                    egl, kegl = sm(4)
                    em.op('act', lambda e, egl=egl, glv=glv: e.activation(out=egl, in_=glv, func=AF.Exp), r=[kgcc], w=[kegl])
                    nb4, knb4 = sm(4)
                    em.op('dve', lambda e, nb4=nb4: e.tensor_scalar(out=nb4, in0=b4, scalar1=-1.0, scalar2=None, op0=ALU.mult), r=[kg], w=[knb4])
                    if b == 0 and c == 0:
                        chk('D1b')
                    kbd, kdec, vb = cq['kbd'], cq['kdec'], cq['vb']
                    for h in range(4):
                        hs = slice(h * 128, (h + 1) * 128)
                        em.op('act', lambda e, h=h, hs=hs, pkb=pkb, kbs=kbs: e.activation(out=kbd[:, h, :], in_=pkb[:, hs], func=AF.Identity, scale=kbs[:, h:h + 1], bias=epsc[:, 1:2]),
                              r=[kpk, kkbs], w=['kbd'])
                        em.op('act', lambda e, h=h, hs=hs, pkb=pkb, ekd=ekd: e.activation(out=kdec[:, h, :], in_=pkb[:, hs], func=AF.Identity, scale=ekd[:, h:h + 1], bias=epsc[:, 1:2]),
                              r=[kpk, kekd], w=['kdec'])
                        em.op('act', lambda e, h=h, hs=hs, pvb=pvb: e.activation(out=vb[:, h, :], in_=pvb[:, hs], func=AF.Identity, scale=b4[:, h:h + 1], bias=epsc[:, 1:2]),
                              r=[kpv, kg], w=['vb'])
                    if b == 0 and c == 0:
                        chk('D2')
                    pkk, _, kpkk = ps()
                    em.mm([lambda e, h=h, pkk=pkk: e.matmul(pkk[:, h * 128:(h + 1) * 128], lhsT=kT[:, h, tsl], rhs=kT[:, h, tsl], start=True, stop=True) for h in range(4)],
                          r=['kT'], w=[kpkk])
                    N0 = cq['N0']
                    for h in range(4):
                        hs = slice(h * 128, (h + 1) * 128)
                        em.op('dve', lambda e, h=h, hs=hs, pkk=pkk, nb4=nb4: e.scalar_tensor_tensor(
                            out=N0[:, h, :], in0=pkk[:, hs], scalar=nb4[:, h:h + 1], in1=Ds[:, h, :], op0=ALU.mult, op1=ALU.mult),
                            r=[kpkk, knb4, 'Ds'], w=['N0'])
                    ptf, ptb, kpt = ps()
                    em.mm([lambda e, h=h, ptb=ptb: e.transpose(ptb[:, h * 128:(h + 1) * 128], N0[:, h, :], identb[:]) for h in range(4)],
                          r=['N0'] + ck(identb), w=[kpt])
                    Pt0 = cq['Pt0']
                    em.op('act', lambda e, ptb=ptb: e.activation(out=fl(Pt0), in_=ptb[:, 0:512], func=AF.Identity, scale=1.0, bias=epsc[:, 1:2]), r=[kpt, 'epsc'], w=['Pt0'])
                    Tt, kTt = nq.get(BF16)
                    if b == 0 and c == 0 and dbg_t is not None and 'D3' in DBGSEL:
                        dump_any(fl(DT), ['DT'], 512)
                        dump_any(fl(Ds), ['Ds'], 512)
                        dump_any(fl(N0), ['N0'], 512)
                    if b == 0 and c == 0:
                        chk('D3')
                    Tq, kTq = nq.get(BF16)
                    Cm, kCm = nq.get(BF16)
                    for h in range(4):
                        hs = slice(h * 128, (h + 1) * 128)
                        em.op('dve', lambda e, h=h, hs=hs, Cm=Cm: e.tensor_tensor(out=Cm[:, hs], in0=N0[:, h, :], in1=bmask[:, 0, :], op=ALU.mult), r=['N0'] + ck(bmask), w=[kCm])
                    em.op('dve', lambda e, Tq=Tq, Cm=Cm: e.tensor_tensor(out=Tq, in0=Cm, in1=identq[:], op=ALU.add), r=[kCm] + ck(identq), w=[kTq])
                    Cmt, kCmt = nq.get(BF16)
                    for h in range(4):
                        hs = slice(h * 128, (h + 1) * 128)
                        em.op('dve', lambda e, h=h, hs=hs, Cmt=Cmt: e.tensor_tensor(out=Cmt[:, hs], in0=Pt0[:, h, :], in1=bmaskT[:, 0, :], op=ALU.mult), r=['Pt0'] + ck(bmaskT), w=[kCmt])
                    em.op('dve', lambda e, Tt=Tt, Cmt=Cmt: e.tensor_tensor(out=Tt, in0=Cmt, in1=identq[:], op=ALU.add), r=[kCmt, kTt] + ck(identq), w=[kTt])
                    if b == 0 and c == 0 and dbg_t is not None and 'D3b' in DBGSEL:
                        dump_any(Cm, [kCm], 512)
                        dump_any(Tq, [kTq], 512)
                        dump_any(Tt, [kTt], 512)
                        dump_any(bmask[:, :, :].rearrange('p a b -> p (a b)')[:, 0:512], ck(bmask), 512)
                    if b == 0 and c == 0:
                        chk('D3b')
                    def masks(lev_):
                        Cm_, kCm_ = nq.get(BF16)
                        Cmt_, kCmt_ = nq.get(BF16)
                        for h in range(4):
                            hs = slice(h * 128, (h + 1) * 128)
                            em.op('dve', lambda e, h=h, hs=hs: e.tensor_tensor(out=Cm_[:, hs], in0=N0[:, h, :], in1=bmask[:, lev_, :], op=ALU.mult), r=['N0'] + ck(bmask), w=[kCm_])
                            if lev_ < 6:
                                em.op('dve', lambda e, h=h, hs=hs: e.tensor_tensor(out=Cmt_[:, hs], in0=Pt0[:, h, :], in1=bmaskT[:, lev_, :], op=ALU.mult), r=['Pt0'] + ck(bmaskT), w=[kCmt_])
                        return Cm_, kCm_, Cmt_, kCmt_

                    nxt_masks = masks(1)
                    for lev in range(1, 7):
                        Cm, kCm, Cmt, kCmt = nxt_masks
                        last = (lev == 6)
                        px2, _, kpx2 = ps()
                        em.mm([lambda e, h=h, px2=px2, Cm=Cm, Tt=Tt: e.matmul(px2[:, h * 128:(h + 1) * 128], lhsT=Cm[:, h * 128:(h + 1) * 128], rhs=Tt[:, h * 128:(h + 1) * 128], start=True, stop=True)
                               for h in range(4)], r=[kCm, kTt], w=[kpx2])
                        if lev < 6:
                            nxt_masks = masks(lev + 1)
                        Xs2, kXs2 = nq.get(BF16)
                        em.op('act', lambda e, Xs2=Xs2, px2=px2: e.activation(out=Xs2, in_=px2, func=AF.Copy), r=[kpx2], w=[kXs2])
                        if not last:
                            px1, _, kpx1 = ps()
                            em.mm([lambda e, h=h, px1=px1, Cmt=Cmt, Tq=Tq: e.matmul(px1[:, h * 128:(h + 1) * 128], lhsT=Cmt[:, h * 128:(h + 1) * 128], rhs=Tq[:, h * 128:(h + 1) * 128], start=True, stop=True)
                                   for h in range(4)], r=[kCmt, kTq], w=[kpx1])
                            Xs1, kXs1 = nq.get(BF16)
                            em.op('act', lambda e, Xs1=Xs1, px1=px1: e.activation(out=Xs1, in_=px1, func=AF.Copy), r=[kpx1], w=[kXs1])
                        py2, _, kpy2 = ps()
                        em.mm([lambda e, h=h, py2=py2, Tq=Tq, Xs2=Xs2: e.matmul(py2[:, h * 128:(h + 1) * 128], lhsT=Tq[:, h * 128:(h + 1) * 128], rhs=Xs2[:, h * 128:(h + 1) * 128], start=True, stop=True)
                               for h in range(4)], r=[kTq, kXs2], w=[kpy2])
                        if not last:
                            py1, _, kpy1 = ps()
                            em.mm([lambda e, h=h, py1=py1, Tt=Tt, Xs1=Xs1: e.matmul(py1[:, h * 128:(h + 1) * 128], lhsT=Tt[:, h * 128:(h + 1) * 128], rhs=Xs1[:, h * 128:(h + 1) * 128], start=True, stop=True)
                                   for h in range(4)], r=[kTt, kXs1], w=[kpy1])
                        Ttn, kTtn = nq.get(BF16)
                        em.op('dve', lambda e, Ttn=Ttn, py2=py2, Tt=Tt: e.tensor_tensor(out=Ttn, in0=py2, in1=Tt, op=ALU.add), r=[kpy2, kTt], w=[kTtn])
                        if not last:
                            Tqn, kTqn = nq.get(BF16)
                            em.op('dve', lambda e, Tqn=Tqn, py1=py1, Tq=Tq: e.tensor_tensor(out=Tqn, in0=py1, in1=Tq, op=ALU.add), r=[kpy1, kTq], w=[kTqn])
                            Tq, kTq = Tqn, kTqn
                        Tt, kTt = Ttn, kTtn
                    if b == 0 and c == 0:
                        chk('D4')
                    pw, _, kpw = ps()
                    em.mm([lambda e, h=h, pw=pw, Tt=Tt: e.matmul(pw[:, h * 128:(h + 1) * 128], lhsT=kbd[:, h, :], rhs=Tt[:, h * 128:(h + 1) * 128], start=True, stop=True)
                           for h in range(4)], r=['kbd', kTt], w=[kpw])
                    nwT, knwT = r2k.get(BF16)
                    em.op('act', lambda e, nwT=nwT, pw=pw: e.activation(out=nwT[:, 0:512], in_=pw, func=AF.Copy, scale=-1.0), r=[kpw], w=[knwT])
                    pvn, _, kpvn = ps()
                    fns = []
                    for h in range(4):
                        hs = slice(h * 128, (h + 1) * 128)
                        fns.append(lambda e, h=h, hs=hs, pvn=pvn, Tt=Tt: e.matmul(pvn[:, hs], lhsT=Tt[:, hs], rhs=vb[:, h, :], start=True, stop=False))
                        fns.append(lambda e, h=h, hs=hs, pvn=pvn, nwT=nwT: e.matmul(pvn[:, hs], lhsT=nwT[:, hs], rhs=Sb[:, h, :], start=False, stop=True))
                    em.mm(fns, r=[kTt, 'vb', knwT, 'Sb'], w=[kpvn])
                    vnew, kvnew = r2k.get(BF16)
                    em.op('act', lambda e, vnew=vnew, pvn=pvn: e.activation(out=vnew[:, 0:512], in_=pvn, func=AF.Copy), r=[kpvn], w=[kvnew])
                    if b == 0 and c == 0:
                        chk('D5')
                    pqk, _, kpqk = ps()
                    em.mm([lambda e, h=h, pqk=pqk: e.matmul(pqk[:, h * 128:(h + 1) * 128], lhsT=kT[:, h, tsl], rhs=qT[:, h, tsl], start=True, stop=True) for h in range(4)],
                          r=['kT', 'qT'], w=[kpqk])
                    attnT, kat = r2k.get(BF16)
                    em.op('dve', lambda e, attnT=attnT, pqk=pqk: e.tensor_tensor(out=attnT[:, 0:512], in0=pqk, in1=fl(DT), op=ALU.mult), r=[kpqk, 'DT'], w=[kat])
                    qdT, kqd = r2k.get(BF16)
                    em.op('dve', lambda e, qdT=qdT: e.tensor_tensor(out=qdT[:, 0:512].rearrange("p (h n) -> p h n", n=128), in0=qT[:, :, tsl], in1=Er[:], op=ALU.mult),
                          r=['qT', 'Er'], w=[kqd])
                    po, _, kpo = ps()
                    fns = []
                    for h in range(4):
                        hs = slice(h * 128, (h + 1) * 128)
                        fns.append(lambda e, h=h, hs=hs, po=po, qdT=qdT: e.matmul(po[:, hs], lhsT=Sb[:, h, :], rhs=qdT[:, hs], start=True, stop=False))
                        fns.append(lambda e, h=h, hs=hs, po=po, vnew=vnew, attnT=attnT: e.matmul(po[:, hs], lhsT=vnew[:, hs], rhs=attnT[:, hs], start=False, stop=True))
                    em.mm(fns, r=['Sb', kqd, kvnew, kat], w=[kpo])
                    osq, kosq = r2k.get(BF16)
                    em.op('act', lambda e, osq=osq, po=po: e.activation(out=osq[:, 0:512], in_=po, func=AF.Square), r=[kpo], w=[kosq])
                    pss, _, kpss = ps()
                    em.mm([lambda e, pss=pss, osq=osq: e.matmul(pss, lhsT=onesb[:], rhs=osq[:, 0:512], start=True, stop=True)], r=[kosq] + ck(onesb), w=[kpss])
                    lno, klno = r2k.get()
                    em.op('act', lambda e, lno=lno, pss=pss: e.activation(out=lno, in_=pss, func=AF.Ln, bias=epsc[:, 0:1], scale=1.0 / 128), r=[kpss, 'epsc'], w=[klno])
                    rso, krso = r2k.get()
                    em.op('act', lambda e, rso=rso, lno=lno: e.activation(out=rso, in_=lno, func=AF.Exp, scale=-0.5), r=[klno], w=[krso])
                    t1, kt1 = r2k.get()
                    em.op('dve', lambda e, t1=t1, po=po, rso=rso: e.tensor_tensor(out=t1, in0=po, in1=rso, op=ALU.mult), r=[kpo, krso], w=[kt1])
                    em.op('dve', lambda e, t1=t1: e.scalar_tensor_tensor(out=yaT[:, :, tsl], in0=t1.rearrange("p (h n) -> p h n", n=128), scalar=dng[:, 0:1],
                                                                          in1=szT[:, :, tsl], op0=ALU.mult, op1=ALU.mult),
                          r=[kt1, 'szT'] + ck(dng), w=['yaT'])
                    if b == 0 and c == 0 and dbg_t is not None and 'D6' in DBGSEL:
                        dump_any(Tt, [kTt], 512)
                        dump_any(vnew[:, 0:512], [kvnew], 512)
                        dump_any(attnT[:, 0:512], [kat], 512)
                        dump_any(po, [kpo], 512)
                        dump_any(rso, [krso], 512)
                    if b == 0 and c == 0:
                        chk('D6')
                    pst, _, kpst = ps()
                    em.mm([lambda e, h=h, pst=pst, vnew=vnew: e.matmul(pst[:, h * 128:(h + 1) * 128], lhsT=kdec[:, h, :], rhs=vnew[:, h * 128:(h + 1) * 128], start=True, stop=True)
                           for h in range(4)], r=['kdec', kvnew], w=[kpst])
                    for h in range(4):
                        hs = slice(h * 128, (h + 1) * 128)
                        em.op('dve', lambda e, h=h, hs=hs, pst=pst, egl=egl: e.scalar_tensor_tensor(
                            out=S[:, h, :], in0=S[:, h, :], scalar=egl[:, h:h + 1], in1=pst[:, hs], op0=ALU.mult, op1=ALU.add),
                            r=['S', kegl, kpst, kpo, kpvn], w=['S'])
                    em.op('act', lambda e: e.activation(out=fl(Sb), in_=fl(S), func=AF.Copy), r=['S', kpo, kpvn], w=['Sb'])
                    if b == 0 and c == 0 and dbg_t is not None and 'S1' in DBGSEL:
                        dump(fl(S), ['S'], 512)
                        dump_any(fl(Sb), ['Sb'], 512)
                        dump_any(fl(kdec), ['kdec'], 512)
                    if b == 0 and c == 0:
                        chk('S1')
                if dbg_t is not None and b == 0 and 'sz' in DBGSEL:
                    for h in range(4):
                        dump_any(szT[:, h, :], ['szT'], 512)
                if dbg_t is not None and b == 0 and 'ya' in DBGSEL:
                    for h in range(4):
                        dump_any(yaT[:, h, :], ['yaT'], 512)
                    for h in range(4):
                        dump_any(ybT[:, h, :], ['ybT'], 512)

                if b == 0:
                    chk('D')
                bufs = {}
                bufs[4] = wpiece(4, 0)
                bufs[5] = wpiece(5, 1)
                bufs[0] = wpiece(0, 2)
                bufs[2] = wpiece(2, 3)
                la_v = bufs[4][0][:].rearrange("p (c n) -> p c n", n=1024)
                lb_v = bufs[5][0][:].rearrange("p (c n) -> p c n", n=1024)
                for mt in range(8):
                    if mt == 4:
                        bufs[1] = wpiece(1, 2)
                        bufs[3] = wpiece(3, 3)
                    gbuf_a, kga = bufs[mt // 4]
                    gbuf_b, kgb = bufs[2 + mt // 4]
                    ga_v = gbuf_a[:].rearrange("p (c n) -> p c n", n=512)
                    gb_v = gbuf_b[:].rearrange("p (c n) -> p c n", n=512)
                    cs = slice((mt % 4) * 128, (mt % 4 + 1) * 128)
                    ms = slice(mt * 128, (mt + 1) * 128)
                    pga, _, kpga = ps()
                    em.mm([lambda e, kc=kc, pga=pga, ga_v=ga_v, cs=cs: e.matmul(pga, lhsT=ga_v[:, kc, cs], rhs=xnT[:, kc, :], start=(kc == 0), stop=(kc == 7)) for kc in range(8)],
                          r=[kga, 'xnT'], w=[kpga])
                    pgb, _, kpgb = ps()
                    em.mm([lambda e, kc=kc, pgb=pgb, gb_v=gb_v, cs=cs: e.matmul(pgb, lhsT=gb_v[:, kc, cs], rhs=xnT[:, kc, :], start=(kc == 0), stop=(kc == 7)) for kc in range(8)],
                          r=[kgb, 'xnT'], w=[kpgb])
                    pla, _, kpla = ps()
                    em.mm([lambda e, kc=kc, pla=pla, ms=ms: e.matmul(pla, lhsT=la_v[:, kc, ms], rhs=yaT[:, kc, :], start=(kc == 0), stop=(kc == 3)) for kc in range(4)],
                          r=[bufs[4][1], 'yaT'], w=[kpla])
                    plb_, _, kplb_ = ps()
                    em.mm([lambda e, kc=kc, plb_=plb_, ms=ms: e.matmul(plb_, lhsT=lb_v[:, kc, ms], rhs=ybT[:, kc, :], start=(kc == 0), stop=(kc == 3)) for kc in range(4)],
                          r=[bufs[5][1], 'ybT'], w=[kplb_])
                    sga, ksga = r2k.get()
                    em.op('act', lambda e, sga=sga, pga=pga: e.activation(out=sga, in_=pga, func=AF.Sigmoid), r=[kpga], w=[ksga])
                    sgb, ksgb = r2k.get()
                    em.op('act', lambda e, sgb=sgb, pgb=pgb: e.activation(out=sgb, in_=pgb, func=AF.Sigmoid), r=[kpgb], w=[ksgb])
                    ma, kma = r2k.get()
                    em.op('dve', lambda e, ma=ma, pla=pla, sga=sga: e.tensor_tensor(out=ma, in0=pla, in1=sga, op=ALU.mult), r=[kpla, ksga], w=[kma])
                    mb, kmb = r2k.get()
                    em.op('dve', lambda e, mb=mb, plb_=plb_, sgb=sgb: e.tensor_tensor(out=mb, in0=plb_, in1=sgb, op=ALU.mult), r=[kplb_, ksgb], w=[kmb])
                    em.op('pool', lambda e, mt=mt, ma=ma, mb=mb: e.tensor_tensor(out=mixedT[:, mt, :], in0=ma, in1=mb, op=ALU.add), r=[kma, kmb], w=['mixedT'])
                if b == 0 and dbg_t is not None and 'E1' in DBGSEL:
                    for h_ in range(8):
                        dump_any(mixedT[:, h_, :], ['mixedT'], 512)
                if b == 0:
                    chk('E1')
                wo = [wpiece(6, 2), wpiece(7, 3)]
                xn2bs = []
                def fetch_x(j_):
                    xin_, kx_ = r4k.get()
                    em.dma('sp', lambda e: e.dma_start(out=xin_, in_=x[t0 + j_ * 128:t0 + (j_ + 1) * 128, :]), w=[kx_])
                    return xin_, kx_

                def e2_H(j):
                    tile_idx = b * 4 + j
                    js = slice(j * 128, (j + 1) * 128)
                    xin, kx = fetch_x(j)
                    h1t, kh1 = r4k.get()
                    for hf in range(2):
                        wv = wo[hf][0][:].rearrange("p (c n) -> p c n", n=512)
                        pw_, _, kpw_ = ps()
                        em.mm([lambda e, kc=kc, pw_=pw_, wv=wv: e.matmul(pw_, lhsT=mixedT[:, kc, js], rhs=wv[:, kc, :], start=(kc == 0), stop=(kc == 7)) for kc in range(8)],
                              r=[wo[hf][1], 'mixedT'], w=[kpw_])
                        fs = slice(hf * 512, (hf + 1) * 512)
                        em.op('dve', lambda e, pw_=pw_, fs=fs: e.tensor_tensor(out=h1t[:, fs], in0=pw_, in1=GT1b[:, fs], op=ALU.mult), r=[kpw_, 'GT1b'], w=[kh1])
                        em.op('pool', lambda e, fs=fs: e.tensor_tensor(out=h1t[:, fs], in0=h1t[:, fs], in1=xin[:, fs], op=ALU.add), r=[kh1, kx], w=[kh1])
                    em.dma('sp', lambda e: e.dma_start(out=h1D[t0 + j * 128:t0 + (j + 1) * 128, :], in_=h1t), r=[kh1], w=[('h1D', tile_idx)])
                    junk, kj = r2k.get(BF16)
                    ss, kss = sm(1)
                    em.op('act', lambda e: e.activation(out=junk, in_=h1t, func=AF.Square, accum_out=ss), r=[kh1], w=[kj, kss])
                    rs, krs = rstd_from_ss(ss, kss, 1.0 / D)
                    xn2f, kxf = r4k.get()
                    em.op('dve', lambda e: e.scalar_tensor_tensor(out=xn2f, in0=h1t, scalar=rs, in1=G2b[:], op0=ALU.mult, op1=ALU.mult),
                          r=[kh1, krs, 'G2b'], w=[kxf])
                    em.op('pool', lambda e: e.tensor_tensor(out=xn2f, in0=xn2f, in1=SH2b[:], op=ALU.add), r=[kxf, 'SH2b'], w=[kxf])
                    xhost, kxb = (yaT, 'yaT') if j < 2 else (ybT, 'ybT')
                    xn2b = xhost[:].rearrange("p h n -> p (h n)")[:, (j % 2) * 1024:(j % 2 + 1) * 1024]
                    em.op('act', lambda e: e.activation(out=xn2b, in_=xn2f, func=AF.Copy), r=[kxf], w=[kxb])
                    xn2bs.append((xn2b, kxb))
                    return dict(j=j, xn2f=xn2f, kxf=kxf)

                def e2_T(st):
                    j, xn2f, kxf = st['j'], st['xn2f'], st['kxf']
                    for half in range(2):
                        ptr, _, kptr = ps()
                        em.mm([lambda e, q=q, ptr=ptr, half=half: e.transpose(ptr[:, q * 128:(q + 1) * 128], xn2f[:, (half * 4 + q) * 128:(half * 4 + q + 1) * 128], identf[:])
                               for q in range(4)], r=[kxf] + ck(identf), w=[kptr])
                        if half == 0:
                            em.op('act', lambda e, ptr=ptr, half=half: e.activation(out=xn2T[:, half * 4:half * 4 + 4, :].rearrange("p c n -> p (c n)"), in_=ptr, func=AF.Copy),
                                  r=[kptr], w=[('xn2T', half)])
                        else:
                            em.op('dve', lambda e, ptr=ptr, half=half: e.tensor_copy(out=xn2T[:, half * 4:half * 4 + 4, :].rearrange("p c n -> p (c n)"), in_=ptr),
                                  r=[kptr], w=[('xn2T', half)])
                    plg, _, kplg = ps()
                    em.mm([lambda e, kc=kc: e.matmul(plg[:, 0:36], lhsT=xn2T[:, kc, :], rhs=wr[:, kc, :], start=(kc == 0), stop=(kc == 7)) for kc in range(8)],
                          r=[('xn2T', 0), ('xn2T', 1)] + ck(wr), w=[kplg])
                    em.op('dve', lambda e: e.tensor_tensor(out=lgb[:, j, :], in0=plg[:, 0:36], in1=brb[:], op=ALU.add), r=[kplg] + ck(brb), w=['lgb'])

                e2s = [e2_H(0), e2_H(1)]
                e2_T(e2s[0])
                e2s.append(e2_H(2))
                e2_T(e2s[1])
                e2s.append(e2_H(3))
                e2_T(e2s[2])
                e2_T(e2s[3])
                routing4(em, sm, lgb, onesb, triS, cum, elim, destall, gidxall, wall, b, ps, ck, r2k)
                for j in range(4):
                    tile_idx = b * 4 + j
                    xn2b, kxb = xn2bs[j]
                    for k in range(2):
                        em.dma('pool', lambda e, xn2b=xn2b, k=k, tile_idx=tile_idx: e.indirect_dma_start(
                            out=xsD[:, :], out_offset=IndirectOffsetOnAxis(ap=destall[:, tile_idx * 2 + k:tile_idx * 2 + k + 1], axis=0),
                            in_=xn2b, in_offset=None, bounds_check=pregs['bc'], oob_is_err=False),
                            r=[kxb, ('dest', b)], w=['xsD'])
                if b == 0:
                    chk('E2')
            if dbg_t is not None and 'route' in DBGSEL:
                t, kt = r2k.get()
                em.op('dve', lambda e: e.tensor_copy(out=t[:, 0:64], in_=destall[:]), r=[('dest', i) for i in range(8)], w=[kt])
                dump(t[:, 0:64], [kt], 64)
                dump(wall[:].rearrange("p a b -> p (a b)"), [('dest', i) for i in range(32)], 64)
            chk('P1')
            p1.close()

            p2 = ExitStack()
            es.enter_context(p2)
            wgu = [sb('wgu%d' % i, [128, 8, 512], BF16, p2) for i in range(3)]
            wdn = [sb('wdn%d' % i, [128, 2, D], BF16, p2) for i in range(3)]
            xst = [sb('xst%d' % i, [128, 4, D], BF16, p2) for i in range(2)]
            xsT = [sb('xsT%d' % i, [128, 8, 512], BF16, p2) for i in range(2)]
            hT = [sb('hT%d' % i, [128, 2, 512], BF16, p2) for i in range(2)]
            yst = [sb('yst%d' % i, [128, 4, D], F32, p2) for i in range(2)]
            sgr = Ring(nc, p2, 'sgr', 4, 2048)
            zrow = sb('zrow', [128, D], F32, p2)
            em.op('pool', lambda e: e.memset(zrow[:], 0.0), w=['zrow'])
            em.dma('sp', lambda e: e.dma_start(out=ysD[E * CAP:E * CAP + 128, :], in_=zrow[:]), r=['zrow'], w=[('ysD', 'z')])
            def load_xst(ex_):
                em.dma('sp', lambda e: e.dma_start(out=xst[ex_ % 2][:], in_=xsD[ex_ * CAP:(ex_ + 1) * CAP, :].rearrange("(j p) d -> p j d", p=128)),
                       r=['xsD'], w=[('xst', ex_ % 2)])

            for ex in range(E):
                i2 = ex % 2
                wg_v = w_gate[ex].rearrange("(c p) n -> p c n", p=128)
                wu_v = w_up[ex].rearrange("(c p) n -> p c n", p=128)
                wd_v = w_down[ex].rearrange("(c p) n -> p c n", p=128)
                i3 = ex % 3
                em.dma('pool', lambda e, i3=i3, wg_v=wg_v: e.dma_start(out=wgu[i3][:, :, 0:256], in_=wg_v), w=[('wgu', i3, 0)])
                em.dma('pool', lambda e, i3=i3, wu_v=wu_v: e.dma_start(out=wgu[i3][:, :, 256:512], in_=wu_v), w=[('wgu', i3, 1)])
                em.dma('pool', lambda e, i3=i3, wd_v=wd_v: e.dma_start(out=wdn[i3][:], in_=wd_v), w=[('wdn', i3)])
                if ex == 0:
                    load_xst(0)
                if ex + 1 < E:
                    load_xst(ex + 1)
                for kp_ in range(4):
                    pf, pb, kp = ps()
                    fns = []
                    for q in range(2):
                        kc = kp_ * 2 + q
                        for j in range(4):
                            fns.append(lambda e, q=q, j=j, kc=kc, pb=pb, i2=i2: e.transpose(pb[:, q * 512 + j * 128:q * 512 + (j + 1) * 128], xst[i2][:, j, kc * 128:(kc + 1) * 128], identb[:]))
                    em.mm(fns, r=[('xst', i2)] + ck(identb), w=[kp])
                    dst = xsT[i2][:, kp_ * 2:kp_ * 2 + 2, :].rearrange("p c n -> p (c n)")
                    em.op('act', lambda e, dst=dst, pb=pb: e.activation(out=dst, in_=pb, func=AF.Identity, scale=1.0, bias=epsc[:, 1:2]), r=[kp, 'epsc'], w=[('xsT', i2)])
                pgs = []
                for ft in range(4):
                    pf, pb, kp = ps()
                    em.mm([lambda e, kc=kc, pf=pf, ft=ft, i2=i2, i3=i3: e.matmul(pf, lhsT=wgu[i3][:, kc, ft * 128:(ft + 1) * 128], rhs=xsT[i2][:, kc, :], start=(kc == 0), stop=(kc == 7))
                           for kc in range(8)], r=[('wgu', i3, ft // 2), ('xsT', i2)], w=[kp])
                    pgs.append((pf, kp))
                for f in range(2):
                    sg, ksg = sgr.get()
                    em.op('act', lambda e, sg=sg, f=f, pgs=pgs: e.activation(out=sg, in_=pgs[f][0], func=AF.Silu), r=[pgs[f][1]], w=[ksg])
                    em.op('dve', lambda e, sg=sg, f=f, pgs=pgs, i2=i2: e.tensor_tensor(out=hT[i2][:, f, :], in0=pgs[2 + f][0], in1=sg, op=ALU.mult),
                          r=[pgs[2 + f][1], ksg], w=[('hT', i2)])
                for j in range(4):
                    for hf in range(2):
                        pf, pb, kp = ps()
                        em.mm([lambda e, f=f, pf=pf, j=j, hf=hf, i2=i2, i3=i3: e.matmul(pf, lhsT=hT[i2][:, f, j * 128:(j + 1) * 128], rhs=wdn[i3][:, f, hf * 512:(hf + 1) * 512],
                                                                                  start=(f == 0), stop=(f == 1)) for f in range(2)], r=[('hT', i2), ('wdn', i3)], w=[kp])
                        if (j * 2 + hf) % 2 == 0:
                            em.op('act', lambda e, pf=pf, j=j, hf=hf, i2=i2: e.activation(out=yst[i2][:, j, hf * 512:(hf + 1) * 512], in_=pf, func=AF.Copy), r=[kp], w=[('yst', i2)])
                        else:
                            em.op('dve', lambda e, pf=pf, j=j, hf=hf, i2=i2: e.tensor_copy(out=yst[i2][:, j, hf * 512:(hf + 1) * 512], in_=pf), r=[kp], w=[('yst', i2)])
                em.dma('sp', lambda e, i2=i2, ex=ex: e.dma_start(out=ysD[ex * CAP:(ex + 1) * CAP, :].rearrange("(j p) d -> p j d", p=128), in_=yst[i2][:]),
                       r=[('yst', i2)], w=[('ysD', ex)])
            chk('P2')
            p2.close()

            p3 = ExitStack()
            es.enter_context(p3)
            GT2b = sb('GT2b', [128, D], F32, p3)
            fngb = sb('fngb', [128, D], F32, p3)
            em.dma('sp', lambda e: e.dma_start(out=fngb[:], in_=p_fng), w=['fngb'])
            q4 = Ring(nc, p3, 'q4', 20, 4096)
            q2 = Ring(nc, p3, 'q2', 2, 2048)
            out_toks = []
            def fetch(ti):
                y0, ky0 = q4.get()
                y1, ky1 = q4.get()
                for k, (yy, kyy) in enumerate(((y0, ky0), (y1, ky1))):
                    em.dma('pool', lambda e, yy=yy, k=k, ti=ti: e.indirect_dma_start(
                        out=yy, out_offset=None, in_=ysD[:, :], in_offset=IndirectOffsetOnAxis(ap=gidxall[:, ti * 2 + k:ti * 2 + k + 1], axis=0)),
                        r=[('ysD', 'z')] + [('ysD', ex_) for ex_ in range(E)] + [('dest', ti // 4)], w=[kyy])
                h1t, kh1 = q4.get()
                em.dma('sp', lambda e, h1t=h1t, ti=ti: e.dma_start(out=h1t, in_=h1D[ti * 128:(ti + 1) * 128, :]), r=[('h1D', ti)], w=[kh1])
                return y0, ky0, y1, ky1, h1t, kh1

            pend = [fetch(0), fetch(1)]
            for ti in range(32):
                seq = ti // 16
                if ti % 16 == 0:
                    em.dma('sp', lambda e, seq=seq: e.dma_start(out=GT2b[:], in_=modD[seq:seq + 1, 5120:6144].partition_broadcast(128)), r=MODK, w=['GT2b'])
                y0, ky0, y1, ky1, h1t, kh1 = pend.pop(0)
                if ti + 2 < 32:
                    pend.append(fetch(ti + 2))
                m, km = q4.get()
                em.op('act', lambda e, m=m, y0=y0, ti=ti: e.activation(out=m, in_=y0, func=AF.Copy, scale=wall[:, ti, 0:1]), r=[ky0, ('dest', ti // 4)], w=[km])
                em.op('dve', lambda e, m=m, y1=y1, ti=ti: e.scalar_tensor_tensor(out=m, in0=y1, scalar=wall[:, ti, 1:2], in1=m, op0=ALU.mult, op1=ALU.add),
                      r=[ky1, km, ('dest', ti // 4)], w=[km])
                em.op('pool', lambda e, m=m: e.tensor_tensor(out=m, in0=m, in1=GT2b[:], op=ALU.mult), r=[km, 'GT2b'], w=[km])
                em.op('dve', lambda e, m=m, h1t=h1t: e.tensor_tensor(out=m, in0=m, in1=h1t, op=ALU.add), r=[km, kh1], w=[km])
                junk, kj = q2.get(BF16)
                ss, kss = sm(1)
                em.op('act', lambda e, m=m, junk=junk, ss=ss: e.activation(out=junk, in_=m, func=AF.Square, accum_out=ss), r=[km], w=[kj, kss])
                rs, krs = rstd_from_ss(ss, kss, 1.0 / D)
                o_, ko = q4.get()
                em.op('dve', lambda e, o_=o_, m=m, rs=rs: e.scalar_tensor_tensor(out=o_, in0=m, scalar=rs, in1=fngb[:], op0=ALU.mult, op1=ALU.mult), r=[km, krs, 'fngb'], w=[ko])
                out_toks.append(em.dma('sp', lambda e, o_=o_, ti=ti: e.dma_start(out=out[ti * 128:(ti + 1) * 128, :], in_=o_), r=[ko], w=[('out', ti)]))
        except StopBuild:
            pass
        em.wait_keys('sp', [k for k in em.lastw if isinstance(k, tuple) and k[0] in ('dbg', 'out')])
        for q_ in ('sp', 'pool'):
            d_ = em.dq[q_]
            for i_, v_ in enumerate(d_['vals']):
                if v_ > 0:
                    em._wait('sp', (q_, i_), v_)
        em.finish()
    return nc


DBGSEL = ()
STOP = None
SERIAL = False


class StopBuild(Exception):
    pass


def chk(name):
    if STOP == name:
        raise StopBuild()


def routing(em, sm, lg, onesb, triS, cum, elim, destall, gidxall, wall, ti, ps, ck, r2k):
    kl = 'lg'
    gmax, kgm = sm(1)
    em.op('dve', lambda e: e.tensor_reduce(out=gmax, in_=lg[:, 0:4], axis=AX.X, op=ALU.max), r=[kl], w=[kgm])
    ohg, kohg = sm(4)
    em.op('dve', lambda e: e.tensor_scalar(out=ohg, in0=lg[:, 0:4], scalar1=gmax, scalar2=None, op0=ALU.is_equal), r=[kl, kgm], w=[kohg])
    ngm, kngm = sm(1)
    em.op('dve', lambda e: e.tensor_scalar(out=ngm, in0=gmax, scalar1=-1.0, scalar2=None, op0=ALU.mult), r=[kgm], w=[kngm])
    eg, keg = sm(4)
    sg, ksg = sm(1)
    em.op('act', lambda e: e.activation(out=eg, in_=lg[:, 0:4], func=AF.Exp, bias=ngm, accum_out=sg), r=[kl, kngm], w=[keg, ksg])
    pg, kpg = sm(1)
    em.op('dve', lambda e: e.reciprocal(out=pg, in_=sg), r=[ksg], w=[kpg])
    les, kles = sm(8)
    em.op('dve', lambda e: e.tensor_scalar(out=les, in0=lg[:, 4:12], scalar1=ohg[:, 0:1], scalar2=None, op0=ALU.mult), r=[kl, kohg], w=[kles])
    for g in range(1, 4):
        em.op('dve', lambda e, g=g: e.scalar_tensor_tensor(out=les, in0=lg[:, 4 + 8 * g:12 + 8 * g], scalar=ohg[:, g:g + 1], in1=les, op0=ALU.mult, op1=ALU.add),
              r=[kl, kohg, kles], w=[kles])
    m8, km8 = sm(8)
    em.op('dve', lambda e: e.max(out=m8, in_=les), r=[kles], w=[km8])
    d21, kd21 = sm(1)
    em.op('dve', lambda e: e.tensor_tensor(out=d21, in0=m8[:, 1:2], in1=m8[:, 0:1], op=ALU.subtract), r=[km8], w=[kd21])
    e21, ke21 = sm(1)
    em.op('act', lambda e: e.activation(out=e21, in_=d21, func=AF.Exp), r=[kd21], w=[ke21])
    den, kden = sm(1)
    em.op('dve', lambda e: e.tensor_scalar(out=den, in0=e21, scalar1=1.0, scalar2=None, op0=ALU.add), r=[ke21], w=[kden])
    rden, krden = sm(1)
    em.op('dve', lambda e: e.reciprocal(out=rden, in_=den), r=[kden], w=[krden])
    w1, kw1 = sm(1)
    em.op('dve', lambda e: e.tensor_tensor(out=w1, in0=pg, in1=rden, op=ALU.mult), r=[kpg, krden], w=[kw1])
    w2, kw2 = sm(1)
    em.op('dve', lambda e: e.tensor_tensor(out=w2, in0=w1, in1=e21, op=ALU.mult), r=[kw1, ke21], w=[kw2])
    ohs = []
    for k in range(2):
        sel, ksel = sm(8)
        em.op('dve', lambda e, k=k, sel=sel: e.tensor_scalar(out=sel, in0=les, scalar1=m8[:, k:k + 1], scalar2=None, op0=ALU.is_equal), r=[kles, km8], w=[ksel])
        oh, koh = sm(32)
        for g in range(4):
            em.op('dve', lambda e, g=g, oh=oh, sel=sel: e.tensor_scalar(out=oh[:, g * 8:(g + 1) * 8], in0=sel, scalar1=ohg[:, g:g + 1], scalar2=None, op0=ALU.mult),
                  r=[ksel, kohg], w=[koh])
        ohs.append((oh, koh))
    ohsum, kohs = r2k.get(BF16)
    em.op('dve', lambda e: e.tensor_tensor(out=ohsum[:, 0:32], in0=ohs[0][0], in1=ohs[1][0], op=ALU.add), r=[ohs[0][1], ohs[1][1]], w=[kohs])
    pr, _, kpr = ps()
    em.mm([lambda e: e.matmul(pr[:, 0:32], lhsT=triS[:], rhs=ohsum[:, 0:32], start=True, stop=True),
           lambda e: e.matmul(pr[:, 32:64], lhsT=onesb[:], rhs=ohsum[:, 0:32], start=True, stop=True)], r=[kohs] + ck(triS, onesb), w=[kpr])
    rk, krk = sm(32)
    em.op('dve', lambda e: e.tensor_tensor(out=rk, in0=pr[:, 0:32], in1=cum[:], op=ALU.add), r=[kpr, 'cum'], w=[krk])
    em.op('dve', lambda e: e.tensor_tensor(out=cum[:], in0=pr[:, 32:64], in1=cum[:], op=ALU.add), r=[kpr, 'cum', krk], w=['cum'])
    for k in range(2):
        oh, koh = ohs[k]
        t32, kt32 = sm(32)
        dst, kdst = sm(1)
        em.op('dve', lambda e, t32=t32, oh=oh: e.tensor_tensor(out=t32, in0=oh, in1=rk, op=ALU.mult), r=[koh, krk], w=[kt32])
        em.op('dve', lambda e, t32=t32, dst=dst: e.tensor_reduce(out=dst, in_=t32, axis=AX.X, op=ALU.add), r=[kt32], w=[kdst])
        l32, kl32 = sm(32)
        lim, klim = sm(1)
        em.op('dve', lambda e, l32=l32, oh=oh: e.tensor_tensor(out=l32, in0=oh, in1=elim[:], op=ALU.mult), r=[koh] + ck(elim), w=[kl32])
        em.op('dve', lambda e, l32=l32, lim=lim: e.tensor_reduce(out=lim, in_=l32, axis=AX.X, op=ALU.add), r=[kl32], w=[klim])
        ok, kok = sm(1)
        em.op('dve', lambda e, ok=ok, dst=dst, lim=lim: e.tensor_tensor(out=ok, in0=dst, in1=lim, op=ALU.is_lt), r=[kdst, klim], w=[kok])
        nok, knok = sm(1)
        em.op('dve', lambda e, nok=nok, ok=ok: e.tensor_scalar(out=nok, in0=ok, scalar1=-1.0, scalar2=1.0, op0=ALU.mult, op1=ALU.add), r=[kok], w=[knok])
        dv, kdv = sm(1)
        em.op('dve', lambda e, dv=dv, dst=dst, ok=ok: e.tensor_tensor(out=dv, in0=dst, in1=ok, op=ALU.mult), r=[kdst, kok], w=[kdv])
        si, ksi = sm(1)
        em.op('dve', lambda e, si=si, nok=nok, dv=dv: e.scalar_tensor_tensor(out=si, in0=nok, scalar=float(E * CAP + 64), in1=dv, op0=ALU.mult, op1=ALU.add), r=[knok, kdv], w=[ksi])
        gi_, kgi = sm(1)
        em.op('dve', lambda e, gi_=gi_, nok=nok, dv=dv: e.scalar_tensor_tensor(out=gi_, in0=nok, scalar=float(E * CAP), in1=dv, op0=ALU.mult, op1=ALU.add), r=[knok, kdv], w=[kgi])
        em.op('dve', lambda e, k=k, si=si: e.tensor_copy(out=destall[:, ti * 2 + k:ti * 2 + k + 1], in_=si), r=[ksi], w=[('dest', ti)])
        em.op('dve', lambda e, k=k, gi_=gi_: e.tensor_copy(out=gidxall[:, ti * 2 + k:ti * 2 + k + 1], in_=gi_), r=[kgi], w=[('dest', ti)])
        wk, kwk = (w1, kw1) if k == 0 else (w2, kw2)
        em.op('dve', lambda e, k=k, wk=wk, ok=ok: e.tensor_tensor(out=wall[:, ti, k:k + 1], in0=wk, in1=ok, op=ALU.mult), r=[kwk, kok], w=[('dest', ti)])


def _consts():
    bf = ml_dtypes.bfloat16
    i = np.arange(128)
    c = {}
    c['k_identb'] = np.eye(128, dtype=np.float32).astype(bf)
    c['k_identq'] = np.tile(np.eye(128, dtype=np.float32), (1, 4)).astype(bf)
    c['k_identf'] = np.eye(128, dtype=np.float32)
    c['k_onesb'] = np.ones((128, 128), np.float32).astype(bf)
    c['k_onesf'] = np.ones((128, 128), np.float32)
    c['k_triU'] = (i[:, None] <= i[None, :]).astype(np.float32)
    c['k_maskT'] = np.where(i[None, :] >= i[:, None], 0.0, NEG).astype(np.float32)
    c['k_maskS'] = np.where(i[:, None] > i[None, :], 0.0, -NEG).astype(np.float32)
    c['k_triS'] = (i[:, None] < i[None, :]).astype(np.float32).astype(bf)
    pc = np.zeros((128, 4, 16), np.float32)
    for gi, win in enumerate((2, 4, 8, 16)):
        t = np.arange(16)
        pc[:, gi, :] = 1.0 / np.minimum(t + 1, win)
    c['k_pcorr'] = pc
    bm = np.zeros((128, 7, 128), np.float32)
    for l in range(7):
        s_ = 1 << l
        bm[:, l, :] = ((i[:, None] // (2 * s_) == i[None, :] // (2 * s_)) & (i[:, None] % (2 * s_) >= s_) & (i[None, :] % (2 * s_) < s_))
    c['k_bmask'] = bm.astype(bf)
    c['k_bmaskT'] = np.ascontiguousarray(bm.transpose(2, 1, 0)).astype(bf)
    c['k_ebase'] = np.tile((np.arange(32) * CAP).astype(np.float32), (128, 1))
    c['k_elim'] = np.tile(((np.arange(32) + 1) * CAP).astype(np.float32), (128, 1))
    return c


def _prep_inputs(inp):
    f = lambda a: np.ascontiguousarray(np.asarray(a, dtype=np.float32))
    shared = {}
    shared['w_ada'] = f(inp['w_ada'][0])
    shared['b_ada'] = f(inp['b_ada'][0]).reshape(1, -1)
    shared['w_in'] = f(inp['w_in'][0])
    shared['w_lift_a'] = f(inp['w_lift_a'][0])
    shared['w_lift_b'] = f(inp['w_lift_b'][0])
    shared['w_out'] = f(inp['w_out'][0])
    shared['pool_w'] = f(inp['pool_w'][0])
    shared['w_gate'] = f(inp['w_gate'][0])
    shared['w_up'] = f(inp['w_up'][0])
    shared['w_down'] = f(inp['w_down'][0])
    shared['p_n1g'] = f(np.asarray(inp['norm1_g'][0]).reshape(8, 128).T)
    shared['p_convw'] = f(np.asarray(inp['conv_w'][0]).reshape(4, 12, 128).transpose(2, 1, 0))
    shared['p_alog'] = f(np.broadcast_to(np.asarray(inp['a_log'][0]).reshape(1, 4), (128, 4)))
    shared['p_dtb'] = f(np.broadcast_to(np.asarray(inp['dt_bias'][0]).reshape(1, 4), (128, 4)))
    shared['p_dng'] = f(np.asarray(inp['dn_norm_g'][0]).reshape(128, 1))
    shared['p_pscale'] = f(np.asarray(inp['pool_scale'][0]).reshape(4, 128).T)
    shared['p_n2g'] = f(np.broadcast_to(np.asarray(inp['norm2_g'][0]).reshape(1, D), (128, D)))
    shared['p_fng'] = f(np.broadcast_to(np.asarray(inp['final_norm_g']).reshape(1, D), (128, D)))
    br = np.concatenate([np.asarray(inp['b_router_group'][0]), np.asarray(inp['b_router_expert'][0])]).reshape(1, 36)
    shared['p_brb'] = f(np.broadcast_to(br, (128, 36)))
    wrc = np.concatenate([np.asarray(inp['w_router_group'][0]), np.asarray(inp['w_router_expert'][0])], axis=1)
    shared['p_wr'] = f(wrc.reshape(8, 128, 36).transpose(1, 0, 2))
    shared.update(_consts())
    xs = np.asarray(inp['x'], dtype=np.float32)
    cs = np.asarray(inp['c'], dtype=np.float32)
    in_maps = []
    for i in range(NCORES):
        m = dict(shared)
        m['x'] = np.ascontiguousarray(xs[2 * i:2 * i + 2].reshape(TOK, D))
        m['cT'] = np.ascontiguousarray(cs[2 * i:2 * i + 2].reshape(2, 8, 128).transpose(2, 1, 0))
        in_maps.append(m)
    return in_maps


_NC_CACHE = {}


def kernel(**inputs):
    in_maps = _prep_inputs(inputs)
    if 'nc' not in _NC_CACHE:
        _NC_CACHE['nc'] = build_nc()
    nc = _NC_CACHE['nc']
    res = run_bass_kernel_spmd(nc, in_maps, core_ids=list(range(NCORES)))
    outs = [np.asarray(r['out'], dtype=np.float32).reshape(2, 2048, D) for r in res.results]
    return np.concatenate(outs, axis=0)


def routing4(em, sm, lgb, onesb, triS, cum, elim, destall, gidxall, wall, b, ps, ck, r2k):
    kl = 'lgb'
    T = 4

    def v3(ap, n):
        return ap.rearrange("p (t n) -> p t n", n=n)

    def bc_last(ap, n):
        return ap.unsqueeze(2).broadcast_to([128, T, n])
    lgg = lgb[:, :, 0:4]
    gmax, kgm = sm(T)
    em.op('dve', lambda e: e.tensor_reduce(out=gmax, in_=lgg, axis=AX.X, op=ALU.max), r=[kl], w=[kgm])
    ohg_, kohg = sm(16)
    ohg = v3(ohg_, 4)
    em.op('dve', lambda e: e.tensor_tensor(out=ohg, in0=lgg, in1=bc_last(gmax, 4), op=ALU.is_equal), r=[kl, kgm], w=[kohg])
    sub_, ksub = sm(16)
    em.op('dve', lambda e: e.tensor_tensor(out=v3(sub_, 4), in0=lgg, in1=bc_last(gmax, 4), op=ALU.subtract), r=[kl, kgm], w=[ksub])
    eg_, keg = sm(16)
    em.op('act', lambda e: e.activation(out=eg_, in_=sub_, func=AF.Exp), r=[ksub], w=[keg])
    sg, ksg = sm(T)
    em.op('dve', lambda e: e.tensor_reduce(out=sg, in_=v3(eg_, 4), axis=AX.X, op=ALU.add), r=[keg], w=[ksg])
    pg, kpg = sm(T)
    em.op('dve', lambda e: e.reciprocal(out=pg, in_=sg), r=[ksg], w=[kpg])
    prod_, kprod = r2k.get()
    prod = prod_[:, 0:T * 32]
    le4 = lgb[:, :, 4:36].rearrange("p t (g j) -> p t g j", j=8)
    em.op('dve', lambda e: e.tensor_tensor(out=prod.rearrange("p (t g j) -> p t g j", g=4, j=8), in0=le4,
                                           in1=ohg.unsqueeze(3).broadcast_to([128, T, 4, 8]), op=ALU.mult), r=[kl, kohg], w=[kprod])
    les_, kles = sm(32)
    les = v3(les_, 8)
    em.op('dve', lambda e: e.tensor_reduce(out=les, in_=prod.rearrange("p (t g j) -> p t j g", g=4, j=8), axis=AX.X, op=ALU.add), r=[kprod], w=[kles])
    m1, km1 = sm(T)
    em.op('dve', lambda e: e.tensor_reduce(out=m1, in_=les, axis=AX.X, op=ALU.max), r=[kles], w=[km1])
    sel1_, ksel1 = sm(32)
    sel1 = v3(sel1_, 8)
    em.op('dve', lambda e: e.tensor_tensor(out=sel1, in0=les, in1=bc_last(m1, 8), op=ALU.is_equal), r=[kles, km1], w=[ksel1])
    les2_, kles2 = sm(32)
    les2 = v3(les2_, 8)
    em.op('dve', lambda e: e.scalar_tensor_tensor(out=les2, in0=sel1, scalar=NEG, in1=les, op0=ALU.mult, op1=ALU.add), r=[ksel1, kles], w=[kles2])
    m2, km2 = sm(T)
    em.op('dve', lambda e: e.tensor_reduce(out=m2, in_=les2, axis=AX.X, op=ALU.max), r=[kles2], w=[km2])
    sel2_, ksel2 = sm(32)
    sel2 = v3(sel2_, 8)
    em.op('dve', lambda e: e.tensor_tensor(out=sel2, in0=les2, in1=bc_last(m2, 8), op=ALU.is_equal), r=[kles2, km2], w=[ksel2])
    d21, kd21 = sm(T)
    em.op('dve', lambda e: e.tensor_tensor(out=d21, in0=m2, in1=m1, op=ALU.subtract), r=[km1, km2], w=[kd21])
    e21, ke21 = sm(T)
    em.op('act', lambda e: e.activation(out=e21, in_=d21, func=AF.Exp), r=[kd21], w=[ke21])
    den, kden = sm(T)
    em.op('dve', lambda e: e.tensor_scalar(out=den, in0=e21, scalar1=1.0, scalar2=None, op0=ALU.add), r=[ke21], w=[kden])
    rden, krden = sm(T)
    em.op('dve', lambda e: e.reciprocal(out=rden, in_=den), r=[kden], w=[krden])
    w1, kw1 = sm(T)
    em.op('dve', lambda e: e.tensor_tensor(out=w1, in0=pg, in1=rden, op=ALU.mult), r=[kpg, krden], w=[kw1])
    w2, kw2 = sm(T)
    em.op('dve', lambda e: e.tensor_tensor(out=w2, in0=w1, in1=e21, op=ALU.mult), r=[kw1, ke21], w=[kw2])
    ohs = []
    for k, (sel, ksel) in enumerate(((sel1, ksel1), (sel2, ksel2))):
        oh_, koh = r2k.get()
        oh = oh_[:, 0:T * 32]
        em.op('dve', lambda e, oh=oh, sel=sel: e.tensor_tensor(out=oh.rearrange("p (t g j) -> p t g j", g=4, j=8),
                                                               in0=ohg.unsqueeze(3).broadcast_to([128, T, 4, 8]),
                                                               in1=sel.unsqueeze(2).broadcast_to([128, T, 4, 8]), op=ALU.mult), r=[kohg, ksel], w=[koh])
        ohs.append((oh, koh))
    ohsum_, kohs = r2k.get(BF16)
    ohsum = ohsum_[:, 0:T * 32]
    em.op('dve', lambda e: e.tensor_tensor(out=ohsum, in0=ohs[0][0], in1=ohs[1][0], op=ALU.add), r=[ohs[0][1], ohs[1][1]], w=[kohs])
    pr, _, kpr = ps()
    fns = []
    for t in range(T):
        terms = [(triS, t)] + [(onesb, t2) for t2 in range(t)]
        for i_, (lt, t2) in enumerate(terms):
            fns.append(lambda e, t=t, lt=lt, t2=t2, i_=i_, n_=len(terms): e.matmul(pr[:, t * 32:(t + 1) * 32], lhsT=lt[:], rhs=ohsum[:, t2 * 32:(t2 + 1) * 32],
                                                                                  start=(i_ == 0), stop=(i_ == n_ - 1)))
    for t in range(T):
        fns.append(lambda e, t=t: e.matmul(pr[:, 128:160], lhsT=onesb[:], rhs=ohsum[:, t * 32:(t + 1) * 32], start=(t == 0), stop=(t == T - 1)))
    em.mm(fns, r=[kohs] + ck(triS, onesb), w=[kpr])
    rk_, krk = r2k.get()
    rk = rk_[:, 0:T * 32]
    em.op('dve', lambda e: e.tensor_tensor(out=v3(rk, 32), in0=v3(pr[:, 0:128], 32), in1=cum[:].unsqueeze(1).broadcast_to([128, T, 32]), op=ALU.add),
          r=[kpr, 'cum'], w=[krk])
    em.op('dve', lambda e: e.tensor_tensor(out=cum[:], in0=pr[:, 128:160], in1=cum[:], op=ALU.add), r=[kpr, 'cum', krk], w=['cum'])
    dsl = slice(b * 8, (b + 1) * 8)
    for k in range(2):
        oh, koh = ohs[k]
        t32_, kt32 = r2k.get()
        t32 = t32_[:, 0:T * 32]
        em.op('dve', lambda e, t32=t32, oh=oh: e.tensor_tensor(out=t32, in0=oh, in1=rk, op=ALU.mult), r=[koh, krk], w=[kt32])
        dst, kdst = sm(T)
        em.op('dve', lambda e, t32=t32, dst=dst: e.tensor_reduce(out=dst, in_=v3(t32, 32), axis=AX.X, op=ALU.add), r=[kt32], w=[kdst])
        l32_, kl32 = r2k.get()
        l32 = l32_[:, 0:T * 32]
        em.op('dve', lambda e, l32=l32, oh=oh: e.tensor_tensor(out=v3(l32, 32), in0=v3(oh, 32), in1=elim[:].unsqueeze(1).broadcast_to([128, T, 32]), op=ALU.mult),
              r=[koh] + ck(elim), w=[kl32])
        lim, klim = sm(T)
        em.op('dve', lambda e, l32=l32, lim=lim: e.tensor_reduce(out=lim, in_=v3(l32, 32), axis=AX.X, op=ALU.add), r=[kl32], w=[klim])
        ok, kok = sm(T)
        em.op('dve', lambda e, ok=ok, dst=dst, lim=lim: e.tensor_tensor(out=ok, in0=dst, in1=lim, op=ALU.is_lt), r=[kdst, klim], w=[kok])
        nok, knok = sm(T)
        em.op('dve', lambda e, nok=nok, ok=ok: e.tensor_scalar(out=nok, in0=ok, scalar1=-1.0, scalar2=1.0, op0=ALU.mult, op1=ALU.add), r=[kok], w=[knok])
        dv, kdv = sm(T)
        em.op('dve', lambda e, dv=dv, dst=dst, ok=ok: e.tensor_tensor(out=dv, in0=dst, in1=ok, op=ALU.mult), r=[kdst, kok], w=[kdv])
        si, ksi = sm(T)
        em.op('dve', lambda e, si=si, nok=nok, dv=dv: e.scalar_tensor_tensor(out=si, in0=nok, scalar=float(E * CAP + 64), in1=dv, op0=ALU.mult, op1=ALU.add), r=[knok, kdv], w=[ksi])
        gi_, kgi = sm(T)
        em.op('dve', lambda e, gi_=gi_, nok=nok, dv=dv: e.scalar_tensor_tensor(out=gi_, in0=nok, scalar=float(E * CAP), in1=dv, op0=ALU.mult, op1=ALU.add), r=[knok, kdv], w=[kgi])
        em.op('dve', lambda e, k=k, si=si: e.tensor_copy(out=destall[:, dsl].rearrange("p (t k) -> p t k", k=2)[:, :, k], in_=si), r=[ksi], w=[('dest', b)])
        em.op('dve', lambda e, k=k, gi_=gi_: e.tensor_copy(out=gidxall[:, dsl].rearrange("p (t k) -> p t k", k=2)[:, :, k], in_=gi_), r=[kgi], w=[('dest', b)])
        wk, kwk = (w1, kw1) if k == 0 else (w2, kw2)
        em.op('dve', lambda e, k=k, wk=wk, ok=ok: e.tensor_tensor(out=wall[:, b * 4:(b + 1) * 4, k], in0=wk, in1=ok, op=ALU.mult), r=[kwk, kok], w=[('dest', b)])
```

```python
import types
import numpy as np
import ml_dtypes
from contextlib import ExitStack
import concourse.bass as bass
import concourse.mybir as mybir
from concourse.bass import IndirectOffsetOnAxis
from concourse.bass_utils import run_bass_kernel_spmd

F32 = mybir.dt.float32
BF16 = mybir.dt.bfloat16
I32 = mybir.dt.int32
U32 = mybir.dt.uint32
AF = mybir.ActivationFunctionType
ALU = mybir.AluOpType
AX = mybir.AxisListType

NCORES = 8
D = 1024
TOK = 4096
BLK = 512
NBLK = TOK // BLK
E = 32
CAP = 512
DFF = 256
EPS = 1e-6
NEG = -1.0e30
WIN_RES = 2568
ENGS = ('pe', 'act', 'dve', 'pool', 'sp')


def _freeze(fn):
    if fn.__closure__ is None:
        return fn
    cells = []
    for c in fn.__closure__:
        try:
            cells.append(types.CellType(c.cell_contents))
        except ValueError:
            cells.append(c)
    g = types.FunctionType(fn.__code__, fn.__globals__, fn.__name__, fn.__defaults__, tuple(cells))
    g.__kwdefaults__ = fn.__kwdefaults__
    return g


class Em:
    def __init__(self, nc, es):
        self.nc = nc
        self.streams = {e: [] for e in ENGS}
        self.sem = {e: es.enter_context(nc.semaphore('s_' + e)) for e in ('pe', 'act', 'dve', 'pool')}
        self.cnt = {e: 0 for e in self.sem}
        self.waited = {e: {} for e in ENGS}
        self.lastw = {}
        self.reads = {}
        self.dq = {}
        for q, n in (('sp', 28), ('pool', 14), ('act', 4)):
            sems = [es.enter_context(nc.semaphore('d_%s%d' % (q, i))) for i in range(n)]
            self.dq[q] = dict(sems=sems, vals=[0] * n, nxt=0)

    def _semh(self, key):
        return self.sem[key] if isinstance(key, str) else self.dq[key[0]]['sems'][key[1]]

    def _wait(self, eng, key, val):
        if self.waited[eng].get(key, 0) >= val:
            return
        self.waited[eng][key] = val
        s = self._semh(key)
        self.streams[eng].append(lambda e, s=s, v=val: e.wait_ge(s, v))

    def _deps(self, eng, r, w, pe_inorder=False):
        need = {}

        def add(tok):
            if tok is None:
                return
            k, v = tok
            if need.get(k, 0) < v:
                need[k] = v
        for key in r:
            add(self.lastw.get(key))
        for key in w:
            add(self.lastw.get(key))
            for t in self.reads.get(key, {}).items():
                add(t)
        if SERIAL:
            for k2 in ('pe', 'act', 'dve', 'pool'):
                if self.cnt[k2] > 0:
                    need[k2] = self.cnt[k2]
            for q2, d2 in self.dq.items():
                for i2, v2 in enumerate(d2['vals']):
                    if v2 > 0:
                        need[(q2, i2)] = max(need.get((q2, i2), 0), v2)
        for k, v in need.items():
            if pe_inorder and k == 'pe':
                continue
            self._wait(eng, k, v)

    def _track(self, tok, r, w):
        for key in w:
            self.lastw[key] = tok
            self.reads[key] = {}
        for key in r:
            d = self.reads.setdefault(key, {})
            if d.get(tok[0], 0) < tok[1]:
                d[tok[0]] = tok[1]

    def op(self, eng, fn, r=(), w=()):
        fn = _freeze(fn)
        self._deps(eng, r, w)
        self.cnt[eng] += 1
        tok = (eng, self.cnt[eng])
        s = self.sem[eng]
        self.streams[eng].append(lambda e, fn=fn, s=s: fn(e).then_inc(s, 1))
        self._track(tok, r, w)
        return tok

    def mm(self, fns, r=(), w=()):
        fns = [_freeze(f) for f in fns]
        self._deps('pe', r, w, pe_inorder=True)
        self.cnt['pe'] += 1
        tok = ('pe', self.cnt['pe'])
        s = self.sem['pe']
        for fn in fns[:-1]:
            self.streams['pe'].append(lambda e, fn=fn: fn(e))
        self.streams['pe'].append(lambda e, fn=fns[-1], s=s: fn(e).then_inc(s, 1))
        self._track(tok, r, w)
        return tok

    def dma(self, q, fn, r=(), w=()):
        fn = _freeze(fn)
        d = self.dq[q]
        i = d['nxt']
        d['nxt'] = (i + 1) % len(d['sems'])
        key = (q, i)
        if d['vals'][i] > 0:
            self._wait(q, key, d['vals'][i])
        self._deps(q, r, w)
        d['vals'][i] += 16
        tok = (key, d['vals'][i])
        s = d['sems'][i]
        self.streams[q].append(lambda e, fn=fn, s=s: fn(e).then_inc(s, 16))
        self._track(tok, r, w)
        return tok

    def wait_keys(self, eng, keys):
        self._deps(eng, keys, keys)

    def finish(self):
        nc = self.nc
        st = self.streams
        with nc.Block() as block:
            @block.tensor
            def _(e):
                for f in st['pe']:
                    f(e)

            @block.scalar
            def _(e):
                for f in st['act']:
                    f(e)

            @block.vector
            def _(e):
                for f in st['dve']:
                    f(e)

            @block.gpsimd
            def _(e):
                for f in st['pool']:
                    f(e)

            @block.sync
            def _(e):
                for f in st['sp']:
                    f(e)


class Ring:
    def __init__(self, nc, es, name, n, nbytes):
        self.t = [es.enter_context(nc.sbuf_tensor('%s%d' % (name, i), [128, nbytes // 4], F32)) for i in range(n)]
        self.name = name
        self.n = n
        self.i = 0

    def get(self, dt=F32):
        i = self.i
        self.i = (i + 1) % self.n
        ap = self.t[i][:]
        if dt != F32:
            ap = ap.bitcast(dt)
        return ap, (self.name, i)


def build_nc(dbg=None):
    nc = bass.Bass("TRN2", target_bir_lowering=False)

    def din(name, shape, dt=F32):
        return nc.dram_tensor(name, list(shape), dt, kind="ExternalInput").ap()

    x = din("x", [TOK, D])
    cT = din("cT", [128, 8, 2])
    w_ada = din("w_ada", [D, 6 * D])
    b_ada = din("b_ada", [1, 6 * D])
    w_in = din("w_in", [D, 4616])
    w_la = din("w_lift_a", [512, D])
    w_lb = din("w_lift_b", [512, D])
    w_out = din("w_out", [D, D])
    pool_w = din("pool_w", [4, 128, 128])
    w_gate = din("w_gate", [E, D, DFF])
    w_up = din("w_up", [E, D, DFF])
    w_down = din("w_down", [E, DFF, D])
    p_n1g = din("p_n1g", [128, 8])
    p_convw = din("p_convw", [128, 12, 4])
    p_alog = din("p_alog", [128, 4])
    p_dtb = din("p_dtb", [128, 4])
    p_dng = din("p_dng", [128, 1])
    p_pscale = din("p_pscale", [128, 4])
    p_n2g = din("p_n2g", [128, D])
    p_fng = din("p_fng", [128, D])
    p_brb = din("p_brb", [128, 36])
    p_wr = din("p_wr", [128, 8, 36])
    k_identb = din("k_identb", [128, 128], BF16)
    k_identq = din("k_identq", [128, 512], BF16)
    k_identf = din("k_identf", [128, 128])
    k_onesb = din("k_onesb", [128, 128], BF16)
    k_onesf = din("k_onesf", [128, 128])
    k_triU = din("k_triU", [128, 128])
    k_maskT = din("k_maskT", [128, 128])
    k_maskS = din("k_maskS", [128, 128])
    k_triS = din("k_triS", [128, 128], BF16)
    k_pcorr = din("k_pcorr", [128, 4, 16])
    k_ebase = din("k_ebase", [128, 32])
    k_bmask = din("k_bmask", [128, 7, 128], BF16)
    k_bmaskT = din("k_bmaskT", [128, 7, 128], BF16)
    k_elim = din("k_elim", [128, 32])

    out = nc.dram_tensor("out", [TOK, D], F32, kind="ExternalOutput").ap()
    dbg_t = None
    if dbg is not None:
        dbg_t = nc.dram_tensor("dbg", list(dbg), F32, kind="ExternalOutput").ap()
    modD = nc.dram_tensor("modD", [2, 6 * D], F32).ap()
    wsD = nc.dram_tensor("wsD", [8, 128, 4096], BF16).ap()
    h1D = nc.dram_tensor("h1D", [TOK, D], F32).ap()
    xsD = nc.dram_tensor("xsD", [E * CAP, D], BF16).ap()
    ysD = nc.dram_tensor("ysD", [E * CAP + 128, D], F32).ap()

    es = ExitStack()
    with es:
        em = Em(nc, es)

        def sb(name, shape, dt=F32, stack=es):
            return stack.enter_context(nc.sbuf_tensor(name, list(shape), dt))

        pregs = {}
        em.streams['pool'].append(lambda e: pregs.__setitem__('bc', e.to_reg(E * CAP - 1)))

        psb = [es.enter_context(nc.psum_tensor('ps%d' % i, [128, 512], F32)) for i in range(8)]
        pstate = {'i': 0}

        def ps():
            i = pstate['i']
            pstate['i'] = (i + 1) % 8
            return psb[i][:], psb[i][:].bitcast(BF16), ('ps', i)

        identb = sb('identb', [128, 128], BF16)
        identq = sb('identq', [128, 512], BF16)
        identf = sb('identf', [128, 128])
        onesb = sb('onesb', [128, 128], BF16)
        onesf = sb('onesf', [128, 128])
        triU = sb('triU', [128, 128])
        maskT = sb('maskT', [128, 128])
        maskS = sb('maskS', [128, 128])
        triS = sb('triS', [128, 128], BF16)
        pcorr = sb('pcorr', [128, 4, 16])
        bmask = sb('bmask', [128, 7, 128], BF16)
        bmaskT = sb('bmaskT', [128, 7, 128], BF16)
        n1g = sb('n1g', [128, 8])
        convw = sb('convw', [128, 12, 4])
        alog = sb('alog', [128, 4])
        dtb = sb('dtb', [128, 4])
        dng = sb('dng', [128, 1])
        pscale = sb('pscale', [128, 4])
        brb = sb('brb', [128, 36])
        wr = sb('wr', [128, 8, 36])
        cum = sb('cum', [128, 32])
        elim = sb('elim', [128, 32])
        destall = sb('destall', [128, 64], I32)
        gidxall = sb('gidxall', [128, 64], I32)
        wall = sb('wall', [128, 32, 2])
        cst = [(identb, k_identb), (identq, k_identq), (identf, k_identf), (onesb, k_onesb), (onesf, k_onesf),
               (triU, k_triU), (maskT, k_maskT), (maskS, k_maskS), (triS, k_triS), (pcorr, k_pcorr),
               (n1g, p_n1g), (convw, p_convw), (alog, p_alog), (dtb, p_dtb), (dng, p_dng), (pscale, p_pscale),
               (brb, p_brb), (wr, p_wr), (cum, k_ebase), (elim, k_elim), (bmask, k_bmask), (bmaskT, k_bmaskT)]
        for n_, (t_, src_) in enumerate(cst):
            em.dma('sp', lambda e, t_=t_, src_=src_: e.dma_start(out=t_[:], in_=src_), w=[('c', n_)])
        CK = [('c', n_) for n_ in range(len(cst))]
        cidx = {id(t_): ('c', n_) for n_, (t_, _) in enumerate(cst)}

        def ck(*ts):
            return [cidx[id(t)] for t in ts]

        small = sb('small', [128, 40 * 32])
        smi = {'i': 0}

        def sm(n=1):
            assert n <= 32
            i = smi['i']
            smi['i'] = (i + 1) % 40
            return small[:, i * 32:i * 32 + n], ('sm', i)

        epsc = sb('epsc', [128, 4])
        ctf_t = sb('ctf_t', [128, 16])
        scb_t = sb('scb_t', [128, 16], BF16)
        em.op('dve', lambda e: e.memset(epsc[:, 0:1], EPS), w=['epsc'])
        em.op('dve', lambda e: e.memset(epsc[:, 1:2], 0.0), w=['epsc'])
        em.op('dve', lambda e: e.memset(epsc[:, 2:3], 1.0), w=['epsc'])

        p1 = ExitStack()
        es.enter_context(p1)
        winb = sb('winb', [128, 8, WIN_RES], BF16, p1)
        poolwb = sb('poolwb', [128, 4, 128], BF16, p1)
        wst = [sb('wst%d' % i, [128, 4096], BF16, p1) for i in range(4)]
        wsi = {'i': 0}
        G2b = sb('G2b', [128, D], F32, p1)
        SH2b = sb('SH2b', [128, D], F32, p1)
        GT1b = sb('GT1b', [128, D], F32, p1)
        modp = sb('modp', [128, 2, 2, 8], F32, p1)
        r2k = Ring(nc, p1, 'r2k', 8, 2048)
        r4k = Ring(nc, p1, 'r4k', 4, 4096)
        nq = Ring(nc, p1, 'nq', 10, 1024)
        xnT = sb('xnT', [128, 8, BLK], BF16, p1)
        qT = sb('qT', [128, 4, BLK], BF16, p1)
        kT = sb('kT', [128, 4, BLK], BF16, p1)
        vT = sb('vT', [128, 4, BLK], BF16, p1)
        szT = sb('szT', [128, 4, BLK], BF16, p1)
        puT = sb('puT', [128, 4, 16 + BLK], F32, p1)
        ybT = sb('ybT', [128, 4, BLK], BF16, p1)
        yaT = sb('yaT', [128, 4, BLK], BF16, p1)
        mixedT = sb('mixedT', [128, 8, BLK], BF16, p1)
        halo = sb('halo', [128, 12, 4], F32, p1)
        gtm = sb('gtm', [128, 4, 8], F32, p1)
        S = sb('S', [128, 4, 128], F32, p1)
        Sb = sb('Sb', [128, 4, 128], BF16, p1)
        cq = {n_: sb('cq_' + n_, [128, 4, 128], BF16, p1) for n_ in ('DT', 'Ds', 'Er', 'kbd', 'kdec', 'vb', 'N0', 'Pt0')}
        xn2T = sb('xn2T', [128, 8, 128], F32, p1)
        lgb = sb('lgb', [128, 4, 36], F32, p1)

        w_in_v = w_in.rearrange("(c p) n -> p c n", p=128)
        for kc in range(8):
            for hf in range(2):
                c0 = hf * (WIN_RES // 2)
                c1 = c0 + WIN_RES // 2
                em.dma('pool', lambda e, kc=kc, c0=c0, c1=c1: e.dma_start(out=winb[:, kc, c0:c1], in_=w_in_v[:, kc, c0:c1]),
                       w=[('winb', kc)])
        WINK = [('winb', kc) for kc in range(8)]
        em.dma('pool', lambda e: e.dma_start(out=poolwb[:], in_=pool_w.rearrange("g c d -> c g d")), w=['poolwb'])

        kctf, kscb = 'ctf', 'scb'
        em.dma('sp', lambda e: e.dma_start(out=ctf_t[:, 0:16], in_=cT.rearrange("p c b -> p (c b)")), w=[kctf])
        em.op('act', lambda e: e.activation(out=scb_t[:, 0:16], in_=ctf_t[:, 0:16], func=AF.Silu), r=[kctf], w=[kscb])
        scb = scb_t[:, 0:16].rearrange("p (c b) -> p c b", b=2)

        def wpiece_load(src_ap_fn, wkey):
            i = wsi['i']
            wsi['i'] = (i + 1) % 4
            buf = wst[i]
            em.dma('pool', lambda e: src_ap_fn(e, buf), w=[('wst', i)])
            return buf, ('wst', i)

        w_ada_v = w_ada.rearrange("(c p) n -> p c n", p=128)
        for nt in range(12):
            buf, kb = wpiece_load(lambda e, buf, nt=nt: e.dma_start(
                out=buf[:].rearrange("p (c n) -> p c n", n=512), in_=w_ada_v[:, :, nt * 512:(nt + 1) * 512]), None)
            bt, kbt = r2k.get()
            em.dma('sp', lambda e, bt=bt, nt=nt: e.dma_start(out=bt[0:2, :], in_=b_ada[0:1, nt * 512:(nt + 1) * 512].partition_broadcast(2)),
                   w=[kbt])
            pf, pb, kp = ps()
            bv = buf[:].rearrange("p (c n) -> p c n", n=512)
            em.mm([lambda e, kc=kc, pf=pf, bv=bv: e.matmul(pf[0:2, :], lhsT=scb[:, kc, :], rhs=bv[:, kc, :], start=(kc == 0), stop=(kc == 7))
                   for kc in range(8)], r=[kscb, kb], w=[kp])
            mr, kmr = r2k.get()
            em.op('dve', lambda e, mr=mr, pf=pf, bt=bt: e.tensor_tensor(out=mr[0:2, :], in0=pf[0:2, :], in1=bt[0:2, :], op=ALU.add),
                  r=[kp, kbt], w=[kmr])
            em.dma('sp', lambda e, mr=mr, nt=nt: e.dma_start(out=modD[:, nt * 512:(nt + 1) * 512], in_=mr[0:2, :]), r=[kmr], w=[('modD', nt)])

        w_la_v = w_la.rearrange("(c p) n -> p c n", p=128)
        w_lb_v = w_lb.rearrange("(c p) n -> p c n", p=128)
        w_out_v = w_out.rearrange("(c p) n -> p c n", p=128)
        piece_src = []
        for i in range(4):
            c0 = WIN_RES + i * 512
            piece_src.append((w_in_v[:, :, c0:c0 + 512], 512))
        piece_src.append((w_la_v, 1024))
        piece_src.append((w_lb_v, 1024))
        piece_src.append((w_out_v[:, :, 0:512], 512))
        piece_src.append((w_out_v[:, :, 512:1024], 512))
        for pi, (src_, n_) in enumerate(piece_src):
            buf, kb = wpiece_load(lambda e, buf, src_=src_, n_=n_: e.dma_start(out=buf[:].rearrange("p (c n) -> p c n", n=n_), in_=src_), None)
            em.dma('sp', lambda e, buf=buf, pi=pi: e.dma_start(out=wsD[pi], in_=buf[:]), r=[kb], w=[('wsD', pi)])

        def wpiece(pi, i):
            buf = wst[i]
            em.dma('sp', lambda e: e.dma_start(out=buf[:], in_=wsD[pi]), r=[('wsD', pi)], w=[('wst', i)])
            return buf, ('wst', i)

        MODK = [('modD', nt) for nt in range(12)]

        def load_seq_mod(seq):
            sh1 = modD[seq, 0:1024].rearrange("(c p) -> p c", p=128)
            sc1 = modD[seq, 1024:2048].rearrange("(c p) -> p c", p=128)
            tmp, kt = sm(8)
            em.dma('sp', lambda e: e.dma_start(out=modp[:, seq, 1, :], in_=sh1, allow_slow_non_contiguous=True), r=MODK, w=[('modp', seq, 1)])
            em.dma('sp', lambda e: e.dma_start(out=tmp, in_=sc1, allow_slow_non_contiguous=True), r=MODK, w=[kt])
            em.op('dve', lambda e: e.scalar_tensor_tensor(out=modp[:, seq, 0, :], in0=tmp, scalar=1.0, in1=n1g[:], op0=ALU.add, op1=ALU.mult),
                  r=[kt] + ck(n1g), w=[('modp', seq, 0)])
            em.dma('sp', lambda e: e.dma_start(out=GT1b[:], in_=modD[seq:seq + 1, 2048:3072].partition_broadcast(128)), r=MODK, w=['GT1b'])
            em.dma('sp', lambda e: e.dma_start(out=SH2b[:], in_=modD[seq:seq + 1, 3072:4096].partition_broadcast(128)), r=MODK, w=['SH2b'])
            t4, k4 = r4k.get()
            em.dma('sp', lambda e: e.dma_start(out=t4, in_=modD[seq:seq + 1, 4096:5120].partition_broadcast(128)), r=MODK, w=[k4])
            n2, kn2 = r4k.get()
            em.dma('sp', lambda e: e.dma_start(out=n2, in_=p_n2g), w=[kn2])
            em.op('dve', lambda e: e.scalar_tensor_tensor(out=G2b[:], in0=t4, scalar=1.0, in1=n2, op0=ALU.add, op1=ALU.mult),
                  r=[k4, kn2], w=['G2b'])

        def rstd_from_ss(ss_ap, kss, scale):
            l1, kl1 = sm(1)
            em.op('act', lambda e: e.activation(out=l1, in_=ss_ap, func=AF.Ln, bias=epsc[:, 0:1], scale=scale), r=[kss, 'epsc'], w=[kl1])
            r1, kr1 = sm(1)
            em.op('act', lambda e: e.activation(out=r1, in_=l1, func=AF.Exp, scale=-0.5), r=[kl1], w=[kr1])
            return r1, kr1


        def proj_fm(col0, ncols=128):
            pf, pb, kp = ps()
            em.mm([lambda e, kc=kc, pf=pf: e.matmul(pf[0:ncols, :], lhsT=winb[:, kc, col0:col0 + ncols], rhs=xnT[:, kc, :],
                                                     start=(kc == 0), stop=(kc == 7)) for kc in range(8)],
                  r=WINK + ['xnT'], w=[kp])
            return pf, kp

        dbg_state = {'off': 0}

        def dump(ap_f32_128xN, keys, n):
            if dbg_t is None:
                return
            o = dbg_state['off']
            dbg_state['off'] = o + n
            em.dma('sp', lambda e: e.dma_start(out=dbg_t[:, o:o + n], in_=ap_f32_128xN), r=keys, w=[('dbg', o)])

        def dump_any(ap, keys, n):
            if dbg_t is None:
                return
            t, kt = r2k.get()
            em.op('dve', lambda e: e.tensor_copy(out=t[:, 0:n], in_=ap), r=keys, w=[kt])
            dump(t[:, 0:n], [kt], n)

        try:
            for b in range(NBLK):
                seq, blk = divmod(b, NBLK // 2)
                t0 = b * BLK
                first = (blk == 0)
                if first:
                    load_seq_mod(seq)
                    em.op('pool', lambda e: e.memset(S[:], 0.0), w=['S'])
                    em.op('pool', lambda e: e.memset(Sb[:], 0.0), w=['Sb'])
                    em.op('pool', lambda e: e.memset(halo[:], 0.0), w=[('halo', ct_) for ct_ in range(12)])
                    em.op('pool', lambda e: e.memset(puT[:, :, 0:16], 0.0), w=[('puT', g_) for g_ in range(4)])
                for j in range(4):
                    xin, kx = r4k.get()
                    em.dma('sp', lambda e, xin=xin, j=j: e.dma_start(out=xin, in_=x[t0 + j * 128:t0 + (j + 1) * 128, :]), w=[kx])
                    junk, kj = r2k.get(BF16)
                    ss, kss = sm(1)
                    em.op('act', lambda e, xin=xin, junk=junk, ss=ss: e.activation(out=junk, in_=xin, func=AF.Square, accum_out=ss), r=[kx], w=[kj, kss])
                    rs, krs = rstd_from_ss(ss, kss, 1.0 / D)
                    xsb, kxs = r2k.get(BF16)
                    em.op('dve', lambda e, xsb=xsb, xin=xin, rs=rs: e.tensor_scalar(out=xsb, in0=xin, scalar1=rs, scalar2=None, op0=ALU.mult),
                          r=[kx, krs], w=[kxs])
                    pf, pb, kp = ps()
                    em.mm([lambda e, kc=kc, pb=pb, xsb=xsb: e.transpose(pb[:, kc * 128:(kc + 1) * 128], xsb[:, kc * 128:(kc + 1) * 128], identb[:])
                           for kc in range(8)], r=[kxs] + ck(identb), w=[kp])
                    for kc in range(8):
                        em.op('act', lambda e, kc=kc, pb=pb, j=j: e.activation(
                            out=xnT[:, kc, j * 128:(j + 1) * 128], in_=pb[:, kc * 128:(kc + 1) * 128], func=AF.Identity,
                            scale=modp[:, seq, 0, kc:kc + 1], bias=modp[:, seq, 1, kc:kc + 1]),
                            r=[kp, ('modp', seq, 0), ('modp', seq, 1)], w=['xnT'])
                if dbg_t is not None and b == 0 and 'xnT' in DBGSEL:
                    for kc in range(8):
                        dump_any(xnT[:, kc, :], ['xnT'], 512)

                if b == 0:
                    chk('A')
                def st_C(ct):
                    pf, kp = proj_fm(ct * 128)
                    pre, kpre = r4k.get()
                    em.op('pool', lambda e: e.tensor_copy(out=pre[:, 0:4], in_=halo[:, ct, :]), r=[('halo', ct)], w=[kpre])
                    em.op('act', lambda e: e.activation(out=pre[:, 4:516], in_=pf, func=AF.Copy), r=[kp], w=[kpre])
                    em.op('pool', lambda e: e.tensor_copy(out=halo[:, ct, :], in_=pre[:, 512:516]), r=[kpre], w=[('halo', ct)])
                    acc, kacc = r2k.get()
                    em.op('act', lambda e: e.activation(out=acc, in_=pf, func=AF.Copy, scale=convw[:, ct, 3:4]), r=[kp] + ck(convw), w=[kacc])
                    return dict(ct=ct, pre=pre, kpre=kpre, acc=acc, kacc=kacc)

                def st_M(st):
                    ct, pre, kpre, acc, kacc = st['ct'], st['pre'], st['kpre'], st['acc'], st['kacc']
                    for tap in (2, 1, 0):
                        sh = 3 - tap
                        em.op('dve', lambda e, tap=tap, sh=sh: e.scalar_tensor_tensor(
                            out=acc, in0=pre[:, 4 - sh:516 - sh], scalar=convw[:, ct, tap:tap + 1], in1=acc, op0=ALU.mult, op1=ALU.add),
                            r=[kpre, kacc] + ck(convw), w=[kacc])

                def st_S(st):
                    ct, acc, kacc = st['ct'], st['acc'], st['kacc']
                    h = ct % 4
                    if ct >= 8:
                        em.op('act', lambda e: e.activation(out=vT[:, h, :], in_=acc, func=AF.Silu), r=[kacc], w=['vT'])
                        return
                    em.op('act', lambda e: e.activation(out=acc, in_=acc, func=AF.Silu), r=[kacc], w=[kacc])
                    i_ = r2k.i
                    sqb, ksq = r2k.get(BF16)
                    st['sqb'], st['ksq'], st['sqf'] = sqb, ksq, r2k.t[i_][:]
                    em.op('act', lambda e: e.activation(out=sqb[:, 0:512], in_=acc, func=AF.Square), r=[kacc], w=[ksq])

                def st_N(st):
                    if st['ct'] >= 8:
                        return
                    sqb, ksq = st['sqb'], st['ksq']
                    pf2, pb2, kp2 = ps()
                    st['pf2'], st['kp2'] = pf2, kp2
                    em.mm([lambda e: e.matmul(pf2, lhsT=onesb[:], rhs=sqb[:, 0:512], start=True, stop=True)], r=[ksq] + ck(onesb), w=[kp2])

                def st_L(st):
                    if st['ct'] >= 8:
                        return
                    lnv, ksq, pf2, kp2 = st['sqf'], st['ksq'], st['pf2'], st['kp2']
                    em.op('act', lambda e: e.activation(out=lnv, in_=pf2, func=AF.Ln, bias=epsc[:, 0:1]), r=[kp2, 'epsc'], w=[ksq])
                    em.op('act', lambda e: e.activation(out=lnv, in_=lnv, func=AF.Exp, scale=-0.5), r=[ksq], w=[ksq])

                def st_F(st):
                    ct = st['ct']
                    if ct >= 8:
                        return
                    h = ct % 4
                    acc, kacc, rinv, ksq = st['acc'], st['kacc'], st['sqf'], st['ksq']
                    qs = (128.0 ** -0.5) if ct < 4 else 1.0
                    dst = qT if ct < 4 else kT
                    dk = 'qT' if ct < 4 else 'kT'
                    em.op('dve', lambda e: e.scalar_tensor_tensor(out=dst[:, h, :], in0=acc, scalar=qs, in1=rinv, op0=ALU.mult, op1=ALU.mult),
                          r=[kacc, ksq], w=[dk])

                prev_pair = None
                for pr in ((0, 1), (2, 3), (4, 5), (6, 7), (8, 9), (10, 11)):
                    cur_pair = [st_C(ct) for ct in pr]
                    for st in cur_pair:
                        st_M(st)
                    if prev_pair is not None:
                        for fn_ in (st_S, st_N, st_L, st_F):
                            for st in prev_pair:
                                fn_(st)
                    prev_pair = cur_pair
                for fn_ in (st_S, st_N, st_L, st_F):
                    for st in prev_pair:
                        fn_(st)
                if b == 0 and dbg_t is not None and 'B' in DBGSEL:
                    for h_ in range(4):
                        dump_any(qT[:, h_, :], ['qT'], 512)
                    for h_ in range(4):
                        dump_any(kT[:, h_, :], ['kT'], 512)
                    for h_ in range(4):
                        dump_any(vT[:, h_, :], ['vT'], 512)
                if b == 0:
                    chk('B')
                for h in range(4):
                    pf, kp = proj_fm(1536 + h * 128)
                    em.op('act', lambda e, pf=pf, h=h: e.activation(out=szT[:, h, :], in_=pf, func=AF.Silu), r=[kp], w=['szT'])
                for c in range(4):
                    pf, pb, kp = ps()
                    em.mm([lambda e, kc=kc, pf=pf, c=c: e.matmul(pf[:, 0:8], lhsT=xnT[:, kc, c * 128:(c + 1) * 128], rhs=winb[:, kc, 2048:2056],
                                                                  start=(kc == 0), stop=(kc == 7)) for kc in range(8)],
                          r=WINK + ['xnT'], w=[kp])
                    em.op('act', lambda e, pf=pf, c=c: e.activation(out=gtm[:, c, 4:8], in_=pf[:, 4:8], func=AF.Sigmoid), r=[kp], w=[('gtm', c)])
                    xa, kxa = sm(4)
                    em.op('dve', lambda e, xa=xa, pf=pf: e.tensor_tensor(out=xa, in0=pf[:, 0:4], in1=dtb[:], op=ALU.add), r=[kp] + ck(dtb), w=[kxa])
                    ab, kab = sm(4)
                    em.op('act', lambda e, ab=ab, xa=xa: e.activation(out=ab, in_=xa, func=AF.Abs), r=[kxa], w=[kab])
                    ex, kex = sm(4)
                    em.op('act', lambda e, ex=ex, ab=ab: e.activation(out=ex, in_=ab, func=AF.Exp, scale=-1.0), r=[kab], w=[kex])
                    l1p, kl1p = sm(4)
                    em.op('act', lambda e, l1p=l1p, ex=ex: e.activation(out=l1p, in_=ex, func=AF.Ln, bias=epsc[:, 2:3]), r=[kex, 'epsc'], w=[kl1p])
                    sp_, ksp = sm(4)
                    em.op('dve', lambda e, sp_=sp_, xa=xa, l1p=l1p: e.scalar_tensor_tensor(out=sp_, in0=xa, scalar=0.0, in1=l1p, op0=ALU.max, op1=ALU.add),
                          r=[kxa, kl1p], w=[ksp])
                    ea, kea = sm(4)
                    em.op('act', lambda e, ea=ea: e.activation(out=ea, in_=alog[:], func=AF.Exp), r=ck(alog), w=[kea])
                    em.op('dve', lambda e, c=c, sp_=sp_, ea=ea: e.scalar_tensor_tensor(out=gtm[:, c, 0:4], in0=sp_, scalar=-1.0, in1=ea, op0=ALU.mult, op1=ALU.mult),
                          r=[ksp, kea], w=[('gtm', c)])

                if b == 0 and dbg_t is not None and 'B2' in DBGSEL:
                    dump(gtm[:].rearrange('p a b -> p (a b)'), [('gtm', c_) for c_ in range(4)], 32)
                if b == 0:
                    chk('B2')
                def pc_H(gi, win):
                    pf, kp = proj_fm(2056 + gi * 128)
                    em.op('act', lambda e: e.activation(out=puT[:, gi, 16:16 + BLK], in_=pf, func=AF.Copy), r=[kp], w=[('puT', gi)])
                    cur = puT[:, gi, :]
                    kcur = ('puT', gi)
                    slots = [r4k.get()]
                    if win > 2:
                        slots.append(r4k.get())
                    w_ = 1
                    k_ = 0
                    while w_ < win:
                        nxt, knxt = slots[k_ % len(slots)]
                        em.op('pool', lambda e, nxt=nxt, cur=cur, w_=w_: e.tensor_tensor(out=nxt[:, w_:16 + BLK], in0=cur[:, w_:16 + BLK], in1=cur[:, 0:16 + BLK - w_], op=ALU.add),
                              r=[kcur], w=[knxt])
                        cur, kcur = nxt, knxt
                        w_ *= 2
                        k_ += 1
                    return dict(gi=gi, win=win, cur=cur, kcur=kcur)

                def pc_T(st):
                    gi, win, cur, kcur = st['gi'], st['win'], st['cur'], st['kcur']
                    plb, kplb = r2k.get(BF16)
                    em.op('dve', lambda e: e.scalar_tensor_tensor(
                        out=plb[:, 0:BLK], in0=cur[:, 16:16 + BLK], scalar=1.0 / win, in1=puT[:, gi, 16:16 + BLK], op0=ALU.mult, op1=ALU.subtract),
                        r=[kcur, ('puT', gi)], w=[kplb])
                    if first:
                        t15, k15 = sm(16)
                        em.op('dve', lambda e: e.tensor_tensor(out=t15, in0=cur[:, 16:32], in1=pcorr[:, gi, :], op=ALU.mult), r=[kcur] + ck(pcorr), w=[k15])
                        em.op('dve', lambda e: e.tensor_tensor(out=plb[:, 0:16], in0=t15, in1=puT[:, gi, 16:32], op=ALU.subtract), r=[k15, ('puT', gi)], w=[kplb])
                    pf2, pb2, kp2 = ps()
                    em.mm([lambda e: e.matmul(pf2, lhsT=poolwb[:, gi, :], rhs=plb[:, 0:BLK], start=True, stop=True)], r=[kplb, 'poolwb'], w=[kp2])
                    em.op('act', lambda e: e.activation(out=ybT[:, gi, :], in_=pf2, func=AF.Copy, scale=pscale[:, gi:gi + 1]), r=[kp2] + ck(pscale), w=['ybT'])
                    em.op('pool', lambda e: e.tensor_copy(out=puT[:, gi, 0:16], in_=puT[:, gi, BLK:BLK + 16]), r=[('puT', gi), kplb], w=[('puT', gi)])

                wins = (2, 4, 8, 16)
                pcs = [pc_H(0, wins[0]), pc_H(1, wins[1])]
                pc_T(pcs[0])
                pcs.append(pc_H(2, wins[2]))
                pc_T(pcs[1])
                pcs.append(pc_H(3, wins[3]))
                pc_T(pcs[2])
                pc_T(pcs[3])

                if b == 0:
                    chk('C')
                for c in range(4):
                    tsl = slice(c * 128, (c + 1) * 128)
                    g4 = gtm[:, c, 0:4]
                    b4 = gtm[:, c, 4:8]
                    kg = ('gtm', c)
                    pkf, pkb, kpk = ps()
                    em.mm([lambda e, h=h, pkb=pkb: e.transpose(pkb[:, h * 128:(h + 1) * 128], kT[:, h, tsl], identb[:]) for h in range(4)],
                          r=['kT'] + ck(identb), w=[kpk])
                    pvf, pvb, kpv = ps()
                    em.mm([lambda e, h=h, pvb=pvb: e.transpose(pvb[:, h * 128:(h + 1) * 128], vT[:, h, tsl], identb[:]) for h in range(4)],
                          r=['vT'] + ck(identb), w=[kpv])
                    Gt, kGt = r2k.get()
                    for h in range(4):
                        em.op('pool', lambda e, h=h, Gt=Gt: e.tensor_scalar(out=Gt[:, h * 128:(h + 1) * 128], in0=triU[:], scalar1=g4[:, h:h + 1], scalar2=1.0,
                                                                            op0=ALU.mult, op1=ALU.mult), r=[kg] + ck(triU), w=[kGt])
                    pgr, _, kpgr = ps()
                    em.mm([lambda e, pgr=pgr, Gt=Gt: e.matmul(pgr, lhsT=onesf[:], rhs=Gt, start=True, stop=True)], r=[kGt] + ck(onesf), w=[kpgr])
                    pgc, _, kpgc = ps()
                    em.mm([lambda e, pgc=pgc: e.matmul(pgc[:, 0:4], lhsT=triU[:], rhs=g4, start=True, stop=True)], r=[kg] + ck(triU), w=[kpgc])
                    pgl, _, kpgl = ps()
                    em.mm([lambda e, pgl=pgl: e.matmul(pgl[:, 0:4], lhsT=onesf[:], rhs=g4, start=True, stop=True)], r=[kg] + ck(onesf), w=[kpgl])
                    gcc8, kgcc = sm(8)
                    em.op('act', lambda e, gcc8=gcc8, pgc=pgc: e.activation(out=gcc8[:, 0:4], in_=pgc[:, 0:4], func=AF.Copy), r=[kpgc], w=[kgcc])
                    em.op('act', lambda e, gcc8=gcc8, pgl=pgl: e.activation(out=gcc8[:, 4:8], in_=pgl[:, 0:4], func=AF.Copy), r=[kpgl, kgcc], w=[kgcc])
                    gcc = gcc8[:, 0:4]
                    glv = gcc8[:, 4:8]
                    DTl, kDTl = r2k.get()
                    Dsl, kDsl = r2k.get()
                    for h in range(4):
                        hs = slice(h * 128, (h + 1) * 128)
                        em.op('dve', lambda e, hs=hs, h=h, DTl=DTl, pgr=pgr, gcc=gcc: e.scalar_tensor_tensor(
                            out=DTl[:, hs], in0=pgr[:, hs], scalar=gcc[:, h:h + 1], in1=maskT[:], op0=ALU.subtract, op1=ALU.add),
                            r=[kpgr, kgcc] + ck(maskT), w=[kDTl])
                        em.op('dve', lambda e, hs=hs, h=h, Dsl=Dsl, pgr=pgr, gcc=gcc: e.scalar_tensor_tensor(
                            out=Dsl[:, hs], in0=pgr[:, hs], scalar=gcc[:, h:h + 1], in1=maskS[:], op0=ALU.subtract, op1=ALU.add),
                            r=[kpgr, kgcc] + ck(maskS), w=[kDsl])
                    DT, Ds, Er = cq['DT'], cq['Ds'], cq['Er']
                    fl = lambda t: t[:].rearrange("p h n -> p (h n)")
                    em.op('act', lambda e: e.activation(out=fl(DT), in_=DTl, func=AF.Exp), r=[kDTl], w=['DT'])
                    em.op('act', lambda e: e.activation(out=fl(Ds), in_=Dsl, func=AF.Exp, scale=-1.0), r=[kDsl], w=['Ds'])
                    em.op('act', lambda e, pgr=pgr: e.activation(out=fl(Er), in_=pgr, func=AF.Exp), r=[kpgr], w=['Er'])
                    if b == 0 and c == 0:
                        chk('D1')
                    egc, kegc = sm(4)
                    em.op('act', lambda e, egc=egc, gcc=gcc: e.activation(out=egc, in_=gcc, func=AF.Exp), r=[kgcc], w=[kegc])
                    kbs, kkbs = sm(4)
                    em.op('dve', lambda e, kbs=kbs, egc=egc: e.tensor_tensor(out=kbs, in0=egc, in1=b4, op=ALU.mult), r=[kegc, kg], w=[kkbs])
                    dl, kdl = sm(4)
                    em.op('dve', lambda e, dl=dl, glv=glv, gcc=gcc: e.tensor_tensor(out=dl, in0=glv, in1=gcc, op=ALU.subtract), r=[kgcc], w=[kdl])
                    ekd, kekd = sm(4)
                    em.op('act', lambda e, ekd=ekd, dl=dl: e.activation(out=ekd, in_=dl, func=AF.Exp), r=[kdl], w=[kekd])
                    egl, kegl = sm(4)
                    em.op('act', lambda e, egl=egl, glv=glv: e.activation(out=egl, in_=glv, func=AF.Exp), r=[kgcc], w=[kegl])
                    nb4, knb4 = sm(4)
                    em.op('dve', lambda e, nb4=nb4: e.tensor_scalar(out=nb4, in0=b4, scalar1=-1.0, scalar2=None, op0=ALU.mult), r=[kg], w=[knb4])
                    if b == 0 and c == 0:
                        chk('D1b')
                    kbd, kdec, vb = cq['kbd'], cq['kdec'], cq['vb']
                    for h in range(4):
                        hs = slice(h * 128, (h + 1) * 128)
                        em.op('act', lambda e, h=h, hs=hs, pkb=pkb, kbs=kbs: e.activation(out=kbd[:, h, :], in_=pkb[:, hs], func=AF.Identity, scale=kbs[:, h:h + 1], bias=epsc[:, 1:2]),
                              r=[kpk, kkbs], w=['kbd'])
                        em.op('act', lambda e, h=h, hs=hs, pkb=pkb, ekd=ekd: e.activation(out=kdec[:, h, :], in_=pkb[:, hs], func=AF.Identity, scale=ekd[:, h:h + 1], bias=epsc[:, 1:2]),
                              r=[kpk, kekd], w=['kdec'])
                        em.op('act', lambda e, h=h, hs=hs, pvb=pvb: e.activation(out=vb[:, h, :], in_=pvb[:, hs], func=AF.Identity, scale=b4[:, h:h + 1], bias=epsc[:, 1:2]),
                              r=[kpv, kg], w=['vb'])
                    if b == 0 and c == 0:
                        chk('D2')
                    pkk, _, kpkk = ps()
                    em.mm([lambda e, h=h, pkk=pkk: e.matmul(pkk[:, h * 128:(h + 1) * 128], lhsT=kT[:, h, tsl], rhs=kT[:, h, tsl], start=True, stop=True) for h in range(4)],
                          r=['kT'], w=[kpkk])
                    N0 = cq['N0']
                    for h in range(4):
                        hs = slice(h * 128, (h + 1) * 128)
                        em.op('dve', lambda e, h=h, hs=hs, pkk=pkk, nb4=nb4: e.scalar_tensor_tensor(
                            out=N0[:, h, :], in0=pkk[:, hs], scalar=nb4[:, h:h + 1], in1=Ds[:, h, :], op0=ALU.mult, op1=ALU.mult),
                            r=[kpkk, knb4, 'Ds'], w=['N0'])
                    ptf, ptb, kpt = ps()
                    em.mm([lambda e, h=h, ptb=ptb: e.transpose(ptb[:, h * 128:(h + 1) * 128], N0[:, h, :], identb[:]) for h in range(4)],
                          r=['N0'] + ck(identb), w=[kpt])
                    Pt0 = cq['Pt0']
                    em.op('act', lambda e, ptb=ptb: e.activation(out=fl(Pt0), in_=ptb[:, 0:512], func=AF.Identity, scale=1.0, bias=epsc[:, 1:2]), r=[kpt, 'epsc'], w=['Pt0'])
                    Tt, kTt = nq.get(BF16)
                    if b == 0 and c == 0 and dbg_t is not None and 'D3' in DBGSEL:
                        dump_any(fl(DT), ['DT'], 512)
                        dump_any(fl(Ds), ['Ds'], 512)
                        dump_any(fl(N0), ['N0'], 512)
                    if b == 0 and c == 0:
                        chk('D3')
                    Tq, kTq = nq.get(BF16)
                    Cm, kCm = nq.get(BF16)
                    for h in range(4):
                        hs = slice(h * 128, (h + 1) * 128)
                        em.op('dve', lambda e, h=h, hs=hs, Cm=Cm: e.tensor_tensor(out=Cm[:, hs], in0=N0[:, h, :], in1=bmask[:, 0, :], op=ALU.mult), r=['N0'] + ck(bmask), w=[kCm])
                    em.op('dve', lambda e, Tq=Tq, Cm=Cm: e.tensor_tensor(out=Tq, in0=Cm, in1=identq[:], op=ALU.add), r=[kCm] + ck(identq), w=[kTq])
                    Cmt, kCmt = nq.get(BF16)
                    for h in range(4):
                        hs = slice(h * 128, (h + 1) * 128)
                        em.op('dve', lambda e, h=h, hs=hs, Cmt=Cmt: e.tensor_tensor(out=Cmt[:, hs], in0=Pt0[:, h, :], in1=bmaskT[:, 0, :], op=ALU.mult), r=['Pt0'] + ck(bmaskT), w=[kCmt])
                    em.op('dve', lambda e, Tt=Tt, Cmt=Cmt: e.tensor_tensor(out=Tt, in0=Cmt, in1=identq[:], op=ALU.add), r=[kCmt, kTt] + ck(identq), w=[kTt])
                    if b == 0 and c == 0 and dbg_t is not None and 'D3b' in DBGSEL:
                        dump_any(Cm, [kCm], 512)
                        dump_any(Tq, [kTq], 512)
                        dump_any(Tt, [kTt], 512)
                        dump_any(bmask[:, :, :].rearrange('p a b -> p (a b)')[:, 0:512], ck(bmask), 512)
                    if b == 0 and c == 0:
                        chk('D3b')
                    def masks(lev_):
                        Cm_, kCm_ = nq.get(BF16)
                        Cmt_, kCmt_ = nq.get(BF16)
                        for h in range(4):
                            hs = slice(h * 128, (h + 1) * 128)
                            em.op('dve', lambda e, h=h, hs=hs: e.tensor_tensor(out=Cm_[:, hs], in0=N0[:, h, :], in1=bmask[:, lev_, :], op=ALU.mult), r=['N0'] + ck(bmask), w=[kCm_])
                            if lev_ < 6:
                                em.op('dve', lambda e, h=h, hs=hs: e.tensor_tensor(out=Cmt_[:, hs], in0=Pt0[:, h, :], in1=bmaskT[:, lev_, :], op=ALU.mult), r=['Pt0'] + ck(bmaskT), w=[kCmt_])
                        return Cm_, kCm_, Cmt_, kCmt_

                    nxt_masks = masks(1)
                    for lev in range(1, 7):
                        Cm, kCm, Cmt, kCmt = nxt_masks
                        last = (lev == 6)
                        px2, _, kpx2 = ps()
                        em.mm([lambda e, h=h, px2=px2, Cm=Cm, Tt=Tt: e.matmul(px2[:, h * 128:(h + 1) * 128], lhsT=Cm[:, h * 128:(h + 1) * 128], rhs=Tt[:, h * 128:(h + 1) * 128], start=True, stop=True)
                               for h in range(4)], r=[kCm, kTt], w=[kpx2])
                        if lev < 6:
                            nxt_masks = masks(lev + 1)
                        Xs2, kXs2 = nq.get(BF16)
                        em.op('act', lambda e, Xs2=Xs2, px2=px2: e.activation(out=Xs2, in_=px2, func=AF.Copy), r=[kpx2], w=[kXs2])
                        if not last:
                            px1, _, kpx1 = ps()
                            em.mm([lambda e, h=h, px1=px1, Cmt=Cmt, Tq=Tq: e.matmul(px1[:, h * 128:(h + 1) * 128], lhsT=Cmt[:, h * 128:(h + 1) * 128], rhs=Tq[:, h * 128:(h + 1) * 128], start=True, stop=True)
                                   for h in range(4)], r=[kCmt, kTq], w=[kpx1])
                            Xs1, kXs1 = nq.get(BF16)
                            em.op('act', lambda e, Xs1=Xs1, px1=px1: e.activation(out=Xs1, in_=px1, func=AF.Copy), r=[kpx1], w=[kXs1])
                        py2, _, kpy2 = ps()
                        em.mm([lambda e, h=h, py2=py2, Tq=Tq, Xs2=Xs2: e.matmul(py2[:, h * 128:(h + 1) * 128], lhsT=Tq[:, h * 128:(h + 1) * 128], rhs=Xs2[:, h * 128:(h + 1) * 128], start=True, stop=True)
                               for h in range(4)], r=[kTq, kXs2], w=[kpy2])
                        if not last:
                            py1, _, kpy1 = ps()
                            em.mm([lambda e, h=h, py1=py1, Tt=Tt, Xs1=Xs1: e.matmul(py1[:, h * 128:(h + 1) * 128], lhsT=Tt[:, h * 128:(h + 1) * 128], rhs=Xs1[:, h * 128:(h + 1) * 128], start=True, stop=True)
                                   for h in range(4)], r=[kTt, kXs1], w=[kpy1])
                        Ttn, kTtn = nq.get(BF16)
                        em.op('dve', lambda e, Ttn=Ttn, py2=py2, Tt=Tt: e.tensor_tensor(out=Ttn, in0=py2, in1=Tt, op=ALU.add), r=[kpy2, kTt], w=[kTtn])
                        if not last:
                            Tqn, kTqn = nq.get(BF16)
                            em.op('dve', lambda e, Tqn=Tqn, py1=py1, Tq=Tq: e.tensor_tensor(out=Tqn, in0=py1, in1=Tq, op=ALU.add), r=[kpy1, kTq], w=[kTqn])
                            Tq, kTq = Tqn, kTqn
                        Tt, kTt = Ttn, kTtn
                    if b == 0 and c == 0:
                        chk('D4')
                    pw, _, kpw = ps()
                    em.mm([lambda e, h=h, pw=pw, Tt=Tt: e.matmul(pw[:, h * 128:(h + 1) * 128], lhsT=kbd[:, h, :], rhs=Tt[:, h * 128:(h + 1) * 128], start=True, stop=True)
                           for h in range(4)], r=['kbd', kTt], w=[kpw])
                    nwT, knwT = r2k.get(BF16)
                    em.op('act', lambda e, nwT=nwT, pw=pw: e.activation(out=nwT[:, 0:512], in_=pw, func=AF.Copy, scale=-1.0), r=[kpw], w=[knwT])
                    pvn, _, kpvn = ps()
                    fns = []
                    for h in range(4):
                        hs = slice(h * 128, (h + 1) * 128)
                        fns.append(lambda e, h=h, hs=hs, pvn=pvn, Tt=Tt: e.matmul(pvn[:, hs], lhsT=Tt[:, hs], rhs=vb[:, h, :], start=True, stop=False))
                        fns.append(lambda e, h=h, hs=hs, pvn=pvn, nwT=nwT: e.matmul(pvn[:, hs], lhsT=nwT[:, hs], rhs=Sb[:, h, :], start=False, stop=True))
                    em.mm(fns, r=[kTt, 'vb', knwT, 'Sb'], w=[kpvn])
                    vnew, kvnew = r2k.get(BF16)
                    em.op('act', lambda e, vnew=vnew, pvn=pvn: e.activation(out=vnew[:, 0:512], in_=pvn, func=AF.Copy), r=[kpvn], w=[kvnew])
                    if b == 0 and c == 0:
                        chk('D5')
                    pqk, _, kpqk = ps()
                    em.mm([lambda e, h=h, pqk=pqk: e.matmul(pqk[:, h * 128:(h + 1) * 128], lhsT=kT[:, h, tsl], rhs=qT[:, h, tsl], start=True, stop=True) for h in range(4)],
                          r=['kT', 'qT'], w=[kpqk])
                    attnT, kat = r2k.get(BF16)
                    em.op('dve', lambda e, attnT=attnT, pqk=pqk: e.tensor_tensor(out=attnT[:, 0:512], in0=pqk, in1=fl(DT), op=ALU.mult), r=[kpqk, 'DT'], w=[kat])
                    qdT, kqd = r2k.get(BF16)
                    em.op('dve', lambda e, qdT=qdT: e.tensor_tensor(out=qdT[:, 0:512].rearrange("p (h n) -> p h n", n=128), in0=qT[:, :, tsl], in1=Er[:], op=ALU.mult),
                          r=['qT', 'Er'], w=[kqd])
                    po, _, kpo = ps()
                    fns = []
                    for h in range(4):
                        hs = slice(h * 128, (h + 1) * 128)
                        fns.append(lambda e, h=h, hs=hs, po=po, qdT=qdT: e.matmul(po[:, hs], lhsT=Sb[:, h, :], rhs=qdT[:, hs], start=True, stop=False))
                        fns.append(lambda e, h=h, hs=hs, po=po, vnew=vnew, attnT=attnT: e.matmul(po[:, hs], lhsT=vnew[:, hs], rhs=attnT[:, hs], start=False, stop=True))
                    em.mm(fns, r=['Sb', kqd, kvnew, kat], w=[kpo])
                    osq, kosq = r2k.get(BF16)
                    em.op('act', lambda e, osq=osq, po=po: e.activation(out=osq[:, 0:512], in_=po, func=AF.Square), r=[kpo], w=[kosq])
                    pss, _, kpss = ps()
                    em.mm([lambda e, pss=pss, osq=osq: e.matmul(pss, lhsT=onesb[:], rhs=osq[:, 0:512], start=True, stop=True)], r=[kosq] + ck(onesb), w=[kpss])
                    lno, klno = r2k.get()
                    em.op('act', lambda e, lno=lno, pss=pss: e.activation(out=lno, in_=pss, func=AF.Ln, bias=epsc[:, 0:1], scale=1.0 / 128), r=[kpss, 'epsc'], w=[klno])
                    rso, krso = r2k.get()
                    em.op('act', lambda e, rso=rso, lno=lno: e.activation(out=rso, in_=lno, func=AF.Exp, scale=-0.5), r=[klno], w=[krso])
                    t1, kt1 = r2k.get()
                    em.op('dve', lambda e, t1=t1, po=po, rso=rso: e.tensor_tensor(out=t1, in0=po, in1=rso, op=ALU.mult), r=[kpo, krso], w=[kt1])
                    em.op('dve', lambda e, t1=t1: e.scalar_tensor_tensor(out=yaT[:, :, tsl], in0=t1.rearrange("p (h n) -> p h n", n=128), scalar=dng[:, 0:1],
                                                                          in1=szT[:, :, tsl], op0=ALU.mult, op1=ALU.mult),
                          r=[kt1, 'szT'] + ck(dng), w=['yaT'])
                    if b == 0 and c == 0 and dbg_t is not None and 'D6' in DBGSEL:
                        dump_any(Tt, [kTt], 512)
                        dump_any(vnew[:, 0:512], [kvnew], 512)
                        dump_any(attnT[:, 0:512], [kat], 512)
                        dump_any(po, [kpo], 512)
                        dump_any(rso, [krso], 512)
                    if b == 0 and c == 0:
                        chk('D6')
                    pst, _, kpst = ps()
                    em.mm([lambda e, h=h, pst=pst, vnew=vnew: e.matmul(pst[:, h * 128:(h + 1) * 128], lhsT=kdec[:, h, :], rhs=vnew[:, h * 128:(h + 1) * 128], start=True, stop=True)
                           for h in range(4)], r=['kdec', kvnew], w=[kpst])
                    for h in range(4):
                        hs = slice(h * 128, (h + 1) * 128)
                        em.op('dve', lambda e, h=h, hs=hs, pst=pst, egl=egl: e.scalar_tensor_tensor(
                            out=S[:, h, :], in0=S[:, h, :], scalar=egl[:, h:h + 1], in1=pst[:, hs], op0=ALU.mult, op1=ALU.add),
                            r=['S', kegl, kpst, kpo, kpvn], w=['S'])
                    em.op('act', lambda e: e.activation(out=fl(Sb), in_=fl(S), func=AF.Copy), r=['S', kpo, kpvn], w=['Sb'])
                    if b == 0 and c == 0 and dbg_t is not None and 'S1' in DBGSEL:
                        dump(fl(S), ['S'], 512)
                        dump_any(fl(Sb), ['Sb'], 512)
                        dump_any(fl(kdec), ['kdec'], 512)
                    if b == 0 and c == 0:
                        chk('S1')
                if dbg_t is not None and b == 0 and 'sz' in DBGSEL:
                    for h in range(4):
                        dump_any(szT[:, h, :], ['szT'], 512)
                if dbg_t is not None and b == 0 and 'ya' in DBGSEL:
                    for h in range(4):
                        dump_any(yaT[:, h, :], ['yaT'], 512)
                    for h in range(4):
                        dump_any(ybT[:, h, :], ['ybT'], 512)

                if b == 0:
                    chk('D')
                bufs = {}
                bufs[4] = wpiece(4, 0)
                bufs[5] = wpiece(5, 1)
                bufs[0] = wpiece(0, 2)
                bufs[2] = wpiece(2, 3)
                la_v = bufs[4][0][:].rearrange("p (c n) -> p c n", n=1024)
                lb_v = bufs[5][0][:].rearrange("p (c n) -> p c n", n=1024)
                for mt in range(8):
                    if mt == 4:
                        bufs[1] = wpiece(1, 2)
                        bufs[3] = wpiece(3, 3)
                    gbuf_a, kga = bufs[mt // 4]
                    gbuf_b, kgb = bufs[2 + mt // 4]
                    ga_v = gbuf_a[:].rearrange("p (c n) -> p c n", n=512)
                    gb_v = gbuf_b[:].rearrange("p (c n) -> p c n", n=512)
                    cs = slice((mt % 4) * 128, (mt % 4 + 1) * 128)
                    ms = slice(mt * 128, (mt + 1) * 128)
                    pga, _, kpga = ps()
                    em.mm([lambda e, kc=kc, pga=pga, ga_v=ga_v, cs=cs: e.matmul(pga, lhsT=ga_v[:, kc, cs], rhs=xnT[:, kc, :], start=(kc == 0), stop=(kc == 7)) for kc in range(8)],
                          r=[kga, 'xnT'], w=[kpga])
                    pgb, _, kpgb = ps()
                    em.mm([lambda e, kc=kc, pgb=pgb, gb_v=gb_v, cs=cs: e.matmul(pgb, lhsT=gb_v[:, kc, cs], rhs=xnT[:, kc, :], start=(kc == 0), stop=(kc == 7)) for kc in range(8)],
                          r=[kgb, 'xnT'], w=[kpgb])
                    pla, _, kpla = ps()
                    em.mm([lambda e, kc=kc, pla=pla, ms=ms: e.matmul(pla, lhsT=la_v[:, kc, ms], rhs=yaT[:, kc, :], start=(kc == 0), stop=(kc == 3)) for kc in range(4)],
                          r=[bufs[4][1], 'yaT'], w=[kpla])
                    plb_, _, kplb_ = ps()
                    em.mm([lambda e, kc=kc, plb_=plb_, ms=ms: e.matmul(plb_, lhsT=lb_v[:, kc, ms], rhs=ybT[:, kc, :], start=(kc == 0), stop=(kc == 3)) for kc in range(4)],
                          r=[bufs[5][1], 'ybT'], w=[kplb_])
                    sga, ksga = r2k.get()
                    em.op('act', lambda e, sga=sga, pga=pga: e.activation(out=sga, in_=pga, func=AF.Sigmoid), r=[kpga], w=[ksga])
                    sgb, ksgb = r2k.get()
                    em.op('act', lambda e, sgb=sgb, pgb=pgb: e.activation(out=sgb, in_=pgb, func=AF.Sigmoid), r=[kpgb], w=[ksgb])
                    ma, kma = r2k.get()
                    em.op('dve', lambda e, ma=ma, pla=pla, sga=sga: e.tensor_tensor(out=ma, in0=pla, in1=sga, op=ALU.mult), r=[kpla, ksga], w=[kma])
                    mb, kmb = r2k.get()
                    em.op('dve', lambda e, mb=mb, plb_=plb_, sgb=sgb: e.tensor_tensor(out=mb, in0=plb_, in1=sgb, op=ALU.mult), r=[kplb_, ksgb], w=[kmb])
                    em.op('pool', lambda e, mt=mt, ma=ma, mb=mb: e.tensor_tensor(out=mixedT[:, mt, :], in0=ma, in1=mb, op=ALU.add), r=[kma, kmb], w=['mixedT'])
                if b == 0 and dbg_t is not None and 'E1' in DBGSEL:
                    for h_ in range(8):
                        dump_any(mixedT[:, h_, :], ['mixedT'], 512)
                if b == 0:
                    chk('E1')
                wo = [wpiece(6, 2), wpiece(7, 3)]
                xn2bs = []
                def fetch_x(j_):
                    xin_, kx_ = r4k.get()
                    em.dma('sp', lambda e: e.dma_start(out=xin_, in_=x[t0 + j_ * 128:t0 + (j_ + 1) * 128, :]), w=[kx_])
                    return xin_, kx_

                def e2_H(j):
                    tile_idx = b * 4 + j
                    js = slice(j * 128, (j + 1) * 128)
                    xin, kx = fetch_x(j)
                    h1t, kh1 = r4k.get()
                    for hf in range(2):
                        wv = wo[hf][0][:].rearrange("p (c n) -> p c n", n=512)
                        pw_, _, kpw_ = ps()
                        em.mm([lambda e, kc=kc, pw_=pw_, wv=wv: e.matmul(pw_, lhsT=mixedT[:, kc, js], rhs=wv[:, kc, :], start=(kc == 0), stop=(kc == 7)) for kc in range(8)],
                              r=[wo[hf][1], 'mixedT'], w=[kpw_])
                        fs = slice(hf * 512, (hf + 1) * 512)
                        em.op('dve', lambda e, pw_=pw_, fs=fs: e.tensor_tensor(out=h1t[:, fs], in0=pw_, in1=GT1b[:, fs], op=ALU.mult), r=[kpw_, 'GT1b'], w=[kh1])
                        em.op('pool', lambda e, fs=fs: e.tensor_tensor(out=h1t[:, fs], in0=h1t[:, fs], in1=xin[:, fs], op=ALU.add), r=[kh1, kx], w=[kh1])
                    em.dma('sp', lambda e: e.dma_start(out=h1D[t0 + j * 128:t0 + (j + 1) * 128, :], in_=h1t), r=[kh1], w=[('h1D', tile_idx)])
                    junk, kj = r2k.get(BF16)
                    ss, kss = sm(1)
                    em.op('act', lambda e: e.activation(out=junk, in_=h1t, func=AF.Square, accum_out=ss), r=[kh1], w=[kj, kss])
                    rs, krs = rstd_from_ss(ss, kss, 1.0 / D)
                    xn2f, kxf = r4k.get()
                    em.op('dve', lambda e: e.scalar_tensor_tensor(out=xn2f, in0=h1t, scalar=rs, in1=G2b[:], op0=ALU.mult, op1=ALU.mult),
                          r=[kh1, krs, 'G2b'], w=[kxf])
                    em.op('pool', lambda e: e.tensor_tensor(out=xn2f, in0=xn2f, in1=SH2b[:], op=ALU.add), r=[kxf, 'SH2b'], w=[kxf])
                    xhost, kxb = (yaT, 'yaT') if j < 2 else (ybT, 'ybT')
                    xn2b = xhost[:].rearrange("p h n -> p (h n)")[:, (j % 2) * 1024:(j % 2 + 1) * 1024]
                    em.op('act', lambda e: e.activation(out=xn2b, in_=xn2f, func=AF.Copy), r=[kxf], w=[kxb])
                    xn2bs.append((xn2b, kxb))
                    return dict(j=j, xn2f=xn2f, kxf=kxf)

                def e2_T(st):
                    j, xn2f, kxf = st['j'], st['xn2f'], st['kxf']
                    for half in range(2):
                        ptr, _, kptr = ps()
                        em.mm([lambda e, q=q, ptr=ptr, half=half: e.transpose(ptr[:, q * 128:(q + 1) * 128], xn2f[:, (half * 4 + q) * 128:(half * 4 + q + 1) * 128], identf[:])
                               for q in range(4)], r=[kxf] + ck(identf), w=[kptr])
                        if half == 0:
                            em.op('act', lambda e, ptr=ptr, half=half: e.activation(out=xn2T[:, half * 4:half * 4 + 4, :].rearrange("p c n -> p (c n)"), in_=ptr, func=AF.Copy),
                                  r=[kptr], w=[('xn2T', half)])
                        else:
                            em.op('dve', lambda e, ptr=ptr, half=half: e.tensor_copy(out=xn2T[:, half * 4:half * 4 + 4, :].rearrange("p c n -> p (c n)"), in_=ptr),
                                  r=[kptr], w=[('xn2T', half)])
                    plg, _, kplg = ps()
                    em.mm([lambda e, kc=kc: e.matmul(plg[:, 0:36], lhsT=xn2T[:, kc, :], rhs=wr[:, kc, :], start=(kc == 0), stop=(kc == 7)) for kc in range(8)],
                          r=[('xn2T', 0), ('xn2T', 1)] + ck(wr), w=[kplg])
                    em.op('dve', lambda e: e.tensor_tensor(out=lgb[:, j, :], in0=plg[:, 0:36], in1=brb[:], op=ALU.add), r=[kplg] + ck(brb), w=['lgb'])

                e2s = [e2_H(0), e2_H(1)]
                e2_T(e2s[0])
                e2s.append(e2_H(2))
                e2_T(e2s[1])
                e2s.append(e2_H(3))
                e2_T(e2s[2])
                e2_T(e2s[3])
                routing4(em, sm, lgb, onesb, triS, cum, elim, destall, gidxall, wall, b, ps, ck, r2k)
                for j in range(4):
                    tile_idx = b * 4 + j
                    xn2b, kxb = xn2bs[j]
                    for k in range(2):
                        em.dma('pool', lambda e, xn2b=xn2b, k=k, tile_idx=tile_idx: e.indirect_dma_start(
                            out=xsD[:, :], out_offset=IndirectOffsetOnAxis(ap=destall[:, tile_idx * 2 + k:tile_idx * 2 + k + 1], axis=0),
                            in_=xn2b, in_offset=None, bounds_check=pregs['bc'], oob_is_err=False),
                            r=[kxb, ('dest', b)], w=['xsD'])
                if b == 0:
                    chk('E2')
            if dbg_t is not None and 'route' in DBGSEL:
                t, kt = r2k.get()
                em.op('dve', lambda e: e.tensor_copy(out=t[:, 0:64], in_=destall[:]), r=[('dest', i) for i in range(8)], w=[kt])
                dump(t[:, 0:64], [kt], 64)
                dump(wall[:].rearrange("p a b -> p (a b)"), [('dest', i) for i in range(32)], 64)
            chk('P1')
            p1.close()

            p2 = ExitStack()
            es.enter_context(p2)
            wgu = [sb('wgu%d' % i, [128, 8, 512], BF16, p2) for i in range(3)]
            wdn = [sb('wdn%d' % i, [128, 2, D], BF16, p2) for i in range(3)]
            xst = [sb('xst%d' % i, [128, 4, D], BF16, p2) for i in range(2)]
            xsT = [sb('xsT%d' % i, [128, 8, 512], BF16, p2) for i in range(2)]
            hT = [sb('hT%d' % i, [128, 2, 512], BF16, p2) for i in range(2)]
            yst = [sb('yst%d' % i, [128, 4, D], F32, p2) for i in range(2)]
            sgr = Ring(nc, p2, 'sgr', 4, 2048)
            zrow = sb('zrow', [128, D], F32, p2)
            em.op('pool', lambda e: e.memset(zrow[:], 0.0), w=['zrow'])
            em.dma('sp', lambda e: e.dma_start(out=ysD[E * CAP:E * CAP + 128, :], in_=zrow[:]), r=['zrow'], w=[('ysD', 'z')])
            def load_xst(ex_):
                em.dma('sp', lambda e: e.dma_start(out=xst[ex_ % 2][:], in_=xsD[ex_ * CAP:(ex_ + 1) * CAP, :].rearrange("(j p) d -> p j d", p=128)),
                       r=['xsD'], w=[('xst', ex_ % 2)])

            def p2_T(ex):
                i2 = ex % 2
                wg_v = w_gate[ex].rearrange("(c p) n -> p c n", p=128)
                wu_v = w_up[ex].rearrange("(c p) n -> p c n", p=128)
                wd_v = w_down[ex].rearrange("(c p) n -> p c n", p=128)
                i3 = ex % 3
                em.dma('pool', lambda e, i3=i3, wg_v=wg_v: e.dma_start(out=wgu[i3][:, :, 0:256], in_=wg_v), w=[('wgu', i3, 0)])
                em.dma('pool', lambda e, i3=i3, wu_v=wu_v: e.dma_start(out=wgu[i3][:, :, 256:512], in_=wu_v), w=[('wgu', i3, 1)])
                em.dma('pool', lambda e, i3=i3, wd_v=wd_v: e.dma_start(out=wdn[i3][:], in_=wd_v), w=[('wdn', i3)])
                if ex == 0:
                    load_xst(0)
                if ex + 1 < E:
                    load_xst(ex + 1)
                for kp_ in range(4):
                    pf, pb, kp = ps()
                    fns = []
                    for q in range(2):
                        kc = kp_ * 2 + q
                        for j in range(4):
                            fns.append(lambda e, q=q, j=j, kc=kc, pb=pb, i2=i2: e.transpose(pb[:, q * 512 + j * 128:q * 512 + (j + 1) * 128], xst[i2][:, j, kc * 128:(kc + 1) * 128], identb[:]))
                    em.mm(fns, r=[('xst', i2)] + ck(identb), w=[kp])
                    dst = xsT[i2][:, kp_ * 2:kp_ * 2 + 2, :].rearrange("p c n -> p (c n)")
                    em.op('act', lambda e, dst=dst, pb=pb: e.activation(out=dst, in_=pb, func=AF.Identity, scale=1.0, bias=epsc[:, 1:2]), r=[kp, 'epsc'], w=[('xsT', i2)])

            def p2_C(ex):
                i2 = ex % 2
                i3 = ex % 3
                pgs = []
                for ft in range(4):
                    pf, pb, kp = ps()
                    em.mm([lambda e, kc=kc, pf=pf, ft=ft, i2=i2, i3=i3: e.matmul(pf, lhsT=wgu[i3][:, kc, ft * 128:(ft + 1) * 128], rhs=xsT[i2][:, kc, :], start=(kc == 0), stop=(kc == 7))
                           for kc in range(8)], r=[('wgu', i3, ft // 2), ('xsT', i2)], w=[kp])
                    pgs.append((pf, kp))
                for f in range(2):
                    sg, ksg = sgr.get()
                    em.op('act', lambda e, sg=sg, f=f, pgs=pgs: e.activation(out=sg, in_=pgs[f][0], func=AF.Silu), r=[pgs[f][1]], w=[ksg])
                    em.op('dve', lambda e, sg=sg, f=f, pgs=pgs, i2=i2: e.tensor_tensor(out=hT[i2][:, f, :], in0=pgs[2 + f][0], in1=sg, op=ALU.mult),
                          r=[pgs[2 + f][1], ksg], w=[('hT', i2)])
                for j in range(4):
                    for hf in range(2):
                        pf, pb, kp = ps()
                        em.mm([lambda e, f=f, pf=pf, j=j, hf=hf, i2=i2, i3=i3: e.matmul(pf, lhsT=hT[i2][:, f, j * 128:(j + 1) * 128], rhs=wdn[i3][:, f, hf * 512:(hf + 1) * 512],
                                                                                  start=(f == 0), stop=(f == 1)) for f in range(2)], r=[('hT', i2), ('wdn', i3)], w=[kp])
                        if (j * 2 + hf) % 2 == 0:
                            em.op('act', lambda e, pf=pf, j=j, hf=hf, i2=i2: e.activation(out=yst[i2][:, j, hf * 512:(hf + 1) * 512], in_=pf, func=AF.Copy), r=[kp], w=[('yst', i2)])
                        else:
                            em.op('dve', lambda e, pf=pf, j=j, hf=hf, i2=i2: e.tensor_copy(out=yst[i2][:, j, hf * 512:(hf + 1) * 512], in_=pf), r=[kp], w=[('yst', i2)])
                em.dma('sp', lambda e, i2=i2, ex=ex: e.dma_start(out=ysD[ex * CAP:(ex + 1) * CAP, :].rearrange("(j p) d -> p j d", p=128), in_=yst[i2][:]),
                       r=[('yst', i2)], w=[('ysD', ex)])
            p2_T(0)
            for ex in range(E):
                if ex + 1 < E:
                    p2_T(ex + 1)
                p2_C(ex)
            chk('P2')
            p2.close()

            p3 = ExitStack()
            es.enter_context(p3)
            GT2b = sb('GT2b', [128, D], F32, p3)
            fngb = sb('fngb', [128, D], F32, p3)
            em.dma('sp', lambda e: e.dma_start(out=fngb[:], in_=p_fng), w=['fngb'])
            q4 = Ring(nc, p3, 'q4', 20, 4096)
            q2 = Ring(nc, p3, 'q2', 2, 2048)
            out_toks = []
            def fetch(ti):
                y0, ky0 = q4.get()
                y1, ky1 = q4.get()
                for k, (yy, kyy) in enumerate(((y0, ky0), (y1, ky1))):
                    em.dma('pool', lambda e, yy=yy, k=k, ti=ti: e.indirect_dma_start(
                        out=yy, out_offset=None, in_=ysD[:, :], in_offset=IndirectOffsetOnAxis(ap=gidxall[:, ti * 2 + k:ti * 2 + k + 1], axis=0)),
                        r=[('ysD', 'z')] + [('ysD', ex_) for ex_ in range(E)] + [('dest', ti // 4)], w=[kyy])
                h1t, kh1 = q4.get()
                em.dma('sp', lambda e, h1t=h1t, ti=ti: e.dma_start(out=h1t, in_=h1D[ti * 128:(ti + 1) * 128, :]), r=[('h1D', ti)], w=[kh1])
                return y0, ky0, y1, ky1, h1t, kh1

            def p3_H(ti, f):
                y0, ky0, y1, ky1, h1t, kh1 = f
                seq = ti // 16
                if ti % 16 == 0:
                    em.dma('sp', lambda e: e.dma_start(out=GT2b[:], in_=modD[seq:seq + 1, 5120:6144].partition_broadcast(128)), r=MODK, w=['GT2b'])
                m, km = q4.get()
                em.op('act', lambda e: e.activation(out=m, in_=y0, func=AF.Copy, scale=wall[:, ti, 0:1]), r=[ky0, ('dest', ti // 4)], w=[km])
                em.op('dve', lambda e: e.scalar_tensor_tensor(out=m, in0=y1, scalar=wall[:, ti, 1:2], in1=m, op0=ALU.mult, op1=ALU.add),
                      r=[ky1, km, ('dest', ti // 4)], w=[km])
                em.op('pool', lambda e: e.tensor_tensor(out=m, in0=m, in1=GT2b[:], op=ALU.mult), r=[km, 'GT2b'], w=[km])
                return m, km, h1t, kh1

            def p3_T(ti, hst):
                m, km, h1t, kh1 = hst
                em.op('dve', lambda e: e.tensor_tensor(out=m, in0=m, in1=h1t, op=ALU.add), r=[km, kh1], w=[km])
                junk, kj = q2.get(BF16)
                ss, kss = sm(1)
                em.op('act', lambda e: e.activation(out=junk, in_=m, func=AF.Square, accum_out=ss), r=[km], w=[kj, kss])
                rs, krs = rstd_from_ss(ss, kss, 1.0 / D)
                o_, ko = q4.get()
                em.op('dve', lambda e: e.scalar_tensor_tensor(out=o_, in0=m, scalar=rs, in1=fngb[:], op0=ALU.mult, op1=ALU.mult), r=[km, krs, 'fngb'], w=[ko])
                em.dma('sp', lambda e: e.dma_start(out=out[ti * 128:(ti + 1) * 128, :], in_=o_), r=[ko], w=[('out', ti)])

            fts = {0: fetch(0), 1: fetch(1)}
            hs_ = {0: p3_H(0, fts[0])}
            for ti in range(32):
                if ti + 2 < 32:
                    fts[ti + 2] = fetch(ti + 2)
                if ti + 1 < 32:
                    hs_[ti + 1] = p3_H(ti + 1, fts[ti + 1])
                p3_T(ti, hs_[ti])
        except StopBuild:
            pass
        em.wait_keys('sp', [k for k in em.lastw if isinstance(k, tuple) and k[0] in ('dbg', 'out')])
        for q_ in ('sp', 'pool'):
            d_ = em.dq[q_]
            for i_, v_ in enumerate(d_['vals']):
                if v_ > 0:
                    em._wait('sp', (q_, i_), v_)
        em.finish()
    return nc


DBGSEL = ()
STOP = None
SERIAL = False


class StopBuild(Exception):
    pass


def chk(name):
    if STOP == name:
        raise StopBuild()


def routing(em, sm, lg, onesb, triS, cum, elim, destall, gidxall, wall, ti, ps, ck, r2k):
    kl = 'lg'
    gmax, kgm = sm(1)
    em.op('dve', lambda e: e.tensor_reduce(out=gmax, in_=lg[:, 0:4], axis=AX.X, op=ALU.max), r=[kl], w=[kgm])
    ohg, kohg = sm(4)
    em.op('dve', lambda e: e.tensor_scalar(out=ohg, in0=lg[:, 0:4], scalar1=gmax, scalar2=None, op0=ALU.is_equal), r=[kl, kgm], w=[kohg])
    ngm, kngm = sm(1)
    em.op('dve', lambda e: e.tensor_scalar(out=ngm, in0=gmax, scalar1=-1.0, scalar2=None, op0=ALU.mult), r=[kgm], w=[kngm])
    eg, keg = sm(4)
    sg, ksg = sm(1)
    em.op('act', lambda e: e.activation(out=eg, in_=lg[:, 0:4], func=AF.Exp, bias=ngm, accum_out=sg), r=[kl, kngm], w=[keg, ksg])
    pg, kpg = sm(1)
    em.op('dve', lambda e: e.reciprocal(out=pg, in_=sg), r=[ksg], w=[kpg])
    les, kles = sm(8)
    em.op('dve', lambda e: e.tensor_scalar(out=les, in0=lg[:, 4:12], scalar1=ohg[:, 0:1], scalar2=None, op0=ALU.mult), r=[kl, kohg], w=[kles])
    for g in range(1, 4):
        em.op('dve', lambda e, g=g: e.scalar_tensor_tensor(out=les, in0=lg[:, 4 + 8 * g:12 + 8 * g], scalar=ohg[:, g:g + 1], in1=les, op0=ALU.mult, op1=ALU.add),
              r=[kl, kohg, kles], w=[kles])
    m8, km8 = sm(8)
    em.op('dve', lambda e: e.max(out=m8, in_=les), r=[kles], w=[km8])
    d21, kd21 = sm(1)
    em.op('dve', lambda e: e.tensor_tensor(out=d21, in0=m8[:, 1:2], in1=m8[:, 0:1], op=ALU.subtract), r=[km8], w=[kd21])
    e21, ke21 = sm(1)
    em.op('act', lambda e: e.activation(out=e21, in_=d21, func=AF.Exp), r=[kd21], w=[ke21])
    den, kden = sm(1)
    em.op('dve', lambda e: e.tensor_scalar(out=den, in0=e21, scalar1=1.0, scalar2=None, op0=ALU.add), r=[ke21], w=[kden])
    rden, krden = sm(1)
    em.op('dve', lambda e: e.reciprocal(out=rden, in_=den), r=[kden], w=[krden])
    w1, kw1 = sm(1)
    em.op('dve', lambda e: e.tensor_tensor(out=w1, in0=pg, in1=rden, op=ALU.mult), r=[kpg, krden], w=[kw1])
    w2, kw2 = sm(1)
    em.op('dve', lambda e: e.tensor_tensor(out=w2, in0=w1, in1=e21, op=ALU.mult), r=[kw1, ke21], w=[kw2])
    ohs = []
    for k in range(2):
        sel, ksel = sm(8)
        em.op('dve', lambda e, k=k, sel=sel: e.tensor_scalar(out=sel, in0=les, scalar1=m8[:, k:k + 1], scalar2=None, op0=ALU.is_equal), r=[kles, km8], w=[ksel])
        oh, koh = sm(32)
        for g in range(4):
            em.op('dve', lambda e, g=g, oh=oh, sel=sel: e.tensor_scalar(out=oh[:, g * 8:(g + 1) * 8], in0=sel, scalar1=ohg[:, g:g + 1], scalar2=None, op0=ALU.mult),
                  r=[ksel, kohg], w=[koh])
        ohs.append((oh, koh))
    ohsum, kohs = r2k.get(BF16)
    em.op('dve', lambda e: e.tensor_tensor(out=ohsum[:, 0:32], in0=ohs[0][0], in1=ohs[1][0], op=ALU.add), r=[ohs[0][1], ohs[1][1]], w=[kohs])
    pr, _, kpr = ps()
    em.mm([lambda e: e.matmul(pr[:, 0:32], lhsT=triS[:], rhs=ohsum[:, 0:32], start=True, stop=True),
           lambda e: e.matmul(pr[:, 32:64], lhsT=onesb[:], rhs=ohsum[:, 0:32], start=True, stop=True)], r=[kohs] + ck(triS, onesb), w=[kpr])
    rk, krk = sm(32)
    em.op('dve', lambda e: e.tensor_tensor(out=rk, in0=pr[:, 0:32], in1=cum[:], op=ALU.add), r=[kpr, 'cum'], w=[krk])
    em.op('dve', lambda e: e.tensor_tensor(out=cum[:], in0=pr[:, 32:64], in1=cum[:], op=ALU.add), r=[kpr, 'cum', krk], w=['cum'])
    for k in range(2):
        oh, koh = ohs[k]
        t32, kt32 = sm(32)
        dst, kdst = sm(1)
        em.op('dve', lambda e, t32=t32, oh=oh: e.tensor_tensor(out=t32, in0=oh, in1=rk, op=ALU.mult), r=[koh, krk], w=[kt32])
        em.op('dve', lambda e, t32=t32, dst=dst: e.tensor_reduce(out=dst, in_=t32, axis=AX.X, op=ALU.add), r=[kt32], w=[kdst])
        l32, kl32 = sm(32)
        lim, klim = sm(1)
        em.op('dve', lambda e, l32=l32, oh=oh: e.tensor_tensor(out=l32, in0=oh, in1=elim[:], op=ALU.mult), r=[koh] + ck(elim), w=[kl32])
        em.op('dve', lambda e, l32=l32, lim=lim: e.tensor_reduce(out=lim, in_=l32, axis=AX.X, op=ALU.add), r=[kl32], w=[klim])
        ok, kok = sm(1)
        em.op('dve', lambda e, ok=ok, dst=dst, lim=lim: e.tensor_tensor(out=ok, in0=dst, in1=lim, op=ALU.is_lt), r=[kdst, klim], w=[kok])
        nok, knok = sm(1)
        em.op('dve', lambda e, nok=nok, ok=ok: e.tensor_scalar(out=nok, in0=ok, scalar1=-1.0, scalar2=1.0, op0=ALU.mult, op1=ALU.add), r=[kok], w=[knok])
        dv, kdv = sm(1)
        em.op('dve', lambda e, dv=dv, dst=dst, ok=ok: e.tensor_tensor(out=dv, in0=dst, in1=ok, op=ALU.mult), r=[kdst, kok], w=[kdv])
        si, ksi = sm(1)
        em.op('dve', lambda e, si=si, nok=nok, dv=dv: e.scalar_tensor_tensor(out=si, in0=nok, scalar=float(E * CAP + 64), in1=dv, op0=ALU.mult, op1=ALU.add), r=[knok, kdv], w=[ksi])
        gi_, kgi = sm(1)
        em.op('dve', lambda e, gi_=gi_, nok=nok, dv=dv: e.scalar_tensor_tensor(out=gi_, in0=nok, scalar=float(E * CAP), in1=dv, op0=ALU.mult, op1=ALU.add), r=[knok, kdv], w=[kgi])
        em.op('dve', lambda e, k=k, si=si: e.tensor_copy(out=destall[:, ti * 2 + k:ti * 2 + k + 1], in_=si), r=[ksi], w=[('dest', ti)])
        em.op('dve', lambda e, k=k, gi_=gi_: e.tensor_copy(out=gidxall[:, ti * 2 + k:ti * 2 + k + 1], in_=gi_), r=[kgi], w=[('dest', ti)])
        wk, kwk = (w1, kw1) if k == 0 else (w2, kw2)
        em.op('dve', lambda e, k=k, wk=wk, ok=ok: e.tensor_tensor(out=wall[:, ti, k:k + 1], in0=wk, in1=ok, op=ALU.mult), r=[kwk, kok], w=[('dest', ti)])


def _consts():
    bf = ml_dtypes.bfloat16
    i = np.arange(128)
    c = {}
    c['k_identb'] = np.eye(128, dtype=np.float32).astype(bf)
    c['k_identq'] = np.tile(np.eye(128, dtype=np.float32), (1, 4)).astype(bf)
    c['k_identf'] = np.eye(128, dtype=np.float32)
    c['k_onesb'] = np.ones((128, 128), np.float32).astype(bf)
    c['k_onesf'] = np.ones((128, 128), np.float32)
    c['k_triU'] = (i[:, None] <= i[None, :]).astype(np.float32)
    c['k_maskT'] = np.where(i[None, :] >= i[:, None], 0.0, NEG).astype(np.float32)
    c['k_maskS'] = np.where(i[:, None] > i[None, :], 0.0, -NEG).astype(np.float32)
    c['k_triS'] = (i[:, None] < i[None, :]).astype(np.float32).astype(bf)
    pc = np.zeros((128, 4, 16), np.float32)
    for gi, win in enumerate((2, 4, 8, 16)):
        t = np.arange(16)
        pc[:, gi, :] = 1.0 / np.minimum(t + 1, win)
    c['k_pcorr'] = pc
    bm = np.zeros((128, 7, 128), np.float32)
    for l in range(7):
        s_ = 1 << l
        bm[:, l, :] = ((i[:, None] // (2 * s_) == i[None, :] // (2 * s_)) & (i[:, None] % (2 * s_) >= s_) & (i[None, :] % (2 * s_) < s_))
    c['k_bmask'] = bm.astype(bf)
    c['k_bmaskT'] = np.ascontiguousarray(bm.transpose(2, 1, 0)).astype(bf)
    c['k_ebase'] = np.tile((np.arange(32) * CAP).astype(np.float32), (128, 1))
    c['k_elim'] = np.tile(((np.arange(32) + 1) * CAP).astype(np.float32), (128, 1))
    return c


def _prep_inputs(inp):
    f = lambda a: np.ascontiguousarray(np.asarray(a, dtype=np.float32))
    shared = {}
    shared['w_ada'] = f(inp['w_ada'][0])
    shared['b_ada'] = f(inp['b_ada'][0]).reshape(1, -1)
    shared['w_in'] = f(inp['w_in'][0])
    shared['w_lift_a'] = f(inp['w_lift_a'][0])
    shared['w_lift_b'] = f(inp['w_lift_b'][0])
    shared['w_out'] = f(inp['w_out'][0])
    shared['pool_w'] = f(inp['pool_w'][0])
    shared['w_gate'] = f(inp['w_gate'][0])
    shared['w_up'] = f(inp['w_up'][0])
    shared['w_down'] = f(inp['w_down'][0])
    shared['p_n1g'] = f(np.asarray(inp['norm1_g'][0]).reshape(8, 128).T)
    shared['p_convw'] = f(np.asarray(inp['conv_w'][0]).reshape(4, 12, 128).transpose(2, 1, 0))
    shared['p_alog'] = f(np.broadcast_to(np.asarray(inp['a_log'][0]).reshape(1, 4), (128, 4)))
    shared['p_dtb'] = f(np.broadcast_to(np.asarray(inp['dt_bias'][0]).reshape(1, 4), (128, 4)))
    shared['p_dng'] = f(np.asarray(inp['dn_norm_g'][0]).reshape(128, 1))
    shared['p_pscale'] = f(np.asarray(inp['pool_scale'][0]).reshape(4, 128).T)
    shared['p_n2g'] = f(np.broadcast_to(np.asarray(inp['norm2_g'][0]).reshape(1, D), (128, D)))
    shared['p_fng'] = f(np.broadcast_to(np.asarray(inp['final_norm_g']).reshape(1, D), (128, D)))
    br = np.concatenate([np.asarray(inp['b_router_group'][0]), np.asarray(inp['b_router_expert'][0])]).reshape(1, 36)
    shared['p_brb'] = f(np.broadcast_to(br, (128, 36)))
    wrc = np.concatenate([np.asarray(inp['w_router_group'][0]), np.asarray(inp['w_router_expert'][0])], axis=1)
    shared['p_wr'] = f(wrc.reshape(8, 128, 36).transpose(1, 0, 2))
    shared.update(_consts())
    xs = np.asarray(inp['x'], dtype=np.float32)
    cs = np.asarray(inp['c'], dtype=np.float32)
    in_maps = []
    for i in range(NCORES):
        m = dict(shared)
        m['x'] = np.ascontiguousarray(xs[2 * i:2 * i + 2].reshape(TOK, D))
        m['cT'] = np.ascontiguousarray(cs[2 * i:2 * i + 2].reshape(2, 8, 128).transpose(2, 1, 0))
        in_maps.append(m)
    return in_maps


_NC_CACHE = {}


def kernel(**inputs):
    in_maps = _prep_inputs(inputs)
    if 'nc' not in _NC_CACHE:
        _NC_CACHE['nc'] = build_nc()
    nc = _NC_CACHE['nc']
    res = run_bass_kernel_spmd(nc, in_maps, core_ids=list(range(NCORES)))
    outs = [np.asarray(r['out'], dtype=np.float32).reshape(2, 2048, D) for r in res.results]
    return np.concatenate(outs, axis=0)


def routing4(em, sm, lgb, onesb, triS, cum, elim, destall, gidxall, wall, b, ps, ck, r2k):
    kl = 'lgb'
    T = 4

    def v3(ap, n):
        return ap.rearrange("p (t n) -> p t n", n=n)

    def bc_last(ap, n):
        return ap.unsqueeze(2).broadcast_to([128, T, n])
    lgg = lgb[:, :, 0:4]
    gmax, kgm = sm(T)
    em.op('dve', lambda e: e.tensor_reduce(out=gmax, in_=lgg, axis=AX.X, op=ALU.max), r=[kl], w=[kgm])
    ohg_, kohg = sm(16)
    ohg = v3(ohg_, 4)
    em.op('dve', lambda e: e.tensor_tensor(out=ohg, in0=lgg, in1=bc_last(gmax, 4), op=ALU.is_equal), r=[kl, kgm], w=[kohg])
    sub_, ksub = sm(16)
    em.op('dve', lambda e: e.tensor_tensor(out=v3(sub_, 4), in0=lgg, in1=bc_last(gmax, 4), op=ALU.subtract), r=[kl, kgm], w=[ksub])
    eg_, keg = sm(16)
    em.op('act', lambda e: e.activation(out=eg_, in_=sub_, func=AF.Exp), r=[ksub], w=[keg])
    sg, ksg = sm(T)
    em.op('dve', lambda e: e.tensor_reduce(out=sg, in_=v3(eg_, 4), axis=AX.X, op=ALU.add), r=[keg], w=[ksg])
    pg, kpg = sm(T)
    em.op('dve', lambda e: e.reciprocal(out=pg, in_=sg), r=[ksg], w=[kpg])
    prod_, kprod = r2k.get()
    prod = prod_[:, 0:T * 32]
    le4 = lgb[:, :, 4:36].rearrange("p t (g j) -> p t g j", j=8)
    em.op('dve', lambda e: e.tensor_tensor(out=prod.rearrange("p (t g j) -> p t g j", g=4, j=8), in0=le4,
                                           in1=ohg.unsqueeze(3).broadcast_to([128, T, 4, 8]), op=ALU.mult), r=[kl, kohg], w=[kprod])
    les_, kles = sm(32)
    les = v3(les_, 8)
    em.op('dve', lambda e: e.tensor_reduce(out=les, in_=prod.rearrange("p (t g j) -> p t j g", g=4, j=8), axis=AX.X, op=ALU.add), r=[kprod], w=[kles])
    m1, km1 = sm(T)
    em.op('dve', lambda e: e.tensor_reduce(out=m1, in_=les, axis=AX.X, op=ALU.max), r=[kles], w=[km1])
    sel1_, ksel1 = sm(32)
    sel1 = v3(sel1_, 8)
    em.op('dve', lambda e: e.tensor_tensor(out=sel1, in0=les, in1=bc_last(m1, 8), op=ALU.is_equal), r=[kles, km1], w=[ksel1])
    les2_, kles2 = sm(32)
    les2 = v3(les2_, 8)
    em.op('dve', lambda e: e.scalar_tensor_tensor(out=les2, in0=sel1, scalar=NEG, in1=les, op0=ALU.mult, op1=ALU.add), r=[ksel1, kles], w=[kles2])
    m2, km2 = sm(T)
    em.op('dve', lambda e: e.tensor_reduce(out=m2, in_=les2, axis=AX.X, op=ALU.max), r=[kles2], w=[km2])
    sel2_, ksel2 = sm(32)
    sel2 = v3(sel2_, 8)
    em.op('dve', lambda e: e.tensor_tensor(out=sel2, in0=les2, in1=bc_last(m2, 8), op=ALU.is_equal), r=[kles2, km2], w=[ksel2])
    d21, kd21 = sm(T)
    em.op('dve', lambda e: e.tensor_tensor(out=d21, in0=m2, in1=m1, op=ALU.subtract), r=[km1, km2], w=[kd21])
    e21, ke21 = sm(T)
    em.op('act', lambda e: e.activation(out=e21, in_=d21, func=AF.Exp), r=[kd21], w=[ke21])
    den, kden = sm(T)
    em.op('dve', lambda e: e.tensor_scalar(out=den, in0=e21, scalar1=1.0, scalar2=None, op0=ALU.add), r=[ke21], w=[kden])
    rden, krden = sm(T)
    em.op('dve', lambda e: e.reciprocal(out=rden, in_=den), r=[kden], w=[krden])
    w1, kw1 = sm(T)
    em.op('dve', lambda e: e.tensor_tensor(out=w1, in0=pg, in1=rden, op=ALU.mult), r=[kpg, krden], w=[kw1])
    w2, kw2 = sm(T)
    em.op('dve', lambda e: e.tensor_tensor(out=w2, in0=w1, in1=e21, op=ALU.mult), r=[kw1, ke21], w=[kw2])
    ohs = []
    for k, (sel, ksel) in enumerate(((sel1, ksel1), (sel2, ksel2))):
        oh_, koh = r2k.get()
        oh = oh_[:, 0:T * 32]
        em.op('dve', lambda e, oh=oh, sel=sel: e.tensor_tensor(out=oh.rearrange("p (t g j) -> p t g j", g=4, j=8),
                                                               in0=ohg.unsqueeze(3).broadcast_to([128, T, 4, 8]),
                                                               in1=sel.unsqueeze(2).broadcast_to([128, T, 4, 8]), op=ALU.mult), r=[kohg, ksel], w=[koh])
        ohs.append((oh, koh))
    ohsum_, kohs = r2k.get(BF16)
    ohsum = ohsum_[:, 0:T * 32]
    em.op('dve', lambda e: e.tensor_tensor(out=ohsum, in0=ohs[0][0], in1=ohs[1][0], op=ALU.add), r=[ohs[0][1], ohs[1][1]], w=[kohs])
    pr, _, kpr = ps()
    fns = []
    for t in range(T):
        terms = [(triS, t)] + [(onesb, t2) for t2 in range(t)]
        for i_, (lt, t2) in enumerate(terms):
            fns.append(lambda e, t=t, lt=lt, t2=t2, i_=i_, n_=len(terms): e.matmul(pr[:, t * 32:(t + 1) * 32], lhsT=lt[:], rhs=ohsum[:, t2 * 32:(t2 + 1) * 32],
                                                                                  start=(i_ == 0), stop=(i_ == n_ - 1)))
    for t in range(T):
        fns.append(lambda e, t=t: e.matmul(pr[:, 128:160], lhsT=onesb[:], rhs=ohsum[:, t * 32:(t + 1) * 32], start=(t == 0), stop=(t == T - 1)))
    em.mm(fns, r=[kohs] + ck(triS, onesb), w=[kpr])
    rk_, krk = r2k.get()
    rk = rk_[:, 0:T * 32]
    em.op('dve', lambda e: e.tensor_tensor(out=v3(rk, 32), in0=v3(pr[:, 0:128], 32), in1=cum[:].unsqueeze(1).broadcast_to([128, T, 32]), op=ALU.add),
          r=[kpr, 'cum'], w=[krk])
    em.op('dve', lambda e: e.tensor_tensor(out=cum[:], in0=pr[:, 128:160], in1=cum[:], op=ALU.add), r=[kpr, 'cum', krk], w=['cum'])
    dsl = slice(b * 8, (b + 1) * 8)
    for k in range(2):
        oh, koh = ohs[k]
        t32_, kt32 = r2k.get()
        t32 = t32_[:, 0:T * 32]
        em.op('dve', lambda e, t32=t32, oh=oh: e.tensor_tensor(out=t32, in0=oh, in1=rk, op=ALU.mult), r=[koh, krk], w=[kt32])
        dst, kdst = sm(T)
        em.op('dve', lambda e, t32=t32, dst=dst: e.tensor_reduce(out=dst, in_=v3(t32, 32), axis=AX.X, op=ALU.add), r=[kt32], w=[kdst])
        l32_, kl32 = r2k.get()
        l32 = l32_[:, 0:T * 32]
        em.op('dve', lambda e, l32=l32, oh=oh: e.tensor_tensor(out=v3(l32, 32), in0=v3(oh, 32), in1=elim[:].unsqueeze(1).broadcast_to([128, T, 32]), op=ALU.mult),
              r=[koh] + ck(elim), w=[kl32])
        lim, klim = sm(T)
        em.op('dve', lambda e, l32=l32, lim=lim: e.tensor_reduce(out=lim, in_=v3(l32, 32), axis=AX.X, op=ALU.add), r=[kl32], w=[klim])
        ok, kok = sm(T)
        em.op('dve', lambda e, ok=ok, dst=dst, lim=lim: e.tensor_tensor(out=ok, in0=dst, in1=lim, op=ALU.is_lt), r=[kdst, klim], w=[kok])
        nok, knok = sm(T)
        em.op('dve', lambda e, nok=nok, ok=ok: e.tensor_scalar(out=nok, in0=ok, scalar1=-1.0, scalar2=1.0, op0=ALU.mult, op1=ALU.add), r=[kok], w=[knok])
        dv, kdv = sm(T)
        em.op('dve', lambda e, dv=dv, dst=dst, ok=ok: e.tensor_tensor(out=dv, in0=dst, in1=ok, op=ALU.mult), r=[kdst, kok], w=[kdv])
        si, ksi = sm(T)
        em.op('dve', lambda e, si=si, nok=nok, dv=dv: e.scalar_tensor_tensor(out=si, in0=nok, scalar=float(E * CAP + 64), in1=dv, op0=ALU.mult, op1=ALU.add), r=[knok, kdv], w=[ksi])
        gi_, kgi = sm(T)
        em.op('dve', lambda e, gi_=gi_, nok=nok, dv=dv: e.scalar_tensor_tensor(out=gi_, in0=nok, scalar=float(E * CAP), in1=dv, op0=ALU.mult, op1=ALU.add), r=[knok, kdv], w=[kgi])
        em.op('dve', lambda e, k=k, si=si: e.tensor_copy(out=destall[:, dsl].rearrange("p (t k) -> p t k", k=2)[:, :, k], in_=si), r=[ksi], w=[('dest', b)])
        em.op('dve', lambda e, k=k, gi_=gi_: e.tensor_copy(out=gidxall[:, dsl].rearrange("p (t k) -> p t k", k=2)[:, :, k], in_=gi_), r=[kgi], w=[('dest', b)])
        wk, kwk = (w1, kw1) if k == 0 else (w2, kw2)
        em.op('dve', lambda e, k=k, wk=wk, ok=ok: e.tensor_tensor(out=wall[:, b * 4:(b + 1) * 4, k], in0=wk, in1=ok, op=ALU.mult), r=[kwk, kok], w=[('dest', b)])
```
